# Optimizing a Trainium2 kernel written in Bass

```python
import math
import jax, jax.numpy as jnp
from jax import lax
import numpy as np

D_MODEL = 1024
BATCH = 8
SEQ = 4096
DEPTH = 2

SG_WIDTH = 2 * D_MODEL
SG_GROUPS = 8
SG_CHUNK = 128
DA_HEADS = D_MODEL // 128
DA_HEAD_DIM = 64
ROT_DIM = DA_HEAD_DIM // 4
ROPE_THETA = 500000.0
Q_BLOCK = 128
FFN_DENSE = 2816
N_EXPERTS = 8
TOP_K = 2
FFN_EXPERT = 3584
NORM_EPS = 1e-6

kernel_name = "hybrid_sgmlp_diffattn_moe"


def _rmsnorm(x, gain, eps=NORM_EPS):
    xf = x.astype(jnp.float32)
    y = xf * lax.rsqrt(jnp.mean(xf * xf, axis=-1, keepdims=True) + eps)
    return (y * gain.astype(jnp.float32)).astype(x.dtype)


def _swiglu(h, w_gate, w_up, w_down):
    return (jax.nn.silu(h @ w_gate) * (h @ w_up)) @ w_down


def _spatial_gating_mixer(h, w_in, v_gain, w_spatial, b_spatial, w_out):
    B, S, _ = h.shape
    z = jax.nn.gelu(h @ w_in)
    u, v = jnp.split(z, 2, axis=-1)
    v = _rmsnorm(v, v_gain)
    n_chunks = S // SG_CHUNK
    cg = SG_WIDTH // SG_GROUPS
    v = v.reshape(B, n_chunks, SG_CHUNK, SG_GROUPS, cg)
    causal = jnp.tril(jnp.ones((SG_CHUNK, SG_CHUNK), dtype=w_spatial.dtype))
    w_masked = w_spatial * causal[None]
    mixed = jnp.einsum('gts,bnsgc->bntgc', w_masked, v) + b_spatial.T[None, None, :, :, None]
    y = u * mixed.reshape(B, S, SG_WIDTH)
    return y @ w_out


def _rope_tables(positions):
    inv_freq = 1.0 / (ROPE_THETA ** (jnp.arange(0, ROT_DIM, 2, dtype=jnp.float32) / ROT_DIM))
    ang = positions.astype(jnp.float32)[..., None] * inv_freq
    return jnp.cos(ang), jnp.sin(ang)


def _partial_rope(x, cos, sin):
    half = ROT_DIM // 2
    xf = x.astype(jnp.float32)
    x1 = xf[..., :half]
    x2 = xf[..., half:ROT_DIM]
    out = jnp.concatenate([x1 * cos - x2 * sin, x2 * cos + x1 * sin, xf[..., ROT_DIM:]], axis=-1)
    return out.astype(x.dtype)


def _diff_attention(h, positions, w_qkv, q_gain, k_gain, lam_q1, lam_k1, lam_q2, lam_k2,
                    subln_gain, w_out, lambda_init):
    B, S, _ = h.shape
    H, Dh = DA_HEADS, DA_HEAD_DIM
    qkv = h @ w_qkv
    q, k, v = jnp.split(qkv, 3, axis=-1)
    q = _rmsnorm(q.reshape(B, S, H, 2, Dh), q_gain)
    k = _rmsnorm(k.reshape(B, S, H, 2, Dh), k_gain)
    v = v.reshape(B, S, H, 2 * Dh)
    cos, sin = _rope_tables(positions)
    cos = cos[:, :, None, None, :]
    sin = sin[:, :, None, None, :]
    q = _partial_rope(q, cos, sin)
    k = _partial_rope(k, cos, sin)

    f32 = jnp.float32
    lam = (jnp.exp(jnp.sum(lam_q1.astype(f32) * lam_k1.astype(f32)))
           - jnp.exp(jnp.sum(lam_q2.astype(f32) * lam_k2.astype(f32))) + lambda_init)
    scale = 1.0 / math.sqrt(Dh)

    n_blocks = S // Q_BLOCK
    k_t = k.transpose(0, 2, 3, 1, 4)
    v_t = v.transpose(0, 2, 1, 3)
    q_b = q.transpose(0, 2, 3, 1, 4).reshape(B, H, 2, n_blocks, Q_BLOCK, Dh)
    q_b = jnp.moveaxis(q_b, 3, 0)
    key_pos = jnp.arange(S)[None, :]

    def block(args):
        q_blk, blk = args
        s = jnp.einsum('bhcqd,bhckd->bhcqk', q_blk, k_t,
                       preferred_element_type=jnp.float32) * scale
        q_pos = blk * Q_BLOCK + jnp.arange(Q_BLOCK)[:, None]
        s = jnp.where(key_pos <= q_pos, s, -jnp.inf)
        p = jax.nn.softmax(s, axis=-1)
        a = (p[:, :, 0] - lam * p[:, :, 1]).astype(v_t.dtype)
        return jnp.einsum('bhqk,bhkd->bhqd', a, v_t)

    o = lax.map(block, (q_b, jnp.arange(n_blocks)))
    o = o.transpose(1, 0, 3, 2, 4).reshape(B, S, H, 2 * Dh)
    o = (_rmsnorm(o, subln_gain).astype(jnp.float32) * (1.0 - lambda_init)).astype(h.dtype)
    return o.reshape(B, S, H * 2 * Dh) @ w_out


def _moe_swiglu(h, w_router, w_gate, w_up, w_down):
    logits = (h @ w_router).astype(jnp.float32)
    top_vals, top_idx = lax.top_k(logits, TOP_K)
    top_w = jax.nn.softmax(top_vals, axis=-1)
    gates = jnp.sum(jax.nn.one_hot(top_idx, N_EXPERTS, dtype=jnp.float32) * top_w[..., None], axis=-2)
    out = jnp.zeros_like(h)
    for e in range(N_EXPERTS):
        y = _swiglu(h, w_gate[e], w_up[e], w_down[e])
        out = out + gates[..., e:e + 1].astype(h.dtype) * y
    return out


def setup_inputs(seed: int = 0) -> dict:
    key = jax.random.key(seed)
    ks = iter(jax.random.split(key, 40))
    f32 = jnp.float32

    def w(shape, fan_in):
        return jax.random.normal(next(ks), shape, f32) * (fan_in ** -0.5)

    def gain(shape):
        return 1.0 + 0.02 * jax.random.normal(next(ks), shape, f32)

    x = jax.random.normal(next(ks), (BATCH, SEQ, D_MODEL), f32)
    offset = jax.random.randint(next(ks), (BATCH, 1), 0, 1024, dtype=jnp.int32)
    positions = offset + jnp.arange(SEQ, dtype=jnp.int32)[None, :]
    qkv_width = 3 * DA_HEADS * 2 * DA_HEAD_DIM
    return {
        "x": x,
        "positions": positions,
        "l0_mix_norm": gain((D_MODEL,)),
        "l0_sg_w_in": w((D_MODEL, 2 * SG_WIDTH), D_MODEL),
        "l0_sg_v_norm": gain((SG_WIDTH,)),
        "l0_sg_w_spatial": w((SG_GROUPS, SG_CHUNK, SG_CHUNK), SG_CHUNK),
        "l0_sg_b_spatial": gain((SG_GROUPS, SG_CHUNK)),
        "l0_sg_w_out": w((SG_WIDTH, D_MODEL), SG_WIDTH),
        "l0_ffn_norm": gain((D_MODEL,)),
        "l0_ffn_w_gate": w((D_MODEL, FFN_DENSE), D_MODEL),
        "l0_ffn_w_up": w((D_MODEL, FFN_DENSE), D_MODEL),
        "l0_ffn_w_down": w((FFN_DENSE, D_MODEL), FFN_DENSE),
        "l1_mix_norm": gain((D_MODEL,)),
        "l1_da_w_qkv": w((D_MODEL, qkv_width), D_MODEL),
        "l1_da_q_norm": gain((DA_HEAD_DIM,)),
        "l1_da_k_norm": gain((DA_HEAD_DIM,)),
        "l1_da_lambda_q1": 0.1 * jax.random.normal(next(ks), (DA_HEAD_DIM,), f32),
        "l1_da_lambda_k1": 0.1 * jax.random.normal(next(ks), (DA_HEAD_DIM,), f32),
        "l1_da_lambda_q2": 0.1 * jax.random.normal(next(ks), (DA_HEAD_DIM,), f32),
        "l1_da_lambda_k2": 0.1 * jax.random.normal(next(ks), (DA_HEAD_DIM,), f32),
        "l1_da_subln": gain((2 * DA_HEAD_DIM,)),
        "l1_da_w_out": w((DA_HEADS * 2 * DA_HEAD_DIM, D_MODEL), DA_HEADS * 2 * DA_HEAD_DIM),
        "l1_moe_norm": gain((D_MODEL,)),
        "l1_moe_w_router": w((D_MODEL, N_EXPERTS), D_MODEL),
        "l1_moe_w_gate": w((N_EXPERTS, D_MODEL, FFN_EXPERT), D_MODEL),
        "l1_moe_w_up": w((N_EXPERTS, D_MODEL, FFN_EXPERT), D_MODEL),
        "l1_moe_w_down": w((N_EXPERTS, FFN_EXPERT, D_MODEL), FFN_EXPERT),
    }


def reference(x, positions,
              l0_mix_norm, l0_sg_w_in, l0_sg_v_norm, l0_sg_w_spatial, l0_sg_b_spatial, l0_sg_w_out,
              l0_ffn_norm, l0_ffn_w_gate, l0_ffn_w_up, l0_ffn_w_down,
              l1_mix_norm, l1_da_w_qkv, l1_da_q_norm, l1_da_k_norm,
              l1_da_lambda_q1, l1_da_lambda_k1, l1_da_lambda_q2, l1_da_lambda_k2,
              l1_da_subln, l1_da_w_out,
              l1_moe_norm, l1_moe_w_router, l1_moe_w_gate, l1_moe_w_up, l1_moe_w_down):
    mixers = [
        (l0_mix_norm, (l0_sg_w_in, l0_sg_v_norm, l0_sg_w_spatial, l0_sg_b_spatial, l0_sg_w_out)),
        (l1_mix_norm, (l1_da_w_qkv, l1_da_q_norm, l1_da_k_norm, l1_da_lambda_q1, l1_da_lambda_k1,
                       l1_da_lambda_q2, l1_da_lambda_k2, l1_da_subln, l1_da_w_out)),
    ]
    channel = [
        (l0_ffn_norm, (l0_ffn_w_gate, l0_ffn_w_up, l0_ffn_w_down)),
        (l1_moe_norm, (l1_moe_w_router, l1_moe_w_gate, l1_moe_w_up, l1_moe_w_down)),
    ]
    h = x
    for i in range(DEPTH):
        norm_g, p = mixers[i]
        hn = _rmsnorm(h, norm_g)
        if i % 2 == 0:
            h = h + _spatial_gating_mixer(hn, *p)
        else:
            lambda_init = 0.8 - 0.6 * math.exp(-0.3 * i)
            h = h + _diff_attention(hn, positions, *p[:-1], p[-1], lambda_init)
        norm_g, p = channel[i]
        hn = _rmsnorm(h, norm_g)
        if i % 2 == 0:
            h = h + _swiglu(hn, *p)
        else:
            h = h + _moe_swiglu(hn, *p)
    return h
```

```python
import math
import os
from contextlib import ExitStack

import ml_dtypes
import numpy as np

import concourse.bass as bass
import concourse.mybir as mybir
from concourse.bass_utils import run_bass_kernel_spmd

F32 = mybir.dt.float32
BF16 = mybir.dt.bfloat16
I32 = mybir.dt.int32
U8 = mybir.dt.uint8
AF = mybir.ActivationFunctionType
ALU = mybir.AluOpType
AX = mybir.AxisListType

S = 4096
D = 1024
NT = S // 128
NST = S // 512
EPS = 1e-6
LAMBDA_INIT = 0.8 - 0.6 * math.exp(-0.3 * 1)
FFN = 2816
NF0 = FFN // 128
NE = 8
FE = 3584
SIZEOF = {F32: 4, BF16: 2, I32: 4, U8: 1}
COMPUTE = ("tensor", "vector", "scalar", "gpsimd")
ALLENG = COMPUTE + ("sync",)


class Op:
    __slots__ = ("eng", "fn", "deps", "signal", "ticket", "is_dma", "dkey", "dval", "strict")


class Prog:
    def __init__(self, nc):
        self.nc = nc
        self.ops = {e: [] for e in ALLENG}
        self.lw = {}
        self.rd = {}
        self.dcount = {}
        self.last = {e: None for e in ALLENG}
        self.last_dma = {}

    def add(self, eng, fn, r=(), w=(), dkey=None, strict=False):
        op = Op()
        op.strict = strict
        op.eng = eng
        op.fn = fn
        op.signal = False
        op.ticket = 0
        op.is_dma = dkey is not None
        op.dkey = dkey
        op.dval = 0
        deps = []
        for k in r:
            x = self.lw.get(k)
            if x is not None:
                deps.append(x)
        for k in w:
            x = self.lw.get(k)
            if x is not None:
                deps.append(x)
            rr = self.rd.get(k)
            if rr:
                deps.extend(rr[0].values())
                deps.extend(rr[1])
        self._set_deps(op, deps)
        if op.is_dma:
            c = self.dcount.get(dkey, 0) + 1
            self.dcount[dkey] = c
            op.dval = 16 * c
            self.last_dma[dkey] = op
        for k in r:
            rr = self.rd.setdefault(k, ({}, []))
            if op.is_dma:
                rr[1].append(op)
            else:
                rr[0][eng] = op
        for k in w:
            self.lw[k] = op
            self.rd[k] = ({}, [])
        self.ops[eng].append(op)
        if not op.is_dma:
            self.last[eng] = op
        return op

    def chain(self, eng, fns, r=(), w=()):
        self.nchain = getattr(self, "nchain", 0) + 1
        key = ("chain", self.nchain)
        op = None
        for i, fn in enumerate(fns):
            op = self.add(eng, fn, r=list(r) + [key], w=list(w) + [key], strict=(i > 0))
        return op

    def _set_deps(self, op, deps):
        seen = set()
        out = []
        for d in deps:
            if d is op or id(d) in seen:
                continue
            seen.add(id(d))
            if (not d.is_dma) and (not op.is_dma) and d.eng == op.eng and not op.strict:
                continue
            out.append(d)
            if not d.is_dma:
                d.signal = True
        op.deps = out

    def barrier(self):
        lasts = [self.last[e] for e in COMPUTE if self.last[e] is not None]
        dmas = list(self.last_dma.values())
        for e in ALLENG:
            op = Op()
            op.strict = False
            op.eng = e
            op.fn = None
            op.signal = False
            op.ticket = 0
            op.is_dma = False
            op.dkey = None
            op.dval = 0
            deps = [d for d in lasts if d.eng != e] + dmas
            for d in deps:
                if not d.is_dma:
                    d.signal = True
            op.deps = deps
            self.ops[e].append(op)
        self.lw = {}
        self.rd = {}

    def emit(self, es):
        nc = self.nc
        for e in ALLENG:
            c = 0
            for op in self.ops[e]:
                if op.is_dma:
                    continue
                if op.signal:
                    c += 1
                    op.ticket = c
        psem = {e: es.enter_context(nc.semaphore("pg_" + e)) for e in COMPUTE}
        dsem = {}
        for i, k in enumerate(self.dcount):
            dsem[k] = es.enter_context(nc.semaphore("dq_%d" % i))
        block = es.enter_context(nc.Block())

        def mk(engname):
            def body(eng):
                waited = {}
                for op in self.ops[engname]:
                    for d in op.deps:
                        if d.is_dma:
                            key = ("d", d.dkey)
                            sem = dsem[d.dkey]
                            val = d.dval
                        else:
                            key = ("p", d.eng)
                            sem = psem[d.eng]
                            val = d.ticket
                        if waited.get(key, 0) >= val:
                            continue
                        eng.wait_ge(sem, val)
                        waited[key] = val
                    if op.fn is None:
                        continue
                    ins = op.fn(eng)
                    if op.is_dma:
                        ins.then_inc(dsem[op.dkey], 16)
                    elif op.signal:
                        ins.then_inc(psem[engname], 1)
                if engname == "sync":
                    for k, c in self.dcount.items():
                        if waited.get(("d", k), 0) < 16 * c:
                            eng.wait_ge(dsem[k], 16 * c)
            return body

        block.tensor(mk("tensor"))
        block.vector(mk("vector"))
        block.scalar(mk("scalar"))
        block.gpsimd(mk("gpsimd"))
        block.sync(mk("sync"))


class Arena:
    def __init__(self, base_ap, nbytes):
        self.base = base_ap
        self.nbytes = nbytes
        self.off = 0

    def reset(self, off=0):
        self.off = off

    def alloc(self, shape, dt):
        n = SIZEOF[dt]
        for s in shape:
            n *= s
        start = (self.off + 63) // 64 * 64
        assert start + n <= self.nbytes, ("SBUF arena overflow", start + n, self.nbytes)
        self.off = start + n
        ap = self.base[:, start:start + n].bitcast(dt)
        if len(shape) == 2:
            ap = ap.rearrange("p (a b) -> p a b", a=shape[0])
        elif len(shape) == 3:
            ap = ap.rearrange("p (a b c) -> p a b c", a=shape[0], b=shape[1])
        return ap


class PsRing:
    def __init__(self, banks):
        self.banks = list(banks)
        self.i = 0

    def next(self):
        b = self.banks[self.i % len(self.banks)]
        self.i += 1
        return b


def build_program(stop_after="D"):
    nc = bass.Bass("TRN2", target_bir_lowering=False)
    es = ExitStack()

    def din(name, shape, dt=F32):
        return nc.dram_tensor(name, list(shape), dt, kind="ExternalInput").ap()

    specs = {
        "x": ([S, D], F32), "positions": ([S], I32),
        "l0_mix_norm": [D], "l0_sg_w_in": [D, 4096], "l0_sg_v_norm": [2048],
        "l0_sg_w_spatial": [8, 128, 128], "l0_sg_b_spatial": [8, 128], "l0_sg_w_out": [2048, D],
        "l0_ffn_norm": [D], "l0_ffn_w_gate": [D, FFN], "l0_ffn_w_up": [D, FFN], "l0_ffn_w_down": [FFN, D],
        "l1_mix_norm": [D], "l1_da_w_qkv": [D, 3072], "l1_da_q_norm": [64], "l1_da_k_norm": [64],
        "l1_da_lambda_q1": [64], "l1_da_lambda_k1": [64], "l1_da_lambda_q2": [64], "l1_da_lambda_k2": [64],
        "l1_da_subln": [128], "l1_da_w_out": [D, D],
        "l1_moe_norm": [D], "l1_moe_w_router": [D, 8], "l1_moe_w_gate": [8, D, FE], "l1_moe_w_up": [8, D, FE],
        "l1_moe_w_down": [8, FE, D],
        "c_ident_bf": ([128, 128], BF16), "c_ident_f": ([128, 128], F32), "c_tri": ([128, 128], F32),
        "c_invf": ([8], F32),
    }
    declared = {}

    class _W:
        def __getitem__(self, k):
            if k not in declared:
                sp = specs[k]
                if isinstance(sp, tuple):
                    declared[k] = din(k, sp[0], sp[1])
                else:
                    declared[k] = din(k, sp)
            return declared[k]
    w = _W()
    x_d = w["x"]
    ident_bf_d = w["c_ident_bf"]
    ident_f_d = w["c_ident_f"]
    tri_d = w["c_tri"]
    out_d = nc.dram_tensor("out", [S, D], F32, kind="ExternalOutput").ap()
    h1_d = nc.dram_tensor("h1_scr", [S, D], F32).ap()
    h2_d = nc.dram_tensor("h2_scr", [S, D], F32).ap()
    h3_d = nc.dram_tensor("h3_scr", [S, D], F32).ap()
    dbg_kind = "ExternalOutput" if os.environ.get("KDBG") else "Internal"
    qT_d = nc.dram_tensor("qT_scr", [8, 128, S], BF16, kind=dbg_kind).ap()
    kT_d = nc.dram_tensor("kT_scr", [8, 128, S], BF16, kind=dbg_kind).ap()
    v_d = nc.dram_tensor("v_scr", [S, 8, 128], BF16, kind=dbg_kind).ap()

    dbg_d = nc.dram_tensor("dbg", [128, 8, 512], F32, kind=dbg_kind).ap()
    ARENA_BYTES = 206 * 1024
    arena_t = es.enter_context(nc.sbuf_tensor("arena", [128, ARENA_BYTES], U8))
    ar = Arena(arena_t, ARENA_BYTES)
    ps_t = es.enter_context(nc.psum_tensor("psum", [128, 8, 512], F32))

    def psf(b):
        return ps_t[:, b, :]

    def psb(b):
        return ps_t[:, b, :].bitcast(BF16)

    P = Prog(nc)

    ident_bf = ar.alloc([128], BF16)
    ident_f = ar.alloc([128], F32)
    P.add("sync", lambda e: e.dma_start(out=ident_bf, in_=ident_bf_d[:, :]), w=["ident_bf"], dkey="k0")
    P.add("sync", lambda e: e.dma_start(out=ident_f, in_=ident_f_d[:, :]), w=["ident_f"], dkey="k1")
    neghalf = ar.alloc([64], F32)
    P.add("gpsimd", lambda e: e.memset(neghalf, -0.5), w=["neghalf"])
    CONST_END = ar.off

    def col_load(dst, src, n):
        def f(e):
            with nc.allow_non_contiguous_dma(reason="tiny gain vector"):
                return e.dma_start(out=dst, in_=src.rearrange("(k p) -> p k", p=128))
        return f

    def norm_p1(xt, xs, ss, rstd, tag, keys_r, xs32=None):
        def sq(e):
            return e.activation(out=xs["junk"], in_=xt, func=AF.Square, accum_out=ss)
        P.add("gpsimd", lambda e: e.memset(ss, 0.0), w=[("ss", tag)])
        P.add("scalar", sq, r=keys_r + [("ss", tag)], w=[("ss", tag), ("junk", tag)])

        P.chain("gpsimd", [
            lambda e: e.tensor_scalar(out=rstd, in0=ss, scalar1=1.0 / D, scalar2=EPS, op0=ALU.mult, op1=ALU.add),
            lambda e: e.tensor_tensor(out=rstd, in0=rstd, in1=neghalf[:, 0:1], op=ALU.pow),
        ], r=[("ss", tag)], w=[("rstd", tag)])
        if xs32 is None:
            P.add("vector", lambda e: e.tensor_scalar(out=xs["bf"], in0=xt, scalar1=rstd[:, 0:1], scalar2=None, op0=ALU.mult),
                  r=keys_r + [("rstd", tag)], w=[("xsbf", tag)])
        else:
            P.add("vector", lambda e: e.tensor_scalar(out=xs32, in0=xt, scalar1=rstd[:, 0:1], scalar2=None, op0=ALU.mult),
                  r=keys_r + [("rstd", tag)], w=[("xs32", tag)])
            P.add("gpsimd", lambda e: e.tensor_copy(out=xs["bf"], in_=xs32), r=[("xs32", tag)], w=[("xsbf", tag)])

    def norm_p2(gcol, xs, xnT_dst, tag, psT, keys_w):
        b = psT.next()

        def tr(e):
            last = None
            for k in range(8):
                last = e.transpose(psb(b)[:, k * 128:(k + 1) * 128], xs["bf"][:, k * 128:(k + 1) * 128], ident_bf)
            return last
        P.add("tensor", tr, r=[("xsbf", tag), "ident_bf"], w=[("ps", b)])

        def ev(e):
            return e.tensor_tensor(out=xnT_dst, in0=psb(b).rearrange("p (k t) -> p k t", k=8),
                                   in1=gcol.unsqueeze(2).to_broadcast([128, 8, 128]), op=ALU.mult)
        P.add("vector", ev, r=[("ps", b), "gcol"], w=keys_w)

    def norm_tile(xt, gcol, xs, ss, rstd, xnT_dst, tag, psT, keys_r, keys_w):
        norm_p1(xt, xs, ss, rstd, tag, keys_r)
        norm_p2(gcol, xs, xnT_dst, tag, psT, keys_w)

    dk_ctr = [0]

    def dk():
        dk_ctr[0] += 1
        return "c%d" % dk_ctr[0]

    def phase_A(src_d, dst_d):
        ar.reset(CONST_END)
        win = ar.alloc([8, 4096], BF16)
        wout = ar.alloc([16, 1024], BF16)
        wsp_f = ar.alloc([8, 128], F32)
        wmT = ar.alloc([8, 128], BF16)
        tri = ar.alloc([128], F32)
        biasT = ar.alloc([8, 128], F32)
        gcol = ar.alloc([8], F32)
        gv = ar.alloc([16], F32)
        xt_ring = [ar.alloc([1024], F32) for _ in range(2)]
        xr_ring = [ar.alloc([1024], F32) for _ in range(2)]
        xs_ring = [{"bf": ar.alloc([1024], BF16), "junk": None} for _ in range(2)]
        junk = ar.alloc([1024], BF16)
        for d_ in xs_ring:
            d_["junk"] = junk
        ss_r = [ar.alloc([1], F32) for _ in range(2)]
        rstd_r = [ar.alloc([1], F32) for _ in range(2)]
        xnT = ar.alloc([8, 512], BF16)
        uT = ar.alloc([16, 512], BF16)
        vbf = ar.alloc([4, 2048], BF16)
        ssv_r = [ar.alloc([4], F32) for _ in range(2)]
        rsv_r = [ar.alloc([1], F32) for _ in range(2)]
        wms_r = [ar.alloc([8, 128], BF16) for _ in range(4)]
        yT = ar.alloc([16, 512], BF16)
        tmp_r = [ar.alloc([512], F32) for _ in range(2)]

        w_in = w["l0_sg_w_in"].rearrange("(k p) n -> p k n", p=128)
        for k in range(8):
            P.add("gpsimd", (lambda k: lambda e: e.dma_start(out=win[:, k, :], in_=w_in[:, k, :]))(k),
                  w=[("win", k)], dkey="w%d" % k)
        w_o = w["l0_sg_w_out"].rearrange("(k p) n -> p k n", p=128)
        for q in range(4):
            P.add("gpsimd", (lambda q: lambda e: e.dma_start(out=wout[:, 4 * q:4 * q + 4, :], in_=w_o[:, 4 * q:4 * q + 4, :]))(q),
                  w=[("wout", q)], dkey="w%d" % (8 + q))
        P.add("sync", lambda e: e.dma_start(out=wsp_f, in_=w["l0_sg_w_spatial"].rearrange("g t s -> t g s")), w=["wsp_f"], dkey="c0")
        P.add("sync", lambda e: e.dma_start(out=tri, in_=tri_d[:, :]), w=["tri"], dkey="c1")
        P.add("sync", lambda e: e.dma_start(out=biasT.rearrange("p g t -> p (g t)"),
                                            in_=w["l0_sg_b_spatial"].rearrange("g t -> (g t)").partition_broadcast(128)),
              w=["biasT"], dkey="c2")
        P.add("sync", col_load(gcol, w["l0_mix_norm"], 8), w=["gcol"], dkey="c3")
        P.add("sync", col_load(gv, w["l0_sg_v_norm"], 16), w=["gv"], dkey="c4")

        bT = 0

        def wtr(e):
            last = None
            for g in range(8):
                last = e.transpose(ps_t[:, g // 4, (g % 4) * 128:(g % 4 + 1) * 128], wsp_f[:, g, :], ident_f)
            return last
        P.add("tensor", wtr, r=["wsp_f", "ident_f"], w=[("ps", 0), ("ps", 1)])

        def wmk(e):
            return e.tensor_tensor(out=wmT, in0=ps_t[:, 0:2, :].rearrange("p a (b t) -> p (a b) t", b=4),
                                   in1=tri.unsqueeze(1).to_broadcast([128, 8, 128]), op=ALU.mult)
        P.add("vector", wmk, r=[("ps", 0), ("ps", 1), "tri"], w=["wmT"])

        psT = PsRing([0, 1])
        psM = PsRing([2, 3, 4, 5, 6, 7])

        for st in range(NST):
            for c in range(4):
                T = st * 4 + c
                xt = xt_ring[T % 2]
                P.add("sync", (lambda xt, T: lambda e: e.dma_start(out=xt, in_=src_d[T * 128:(T + 1) * 128, :]))(xt, T),
                      w=[("xt", T % 2)], dkey="ld%d" % (T % 2))
                norm_tile(xt, gcol, xs_ring[T % 2], ss_r[T % 2], rstd_r[T % 2],
                          xnT[:, :, c * 128:(c + 1) * 128], ("A", T % 2), psT,
                          keys_r=[("xt", T % 2)], keys_w=[("xnT", c)])
            for m in range(16):
                b = psM.next()

                def mm(e, m=m, b=b):
                    last = None
                    for k in range(8):
                        last = e.matmul(psf(b), lhsT=win[:, k, m * 128:(m + 1) * 128], rhs=xnT[:, k, :],
                                        start=(k == 0), stop=(k == 7))
                    return last
                P.add("tensor", mm, r=[("win", k) for k in range(8)] + [("xnT", c) for c in range(4)], w=[("ps", b)])
                P.add("scalar", (lambda m, b: lambda e: e.activation(out=uT[:, m, :], in_=psf(b), func=AF.Gelu_apprx_tanh))(m, b),
                      r=[("ps", b)], w=[("uT", m)])
            for c in range(4):
                T = st * 4 + c
                ssv = ssv_r[T % 2]
                rsv = rsv_r[T % 2]
                P.add("gpsimd", (lambda ssv: lambda e: e.memset(ssv, 0.0))(ssv), w=[("ssv", T % 2)])
                for j in range(4):
                    b = psM.next()

                    def mmv(e, c=c, j=j, b=b):
                        last = None
                        for k in range(8):
                            last = e.matmul(psf(b), lhsT=xnT[:, k, c * 128:(c + 1) * 128],
                                            rhs=win[:, k, 2048 + j * 512:2048 + (j + 1) * 512],
                                            start=(k == 0), stop=(k == 7))
                        return last
                    P.add("tensor", mmv, r=[("win", k) for k in range(8)] + [("xnT", c)], w=[("ps", b)])
                    P.add("scalar", (lambda c, j, b: lambda e: e.activation(out=vbf[:, c, j * 512:(j + 1) * 512], in_=psf(b),
                                                                             func=AF.Gelu_apprx_tanh))(c, j, b),
                          r=[("ps", b)], w=[("vbf", c, j)])
                    P.add("scalar", (lambda c, j, ssv: lambda e: e.activation(out=junk[:, 0:512], in_=vbf[:, c, j * 512:(j + 1) * 512],
                                                                               func=AF.Square, accum_out=ssv[:, j:j + 1]))(c, j, ssv),
                          r=[("vbf", c, j), ("ssv", T % 2)], w=[("ssv", T % 2), ("junkA",)])

                P.chain("gpsimd", [
                    (lambda ssv, rsv: lambda e: e.tensor_tensor(out=rsv, in0=ssv[:, 0:1], in1=ssv[:, 1:2], op=ALU.add))(ssv, rsv),
                    (lambda ssv, rsv: lambda e: e.tensor_tensor(out=rsv, in0=rsv, in1=ssv[:, 2:3], op=ALU.add))(ssv, rsv),
                    (lambda ssv, rsv: lambda e: e.tensor_tensor(out=rsv, in0=rsv, in1=ssv[:, 3:4], op=ALU.add))(ssv, rsv),
                    (lambda ssv, rsv: lambda e: e.tensor_scalar(out=rsv, in0=rsv, scalar1=1.0 / 2048, scalar2=EPS, op0=ALU.mult, op1=ALU.add))(ssv, rsv),
                    (lambda ssv, rsv: lambda e: e.tensor_tensor(out=rsv, in0=rsv, in1=neghalf[:, 0:1], op=ALU.pow))(ssv, rsv),
                ], r=[("ssv", T % 2)], w=[("rsv", T % 2)])
                wms = wms_r[T % 4]
                P.add("gpsimd", (lambda wms, rsv: lambda e: e.tensor_scalar(out=wms, in0=wmT, scalar1=rsv[:, 0:1], scalar2=None,
                                                                             op0=ALU.mult))(wms, rsv),
                      r=["wmT", ("rsv", T % 2)], w=[("wms", T % 4)], strict=True)
            for m in range(16):
                g = m // 2
                b = psM.next()

                def mmx(e, m=m, g=g, b=b):
                    last = None
                    for c in range(4):
                        T = st * 4 + c
                        last = e.matmul(psf(b)[:, c * 128:(c + 1) * 128], lhsT=vbf[:, c, m * 128:(m + 1) * 128],
                                        rhs=wms_r[T % 4][:, g, :], start=True, stop=True)
                    return last
                P.add("tensor", mmx, r=[("vbf", c, m // 4) for c in range(4)] + [("wms", (st * 4 + c) % 4) for c in range(4)],
                      w=[("ps", b)])
                tmp = tmp_r[m % 2]

                def e1(e, m=m, g=g, b=b, tmp=tmp):
                    return e.scalar_tensor_tensor(out=tmp.rearrange("p (c t) -> p c t", c=4),
                                                  in0=psf(b).rearrange("p (c t) -> p c t", c=4),
                                                  scalar=gv[:, m:m + 1],
                                                  in1=biasT[:, g, :].unsqueeze(1).to_broadcast([128, 4, 128]),
                                                  op0=ALU.mult, op1=ALU.add)
                P.add("vector", e1, r=[("ps", b), "gv", "biasT"], w=[("tmp", m % 2)])
                P.add("vector", (lambda m, tmp: lambda e: e.tensor_tensor(out=yT[:, m, :], in0=tmp, in1=uT[:, m, :], op=ALU.mult))(m, tmp),
                      r=[("tmp", m % 2), ("uT", m)], w=[("yT", m)])
            for c in range(4):
                T = st * 4 + c
                xr = xr_ring[T % 2]
                P.add("sync", (lambda xr, T: lambda e: e.dma_start(out=xr, in_=src_d[T * 128:(T + 1) * 128, :]))(xr, T),
                      w=[("xr", T % 2)], dkey="lr%d" % (T % 2))
                for j in range(2):
                    b = psM.next()

                    def mmo(e, c=c, j=j, b=b):
                        last = None
                        for k in range(16):
                            last = e.matmul(psf(b), lhsT=yT[:, k, c * 128:(c + 1) * 128], rhs=wout[:, k, j * 512:(j + 1) * 512],
                                            start=(k == 0), stop=(k == 15))
                        return last
                    P.add("tensor", mmo, r=[("yT", m) for m in range(16)] + [("wout", q) for q in range(4)], w=[("ps", b)])
                    P.add("vector", (lambda xr, j, b: lambda e: e.tensor_tensor(out=xr[:, j * 512:(j + 1) * 512], in0=psf(b),
                                                                                 in1=xr[:, j * 512:(j + 1) * 512], op=ALU.add))(xr, j, b),
                          r=[("ps", b), ("xr", T % 2)], w=[("xr", T % 2)])
                P.add("sync", (lambda xr, T: lambda e: e.dma_start(out=dst_d[T * 128:(T + 1) * 128, :], in_=xr))(xr, T),
                      r=[("xr", T % 2)], w=[("hA", T)], dkey="st%d" % (T % 2))


    def phase_B(src_d, dst_d):
        ar.reset(CONST_END)
        dk_ctr[0] = 0
        wg = ar.alloc([8, FFN], BF16)
        wu = ar.alloc([8, FFN], BF16)
        wd = ar.alloc([NF0, 1024], BF16)
        gcol = ar.alloc([8], F32)
        xt_ring = [ar.alloc([1024], F32) for _ in range(2)]
        xr_ring = [ar.alloc([1024], F32) for _ in range(2)]
        junk = ar.alloc([1024], BF16)
        xs_ring = [{"bf": ar.alloc([1024], BF16), "junk": junk} for _ in range(4)]
        ss_r = [ar.alloc([1], F32) for _ in range(4)]
        rstd_r = [ar.alloc([1], F32) for _ in range(4)]
        xnT = ar.alloc([8, 512], BF16)
        HT = ar.alloc([NF0, 512], BF16)
        tmp_r = [ar.alloc([512], F32) for _ in range(2)]

        wgd = w["l0_ffn_w_gate"].rearrange("(k p) n -> p k n", p=128)
        wud = w["l0_ffn_w_up"].rearrange("(k p) n -> p k n", p=128)
        wdd = w["l0_ffn_w_down"].rearrange("(k p) n -> p k n", p=128)
        P.add("sync", col_load(gcol, w["l0_ffn_norm"], 8), w=["gcol"], dkey=dk())
        for k in range(8):
            P.add("gpsimd", (lambda k: lambda e: e.dma_start(out=wg[:, k, :], in_=wgd[:, k, :]))(k), w=[("wg", k)], dkey="w%d" % k)
            P.add("gpsimd", (lambda k: lambda e: e.dma_start(out=wu[:, k, :], in_=wud[:, k, :]))(k), w=[("wu", k)], dkey="w%d" % (8 + k))
        for q in range(2):
            P.add("gpsimd", (lambda q: lambda e: e.dma_start(out=wd[:, 11 * q:11 * q + 11, :], in_=wdd[:, 11 * q:11 * q + 11, :]))(q),
                  w=[("wd", q)], dkey="w%d" % (16 + q))
        psT = PsRing([0, 1])
        psM = PsRing([2, 3, 4, 5, 6, 7])

        def p1(st):
            for c in range(4):
                T = st * 4 + c
                xt = xt_ring[T % 2]
                P.add("sync", (lambda xt, T: lambda e: e.dma_start(out=xt, in_=src_d[T * 128:(T + 1) * 128, :]))(xt, T),
                      w=[("xt", T % 2)], dkey="ld%d" % (T % 2))
                norm_p1(xt, xs_ring[c], ss_r[c], rstd_r[c], ("B", c), [("xt", T % 2)])

        def p2(st):
            for c in range(4):
                norm_p2(gcol, xs_ring[c], xnT[:, :, c * 128:(c + 1) * 128], ("B", c), psT, [("xnT", c)])

        p1(0)
        for st in range(NST):
            p2(st)
            for m in range(NF0):
                bg = psM.next()
                bu = psM.next()

                def mmg(e, m=m, bg=bg):
                    last = None
                    for k in range(8):
                        last = e.matmul(psf(bg), lhsT=wg[:, k, m * 128:(m + 1) * 128], rhs=xnT[:, k, :], start=(k == 0), stop=(k == 7))
                    return last

                def mmu(e, m=m, bu=bu):
                    last = None
                    for k in range(8):
                        last = e.matmul(psf(bu), lhsT=wu[:, k, m * 128:(m + 1) * 128], rhs=xnT[:, k, :], start=(k == 0), stop=(k == 7))
                    return last
                xk = [("xnT", c) for c in range(4)]
                P.add("tensor", mmg, r=[("wg", k) for k in range(8)] + xk, w=[("ps", bg)])
                P.add("tensor", mmu, r=[("wu", k) for k in range(8)] + xk, w=[("ps", bu)])
                tmp = tmp_r[m % 2]
                P.add("scalar", (lambda tmp, bg: lambda e: e.activation(out=tmp, in_=psf(bg), func=AF.Silu))(tmp, bg),
                      r=[("ps", bg)], w=[("tmp", m % 2)])
                P.add("vector", (lambda tmp, bu, m: lambda e: e.tensor_tensor(out=HT[:, m, :], in0=psf(bu), in1=tmp, op=ALU.mult))(tmp, bu, m),
                      r=[("ps", bu), ("tmp", m % 2)], w=[("HT", m)])
            if st + 1 < NST:
                p1(st + 1)
            for c in range(4):
                T = st * 4 + c
                xr = xr_ring[T % 2]
                P.add("sync", (lambda xr, T: lambda e: e.dma_start(out=xr, in_=src_d[T * 128:(T + 1) * 128, :]))(xr, T),
                      w=[("xr", T % 2)], dkey="lr%d" % (T % 2))
                for j in range(2):
                    b = psM.next()

                    def mmd(e, c=c, j=j, b=b):
                        last = None
                        for k in range(NF0):
                            last = e.matmul(psf(b), lhsT=HT[:, k, c * 128:(c + 1) * 128], rhs=wd[:, k, j * 512:(j + 1) * 512],
                                            start=(k == 0), stop=(k == NF0 - 1))
                        return last
                    P.add("tensor", mmd, r=[("HT", m) for m in range(NF0)] + [("wd", 0), ("wd", 1)], w=[("ps", b)])
                    P.add("vector", (lambda xr, j, b: lambda e: e.tensor_tensor(out=xr[:, j * 512:(j + 1) * 512], in0=psf(b),
                                                                                 in1=xr[:, j * 512:(j + 1) * 512], op=ALU.add))(xr, j, b),
                          r=[("ps", b), ("xr", T % 2)], w=[("xr", T % 2)])
                P.add("sync", (lambda xr, T: lambda e: e.dma_start(out=dst_d[T * 128:(T + 1) * 128, :], in_=xr))(xr, T),
                      r=[("xr", T % 2)], w=[("hB", T)], dkey="st%d" % (T % 2))


    def phase_C(src_d, dst_d):
        ar.reset(CONST_END)
        dk_ctr[0] = 0
        wqkv = ar.alloc([8, 3072], BF16)
        gcol = ar.alloc([8], F32)
        gqk = ar.alloc([2, 64], F32)
        posi = ar.alloc([NT], I32)
        posf = ar.alloc([NT], F32)
        invf = ar.alloc([8], F32)
        ang = ar.alloc([NT, 8], F32)
        angk = ar.alloc([NT, 8], I32)
        angf = ar.alloc([NT, 8], F32)
        angm = ar.alloc([NT, 8], F32)
        sinT = ar.alloc([NT, 8], F32)
        cosT = ar.alloc([NT, 8], F32)
        xt_ring = [ar.alloc([1024], F32) for _ in range(2)]
        junk = ar.alloc([1024], BF16)
        xs_ring = [{"bf": ar.alloc([1024], BF16), "junk": junk} for _ in range(2)]
        ss_r = [ar.alloc([1], F32) for _ in range(2)]
        rstd_r = [ar.alloc([1], F32) for _ in range(2)]
        xnT_r = [ar.alloc([8, 128], BF16) for _ in range(2)]
        qk_r = [ar.alloc([32, 64], F32) for _ in range(2)]
        sq = ar.alloc([32, 64], F32)
        ssq_r = [ar.alloc([32], F32) for _ in range(2)]
        qkb_r = [ar.alloc([32, 64], BF16) for _ in range(2)]
        rt = [ar.alloc([32, 8], F32) for _ in range(4)]
        qT_st = [ar.alloc([8, 512], BF16) for _ in range(2)]
        kT_st = [ar.alloc([8, 512], BF16) for _ in range(2)]
        vb_r = [ar.alloc([1024], BF16) for _ in range(2)]

        wq_d = w["l1_da_w_qkv"].rearrange("(k p) n -> p k n", p=128)
        for k in range(8):
            P.add("gpsimd", (lambda k: lambda e: e.dma_start(out=wqkv[:, k, :], in_=wq_d[:, k, :]))(k), w=[("wqkv", k)], dkey="w%d" % k)
        P.add("sync", col_load(gcol, w["l1_mix_norm"], 8), w=["gcol"], dkey=dk())
        P.add("sync", lambda e: e.dma_start(out=gqk[:, 0, :], in_=w["l1_da_q_norm"].partition_broadcast(128)), w=["gqk0"], dkey=dk())
        P.add("sync", lambda e: e.dma_start(out=gqk[:, 1, :], in_=w["l1_da_k_norm"].partition_broadcast(128)), w=["gqk1"], dkey=dk())
        P.add("sync", lambda e: e.dma_start(out=invf, in_=w["c_invf"].partition_broadcast(128)), w=["invf"], dkey=dk())

        def posld(e):
            with nc.allow_non_contiguous_dma(reason="positions to token-major columns"):
                return e.dma_start(out=posi, in_=w["positions"].rearrange("(t p) -> p t", p=128))
        P.add("sync", posld, w=["posi"], dkey=dk())
        P.add("vector", lambda e: e.tensor_scalar(out=gqk[:, 0, :], in0=gqk[:, 0, :], scalar1=0.125, scalar2=None, op0=ALU.mult),
              r=["gqk0"], w=["gqk0"])

        TWO_PI = 2.0 * math.pi

        def trig(dst, shift, tagk):
            def f(e):
                a3 = ang.rearrange("p t i -> p (t i)")
                k3 = angk.rearrange("p t i -> p (t i)")
                f3 = angf.rearrange("p t i -> p (t i)")
                m3 = angm.rearrange("p t i -> p (t i)")
                e.tensor_scalar(out=f3, in0=a3, scalar1=shift, scalar2=None, op0=ALU.add)
                e.tensor_scalar(out=k3, in0=f3, scalar1=1.0 / TWO_PI, scalar2=None, op0=ALU.mult)
                e.tensor_copy(out=m3, in_=k3)
                e.scalar_tensor_tensor(out=f3, in0=m3, scalar=-TWO_PI, in1=f3, op0=ALU.mult, op1=ALU.add)
                e.tensor_scalar(out=m3, in0=f3, scalar1=math.pi, scalar2=-TWO_PI, op0=ALU.is_gt, op1=ALU.mult)
                e.tensor_tensor(out=f3, in0=f3, in1=m3, op=ALU.add)
                e.tensor_scalar(out=m3, in0=f3, scalar1=-math.pi, scalar2=TWO_PI, op0=ALU.is_lt, op1=ALU.mult)
                e.tensor_tensor(out=f3, in0=f3, in1=m3, op=ALU.add)
                return e.tensor_scalar(out=f3, in0=f3, scalar1=3.1415925, scalar2=-3.1415925, op0=ALU.min, op1=ALU.max)
            P.add("vector", f, r=["ang"], w=["angf"])
            P.add("scalar", lambda e: e.activation(out=dst.rearrange("p t i -> p (t i)"), in_=angf.rearrange("p t i -> p (t i)"), func=AF.Sin),
                  r=["angf"], w=[tagk])

        P.add("vector", lambda e: e.tensor_copy(out=posf, in_=posi), r=["posi"], w=["posf"])
        P.add("vector", lambda e: e.tensor_tensor(out=ang, in0=posf.unsqueeze(2).to_broadcast([128, NT, 8]),
                                                  in1=invf.unsqueeze(1).to_broadcast([128, NT, 8]), op=ALU.mult),
              r=["posf", "invf"], w=["ang"])
        trig(sinT, 0.0, "sinT")
        trig(cosT, 0.5 * math.pi, "cosT")

        psT = PsRing([0, 1])
        psQ = PsRing([2, 3])
        psM = PsRing([4, 5, 6, 7])
        qT_v = qT_d.rearrange("h p s -> p h s")
        kT_v = kT_d.rearrange("h p s -> p h s")
        v_v = v_d.rearrange("s h d -> s (h d)")

        for T in range(NT):
            st, c = T // 4, T % 4
            s2 = T % 2
            xt = xt_ring[s2]
            P.add("sync", (lambda xt, T: lambda e: e.dma_start(out=xt, in_=src_d[T * 128:(T + 1) * 128, :]))(xt, T),
                  w=[("xt", s2)], dkey="ld%d" % s2)
            xnT = xnT_r[s2]
            norm_tile(xt, gcol, xs_ring[s2], ss_r[s2], rstd_r[s2], xnT, ("C", s2), psT, [("xt", s2)], [("xnT", s2)])
            qk = qk_r[s2]
            qkf = qk.rearrange("p g d -> p (g d)")
            sqf = sq.rearrange("p g d -> p (g d)")
            vb = vb_r[s2]
            for jb in range(6):
                b = psM.next()

                def mm(e, jb=jb, b=b, xnT=xnT):
                    last = None
                    for k in range(8):
                        last = e.matmul(psf(b), lhsT=xnT[:, k, :], rhs=wqkv[:, k, jb * 512:(jb + 1) * 512], start=(k == 0), stop=(k == 7))
                    return last
                P.add("tensor", mm, r=[("wqkv", k) for k in range(8)] + [("xnT", s2)], w=[("ps", b)])
                if jb < 4:
                    P.add("scalar", (lambda jb, b, qkf: lambda e: e.activation(out=qkf[:, jb * 512:(jb + 1) * 512], in_=psf(b), func=AF.Copy))(jb, b, qkf),
                          r=[("ps", b)], w=[("qk", s2, jb)])
                    P.add("scalar", (lambda jb, b: lambda e: e.activation(out=sqf[:, jb * 512:(jb + 1) * 512], in_=psf(b), func=AF.Square))(jb, b),
                          r=[("ps", b)], w=[("sq", jb)])
                else:
                    P.add("scalar", (lambda jb, b, vb: lambda e: e.activation(out=vb[:, (jb - 4) * 512:(jb - 3) * 512], in_=psf(b), func=AF.Copy))(jb, b, vb),
                          r=[("ps", b)], w=[("vb", s2, jb)])
            ssq = ssq_r[s2]
            P.add("vector", (lambda ssq: lambda e: e.tensor_reduce(out=ssq, in_=sq, axis=AX.X, op=ALU.add))(ssq),
                  r=[("sq", j) for j in range(4)], w=[("ssq", s2)])

            P.chain("gpsimd", [
                (lambda ssq: lambda e: e.tensor_scalar(out=ssq, in0=ssq, scalar1=1.0 / 64, scalar2=EPS, op0=ALU.mult, op1=ALU.add))(ssq),
                (lambda ssq: lambda e: e.tensor_tensor(out=ssq, in0=ssq, in1=neghalf[:, 0:32], op=ALU.pow))(ssq),
            ], r=[("ssq", s2)], w=[("ssq", s2)])
            qkeys = [("qk", s2, j) for j in range(4)]
            P.add("vector", (lambda qk, ssq: lambda e: e.tensor_tensor(out=qk, in0=qk, in1=ssq.unsqueeze(2).to_broadcast([128, 32, 64]), op=ALU.mult))(qk, ssq),
                  r=qkeys + [("ssq", s2)], w=qkeys)

            def gmul(e, qk=qk):
                q4 = qk.rearrange("p (a g) d -> p a g d", a=2)
                return e.tensor_tensor(out=q4, in0=q4, in1=gqk.unsqueeze(2).to_broadcast([128, 2, 16, 64]), op=ALU.mult)
            P.add("gpsimd", gmul, r=qkeys + ["gqk0", "gqk1"], w=qkeys)
            qkb = qkb_r[s2]
            P.add("scalar", (lambda qkb, qkf: lambda e: e.activation(out=qkb.rearrange("p g d -> p (g d)"), in_=qkf, func=AF.Copy))(qkb, qkf),
                  r=qkeys, w=[("qkb", s2)])

            def rope(e, qk=qk, qkb=qkb, T=T):
                cs = cosT[:, T, :].unsqueeze(1).to_broadcast([128, 32, 8])
                sn = sinT[:, T, :].unsqueeze(1).to_broadcast([128, 32, 8])
                x1 = qk[:, :, 0:8]
                x2 = qk[:, :, 8:16]
                e.tensor_tensor(out=rt[0], in0=x1, in1=cs, op=ALU.mult)
                e.tensor_tensor(out=rt[1], in0=x2, in1=sn, op=ALU.mult)
                e.tensor_tensor(out=rt[2], in0=x2, in1=cs, op=ALU.mult)
                e.tensor_tensor(out=rt[3], in0=x1, in1=sn, op=ALU.mult)
                e.tensor_tensor(out=qkb[:, :, 0:8], in0=rt[0], in1=rt[1], op=ALU.subtract)
                return e.tensor_tensor(out=qkb[:, :, 8:16], in0=rt[2], in1=rt[3], op=ALU.add)
            P.add("gpsimd", rope, r=qkeys + [("qkb", s2), "sinT", "cosT"], w=[("qkb", s2)])
            bq = psQ.next()
            bk = psQ.next()

            def trq(e, qkb=qkb, bq=bq, bk=bk):
                last = None
                for h in range(8):
                    e.transpose(psb(bq)[:, h * 128:(h + 1) * 128], qkb[:, 2 * h:2 * h + 2, :].rearrange("p a d -> p (a d)"), ident_bf)
                    last = e.transpose(psb(bk)[:, h * 128:(h + 1) * 128], qkb[:, 16 + 2 * h:18 + 2 * h, :].rearrange("p a d -> p (a d)"), ident_bf)
                return last
            P.add("tensor", trq, r=[("qkb", s2), "ident_bf"], w=[("ps", bq), ("ps", bk)])
            qs = qT_st[st % 2]
            ks = kT_st[st % 2]
            P.add("scalar", (lambda qs, bq, c: lambda e: e.activation(out=qs[:, :, c * 128:(c + 1) * 128],
                                                                       in_=psb(bq).rearrange("p (h t) -> p h t", h=8), func=AF.Copy))(qs, bq, c),
                  r=[("ps", bq)], w=[("qs", st % 2, c)])
            P.add("vector", (lambda ks, bk, c: lambda e: e.tensor_copy(out=ks[:, :, c * 128:(c + 1) * 128],
                                                                        in_=psb(bk).rearrange("p (h t) -> p h t", h=8)))(ks, bk, c),
                  r=[("ps", bk)], w=[("ks", st % 2, c)])
            P.add("sync", (lambda vb, T: lambda e: e.dma_start(out=v_v[T * 128:(T + 1) * 128, :], in_=vb))(vb, T),
                  r=[("vb", s2, 4), ("vb", s2, 5)], w=[("v_d", T)], dkey="sv%d" % s2)
            if c == 3:
                P.add("sync", (lambda qs, st: lambda e: e.dma_start(out=qT_v[:, :, st * 512:(st + 1) * 512], in_=qs))(qs, st),
                      r=[("qs", st % 2, cc) for cc in range(4)], w=[("qT_d", st)], dkey="sq%d" % (st % 2))
                P.add("sync", (lambda ks, st: lambda e: e.dma_start(out=kT_v[:, :, st * 512:(st + 1) * 512], in_=ks))(ks, st),
                      r=[("ks", st % 2, cc) for cc in range(4)], w=[("kT_d", st)], dkey="sk%d" % (st % 2))
        P.barrier()

        ar.reset(CONST_END)
        dk_ctr[0] = 0
        OnT = ar.alloc([8, S], BF16)
        KT_r = [ar.alloc([S], BF16) for _ in range(2)]
        QT_r = [ar.alloc([S], BF16) for _ in range(2)]
        VA_r = [ar.alloc([NT, 132], BF16) for _ in range(2)]
        E_r = [ar.alloc([512], BF16) for _ in range(4)]
        tri_f = ar.alloc([128], F32)
        tri_b = ar.alloc([128], BF16)
        maskneg = tri_b
        lamv = ar.alloc([4, 64], F32)
        lamp = ar.alloc([2, 64], F32)
        lsum = [ar.alloc([1], F32) for _ in range(2)]
        lexp = [ar.alloc([1], F32) for _ in range(2)]
        nlam = ar.alloc([1], F32)
        gb = ar.alloc([2, 64], F32)
        mx = [ar.alloc([1], F32) for _ in range(2)]
        negB = ar.alloc([1], F32)
        sg = ar.alloc([1], F32)
        O_r = [ar.alloc([128], F32) for _ in range(4)]
        rr_r = [[ar.alloc([1], F32) for _ in range(4)] for _ in range(4)]
        Onb_r = [ar.alloc([128], BF16) for _ in range(2)]
        junkO = ar.alloc([128], BF16)
        wo = ar.alloc([8, 1024], BF16)
        xr_ring = [ar.alloc([1024], F32) for _ in range(2)]
        dbg_s = ar.alloc([8, 512], F32)
        P.add("gpsimd", lambda e: e.memset(dbg_s.rearrange("p a b -> p (a b)"), 0.0), w=["dbg%d" % z for z in range(7)])

        wo_d = w["l1_da_w_out"].rearrange("(k p) n -> p k n", p=128)
        P.add("gpsimd", lambda e: e.dma_start(out=wo, in_=wo_d), w=["wo"], dkey="w0")
        P.add("sync", lambda e: e.dma_start(out=tri_f, in_=tri_d[:, :]), w=["tri_f"], dkey=dk())
        P.add("vector", lambda e: e.tensor_scalar(out=tri_b, in0=tri_f, scalar1=-1.0, scalar2=30000.0, op0=ALU.add, op1=ALU.mult),
              r=["tri_f"], w=["maskneg"])
        for i, nm in enumerate(["l1_da_lambda_q1", "l1_da_lambda_q2", "l1_da_lambda_k1", "l1_da_lambda_k2"]):
            P.add("sync", (lambda i, nm: lambda e: e.dma_start(out=lamv[:, i, :], in_=w[nm].partition_broadcast(128)))(i, nm),
                  w=[("lamv", i)], dkey=dk())
        P.add("sync", lambda e: e.dma_start(out=gb[:, 0, :], in_=w["l1_da_q_norm"].partition_broadcast(128)), w=[("gb", 0)], dkey=dk())
        P.add("sync", lambda e: e.dma_start(out=gb[:, 1, :], in_=w["l1_da_k_norm"].partition_broadcast(128)), w=[("gb", 1)], dkey=dk())

        def sgld(e):
            with nc.allow_non_contiguous_dma(reason="tiny gain column"):
                return e.dma_start(out=sg, in_=w["l1_da_subln"].rearrange("(p o) -> p o", o=1))
        P.add("sync", sgld, w=["sg"], dkey=dk())
        P.add("vector", lambda e: e.tensor_scalar(out=sg, in0=sg, scalar1=1.0 - LAMBDA_INIT, scalar2=None, op0=ALU.mult), r=["sg"], w=["sg"])

        P.chain("vector", [
            lambda e: e.tensor_tensor(out=lamp[:, 0, :], in0=lamv[:, 0, :], in1=lamv[:, 2, :], op=ALU.mult),
            lambda e: e.tensor_tensor(out=lamp[:, 1, :], in0=lamv[:, 1, :], in1=lamv[:, 3, :], op=ALU.mult),
            lambda e: e.tensor_reduce(out=lsum[0], in_=lamp[:, 0, :], axis=AX.X, op=ALU.add),
            lambda e: e.tensor_reduce(out=lsum[1], in_=lamp[:, 1, :], axis=AX.X, op=ALU.add),
        ], r=[("lamv", i) for i in range(4)], w=["lsum"])
        def lexpf(e):
            e.activation(out=lexp[0], in_=lsum[0], func=AF.Exp)
            return e.activation(out=lexp[1], in_=lsum[1], func=AF.Exp)
        P.add("scalar", lexpf, r=["lsum"], w=["lexp"])

        P.chain("vector", [
            lambda e: e.tensor_tensor(out=nlam, in0=lexp[1], in1=lexp[0], op=ALU.subtract),
            lambda e: e.tensor_scalar(out=nlam, in0=nlam, scalar1=-LAMBDA_INIT, scalar2=None, op0=ALU.add),
        ], r=["lexp"], w=["nlam"])

        P.chain("vector", [
            lambda e: e.tensor_tensor(out=gb, in0=gb, in1=gb, op=ALU.mult),
            lambda e: e.tensor_reduce(out=mx[0], in_=gb[:, 0, :], axis=AX.X, op=ALU.max),
            lambda e: e.tensor_reduce(out=mx[1], in_=gb[:, 1, :], axis=AX.X, op=ALU.max),
            lambda e: e.tensor_tensor(out=mx[0], in0=mx[0], in1=mx[1], op=ALU.max),
            lambda e: e.tensor_scalar(out=negB, in0=mx[0], scalar1=1.0, scalar2=-8.0, op0=ALU.max, op1=ALU.mult),
        ], r=[("gb", 0), ("gb", 1)], w=["negB"])
        for s2 in range(2):
            P.add("gpsimd", (lambda s2: lambda e: e.memset(VA_r[s2].rearrange("p j d -> p (j d)"), 1.0))(s2), w=[("VA", s2)])

        psS = PsRing([4, 5, 6, 7])
        v_h = v_d.rearrange("(j p) h d -> p j h d", p=128)
        def head_loads(h):
            s2 = h % 2
            KT, QT, VA = KT_r[s2], QT_r[s2], VA_r[s2]
            P.add("sync", (lambda KT, h: lambda e: e.dma_start(out=KT, in_=kT_d[h]))(KT, h), w=[("KT", s2)], dkey="lk%d" % s2)
            P.add("sync", (lambda QT, h: lambda e: e.dma_start(out=QT, in_=qT_d[h]))(QT, h), w=[("QT", s2)], dkey="lq%d" % s2)
            P.add("sync", (lambda VA, h: lambda e: e.dma_start(out=VA[:, :, 0:128], in_=v_h[:, :, h, :]))(VA, h), w=[("VA", s2)], dkey="lv%d" % s2)

        items = [(h, Q, comp, j) for h in range(8) for Q in range(NST) for comp in range(2) for j in range(4 * Q + 4)]
        slot_of = {}
        ectr = [0]

        def front(it):
            h, Q, comp, j = it
            s2 = h % 2
            KT, QT = KT_r[s2], QT_r[s2]
            if Q == 0 and comp == 0 and j == 0:
                if h == 0:
                    head_loads(0)
                if h + 1 < 8:
                    head_loads(h + 1)
            lo_p, hi_p = comp * 64, comp * 64 + 64
            lo = max(0, j - 4 * Q)
            bS = psS.next()

            def smm(e):
                diag = j >= 4 * Q
                ins = e.matmul(psf(bS)[:, lo * 128:512], lhsT=KT[lo_p:hi_p, j * 128:(j + 1) * 128],
                               rhs=QT[lo_p:hi_p, Q * 512 + lo * 128:(Q + 1) * 512], start=True, stop=not diag)
                if diag:
                    ins = e.matmul(psf(bS)[:, lo * 128:(lo + 1) * 128], lhsT=ident_bf, rhs=maskneg, start=False, stop=True)
                return ins
            P.add("tensor", smm, r=[("KT", s2), ("QT", s2), "maskneg", "ident_bf"], w=[("ps", bS)])
            es_ = ectr[0] % 4
            ectr[0] += 1
            slot_of[it] = es_
            E = E_r[es_]
            P.add("scalar", lambda e: e.activation(out=E[:, lo * 128:512], in_=psf(bS)[:, lo * 128:512], func=AF.Exp, bias=negB[:, 0:1]),
                  r=[("ps", bS), "negB"], w=[("E", es_)])

        def back(it):
            h, Q, comp, j = it
            s2 = h % 2
            VA = VA_r[s2]
            lo = max(0, j - 4 * Q)
            es_ = slot_of.pop(it)
            E = E_r[es_]

            def av(e):
                last = None
                for i in range(lo, 4):
                    last = e.matmul(ps_t[:, i, 0:132], lhsT=E[:, i * 128:(i + 1) * 128], rhs=VA[:, j, :],
                                    start=(j == 0), stop=(j == 4 * Q + i))
                return last
            P.add("tensor", av, r=[("E", es_), ("VA", s2)], w=[("ps", i) for i in range(lo, 4)])
            if j != 4 * Q + 3:
                return
            if comp == 0:
                for i in range(4):
                    P.chain("vector", [
                        (lambda i: lambda e: e.reciprocal(out=rr_r[i][0], in_=ps_t[:, i, 128:129]))(i),
                        (lambda i: lambda e: e.tensor_scalar(out=O_r[i], in0=ps_t[:, i, 0:128], scalar1=rr_r[i][0], scalar2=None, op0=ALU.mult))(i),
                    ], r=[("ps", i)], w=[("O", i), ("rr", i)])
                return
            for i in range(4):
                T = 4 * Q + i
                o2 = i
                U2 = ps_t[:, i, 0:132]
                O, rr, Onb = O_r[o2], rr_r[o2], Onb_r[i % 2]
                P.chain("vector", [
                    (lambda U2, rr: lambda e: e.reciprocal(out=rr[1], in_=U2[:, 128:129]))(U2, rr),
                    (lambda rr: lambda e: e.tensor_tensor(out=rr[1], in0=rr[1], in1=nlam, op=ALU.mult))(rr),
                    (lambda U2, O, rr: lambda e: e.scalar_tensor_tensor(out=O, in0=U2[:, 0:128], scalar=rr[1], in1=O, op0=ALU.mult, op1=ALU.add))(U2, O, rr),
                ], r=[("ps", i), "nlam", ("O", o2), ("rr", o2)], w=[("O", o2), ("rr", o2)])
                P.add("gpsimd", (lambda rr: lambda e: e.memset(rr[2], 0.0))(rr), r=[("rr", o2)], w=[("rr2", o2)])
                P.add("scalar", (lambda O, rr: lambda e: e.activation(out=junkO, in_=O, func=AF.Square, accum_out=rr[2]))(O, rr),
                      r=[("O", o2), ("rr2", o2)], w=[("rr2", o2)])
                P.chain("gpsimd", [
                    (lambda rr: lambda e: e.tensor_scalar(out=rr[3], in0=rr[2], scalar1=1.0 / 128, scalar2=EPS, op0=ALU.mult, op1=ALU.add))(rr),
                    (lambda rr: lambda e: e.tensor_tensor(out=rr[3], in0=rr[3], in1=neghalf[:, 0:1], op=ALU.pow))(rr),
                ], r=[("rr2", o2)], w=[("rr3", o2)])
                P.add("vector", (lambda Onb, O, rr: lambda e: e.tensor_scalar(out=Onb, in0=O, scalar1=rr[3], scalar2=None, op0=ALU.mult))(Onb, O, rr),
                      r=[("O", o2), ("rr3", o2)], w=[("Onb", i % 2)])
                bt = psS.next()
                P.add("tensor", (lambda bt, Onb: lambda e: e.transpose(psb(bt)[:, 0:128], Onb, ident_bf))(bt, Onb),
                      r=[("Onb", i % 2), "ident_bf"], w=[("ps", bt)])
                P.add("vector", (lambda bt, h, T: lambda e: e.tensor_scalar(out=OnT[:, h, T * 128:(T + 1) * 128], in0=psb(bt)[:, 0:128],
                                                                             scalar1=sg[:, 0:1], scalar2=None, op0=ALU.mult))(bt, h, T),
                      r=[("ps", bt), "sg"], w=[("OnT", h, T)])

        LA = 2
        for n in range(len(items) + LA):
            if n < len(items):
                front(items[n])
            if n >= LA:
                back(items[n - LA])

        for T in range(NT):
            s2 = T % 2
            xr = xr_ring[s2]
            P.add("sync", (lambda xr, T: lambda e: e.dma_start(out=xr, in_=src_d[T * 128:(T + 1) * 128, :]))(xr, T),
                  w=[("xr", s2)], dkey="lr%d" % s2)
            for j in range(2):
                b = psS.next()

                def mmo(e, T=T, j=j, b=b):
                    last = None
                    for k in range(8):
                        last = e.matmul(psf(b), lhsT=OnT[:, k, T * 128:(T + 1) * 128], rhs=wo[:, k, j * 512:(j + 1) * 512],
                                        start=(k == 0), stop=(k == 7))
                    return last
                P.add("tensor", mmo, r=[("OnT", k, T) for k in range(8)] + ["wo"], w=[("ps", b)])
                P.add("vector", (lambda xr, j, b: lambda e: e.tensor_tensor(out=xr[:, j * 512:(j + 1) * 512], in0=psf(b),
                                                                             in1=xr[:, j * 512:(j + 1) * 512], op=ALU.add))(xr, j, b),
                      r=[("ps", b), ("xr", s2)], w=[("xr", s2)])
            P.add("sync", (lambda xr, T: lambda e: e.dma_start(out=dst_d[T * 128:(T + 1) * 128, :], in_=xr))(xr, T),
                  r=[("xr", s2)], w=[("hC", T)], dkey="st%d" % s2)


    def phase_D(src_d, dst_d):
        ar.reset(CONST_END)
        dk_ctr[0] = 0
        gcol = ar.alloc([8], F32)
        wr = ar.alloc([8, 8], F32)
        acc = ar.alloc([16, 1024], F32)
        xnT = ar.alloc([8, 2048], BF16)
        gates = ar.alloc([16, 8], F32)
        xs32_r = [ar.alloc([1024], F32) for _ in range(2)]
        junk = ar.alloc([1024], BF16)
        xs_ring = [{"bf": ar.alloc([1024], BF16), "junk": junk} for _ in range(2)]
        ss_r = [ar.alloc([1], F32) for _ in range(2)]
        rstd_r = [ar.alloc([1], F32) for _ in range(2)]
        xn32T = ar.alloc([8, 128], F32)
        lg = ar.alloc([8], F32)
        eq = ar.alloc([8], F32)
        l2 = ar.alloc([8], F32)
        sel = ar.alloc([8], F32)
        ex = ar.alloc([8], F32)
        m1 = ar.alloc([1], F32)
        m2 = ar.alloc([1], F32)
        nm1 = ar.alloc([1], F32)
        den = ar.alloc([1], F32)
        rden = ar.alloc([1], F32)
        wg_r = [ar.alloc([8, 512], BF16) for _ in range(2)]
        wu_r = [ar.alloc([8, 512], BF16) for _ in range(2)]
        wd_r = [ar.alloc([4, 1024], BF16) for _ in range(2)]
        HT_r = [ar.alloc([4, 512], BF16) for _ in range(2)]
        tmp_r = [ar.alloc([512], F32) for _ in range(2)]

        P.add("sync", col_load(gcol, w["l1_moe_norm"], 8), w=["gcol"], dkey=dk())
        P.add("sync", lambda e: e.dma_start(out=wr, in_=w["l1_moe_w_router"].rearrange("(k p) e -> p k e", p=128)), w=["wr"], dkey=dk())
        wg_d = w["l1_moe_w_gate"]
        wu_d = w["l1_moe_w_up"]
        wd_d = w["l1_moe_w_down"]
        psT = PsRing([0, 1])

        groups = [(hf, e_, fg) for hf in range(2) for e_ in range(NE) for fg in range(FE // 512)]

        def load_group(gi):
            hf, e_, fg = groups[gi]
            sl = gi % 2
            P.add("gpsimd", (lambda sl, e_, fg: lambda e: e.dma_start(
                out=wg_r[sl], in_=wg_d[e_].rearrange("(k p) f -> p k f", p=128)[:, :, fg * 512:(fg + 1) * 512]))(sl, e_, fg),
                w=[("wg", sl)], dkey="wg%d" % sl)
            P.add("gpsimd", (lambda sl, e_, fg: lambda e: e.dma_start(
                out=wu_r[sl], in_=wu_d[e_].rearrange("(k p) f -> p k f", p=128)[:, :, fg * 512:(fg + 1) * 512]))(sl, e_, fg),
                w=[("wu", sl)], dkey="wu%d" % sl)
            P.add("gpsimd", (lambda sl, e_, fg: lambda e: e.dma_start(
                out=wd_r[sl], in_=wd_d[e_][fg * 512:(fg + 1) * 512, :].rearrange("(k p) n -> p k n", p=128)))(sl, e_, fg),
                w=[("wd", sl)], dkey="wd%d" % sl)

        gi = 0
        hi = 0
        for hf in range(2):
            for t in range(16):
                T = hf * 16 + t
                s2 = t % 2
                at = acc[:, t, :]
                P.add("sync", (lambda at, T: lambda e: e.dma_start(out=at, in_=src_d[T * 128:(T + 1) * 128, :]))(at, T),
                      w=[("acc", t)], dkey="la%d" % t)
                xs32 = xs32_r[s2]
                norm_p1(at, xs_ring[s2], ss_r[s2], rstd_r[s2], ("D", s2), [("acc", t)], xs32=xs32)
                norm_p2(gcol, xs_ring[s2], xnT[:, :, t * 128:(t + 1) * 128], ("D", s2), psT, [("xnT", t)])

                def trf(e, xs32=xs32):
                    last = None
                    for k in range(8):
                        last = e.transpose(ps_t[:, 2 + k // 4, (k % 4) * 128:(k % 4 + 1) * 128], xs32[:, k * 128:(k + 1) * 128], ident_f)
                    return last
                P.add("tensor", trf, r=[("xs32", ("D", s2)), "ident_f"], w=[("ps", 2), ("ps", 3)])
                P.add("vector", lambda e: e.tensor_tensor(out=xn32T, in0=ps_t[:, 2:4, :].rearrange("p a (b t) -> p (a b) t", b=4),
                                                          in1=gcol.unsqueeze(2).to_broadcast([128, 8, 128]), op=ALU.mult),
                      r=[("ps", 2), ("ps", 3), "gcol"], w=["xn32T"])

                def lgm(e):
                    last = None
                    for k in range(8):
                        last = e.matmul(ps_t[:, 4, 0:8], lhsT=xn32T[:, k, :], rhs=wr[:, k, :], start=(k == 0), stop=(k == 7))
                    return last
                P.add("tensor", lgm, r=["xn32T", "wr"], w=[("ps", 4)])
                P.chain("vector", [
                    lambda e: e.tensor_copy(out=lg, in_=ps_t[:, 4, 0:8]),
                    lambda e: e.tensor_reduce(out=m1, in_=lg, axis=AX.X, op=ALU.max),
                    lambda e: e.tensor_scalar(out=eq, in0=lg, scalar1=m1[:, 0:1], scalar2=None, op0=ALU.is_equal),
                    lambda e: e.scalar_tensor_tensor(out=l2, in0=eq, scalar=-1.0e30, in1=lg, op0=ALU.mult, op1=ALU.add),
                    lambda e: e.tensor_reduce(out=m2, in_=l2, axis=AX.X, op=ALU.max),
                    lambda e: e.tensor_scalar(out=sel, in0=lg, scalar1=m2[:, 0:1], scalar2=None, op0=ALU.is_ge),
                    lambda e: e.tensor_scalar(out=nm1, in0=m1, scalar1=-1.0, scalar2=None, op0=ALU.mult),
                ], r=[("ps", 4)], w=["lg", "sel", "nm1"])
                P.add("scalar", lambda e: e.activation(out=ex, in_=lg, func=AF.Exp, bias=nm1[:, 0:1]), r=["lg", "nm1"], w=["ex"])
                P.chain("vector", [
                    lambda e: e.tensor_tensor(out=ex, in0=ex, in1=sel, op=ALU.mult),
                    lambda e: e.tensor_reduce(out=den, in_=ex, axis=AX.X, op=ALU.add),
                    lambda e: e.reciprocal(out=rden, in_=den),
                    (lambda t: lambda e: e.tensor_scalar(out=gates[:, t, :], in0=ex, scalar1=rden[:, 0:1], scalar2=None, op0=ALU.mult))(t),
                ], r=["ex", "sel"], w=[("gates", t), "ex"])

            psM = PsRing([0, 1, 2, 3, 4, 5, 6, 7])
            if hf == 0:
                load_group(0)
            for e_ in range(NE):
                for fg in range(FE // 512):
                    if gi + 1 < len(groups):
                        load_group(gi + 1)
                    sl = gi % 2
                    wg, wu, wd = wg_r[sl], wu_r[sl], wd_r[sl]
                    for st in range(4):
                        HT = HT_r[hi % 2]
                        hk = hi % 2
                        hi += 1
                        xk = [("xnT", st * 4 + c) for c in range(4)]
                        for fc in range(4):
                            bg = psM.next()
                            bu = psM.next()

                            def mmg(e, fc=fc, bg=bg, wg=wg, st=st):
                                last = None
                                for k in range(8):
                                    last = e.matmul(psf(bg), lhsT=wg[:, k, fc * 128:(fc + 1) * 128], rhs=xnT[:, k, st * 512:(st + 1) * 512],
                                                    start=(k == 0), stop=(k == 7))
                                return last

                            def mmu(e, fc=fc, bu=bu, wu=wu, st=st):
                                last = None
                                for k in range(8):
                                    last = e.matmul(psf(bu), lhsT=wu[:, k, fc * 128:(fc + 1) * 128], rhs=xnT[:, k, st * 512:(st + 1) * 512],
                                                    start=(k == 0), stop=(k == 7))
                                return last
                            P.add("tensor", mmg, r=[("wg", sl)] + xk, w=[("ps", bg)])
                            P.add("tensor", mmu, r=[("wu", sl)] + xk, w=[("ps", bu)])
                            tmp = tmp_r[fc % 2]
                            P.add("scalar", (lambda tmp, bg: lambda e: e.activation(out=tmp, in_=psf(bg), func=AF.Silu))(tmp, bg),
                                  r=[("ps", bg)], w=[("tmp", fc % 2)])
                            P.add("vector", (lambda tmp, bu, fc, HT: lambda e: e.tensor_tensor(out=HT[:, fc, :], in0=psf(bu), in1=tmp, op=ALU.mult))(tmp, bu, fc, HT),
                                  r=[("ps", bu), ("tmp", fc % 2)], w=[("HT", hk, fc)])
                        for c in range(4):
                            t = st * 4 + c
                            for j in range(2):
                                b = psM.next()

                                def mmd(e, c=c, j=j, b=b, HT=HT, wd=wd):
                                    last = None
                                    for k in range(4):
                                        last = e.matmul(psf(b), lhsT=HT[:, k, c * 128:(c + 1) * 128], rhs=wd[:, k, j * 512:(j + 1) * 512],
                                                        start=(k == 0), stop=(k == 3))
                                    return last
                                P.add("tensor", mmd, r=[("HT", hk, fc) for fc in range(4)] + [("wd", sl)], w=[("ps", b)])
                                P.add("vector", (lambda t, j, b, e_: lambda e: e.scalar_tensor_tensor(
                                    out=acc[:, t, j * 512:(j + 1) * 512], in0=psf(b), scalar=gates[:, t, e_:e_ + 1],
                                    in1=acc[:, t, j * 512:(j + 1) * 512], op0=ALU.mult, op1=ALU.add))(t, j, b, e_),
                                    r=[("ps", b), ("gates", t), ("acc", t)], w=[("acc", t)])
                    gi += 1
            for t in range(16):
                T = hf * 16 + t
                P.add("sync", (lambda t, T: lambda e: e.dma_start(out=dst_d[T * 128:(T + 1) * 128, :], in_=acc[:, t, :]))(t, T),
                      r=[("acc", t)], w=[("hD", T)], dkey="sa%d" % t)

    order = ["A", "B", "C", "D"]
    nph = order.index(stop_after) + 1
    srcs = [x_d, h1_d, h2_d, h3_d]
    dsts = [h1_d, h2_d, h3_d, out_d]
    dsts[nph - 1] = out_d
    fns = [phase_A, phase_B, phase_C, phase_D]
    for i in range(nph):
        fns[i](srcs[i], dsts[i])
        P.barrier()

    P.emit(es)
    es.close()
    return nc, list(declared.keys())


_CACHE = {}


def _consts():
    ident = np.eye(128, dtype=np.float32)
    tri = np.triu(np.ones((128, 128), dtype=np.float32))
    invf = (1.0 / (np.float32(500000.0) ** (np.arange(0, 16, 2, dtype=np.float32) / np.float32(16)))).astype(np.float32)
    return {
        "c_ident_bf": ident.astype(ml_dtypes.bfloat16),
        "c_ident_f": ident,
        "c_tri": tri,
        "c_invf": invf,
    }


def kernel(stop_after="D", **inputs):
    key = stop_after
    if key not in _CACHE:
        _CACHE[key] = build_program(stop_after)
    nc, used = _CACHE[key]
    consts = _consts()
    in_maps = []
    for b in range(8):
        m = {}
        for k, v in inputs.items():
            if k not in used:
                continue
            a = np.asarray(v)
            if k == "x":
                m[k] = np.ascontiguousarray(a[b])
            elif k == "positions":
                m[k] = np.ascontiguousarray(a[b]).astype(np.int32, copy=False)
            else:
                m[k] = np.ascontiguousarray(a)
        m.update({k: v for k, v in consts.items() if k in used})
        in_maps.append(m)
    res = run_bass_kernel_spmd(nc, in_maps, core_ids=list(range(8)))
    global _LAST
    _LAST = res
    return np.stack([np.asarray(r["out"]) for r in res.results], axis=0).astype(np.float32, copy=False)
```

```python
import math
import os
from contextlib import ExitStack

import ml_dtypes
import numpy as np

import concourse.bass as bass
import concourse.mybir as mybir
from concourse.bass_utils import run_bass_kernel_spmd

F32 = mybir.dt.float32
BF16 = mybir.dt.bfloat16
I32 = mybir.dt.int32
U8 = mybir.dt.uint8
AF = mybir.ActivationFunctionType
ALU = mybir.AluOpType
AX = mybir.AxisListType

S = 4096
D = 1024
NT = S // 128
NST = S // 512
EPS = 1e-6
LAMBDA_INIT = 0.8 - 0.6 * math.exp(-0.3 * 1)
FFN = 2816
NF0 = FFN // 128
NE = 8
FE = 3584
SIZEOF = {F32: 4, BF16: 2, I32: 4, U8: 1}
COMPUTE = ("tensor", "vector", "scalar", "gpsimd")
ALLENG = COMPUTE + ("sync",)


class Op:
    __slots__ = ("eng", "fn", "deps", "signal", "ticket", "is_dma", "dkey", "dval", "strict", "mark")


class Prog:
    def __init__(self, nc):
        self.nc = nc
        self.ops = {e: [] for e in ALLENG}
        self.lw = {}
        self.rd = {}
        self.dcount = {}
        self.last = {e: None for e in ALLENG}
        self.last_dma = {}

    def add(self, eng, fn, r=(), w=(), dkey=None, strict=False):
        op = Op()
        op.mark = None
        op.strict = strict
        op.eng = eng
        op.fn = fn
        op.signal = False
        op.ticket = 0
        op.is_dma = dkey is not None
        op.dkey = dkey
        op.dval = 0
        deps = []
        for k in r:
            x = self.lw.get(k)
            if x is not None:
                deps.append(x)
        for k in w:
            x = self.lw.get(k)
            if x is not None:
                deps.append(x)
            rr = self.rd.get(k)
            if rr:
                deps.extend(rr[0].values())
                deps.extend(rr[1])
        self._set_deps(op, deps)
        if op.is_dma:
            c = self.dcount.get(dkey, 0) + 1
            self.dcount[dkey] = c
            op.dval = 16 * c
            self.last_dma[dkey] = op
        for k in r:
            rr = self.rd.setdefault(k, ({}, []))
            if op.is_dma:
                rr[1].append(op)
            else:
                rr[0][eng] = op
        for k in w:
            self.lw[k] = op
            self.rd[k] = ({}, [])
        self.ops[eng].append(op)
        if not op.is_dma:
            self.last[eng] = op
        return op

    def _marker(self, mark):
        for e in ALLENG:
            op = Op()
            op.mark = mark
            op.strict = False
            op.eng = e
            op.fn = None
            op.signal = False
            op.ticket = 0
            op.is_dma = False
            op.dkey = None
            op.dval = 0
            op.deps = []
            self.ops[e].append(op)

    def regload(self, key, ap):
        self._marker(("regload", key, ap))

    def pred_begin(self, key, thr):
        self._marker(("begin", key, thr))

    def pred_end(self):
        self._marker(("end",))

    def chain(self, eng, fns, r=(), w=()):
        self.nchain = getattr(self, "nchain", 0) + 1
        key = ("chain", self.nchain)
        op = None
        for i, fn in enumerate(fns):
            op = self.add(eng, fn, r=list(r) + [key], w=list(w) + [key], strict=(i > 0))
        return op

    def _set_deps(self, op, deps):
        seen = set()
        out = []
        for d in deps:
            if d is op or id(d) in seen:
                continue
            seen.add(id(d))
            if (not d.is_dma) and (not op.is_dma) and d.eng == op.eng and not op.strict:
                continue
            out.append(d)
            if not d.is_dma:
                d.signal = True
        op.deps = out

    def barrier(self):
        lasts = [self.last[e] for e in COMPUTE if self.last[e] is not None]
        dmas = list(self.last_dma.values())
        for e in ALLENG:
            op = Op()
            op.mark = None
            op.strict = False
            op.eng = e
            op.fn = None
            op.signal = False
            op.ticket = 0
            op.is_dma = False
            op.dkey = None
            op.dval = 0
            deps = [d for d in lasts if d.eng != e] + dmas
            for d in deps:
                if not d.is_dma:
                    d.signal = True
            op.deps = deps
            self.ops[e].append(op)
        self.lw = {}
        self.rd = {}

    def emit(self, es):
        nc = self.nc
        for e in ALLENG:
            c = 0
            for op in self.ops[e]:
                if op.is_dma:
                    continue
                if op.signal:
                    c += 1
                    op.ticket = c
        psem = {e: es.enter_context(nc.semaphore("pg_" + e)) for e in COMPUTE}
        dsem = {}
        for i, k in enumerate(self.dcount):
            dsem[k] = es.enter_context(nc.semaphore("dq_%d" % i))
        block = es.enter_context(nc.Block())

        def mk(engname):
            def body(eng):
                waited = {}
                regs = {}

                def emit_op(op):
                    for d in op.deps:
                        if d.is_dma:
                            key = ("d", d.dkey)
                            sem = dsem[d.dkey]
                            val = d.dval
                        else:
                            key = ("p", d.eng)
                            sem = psem[d.eng]
                            val = d.ticket
                        if waited.get(key, 0) >= val:
                            continue
                        eng.wait_ge(sem, val)
                        waited[key] = val
                    if op.fn is None:
                        return
                    ins = op.fn(eng)
                    if op.is_dma:
                        ins.then_inc(dsem[op.dkey], 16)
                    elif op.signal:
                        ins.then_inc(psem[engname], 1)

                ops = self.ops[engname]
                i = 0
                n = len(ops)
                while i < n:
                    op = ops[i]
                    if op.mark is None:
                        emit_op(op)
                        i += 1
                        continue
                    kind = op.mark[0]
                    if kind == "regload":
                        _, key, ap = op.mark
                        if key not in regs:
                            regs[key] = eng.alloc_register("rg_%s_%s" % (engname, key))
                        eng.reg_load(regs[key], ap)
                        i += 1
                        continue
                    assert kind == "begin", kind
                    _, key, thr = op.mark
                    j = i + 1
                    while ops[j].mark is None or ops[j].mark[0] != "end":
                        assert ops[j].mark is None, "nested predicated regions are not supported"
                        j += 1
                    region = ops[i + 1:j]
                    nsig = sum(1 for o in region if (not o.is_dma) and o.signal)
                    dfirst = {}
                    dcnt = {}
                    for o in region:
                        if o.is_dma:
                            dfirst.setdefault(o.dkey, o.dval)
                            dcnt[o.dkey] = dcnt.get(o.dkey, 0) + 1
                    if region:
                        snap = dict(waited)
                        with eng.If_lt(regs[key], thr):
                            for k2, c2 in dcnt.items():
                                prior = dfirst[k2] - 16
                                if prior > 0:
                                    eng.wait_ge(dsem[k2], prior)
                                eng.sem_inc(dsem[k2], 16 * c2)
                            if nsig:
                                eng.drain().then_inc(psem[engname], nsig)
                        with eng.Else():
                            for o in region:
                                emit_op(o)
                        waited = snap
                    i = j + 1
                if engname == "sync":
                    for k, c in self.dcount.items():
                        if waited.get(("d", k), 0) < 16 * c:
                            eng.wait_ge(dsem[k], 16 * c)
            return body

        block.tensor(mk("tensor"))
        block.vector(mk("vector"))
        block.scalar(mk("scalar"))
        block.gpsimd(mk("gpsimd"))
        block.sync(mk("sync"))


class Arena:
    def __init__(self, base_ap, nbytes):
        self.base = base_ap
        self.nbytes = nbytes
        self.off = 0

    def reset(self, off=0):
        self.off = off

    def alloc(self, shape, dt):
        n = SIZEOF[dt]
        for s in shape:
            n *= s
        start = (self.off + 63) // 64 * 64
        assert start + n <= self.nbytes, ("SBUF arena overflow", start + n, self.nbytes)
        self.off = start + n
        ap = self.base[:, start:start + n].bitcast(dt)
        if len(shape) == 2:
            ap = ap.rearrange("p (a b) -> p a b", a=shape[0])
        elif len(shape) == 3:
            ap = ap.rearrange("p (a b c) -> p a b c", a=shape[0], b=shape[1])
        return ap


class PsRing:
    def __init__(self, banks):
        self.banks = list(banks)
        self.i = 0

    def next(self):
        b = self.banks[self.i % len(self.banks)]
        self.i += 1
        return b


def build_program(stop_after="D"):
    nc = bass.Bass("TRN2", target_bir_lowering=False)
    es = ExitStack()

    def din(name, shape, dt=F32):
        return nc.dram_tensor(name, list(shape), dt, kind="ExternalInput").ap()

    specs = {
        "x": ([S, D], F32), "positions": ([S], I32),
        "l0_mix_norm": [D], "l0_sg_w_in": [D, 4096], "l0_sg_v_norm": [2048],
        "l0_sg_w_spatial": [8, 128, 128], "l0_sg_b_spatial": [8, 128], "l0_sg_w_out": [2048, D],
        "l0_ffn_norm": [D], "l0_ffn_w_gate": [D, FFN], "l0_ffn_w_up": [D, FFN], "l0_ffn_w_down": [FFN, D],
        "l1_mix_norm": [D], "l1_da_w_qkv": [D, 3072], "l1_da_q_norm": [64], "l1_da_k_norm": [64],
        "l1_da_lambda_q1": [64], "l1_da_lambda_k1": [64], "l1_da_lambda_q2": [64], "l1_da_lambda_k2": [64],
        "l1_da_subln": [128], "l1_da_w_out": [D, D],
        "l1_moe_norm": [D], "l1_moe_w_router": [D, 8], "l1_moe_w_gate": [8, D, FE], "l1_moe_w_up": [8, D, FE],
        "l1_moe_w_down": [8, FE, D],
        "c_ident_bf": ([128, 128], BF16), "c_ident_f": ([128, 128], F32), "c_tri": ([128, 128], F32),
        "c_invf": ([8], F32), "c_tris": ([128, 128], F32), "c_eoff": ([8], F32),
    }
    declared = {}

    class _W:
        def __getitem__(self, k):
            if k not in declared:
                sp = specs[k]
                if isinstance(sp, tuple):
                    declared[k] = din(k, sp[0], sp[1])
                else:
                    declared[k] = din(k, sp)
            return declared[k]
    w = _W()
    x_d = w["x"]
    ident_bf_d = w["c_ident_bf"]
    ident_f_d = w["c_ident_f"]
    tri_d = w["c_tri"]
    out_d = nc.dram_tensor("out", [S, D], F32, kind="ExternalOutput").ap()
    h1_d = nc.dram_tensor("h1_scr", [S, D], F32).ap()
    h2_d = nc.dram_tensor("h2_scr", [S, D], F32).ap()
    h3_d = nc.dram_tensor("h3_scr", [S, D], F32).ap()
    dbg_kind = "ExternalOutput" if os.environ.get("KDBG") else "Internal"
    qT_d = nc.dram_tensor("qT_scr", [8, 128, S], BF16, kind=dbg_kind).ap()
    kT_d = nc.dram_tensor("kT_scr", [8, 128, S], BF16, kind=dbg_kind).ap()
    v_d = nc.dram_tensor("v_scr", [S, 8, 128], BF16, kind=dbg_kind).ap()

    NSLOT = 8 * S
    Xg_d = nc.dram_tensor("xg_scr", [NSLOT, D], BF16).ap()
    Yg_d = nc.dram_tensor("yg_scr", [NSLOT, D], F32).ap()
    cnt_d = nc.dram_tensor("cnt_scr", [1, 8], I32).ap()
    dbg_d = nc.dram_tensor("dbg", [128, 8, 512], F32, kind=dbg_kind).ap()
    ARENA_BYTES = 199 * 1024
    arena_t = es.enter_context(nc.sbuf_tensor("arena", [128, ARENA_BYTES], U8))
    ar = Arena(arena_t, ARENA_BYTES)
    ps_t = es.enter_context(nc.psum_tensor("psum", [128, 8, 512], F32))

    def psf(b):
        return ps_t[:, b, :]

    def psb(b):
        return ps_t[:, b, :].bitcast(BF16)

    P = Prog(nc)

    ident_bf = ar.alloc([128], BF16)
    ident_f = ar.alloc([128], F32)
    P.add("sync", lambda e: e.dma_start(out=ident_bf, in_=ident_bf_d[:, :]), w=["ident_bf"], dkey="k0")
    P.add("sync", lambda e: e.dma_start(out=ident_f, in_=ident_f_d[:, :]), w=["ident_f"], dkey="k1")
    neghalf = ar.alloc([64], F32)
    P.add("gpsimd", lambda e: e.memset(neghalf, -0.5), w=["neghalf"])
    CONST_END = ar.off

    def col_load(dst, src, n):
        def f(e):
            with nc.allow_non_contiguous_dma(reason="tiny gain vector"):
                return e.dma_start(out=dst, in_=src.rearrange("(k p) -> p k", p=128))
        return f

    def norm_p1(xt, xs, ss, rstd, tag, keys_r, xs32=None):
        def sq(e):
            return e.activation(out=xs["junk"], in_=xt, func=AF.Square, accum_out=ss)
        P.add("gpsimd", lambda e: e.memset(ss, 0.0), w=[("ss", tag)])
        P.add("scalar", sq, r=keys_r + [("ss", tag)], w=[("ss", tag), ("junk", tag)])

        P.chain("gpsimd", [
            lambda e: e.tensor_scalar(out=rstd, in0=ss, scalar1=1.0 / D, scalar2=EPS, op0=ALU.mult, op1=ALU.add),
            lambda e: e.tensor_tensor(out=rstd, in0=rstd, in1=neghalf[:, 0:1], op=ALU.pow),
        ], r=[("ss", tag)], w=[("rstd", tag)])
        if xs32 is None:
            P.add("vector", lambda e: e.tensor_scalar(out=xs["bf"], in0=xt, scalar1=rstd[:, 0:1], scalar2=None, op0=ALU.mult),
                  r=keys_r + [("rstd", tag)], w=[("xsbf", tag)])
        else:
            P.add("vector", lambda e: e.tensor_scalar(out=xs32, in0=xt, scalar1=rstd[:, 0:1], scalar2=None, op0=ALU.mult),
                  r=keys_r + [("rstd", tag)], w=[("xs32", tag)])
            P.add("gpsimd", lambda e: e.tensor_copy(out=xs["bf"], in_=xs32), r=[("xs32", tag)], w=[("xsbf", tag)])

    def norm_p2(gcol, xs, xnT_dst, tag, psT, keys_w):
        b = psT.next()

        def tr(e):
            last = None
            for k in range(8):
                last = e.transpose(psb(b)[:, k * 128:(k + 1) * 128], xs["bf"][:, k * 128:(k + 1) * 128], ident_bf)
            return last
        P.add("tensor", tr, r=[("xsbf", tag), "ident_bf"], w=[("ps", b)])

        def ev(e):
            return e.tensor_tensor(out=xnT_dst, in0=psb(b).rearrange("p (k t) -> p k t", k=8),
                                   in1=gcol.unsqueeze(2).to_broadcast([128, 8, 128]), op=ALU.mult)
        P.add("vector", ev, r=[("ps", b), "gcol"], w=keys_w)

    def norm_tile(xt, gcol, xs, ss, rstd, xnT_dst, tag, psT, keys_r, keys_w):
        norm_p1(xt, xs, ss, rstd, tag, keys_r)
        norm_p2(gcol, xs, xnT_dst, tag, psT, keys_w)

    dk_ctr = [0]

    def dk():
        dk_ctr[0] += 1
        return "c%d" % dk_ctr[0]

    def phase_A(src_d, dst_d):
        ar.reset(CONST_END)
        win = ar.alloc([8, 4096], BF16)
        wout = ar.alloc([16, 1024], BF16)
        wsp_f = ar.alloc([8, 128], F32)
        wmT = ar.alloc([8, 128], BF16)
        tri = ar.alloc([128], F32)
        biasT = ar.alloc([8, 128], F32)
        gcol = ar.alloc([8], F32)
        gv = ar.alloc([16], F32)
        xt_ring = [ar.alloc([1024], F32) for _ in range(2)]
        xr_ring = [ar.alloc([1024], F32) for _ in range(2)]
        xs_ring = [{"bf": ar.alloc([1024], BF16), "junk": None} for _ in range(2)]
        junk = ar.alloc([1024], BF16)
        for d_ in xs_ring:
            d_["junk"] = junk
        ss_r = [ar.alloc([1], F32) for _ in range(2)]
        rstd_r = [ar.alloc([1], F32) for _ in range(2)]
        xnT = ar.alloc([8, 512], BF16)
        uT = ar.alloc([16, 512], BF16)
        vbf = ar.alloc([4, 2048], BF16)
        ssv_r = [ar.alloc([4], F32) for _ in range(2)]
        rsv_r = [ar.alloc([1], F32) for _ in range(2)]
        wms_r = [ar.alloc([8, 128], BF16) for _ in range(4)]
        yT = ar.alloc([16, 512], BF16)
        tmp_r = [ar.alloc([512], F32) for _ in range(2)]

        w_in = w["l0_sg_w_in"].rearrange("(k p) n -> p k n", p=128)
        for k in range(8):
            P.add("gpsimd", (lambda k: lambda e: e.dma_start(out=win[:, k, :], in_=w_in[:, k, :]))(k),
                  w=[("win", k)], dkey="w%d" % k)
        w_o = w["l0_sg_w_out"].rearrange("(k p) n -> p k n", p=128)
        for q in range(4):
            P.add("gpsimd", (lambda q: lambda e: e.dma_start(out=wout[:, 4 * q:4 * q + 4, :], in_=w_o[:, 4 * q:4 * q + 4, :]))(q),
                  w=[("wout", q)], dkey="w%d" % (8 + q))
        P.add("sync", lambda e: e.dma_start(out=wsp_f, in_=w["l0_sg_w_spatial"].rearrange("g t s -> t g s")), w=["wsp_f"], dkey="c0")
        P.add("sync", lambda e: e.dma_start(out=tri, in_=tri_d[:, :]), w=["tri"], dkey="c1")
        P.add("sync", lambda e: e.dma_start(out=biasT.rearrange("p g t -> p (g t)"),
                                            in_=w["l0_sg_b_spatial"].rearrange("g t -> (g t)").partition_broadcast(128)),
              w=["biasT"], dkey="c2")
        P.add("sync", col_load(gcol, w["l0_mix_norm"], 8), w=["gcol"], dkey="c3")
        P.add("sync", col_load(gv, w["l0_sg_v_norm"], 16), w=["gv"], dkey="c4")

        bT = 0

        def wtr(e):
            last = None
            for g in range(8):
                last = e.transpose(ps_t[:, g // 4, (g % 4) * 128:(g % 4 + 1) * 128], wsp_f[:, g, :], ident_f)
            return last
        P.add("tensor", wtr, r=["wsp_f", "ident_f"], w=[("ps", 0), ("ps", 1)])

        def wmk(e):
            return e.tensor_tensor(out=wmT, in0=ps_t[:, 0:2, :].rearrange("p a (b t) -> p (a b) t", b=4),
                                   in1=tri.unsqueeze(1).to_broadcast([128, 8, 128]), op=ALU.mult)
        P.add("vector", wmk, r=[("ps", 0), ("ps", 1), "tri"], w=["wmT"])

        psT = PsRing([0, 1])
        psM = PsRing([2, 3, 4, 5, 6, 7])

        for st in range(NST):
            for c in range(4):
                T = st * 4 + c
                xt = xt_ring[T % 2]
                P.add("sync", (lambda xt, T: lambda e: e.dma_start(out=xt, in_=src_d[T * 128:(T + 1) * 128, :]))(xt, T),
                      w=[("xt", T % 2)], dkey="ld%d" % (T % 2))
                norm_tile(xt, gcol, xs_ring[T % 2], ss_r[T % 2], rstd_r[T % 2],
                          xnT[:, :, c * 128:(c + 1) * 128], ("A", T % 2), psT,
                          keys_r=[("xt", T % 2)], keys_w=[("xnT", c)])
            for m in range(16):
                b = psM.next()

                def mm(e, m=m, b=b):
                    last = None
                    for k in range(8):
                        last = e.matmul(psf(b), lhsT=win[:, k, m * 128:(m + 1) * 128], rhs=xnT[:, k, :],
                                        start=(k == 0), stop=(k == 7))
                    return last
                P.add("tensor", mm, r=[("win", k) for k in range(8)] + [("xnT", c) for c in range(4)], w=[("ps", b)])
                P.add("scalar", (lambda m, b: lambda e: e.activation(out=uT[:, m, :], in_=psf(b), func=AF.Gelu_apprx_tanh))(m, b),
                      r=[("ps", b)], w=[("uT", m)])
            for c in range(4):
                T = st * 4 + c
                ssv = ssv_r[T % 2]
                rsv = rsv_r[T % 2]
                P.add("gpsimd", (lambda ssv: lambda e: e.memset(ssv, 0.0))(ssv), w=[("ssv", T % 2)])
                for j in range(4):
                    b = psM.next()

                    def mmv(e, c=c, j=j, b=b):
                        last = None
                        for k in range(8):
                            last = e.matmul(psf(b), lhsT=xnT[:, k, c * 128:(c + 1) * 128],
                                            rhs=win[:, k, 2048 + j * 512:2048 + (j + 1) * 512],
                                            start=(k == 0), stop=(k == 7))
                        return last
                    P.add("tensor", mmv, r=[("win", k) for k in range(8)] + [("xnT", c)], w=[("ps", b)])
                    P.add("scalar", (lambda c, j, b: lambda e: e.activation(out=vbf[:, c, j * 512:(j + 1) * 512], in_=psf(b),
                                                                             func=AF.Gelu_apprx_tanh))(c, j, b),
                          r=[("ps", b)], w=[("vbf", c, j)])
                    P.add("scalar", (lambda c, j, ssv: lambda e: e.activation(out=junk[:, 0:512], in_=vbf[:, c, j * 512:(j + 1) * 512],
                                                                               func=AF.Square, accum_out=ssv[:, j:j + 1]))(c, j, ssv),
                          r=[("vbf", c, j), ("ssv", T % 2)], w=[("ssv", T % 2), ("junkA",)])

                P.chain("gpsimd", [
                    (lambda ssv, rsv: lambda e: e.tensor_tensor(out=rsv, in0=ssv[:, 0:1], in1=ssv[:, 1:2], op=ALU.add))(ssv, rsv),
                    (lambda ssv, rsv: lambda e: e.tensor_tensor(out=rsv, in0=rsv, in1=ssv[:, 2:3], op=ALU.add))(ssv, rsv),
                    (lambda ssv, rsv: lambda e: e.tensor_tensor(out=rsv, in0=rsv, in1=ssv[:, 3:4], op=ALU.add))(ssv, rsv),
                    (lambda ssv, rsv: lambda e: e.tensor_scalar(out=rsv, in0=rsv, scalar1=1.0 / 2048, scalar2=EPS, op0=ALU.mult, op1=ALU.add))(ssv, rsv),
                    (lambda ssv, rsv: lambda e: e.tensor_tensor(out=rsv, in0=rsv, in1=neghalf[:, 0:1], op=ALU.pow))(ssv, rsv),
                ], r=[("ssv", T % 2)], w=[("rsv", T % 2)])
                wms = wms_r[T % 4]
                P.add("gpsimd", (lambda wms, rsv: lambda e: e.tensor_scalar(out=wms, in0=wmT, scalar1=rsv[:, 0:1], scalar2=None,
                                                                             op0=ALU.mult))(wms, rsv),
                      r=["wmT", ("rsv", T % 2)], w=[("wms", T % 4)], strict=True)
            for m in range(16):
                g = m // 2
                b = psM.next()

                def mmx(e, m=m, g=g, b=b):
                    last = None
                    for c in range(4):
                        T = st * 4 + c
                        last = e.matmul(psf(b)[:, c * 128:(c + 1) * 128], lhsT=vbf[:, c, m * 128:(m + 1) * 128],
                                        rhs=wms_r[T % 4][:, g, :], start=True, stop=True)
                    return last
                P.add("tensor", mmx, r=[("vbf", c, m // 4) for c in range(4)] + [("wms", (st * 4 + c) % 4) for c in range(4)],
                      w=[("ps", b)])
                tmp = tmp_r[m % 2]

                def e1(e, m=m, g=g, b=b, tmp=tmp):
                    return e.scalar_tensor_tensor(out=tmp.rearrange("p (c t) -> p c t", c=4),
                                                  in0=psf(b).rearrange("p (c t) -> p c t", c=4),
                                                  scalar=gv[:, m:m + 1],
                                                  in1=biasT[:, g, :].unsqueeze(1).to_broadcast([128, 4, 128]),
                                                  op0=ALU.mult, op1=ALU.add)
                P.add("vector", e1, r=[("ps", b), "gv", "biasT"], w=[("tmp", m % 2)])
                P.add("vector", (lambda m, tmp: lambda e: e.tensor_tensor(out=yT[:, m, :], in0=tmp, in1=uT[:, m, :], op=ALU.mult))(m, tmp),
                      r=[("tmp", m % 2), ("uT", m)], w=[("yT", m)])
            for c in range(4):
                T = st * 4 + c
                xr = xr_ring[T % 2]
                P.add("sync", (lambda xr, T: lambda e: e.dma_start(out=xr, in_=src_d[T * 128:(T + 1) * 128, :]))(xr, T),
                      w=[("xr", T % 2)], dkey="lr%d" % (T % 2))
                for j in range(2):
                    b = psM.next()

                    def mmo(e, c=c, j=j, b=b):
                        last = None
                        for k in range(16):
                            last = e.matmul(psf(b), lhsT=yT[:, k, c * 128:(c + 1) * 128], rhs=wout[:, k, j * 512:(j + 1) * 512],
                                            start=(k == 0), stop=(k == 15))
                        return last
                    P.add("tensor", mmo, r=[("yT", m) for m in range(16)] + [("wout", q) for q in range(4)], w=[("ps", b)])
                    P.add("vector", (lambda xr, j, b: lambda e: e.tensor_tensor(out=xr[:, j * 512:(j + 1) * 512], in0=psf(b),
                                                                                 in1=xr[:, j * 512:(j + 1) * 512], op=ALU.add))(xr, j, b),
                          r=[("ps", b), ("xr", T % 2)], w=[("xr", T % 2)])
                P.add("sync", (lambda xr, T: lambda e: e.dma_start(out=dst_d[T * 128:(T + 1) * 128, :], in_=xr))(xr, T),
                      r=[("xr", T % 2)], w=[("hA", T)], dkey="st%d" % (T % 2))


    def phase_B(src_d, dst_d):
        ar.reset(CONST_END)
        dk_ctr[0] = 0
        wg = ar.alloc([8, FFN], BF16)
        wu = ar.alloc([8, FFN], BF16)
        wd = ar.alloc([NF0, 1024], BF16)
        gcol = ar.alloc([8], F32)
        xt_ring = [ar.alloc([1024], F32) for _ in range(2)]
        xr_ring = [ar.alloc([1024], F32) for _ in range(2)]
        junk = ar.alloc([1024], BF16)
        xs_ring = [{"bf": ar.alloc([1024], BF16), "junk": junk} for _ in range(4)]
        ss_r = [ar.alloc([1], F32) for _ in range(4)]
        rstd_r = [ar.alloc([1], F32) for _ in range(4)]
        xnT = ar.alloc([8, 512], BF16)
        HT = ar.alloc([NF0, 512], BF16)
        tmp_r = [ar.alloc([512], F32) for _ in range(2)]

        wgd = w["l0_ffn_w_gate"].rearrange("(k p) n -> p k n", p=128)
        wud = w["l0_ffn_w_up"].rearrange("(k p) n -> p k n", p=128)
        wdd = w["l0_ffn_w_down"].rearrange("(k p) n -> p k n", p=128)
        P.add("sync", col_load(gcol, w["l0_ffn_norm"], 8), w=["gcol"], dkey=dk())
        for k in range(8):
            P.add("gpsimd", (lambda k: lambda e: e.dma_start(out=wg[:, k, :], in_=wgd[:, k, :]))(k), w=[("wg", k)], dkey="w%d" % k)
            P.add("gpsimd", (lambda k: lambda e: e.dma_start(out=wu[:, k, :], in_=wud[:, k, :]))(k), w=[("wu", k)], dkey="w%d" % (8 + k))
        for q in range(2):
            P.add("gpsimd", (lambda q: lambda e: e.dma_start(out=wd[:, 11 * q:11 * q + 11, :], in_=wdd[:, 11 * q:11 * q + 11, :]))(q),
                  w=[("wd", q)], dkey="w%d" % (16 + q))
        psT = PsRing([0, 1])
        psM = PsRing([2, 3, 4, 5, 6, 7])

        def p1(st):
            for c in range(4):
                T = st * 4 + c
                xt = xt_ring[T % 2]
                P.add("sync", (lambda xt, T: lambda e: e.dma_start(out=xt, in_=src_d[T * 128:(T + 1) * 128, :]))(xt, T),
                      w=[("xt", T % 2)], dkey="ld%d" % (T % 2))
                norm_p1(xt, xs_ring[c], ss_r[c], rstd_r[c], ("B", c), [("xt", T % 2)])

        def p2(st):
            for c in range(4):
                norm_p2(gcol, xs_ring[c], xnT[:, :, c * 128:(c + 1) * 128], ("B", c), psT, [("xnT", c)])

        p1(0)
        for st in range(NST):
            p2(st)
            for m in range(NF0):
                bg = psM.next()
                bu = psM.next()

                def mmg(e, m=m, bg=bg):
                    last = None
                    for k in range(8):
                        last = e.matmul(psf(bg), lhsT=wg[:, k, m * 128:(m + 1) * 128], rhs=xnT[:, k, :], start=(k == 0), stop=(k == 7))
                    return last

                def mmu(e, m=m, bu=bu):
                    last = None
                    for k in range(8):
                        last = e.matmul(psf(bu), lhsT=wu[:, k, m * 128:(m + 1) * 128], rhs=xnT[:, k, :], start=(k == 0), stop=(k == 7))
                    return last
                xk = [("xnT", c) for c in range(4)]
                P.add("tensor", mmg, r=[("wg", k) for k in range(8)] + xk, w=[("ps", bg)])
                P.add("tensor", mmu, r=[("wu", k) for k in range(8)] + xk, w=[("ps", bu)])
                tmp = tmp_r[m % 2]
                P.add("scalar", (lambda tmp, bg: lambda e: e.activation(out=tmp, in_=psf(bg), func=AF.Silu))(tmp, bg),
                      r=[("ps", bg)], w=[("tmp", m % 2)])
                P.add("vector", (lambda tmp, bu, m: lambda e: e.tensor_tensor(out=HT[:, m, :], in0=psf(bu), in1=tmp, op=ALU.mult))(tmp, bu, m),
                      r=[("ps", bu), ("tmp", m % 2)], w=[("HT", m)])
            if st + 1 < NST:
                p1(st + 1)
            for c in range(4):
                T = st * 4 + c
                xr = xr_ring[T % 2]
                P.add("sync", (lambda xr, T: lambda e: e.dma_start(out=xr, in_=src_d[T * 128:(T + 1) * 128, :]))(xr, T),
                      w=[("xr", T % 2)], dkey="lr%d" % (T % 2))
                for j in range(2):
                    b = psM.next()

                    def mmd(e, c=c, j=j, b=b):
                        last = None
                        for k in range(NF0):
                            last = e.matmul(psf(b), lhsT=HT[:, k, c * 128:(c + 1) * 128], rhs=wd[:, k, j * 512:(j + 1) * 512],
                                            start=(k == 0), stop=(k == NF0 - 1))
                        return last
                    P.add("tensor", mmd, r=[("HT", m) for m in range(NF0)] + [("wd", 0), ("wd", 1)], w=[("ps", b)])
                    P.add("vector", (lambda xr, j, b: lambda e: e.tensor_tensor(out=xr[:, j * 512:(j + 1) * 512], in0=psf(b),
                                                                                 in1=xr[:, j * 512:(j + 1) * 512], op=ALU.add))(xr, j, b),
                          r=[("ps", b), ("xr", T % 2)], w=[("xr", T % 2)])
                P.add("sync", (lambda xr, T: lambda e: e.dma_start(out=dst_d[T * 128:(T + 1) * 128, :], in_=xr))(xr, T),
                      r=[("xr", T % 2)], w=[("hB", T)], dkey="st%d" % (T % 2))


    def phase_C(src_d, dst_d):
        ar.reset(CONST_END)
        dk_ctr[0] = 0
        wqkv = ar.alloc([8, 3072], BF16)
        gcol = ar.alloc([8], F32)
        gqk = ar.alloc([2, 64], F32)
        posi = ar.alloc([NT], I32)
        posf = ar.alloc([NT], F32)
        invf = ar.alloc([8], F32)
        ang = ar.alloc([NT, 8], F32)
        angk = ar.alloc([NT, 8], I32)
        angf = ar.alloc([NT, 8], F32)
        angm = ar.alloc([NT, 8], F32)
        sinT = ar.alloc([NT, 8], F32)
        cosT = ar.alloc([NT, 8], F32)
        xt_ring = [ar.alloc([1024], F32) for _ in range(2)]
        junk = ar.alloc([1024], BF16)
        xs_ring = [{"bf": ar.alloc([1024], BF16), "junk": junk} for _ in range(2)]
        ss_r = [ar.alloc([1], F32) for _ in range(2)]
        rstd_r = [ar.alloc([1], F32) for _ in range(2)]
        xnT_r = [ar.alloc([8, 128], BF16) for _ in range(2)]
        qk_r = [ar.alloc([32, 64], F32) for _ in range(2)]
        sq = ar.alloc([32, 64], F32)
        ssq_r = [ar.alloc([32], F32) for _ in range(2)]
        qkb_r = [ar.alloc([32, 64], BF16) for _ in range(2)]
        rt = [ar.alloc([32, 8], F32) for _ in range(4)]
        qT_st = [ar.alloc([8, 512], BF16) for _ in range(2)]
        kT_st = [ar.alloc([8, 512], BF16) for _ in range(2)]
        vb_r = [ar.alloc([1024], BF16) for _ in range(2)]

        wq_d = w["l1_da_w_qkv"].rearrange("(k p) n -> p k n", p=128)
        for k in range(8):
            P.add("gpsimd", (lambda k: lambda e: e.dma_start(out=wqkv[:, k, :], in_=wq_d[:, k, :]))(k), w=[("wqkv", k)], dkey="w%d" % k)
        P.add("sync", col_load(gcol, w["l1_mix_norm"], 8), w=["gcol"], dkey=dk())
        P.add("sync", lambda e: e.dma_start(out=gqk[:, 0, :], in_=w["l1_da_q_norm"].partition_broadcast(128)), w=["gqk0"], dkey=dk())
        P.add("sync", lambda e: e.dma_start(out=gqk[:, 1, :], in_=w["l1_da_k_norm"].partition_broadcast(128)), w=["gqk1"], dkey=dk())
        P.add("sync", lambda e: e.dma_start(out=invf, in_=w["c_invf"].partition_broadcast(128)), w=["invf"], dkey=dk())

        def posld(e):
            with nc.allow_non_contiguous_dma(reason="positions to token-major columns"):
                return e.dma_start(out=posi, in_=w["positions"].rearrange("(t p) -> p t", p=128))
        P.add("sync", posld, w=["posi"], dkey=dk())
        P.add("vector", lambda e: e.tensor_scalar(out=gqk[:, 0, :], in0=gqk[:, 0, :], scalar1=0.125, scalar2=None, op0=ALU.mult),
              r=["gqk0"], w=["gqk0"])

        TWO_PI = 2.0 * math.pi

        def trig(dst, shift, tagk):
            def f(e):
                a3 = ang.rearrange("p t i -> p (t i)")
                k3 = angk.rearrange("p t i -> p (t i)")
                f3 = angf.rearrange("p t i -> p (t i)")
                m3 = angm.rearrange("p t i -> p (t i)")
                e.tensor_scalar(out=f3, in0=a3, scalar1=shift, scalar2=None, op0=ALU.add)
                e.tensor_scalar(out=k3, in0=f3, scalar1=1.0 / TWO_PI, scalar2=None, op0=ALU.mult)
                e.tensor_copy(out=m3, in_=k3)
                e.scalar_tensor_tensor(out=f3, in0=m3, scalar=-TWO_PI, in1=f3, op0=ALU.mult, op1=ALU.add)
                e.tensor_scalar(out=m3, in0=f3, scalar1=math.pi, scalar2=-TWO_PI, op0=ALU.is_gt, op1=ALU.mult)
                e.tensor_tensor(out=f3, in0=f3, in1=m3, op=ALU.add)
                e.tensor_scalar(out=m3, in0=f3, scalar1=-math.pi, scalar2=TWO_PI, op0=ALU.is_lt, op1=ALU.mult)
                e.tensor_tensor(out=f3, in0=f3, in1=m3, op=ALU.add)
                return e.tensor_scalar(out=f3, in0=f3, scalar1=3.1415925, scalar2=-3.1415925, op0=ALU.min, op1=ALU.max)
            P.add("vector", f, r=["ang"], w=["angf"])
            P.add("scalar", lambda e: e.activation(out=dst.rearrange("p t i -> p (t i)"), in_=angf.rearrange("p t i -> p (t i)"), func=AF.Sin),
                  r=["angf"], w=[tagk])

        P.add("vector", lambda e: e.tensor_copy(out=posf, in_=posi), r=["posi"], w=["posf"])
        P.add("vector", lambda e: e.tensor_tensor(out=ang, in0=posf.unsqueeze(2).to_broadcast([128, NT, 8]),
                                                  in1=invf.unsqueeze(1).to_broadcast([128, NT, 8]), op=ALU.mult),
              r=["posf", "invf"], w=["ang"])
        trig(sinT, 0.0, "sinT")
        trig(cosT, 0.5 * math.pi, "cosT")

        psT = PsRing([0, 1])
        psQ = PsRing([2, 3])
        psM = PsRing([4, 5, 6, 7])
        qT_v = qT_d.rearrange("h p s -> p h s")
        kT_v = kT_d.rearrange("h p s -> p h s")
        v_v = v_d.rearrange("s h d -> s (h d)")

        for T in range(NT):
            st, c = T // 4, T % 4
            s2 = T % 2
            xt = xt_ring[s2]
            P.add("sync", (lambda xt, T: lambda e: e.dma_start(out=xt, in_=src_d[T * 128:(T + 1) * 128, :]))(xt, T),
                  w=[("xt", s2)], dkey="ld%d" % s2)
            xnT = xnT_r[s2]
            norm_tile(xt, gcol, xs_ring[s2], ss_r[s2], rstd_r[s2], xnT, ("C", s2), psT, [("xt", s2)], [("xnT", s2)])
            qk = qk_r[s2]
            qkf = qk.rearrange("p g d -> p (g d)")
            sqf = sq.rearrange("p g d -> p (g d)")
            vb = vb_r[s2]
            for jb in range(6):
                b = psM.next()

                def mm(e, jb=jb, b=b, xnT=xnT):
                    last = None
                    for k in range(8):
                        last = e.matmul(psf(b), lhsT=xnT[:, k, :], rhs=wqkv[:, k, jb * 512:(jb + 1) * 512], start=(k == 0), stop=(k == 7))
                    return last
                P.add("tensor", mm, r=[("wqkv", k) for k in range(8)] + [("xnT", s2)], w=[("ps", b)])
                if jb < 4:
                    P.add("scalar", (lambda jb, b, qkf: lambda e: e.activation(out=qkf[:, jb * 512:(jb + 1) * 512], in_=psf(b), func=AF.Copy))(jb, b, qkf),
                          r=[("ps", b)], w=[("qk", s2, jb)])
                    P.add("scalar", (lambda jb, b: lambda e: e.activation(out=sqf[:, jb * 512:(jb + 1) * 512], in_=psf(b), func=AF.Square))(jb, b),
                          r=[("ps", b)], w=[("sq", jb)])
                else:
                    P.add("scalar", (lambda jb, b, vb: lambda e: e.activation(out=vb[:, (jb - 4) * 512:(jb - 3) * 512], in_=psf(b), func=AF.Copy))(jb, b, vb),
                          r=[("ps", b)], w=[("vb", s2, jb)])
            ssq = ssq_r[s2]
            P.add("vector", (lambda ssq: lambda e: e.tensor_reduce(out=ssq, in_=sq, axis=AX.X, op=ALU.add))(ssq),
                  r=[("sq", j) for j in range(4)], w=[("ssq", s2)])

            P.chain("gpsimd", [
                (lambda ssq: lambda e: e.tensor_scalar(out=ssq, in0=ssq, scalar1=1.0 / 64, scalar2=EPS, op0=ALU.mult, op1=ALU.add))(ssq),
                (lambda ssq: lambda e: e.tensor_tensor(out=ssq, in0=ssq, in1=neghalf[:, 0:32], op=ALU.pow))(ssq),
            ], r=[("ssq", s2)], w=[("ssq", s2)])
            qkeys = [("qk", s2, j) for j in range(4)]
            P.add("vector", (lambda qk, ssq: lambda e: e.tensor_tensor(out=qk, in0=qk, in1=ssq.unsqueeze(2).to_broadcast([128, 32, 64]), op=ALU.mult))(qk, ssq),
                  r=qkeys + [("ssq", s2)], w=qkeys)

            def gmul(e, qk=qk):
                q4 = qk.rearrange("p (a g) d -> p a g d", a=2)
                return e.tensor_tensor(out=q4, in0=q4, in1=gqk.unsqueeze(2).to_broadcast([128, 2, 16, 64]), op=ALU.mult)
            P.add("gpsimd", gmul, r=qkeys + ["gqk0", "gqk1"], w=qkeys)
            qkb = qkb_r[s2]
            P.add("scalar", (lambda qkb, qkf: lambda e: e.activation(out=qkb.rearrange("p g d -> p (g d)"), in_=qkf, func=AF.Copy))(qkb, qkf),
                  r=qkeys, w=[("qkb", s2)])

            def rope(e, qk=qk, qkb=qkb, T=T):
                cs = cosT[:, T, :].unsqueeze(1).to_broadcast([128, 32, 8])
                sn = sinT[:, T, :].unsqueeze(1).to_broadcast([128, 32, 8])
                x1 = qk[:, :, 0:8]
                x2 = qk[:, :, 8:16]
                e.tensor_tensor(out=rt[0], in0=x1, in1=cs, op=ALU.mult)
                e.tensor_tensor(out=rt[1], in0=x2, in1=sn, op=ALU.mult)
                e.tensor_tensor(out=rt[2], in0=x2, in1=cs, op=ALU.mult)
                e.tensor_tensor(out=rt[3], in0=x1, in1=sn, op=ALU.mult)
                e.tensor_tensor(out=qkb[:, :, 0:8], in0=rt[0], in1=rt[1], op=ALU.subtract)
                return e.tensor_tensor(out=qkb[:, :, 8:16], in0=rt[2], in1=rt[3], op=ALU.add)
            P.add("gpsimd", rope, r=qkeys + [("qkb", s2), "sinT", "cosT"], w=[("qkb", s2)])
            bq = psQ.next()
            bk = psQ.next()

            def trq(e, qkb=qkb, bq=bq, bk=bk):
                last = None
                for h in range(8):
                    e.transpose(psb(bq)[:, h * 128:(h + 1) * 128], qkb[:, 2 * h:2 * h + 2, :].rearrange("p a d -> p (a d)"), ident_bf)
                    last = e.transpose(psb(bk)[:, h * 128:(h + 1) * 128], qkb[:, 16 + 2 * h:18 + 2 * h, :].rearrange("p a d -> p (a d)"), ident_bf)
                return last
            P.add("tensor", trq, r=[("qkb", s2), "ident_bf"], w=[("ps", bq), ("ps", bk)])
            qs = qT_st[st % 2]
            ks = kT_st[st % 2]
            P.add("scalar", (lambda qs, bq, c: lambda e: e.activation(out=qs[:, :, c * 128:(c + 1) * 128],
                                                                       in_=psb(bq).rearrange("p (h t) -> p h t", h=8), func=AF.Copy))(qs, bq, c),
                  r=[("ps", bq)], w=[("qs", st % 2, c)])
            P.add("vector", (lambda ks, bk, c: lambda e: e.tensor_copy(out=ks[:, :, c * 128:(c + 1) * 128],
                                                                        in_=psb(bk).rearrange("p (h t) -> p h t", h=8)))(ks, bk, c),
                  r=[("ps", bk)], w=[("ks", st % 2, c)])
            P.add("sync", (lambda vb, T: lambda e: e.dma_start(out=v_v[T * 128:(T + 1) * 128, :], in_=vb))(vb, T),
                  r=[("vb", s2, 4), ("vb", s2, 5)], w=[("v_d", T)], dkey="sv%d" % s2)
            if c == 3:
                P.add("sync", (lambda qs, st: lambda e: e.dma_start(out=qT_v[:, :, st * 512:(st + 1) * 512], in_=qs))(qs, st),
                      r=[("qs", st % 2, cc) for cc in range(4)], w=[("qT_d", st)], dkey="sq%d" % (st % 2))
                P.add("sync", (lambda ks, st: lambda e: e.dma_start(out=kT_v[:, :, st * 512:(st + 1) * 512], in_=ks))(ks, st),
                      r=[("ks", st % 2, cc) for cc in range(4)], w=[("kT_d", st)], dkey="sk%d" % (st % 2))
        P.barrier()

        ar.reset(CONST_END)
        dk_ctr[0] = 0
        OnT = ar.alloc([8, S], BF16)
        KT_r = [ar.alloc([S], BF16) for _ in range(2)]
        QT_r = [ar.alloc([S], BF16) for _ in range(2)]
        VA_r = [ar.alloc([NT, 132], BF16) for _ in range(2)]
        E_r = [ar.alloc([512], BF16) for _ in range(4)]
        tri_f = ar.alloc([128], F32)
        tri_b = ar.alloc([128], BF16)
        maskneg = tri_b
        lamv = ar.alloc([4, 64], F32)
        lamp = ar.alloc([2, 64], F32)
        lsum = [ar.alloc([1], F32) for _ in range(2)]
        lexp = [ar.alloc([1], F32) for _ in range(2)]
        nlam = ar.alloc([1], F32)
        gb = ar.alloc([2, 64], F32)
        mx = [ar.alloc([1], F32) for _ in range(2)]
        negB = ar.alloc([1], F32)
        sg = ar.alloc([1], F32)
        O_r = [ar.alloc([128], F32) for _ in range(4)]
        rr_r = [[ar.alloc([1], F32) for _ in range(4)] for _ in range(4)]
        Onb_r = [ar.alloc([128], BF16) for _ in range(2)]
        junkO = ar.alloc([128], BF16)
        wo = ar.alloc([8, 1024], BF16)
        xr_ring = [ar.alloc([1024], F32) for _ in range(2)]
        dbg_s = ar.alloc([8, 512], F32)
        P.add("gpsimd", lambda e: e.memset(dbg_s.rearrange("p a b -> p (a b)"), 0.0), w=["dbg%d" % z for z in range(7)])

        wo_d = w["l1_da_w_out"].rearrange("(k p) n -> p k n", p=128)
        P.add("gpsimd", lambda e: e.dma_start(out=wo, in_=wo_d), w=["wo"], dkey="w0")
        P.add("sync", lambda e: e.dma_start(out=tri_f, in_=tri_d[:, :]), w=["tri_f"], dkey=dk())
        P.add("vector", lambda e: e.tensor_scalar(out=tri_b, in0=tri_f, scalar1=-1.0, scalar2=30000.0, op0=ALU.add, op1=ALU.mult),
              r=["tri_f"], w=["maskneg"])
        for i, nm in enumerate(["l1_da_lambda_q1", "l1_da_lambda_q2", "l1_da_lambda_k1", "l1_da_lambda_k2"]):
            P.add("sync", (lambda i, nm: lambda e: e.dma_start(out=lamv[:, i, :], in_=w[nm].partition_broadcast(128)))(i, nm),
                  w=[("lamv", i)], dkey=dk())
        P.add("sync", lambda e: e.dma_start(out=gb[:, 0, :], in_=w["l1_da_q_norm"].partition_broadcast(128)), w=[("gb", 0)], dkey=dk())
        P.add("sync", lambda e: e.dma_start(out=gb[:, 1, :], in_=w["l1_da_k_norm"].partition_broadcast(128)), w=[("gb", 1)], dkey=dk())

        def sgld(e):
            with nc.allow_non_contiguous_dma(reason="tiny gain column"):
                return e.dma_start(out=sg, in_=w["l1_da_subln"].rearrange("(p o) -> p o", o=1))
        P.add("sync", sgld, w=["sg"], dkey=dk())
        P.add("vector", lambda e: e.tensor_scalar(out=sg, in0=sg, scalar1=1.0 - LAMBDA_INIT, scalar2=None, op0=ALU.mult), r=["sg"], w=["sg"])

        P.chain("vector", [
            lambda e: e.tensor_tensor(out=lamp[:, 0, :], in0=lamv[:, 0, :], in1=lamv[:, 2, :], op=ALU.mult),
            lambda e: e.tensor_tensor(out=lamp[:, 1, :], in0=lamv[:, 1, :], in1=lamv[:, 3, :], op=ALU.mult),
            lambda e: e.tensor_reduce(out=lsum[0], in_=lamp[:, 0, :], axis=AX.X, op=ALU.add),
            lambda e: e.tensor_reduce(out=lsum[1], in_=lamp[:, 1, :], axis=AX.X, op=ALU.add),
        ], r=[("lamv", i) for i in range(4)], w=["lsum"])
        def lexpf(e):
            e.activation(out=lexp[0], in_=lsum[0], func=AF.Exp)
            return e.activation(out=lexp[1], in_=lsum[1], func=AF.Exp)
        P.add("scalar", lexpf, r=["lsum"], w=["lexp"])

        P.chain("vector", [
            lambda e: e.tensor_tensor(out=nlam, in0=lexp[1], in1=lexp[0], op=ALU.subtract),
            lambda e: e.tensor_scalar(out=nlam, in0=nlam, scalar1=-LAMBDA_INIT, scalar2=None, op0=ALU.add),
        ], r=["lexp"], w=["nlam"])

        P.chain("vector", [
            lambda e: e.tensor_tensor(out=gb, in0=gb, in1=gb, op=ALU.mult),
            lambda e: e.tensor_reduce(out=mx[0], in_=gb[:, 0, :], axis=AX.X, op=ALU.max),
            lambda e: e.tensor_reduce(out=mx[1], in_=gb[:, 1, :], axis=AX.X, op=ALU.max),
            lambda e: e.tensor_tensor(out=mx[0], in0=mx[0], in1=mx[1], op=ALU.max),
            lambda e: e.tensor_scalar(out=negB, in0=mx[0], scalar1=1.0, scalar2=-8.0, op0=ALU.max, op1=ALU.mult),
        ], r=[("gb", 0), ("gb", 1)], w=["negB"])
        for s2 in range(2):
            P.add("gpsimd", (lambda s2: lambda e: e.memset(VA_r[s2].rearrange("p j d -> p (j d)"), 1.0))(s2), w=[("VA", s2)])

        psS = PsRing([4, 5, 6, 7])
        v_h = v_d.rearrange("(j p) h d -> p j h d", p=128)
        def head_loads(h):
            s2 = h % 2
            KT, QT, VA = KT_r[s2], QT_r[s2], VA_r[s2]
            P.add("sync", (lambda KT, h: lambda e: e.dma_start(out=KT, in_=kT_d[h]))(KT, h), w=[("KT", s2)], dkey="lk%d" % s2)
            P.add("sync", (lambda QT, h: lambda e: e.dma_start(out=QT, in_=qT_d[h]))(QT, h), w=[("QT", s2)], dkey="lq%d" % s2)
            P.add("sync", (lambda VA, h: lambda e: e.dma_start(out=VA[:, :, 0:128], in_=v_h[:, :, h, :]))(VA, h), w=[("VA", s2)], dkey="lv%d" % s2)

        items = [(h, Q, comp, j) for h in range(8) for Q in range(NST) for comp in range(2) for j in range(4 * Q + 4)]
        slot_of = {}
        ectr = [0]

        def front(it):
            h, Q, comp, j = it
            s2 = h % 2
            KT, QT = KT_r[s2], QT_r[s2]
            if Q == 0 and comp == 0 and j == 0:
                if h == 0:
                    head_loads(0)
                if h + 1 < 8:
                    head_loads(h + 1)
            lo_p, hi_p = comp * 64, comp * 64 + 64
            lo = max(0, j - 4 * Q)
            bS = psS.next()

            def smm(e):
                diag = j >= 4 * Q
                ins = e.matmul(psf(bS)[:, lo * 128:512], lhsT=KT[lo_p:hi_p, j * 128:(j + 1) * 128],
                               rhs=QT[lo_p:hi_p, Q * 512 + lo * 128:(Q + 1) * 512], start=True, stop=not diag)
                if diag:
                    ins = e.matmul(psf(bS)[:, lo * 128:(lo + 1) * 128], lhsT=ident_bf, rhs=maskneg, start=False, stop=True)
                return ins
            P.add("tensor", smm, r=[("KT", s2), ("QT", s2), "maskneg", "ident_bf"], w=[("ps", bS)])
            es_ = ectr[0] % 4
            ectr[0] += 1
            slot_of[it] = es_
            E = E_r[es_]
            P.add("scalar", lambda e: e.activation(out=E[:, lo * 128:512], in_=psf(bS)[:, lo * 128:512], func=AF.Exp, bias=negB[:, 0:1]),
                  r=[("ps", bS), "negB"], w=[("E", es_)])

        def back(it):
            h, Q, comp, j = it
            s2 = h % 2
            VA = VA_r[s2]
            lo = max(0, j - 4 * Q)
            es_ = slot_of.pop(it)
            E = E_r[es_]

            def av(e):
                last = None
                for i in range(lo, 4):
                    last = e.matmul(ps_t[:, i, 0:132], lhsT=E[:, i * 128:(i + 1) * 128], rhs=VA[:, j, :],
                                    start=(j == 0), stop=(j == 4 * Q + i))
                return last
            P.add("tensor", av, r=[("E", es_), ("VA", s2)], w=[("ps", i) for i in range(lo, 4)])
            if j != 4 * Q + 3:
                return
            if comp == 0:
                for i in range(4):
                    P.chain("vector", [
                        (lambda i: lambda e: e.reciprocal(out=rr_r[i][0], in_=ps_t[:, i, 128:129]))(i),
                        (lambda i: lambda e: e.tensor_scalar(out=O_r[i], in0=ps_t[:, i, 0:128], scalar1=rr_r[i][0], scalar2=None, op0=ALU.mult))(i),
                    ], r=[("ps", i)], w=[("O", i), ("rr", i)])
                return
            for i in range(4):
                T = 4 * Q + i
                o2 = i
                U2 = ps_t[:, i, 0:132]
                O, rr, Onb = O_r[o2], rr_r[o2], Onb_r[i % 2]
                P.chain("vector", [
                    (lambda U2, rr: lambda e: e.reciprocal(out=rr[1], in_=U2[:, 128:129]))(U2, rr),
                    (lambda rr: lambda e: e.tensor_tensor(out=rr[1], in0=rr[1], in1=nlam, op=ALU.mult))(rr),
                    (lambda U2, O, rr: lambda e: e.scalar_tensor_tensor(out=O, in0=U2[:, 0:128], scalar=rr[1], in1=O, op0=ALU.mult, op1=ALU.add))(U2, O, rr),
                ], r=[("ps", i), "nlam", ("O", o2), ("rr", o2)], w=[("O", o2), ("rr", o2)])
                P.add("gpsimd", (lambda rr: lambda e: e.memset(rr[2], 0.0))(rr), r=[("rr", o2)], w=[("rr2", o2)])
                P.add("scalar", (lambda O, rr: lambda e: e.activation(out=junkO, in_=O, func=AF.Square, accum_out=rr[2]))(O, rr),
                      r=[("O", o2), ("rr2", o2)], w=[("rr2", o2)])
                P.chain("gpsimd", [
                    (lambda rr: lambda e: e.tensor_scalar(out=rr[3], in0=rr[2], scalar1=1.0 / 128, scalar2=EPS, op0=ALU.mult, op1=ALU.add))(rr),
                    (lambda rr: lambda e: e.tensor_tensor(out=rr[3], in0=rr[3], in1=neghalf[:, 0:1], op=ALU.pow))(rr),
                ], r=[("rr2", o2)], w=[("rr3", o2)])
                P.add("vector", (lambda Onb, O, rr: lambda e: e.tensor_scalar(out=Onb, in0=O, scalar1=rr[3], scalar2=None, op0=ALU.mult))(Onb, O, rr),
                      r=[("O", o2), ("rr3", o2)], w=[("Onb", i % 2)])
                bt = psS.next()
                P.add("tensor", (lambda bt, Onb: lambda e: e.transpose(psb(bt)[:, 0:128], Onb, ident_bf))(bt, Onb),
                      r=[("Onb", i % 2), "ident_bf"], w=[("ps", bt)])
                P.add("vector", (lambda bt, h, T: lambda e: e.tensor_scalar(out=OnT[:, h, T * 128:(T + 1) * 128], in0=psb(bt)[:, 0:128],
                                                                             scalar1=sg[:, 0:1], scalar2=None, op0=ALU.mult))(bt, h, T),
                      r=[("ps", bt), "sg"], w=[("OnT", h, T)])

        LA = 2
        for n in range(len(items) + LA):
            if n < len(items):
                front(items[n])
            if n >= LA:
                back(items[n - LA])

        for T in range(NT):
            s2 = T % 2
            xr = xr_ring[s2]
            P.add("sync", (lambda xr, T: lambda e: e.dma_start(out=xr, in_=src_d[T * 128:(T + 1) * 128, :]))(xr, T),
                  w=[("xr", s2)], dkey="lr%d" % s2)
            for j in range(2):
                b = psS.next()

                def mmo(e, T=T, j=j, b=b):
                    last = None
                    for k in range(8):
                        last = e.matmul(psf(b), lhsT=OnT[:, k, T * 128:(T + 1) * 128], rhs=wo[:, k, j * 512:(j + 1) * 512],
                                        start=(k == 0), stop=(k == 7))
                    return last
                P.add("tensor", mmo, r=[("OnT", k, T) for k in range(8)] + ["wo"], w=[("ps", b)])
                P.add("vector", (lambda xr, j, b: lambda e: e.tensor_tensor(out=xr[:, j * 512:(j + 1) * 512], in0=psf(b),
                                                                             in1=xr[:, j * 512:(j + 1) * 512], op=ALU.add))(xr, j, b),
                      r=[("ps", b), ("xr", s2)], w=[("xr", s2)])
            P.add("sync", (lambda xr, T: lambda e: e.dma_start(out=dst_d[T * 128:(T + 1) * 128, :], in_=xr))(xr, T),
                  r=[("xr", s2)], w=[("hC", T)], dkey="st%d" % s2)


    def phase_D_dense(src_d, dst_d):
        ar.reset(CONST_END)
        dk_ctr[0] = 0
        gcol = ar.alloc([8], F32)
        wr = ar.alloc([8, 8], F32)
        acc = ar.alloc([16, 1024], F32)
        xnT = ar.alloc([8, 2048], BF16)
        gates = ar.alloc([16, 8], F32)
        xs32_r = [ar.alloc([1024], F32) for _ in range(2)]
        junk = ar.alloc([1024], BF16)
        xs_ring = [{"bf": ar.alloc([1024], BF16), "junk": junk} for _ in range(2)]
        ss_r = [ar.alloc([1], F32) for _ in range(2)]
        rstd_r = [ar.alloc([1], F32) for _ in range(2)]
        xn32T = ar.alloc([8, 128], F32)
        lg = ar.alloc([8], F32)
        eq = ar.alloc([8], F32)
        l2 = ar.alloc([8], F32)
        sel = ar.alloc([8], F32)
        ex = ar.alloc([8], F32)
        m1 = ar.alloc([1], F32)
        m2 = ar.alloc([1], F32)
        nm1 = ar.alloc([1], F32)
        den = ar.alloc([1], F32)
        rden = ar.alloc([1], F32)
        wg_r = [ar.alloc([8, 512], BF16) for _ in range(2)]
        wu_r = [ar.alloc([8, 512], BF16) for _ in range(2)]
        wd_r = [ar.alloc([4, 1024], BF16) for _ in range(2)]
        HT_r = [ar.alloc([4, 512], BF16) for _ in range(2)]
        tmp_r = [ar.alloc([512], F32) for _ in range(2)]

        P.add("sync", col_load(gcol, w["l1_moe_norm"], 8), w=["gcol"], dkey=dk())
        P.add("sync", lambda e: e.dma_start(out=wr, in_=w["l1_moe_w_router"].rearrange("(k p) e -> p k e", p=128)), w=["wr"], dkey=dk())
        wg_d = w["l1_moe_w_gate"]
        wu_d = w["l1_moe_w_up"]
        wd_d = w["l1_moe_w_down"]
        psT = PsRing([0, 1])

        groups = [(hf, e_, fg) for hf in range(2) for e_ in range(NE) for fg in range(FE // 512)]

        def load_group(gi):
            hf, e_, fg = groups[gi]
            sl = gi % 2
            P.add("gpsimd", (lambda sl, e_, fg: lambda e: e.dma_start(
                out=wg_r[sl], in_=wg_d[e_].rearrange("(k p) f -> p k f", p=128)[:, :, fg * 512:(fg + 1) * 512]))(sl, e_, fg),
                w=[("wg", sl)], dkey="wg%d" % sl)
            P.add("gpsimd", (lambda sl, e_, fg: lambda e: e.dma_start(
                out=wu_r[sl], in_=wu_d[e_].rearrange("(k p) f -> p k f", p=128)[:, :, fg * 512:(fg + 1) * 512]))(sl, e_, fg),
                w=[("wu", sl)], dkey="wu%d" % sl)
            P.add("gpsimd", (lambda sl, e_, fg: lambda e: e.dma_start(
                out=wd_r[sl], in_=wd_d[e_][fg * 512:(fg + 1) * 512, :].rearrange("(k p) n -> p k n", p=128)))(sl, e_, fg),
                w=[("wd", sl)], dkey="wd%d" % sl)

        gi = 0
        hi = 0
        for hf in range(2):
            for t in range(16):
                T = hf * 16 + t
                s2 = t % 2
                at = acc[:, t, :]
                P.add("sync", (lambda at, T: lambda e: e.dma_start(out=at, in_=src_d[T * 128:(T + 1) * 128, :]))(at, T),
                      w=[("acc", t)], dkey="la%d" % t)
                xs32 = xs32_r[s2]
                norm_p1(at, xs_ring[s2], ss_r[s2], rstd_r[s2], ("D", s2), [("acc", t)], xs32=xs32)
                norm_p2(gcol, xs_ring[s2], xnT[:, :, t * 128:(t + 1) * 128], ("D", s2), psT, [("xnT", t)])

                def trf(e, xs32=xs32):
                    last = None
                    for k in range(8):
                        last = e.transpose(ps_t[:, 2 + k // 4, (k % 4) * 128:(k % 4 + 1) * 128], xs32[:, k * 128:(k + 1) * 128], ident_f)
                    return last
                P.add("tensor", trf, r=[("xs32", ("D", s2)), "ident_f"], w=[("ps", 2), ("ps", 3)])
                P.add("vector", lambda e: e.tensor_tensor(out=xn32T, in0=ps_t[:, 2:4, :].rearrange("p a (b t) -> p (a b) t", b=4),
                                                          in1=gcol.unsqueeze(2).to_broadcast([128, 8, 128]), op=ALU.mult),
                      r=[("ps", 2), ("ps", 3), "gcol"], w=["xn32T"])

                def lgm(e):
                    last = None
                    for k in range(8):
                        last = e.matmul(ps_t[:, 4, 0:8], lhsT=xn32T[:, k, :], rhs=wr[:, k, :], start=(k == 0), stop=(k == 7))
                    return last
                P.add("tensor", lgm, r=["xn32T", "wr"], w=[("ps", 4)])
                P.chain("vector", [
                    lambda e: e.tensor_copy(out=lg, in_=ps_t[:, 4, 0:8]),
                    lambda e: e.tensor_reduce(out=m1, in_=lg, axis=AX.X, op=ALU.max),
                    lambda e: e.tensor_scalar(out=eq, in0=lg, scalar1=m1[:, 0:1], scalar2=None, op0=ALU.is_equal),
                    lambda e: e.scalar_tensor_tensor(out=l2, in0=eq, scalar=-1.0e30, in1=lg, op0=ALU.mult, op1=ALU.add),
                    lambda e: e.tensor_reduce(out=m2, in_=l2, axis=AX.X, op=ALU.max),
                    lambda e: e.tensor_scalar(out=sel, in0=lg, scalar1=m2[:, 0:1], scalar2=None, op0=ALU.is_ge),
                    lambda e: e.tensor_scalar(out=nm1, in0=m1, scalar1=-1.0, scalar2=None, op0=ALU.mult),
                ], r=[("ps", 4)], w=["lg", "sel", "nm1"])
                P.add("scalar", lambda e: e.activation(out=ex, in_=lg, func=AF.Exp, bias=nm1[:, 0:1]), r=["lg", "nm1"], w=["ex"])
                P.chain("vector", [
                    lambda e: e.tensor_tensor(out=ex, in0=ex, in1=sel, op=ALU.mult),
                    lambda e: e.tensor_reduce(out=den, in_=ex, axis=AX.X, op=ALU.add),
                    lambda e: e.reciprocal(out=rden, in_=den),
                    (lambda t: lambda e: e.tensor_scalar(out=gates[:, t, :], in0=ex, scalar1=rden[:, 0:1], scalar2=None, op0=ALU.mult))(t),
                ], r=["ex", "sel"], w=[("gates", t), "ex"])

            psM = PsRing([0, 1, 2, 3, 4, 5, 6, 7])
            if hf == 0:
                load_group(0)
            for e_ in range(NE):
                for fg in range(FE // 512):
                    if gi + 1 < len(groups):
                        load_group(gi + 1)
                    sl = gi % 2
                    wg, wu, wd = wg_r[sl], wu_r[sl], wd_r[sl]
                    for st in range(4):
                        HT = HT_r[hi % 2]
                        hk = hi % 2
                        hi += 1
                        xk = [("xnT", st * 4 + c) for c in range(4)]
                        for fc in range(4):
                            bg = psM.next()
                            bu = psM.next()

                            def mmg(e, fc=fc, bg=bg, wg=wg, st=st):
                                last = None
                                for k in range(8):
                                    last = e.matmul(psf(bg), lhsT=wg[:, k, fc * 128:(fc + 1) * 128], rhs=xnT[:, k, st * 512:(st + 1) * 512],
                                                    start=(k == 0), stop=(k == 7))
                                return last

                            def mmu(e, fc=fc, bu=bu, wu=wu, st=st):
                                last = None
                                for k in range(8):
                                    last = e.matmul(psf(bu), lhsT=wu[:, k, fc * 128:(fc + 1) * 128], rhs=xnT[:, k, st * 512:(st + 1) * 512],
                                                    start=(k == 0), stop=(k == 7))
                                return last
                            P.add("tensor", mmg, r=[("wg", sl)] + xk, w=[("ps", bg)])
                            P.add("tensor", mmu, r=[("wu", sl)] + xk, w=[("ps", bu)])
                            tmp = tmp_r[fc % 2]
                            P.add("scalar", (lambda tmp, bg: lambda e: e.activation(out=tmp, in_=psf(bg), func=AF.Silu))(tmp, bg),
                                  r=[("ps", bg)], w=[("tmp", fc % 2)])
                            P.add("vector", (lambda tmp, bu, fc, HT: lambda e: e.tensor_tensor(out=HT[:, fc, :], in0=psf(bu), in1=tmp, op=ALU.mult))(tmp, bu, fc, HT),
                                  r=[("ps", bu), ("tmp", fc % 2)], w=[("HT", hk, fc)])
                        for c in range(4):
                            t = st * 4 + c
                            for j in range(2):
                                b = psM.next()

                                def mmd(e, c=c, j=j, b=b, HT=HT, wd=wd):
                                    last = None
                                    for k in range(4):
                                        last = e.matmul(psf(b), lhsT=HT[:, k, c * 128:(c + 1) * 128], rhs=wd[:, k, j * 512:(j + 1) * 512],
                                                        start=(k == 0), stop=(k == 3))
                                    return last
                                P.add("tensor", mmd, r=[("HT", hk, fc) for fc in range(4)] + [("wd", sl)], w=[("ps", b)])
                                P.add("vector", (lambda t, j, b, e_: lambda e: e.scalar_tensor_tensor(
                                    out=acc[:, t, j * 512:(j + 1) * 512], in0=psf(b), scalar=gates[:, t, e_:e_ + 1],
                                    in1=acc[:, t, j * 512:(j + 1) * 512], op0=ALU.mult, op1=ALU.add))(t, j, b, e_),
                                    r=[("ps", b), ("gates", t), ("acc", t)], w=[("acc", t)])
                    gi += 1
            for t in range(16):
                T = hf * 16 + t
                P.add("sync", (lambda t, T: lambda e: e.dma_start(out=dst_d[T * 128:(T + 1) * 128, :], in_=acc[:, t, :]))(t, T),
                      r=[("acc", t)], w=[("hD", T)], dkey="sa%d" % t)


    def phase_D(src_d, dst_d):
        BIG = 1.0e6
        ar.reset(CONST_END)
        dk_ctr[0] = 0
        idxA = ar.alloc([NT, 2], I32)
        idxB = ar.alloc([NT, 2], I32)
        gAB = ar.alloc([NT, 2], F32)
        gBB = ar.alloc([NT, 2], F32)
        gcol = ar.alloc([8], F32)
        P_END = ar.off
        wr = ar.alloc([8, 8], F32)
        tris = ar.alloc([128], F32)
        ones_f = ar.alloc([128], F32)
        eoff = ar.alloc([8], F32)
        Mcum = ar.alloc([8], F32)
        xt_ring = [ar.alloc([1024], F32) for _ in range(2)]
        xs32_r = [ar.alloc([1024], F32) for _ in range(2)]
        junk = ar.alloc([1024], BF16)
        xs_ring = [{"bf": ar.alloc([1024], BF16), "junk": junk} for _ in range(2)]
        ss_r = [ar.alloc([1], F32) for _ in range(2)]
        rstd_r = [ar.alloc([1], F32) for _ in range(2)]
        xn32T = ar.alloc([8, 128], F32)
        lg = ar.alloc([8], F32)
        eq = ar.alloc([8], F32)
        l2 = ar.alloc([8], F32)
        sel = ar.alloc([8], F32)
        ex = ar.alloc([8], F32)
        gt = ar.alloc([8], F32)
        rk = ar.alloc([8], F32)
        sf = ar.alloc([8], F32)
        sm = ar.alloc([8], F32)
        t2 = ar.alloc([8], F32)
        oh = ar.alloc([8], F32)
        tg = ar.alloc([8], F32)
        tg2 = ar.alloc([8], F32)
        cnti = ar.alloc([8], I32)
        m1 = ar.alloc([1], F32)
        m2 = ar.alloc([1], F32)
        nm1 = ar.alloc([1], F32)
        den = ar.alloc([1], F32)
        rden = ar.alloc([1], F32)
        sA = ar.alloc([1], F32)
        sB = ar.alloc([1], F32)
        XT = [ar.alloc([8, 512], BF16) for _ in range(4)]
        acc = [ar.alloc([4, 1024], F32) for _ in range(4)]
        wg_r = [ar.alloc([8, 512], BF16) for _ in range(2)]
        wu_r = [ar.alloc([8, 512], BF16) for _ in range(2)]
        wd_r = [ar.alloc([4, 1024], BF16) for _ in range(2)]
        HT_r = [ar.alloc([4, 512], BF16) for _ in range(2)]
        tmp_r = [ar.alloc([512], F32) for _ in range(2)]
        xg_r = [ar.alloc([1024], BF16) for _ in range(2)]

        P.add("sync", col_load(gcol, w["l1_moe_norm"], 8), w=["gcol"], dkey=dk())
        P.add("sync", lambda e: e.dma_start(out=wr, in_=w["l1_moe_w_router"].rearrange("(k p) e -> p k e", p=128)), w=["wr"], dkey=dk())
        P.add("sync", lambda e: e.dma_start(out=tris, in_=w["c_tris"][:, :]), w=["tris"], dkey=dk())
        P.add("sync", lambda e: e.dma_start(out=eoff, in_=w["c_eoff"].partition_broadcast(128)), w=["eoff"], dkey=dk())
        P.add("gpsimd", lambda e: e.memset(ones_f, 1.0), w=["ones_f"])
        P.add("gpsimd", lambda e: e.memset(Mcum, 0.0), w=["Mcum"])
        psT = PsRing([0, 1])

        for T in range(NT):
            s2 = T % 2
            xt = xt_ring[s2]
            P.add("sync", (lambda xt, T: lambda e: e.dma_start(out=xt, in_=src_d[T * 128:(T + 1) * 128, :]))(xt, T),
                  w=[("xt", s2)], dkey="ld%d" % s2)
            xs32 = xs32_r[s2]
            xsb = xs_ring[s2]["bf"]
            norm_p1(xt, xs_ring[s2], ss_r[s2], rstd_r[s2], ("D", s2), [("xt", s2)], xs32=xs32)

            def trf(e, xs32=xs32):
                last = None
                for k in range(8):
                    last = e.transpose(ps_t[:, 2 + k // 4, (k % 4) * 128:(k % 4 + 1) * 128], xs32[:, k * 128:(k + 1) * 128], ident_f)
                return last
            P.add("tensor", trf, r=[("xs32", ("D", s2)), "ident_f"], w=[("ps", 2), ("ps", 3)])
            P.add("vector", lambda e: e.tensor_tensor(out=xn32T, in0=ps_t[:, 2:4, :].rearrange("p a (b t) -> p (a b) t", b=4),
                                                      in1=gcol.unsqueeze(2).to_broadcast([128, 8, 128]), op=ALU.mult),
                  r=[("ps", 2), ("ps", 3), "gcol"], w=["xn32T"])

            def lgm(e):
                last = None
                for k in range(8):
                    last = e.matmul(ps_t[:, 4, 0:8], lhsT=xn32T[:, k, :], rhs=wr[:, k, :], start=(k == 0), stop=(k == 7))
                return last
            P.add("tensor", lgm, r=["xn32T", "wr"], w=[("ps", 4)])
            P.chain("vector", [
                lambda e: e.tensor_copy(out=lg, in_=ps_t[:, 4, 0:8]),
                lambda e: e.tensor_reduce(out=m1, in_=lg, axis=AX.X, op=ALU.max),
                lambda e: e.tensor_scalar(out=eq, in0=lg, scalar1=m1[:, 0:1], scalar2=None, op0=ALU.is_equal),
                lambda e: e.scalar_tensor_tensor(out=l2, in0=eq, scalar=-1.0e30, in1=lg, op0=ALU.mult, op1=ALU.add),
                lambda e: e.tensor_reduce(out=m2, in_=l2, axis=AX.X, op=ALU.max),
                lambda e: e.tensor_scalar(out=sel, in0=lg, scalar1=m2[:, 0:1], scalar2=None, op0=ALU.is_ge),
                lambda e: e.tensor_scalar(out=nm1, in0=m1, scalar1=-1.0, scalar2=None, op0=ALU.mult),
            ], r=[("ps", 4)], w=["lg", "sel", "nm1"])
            P.add("scalar", lambda e: e.activation(out=ex, in_=lg, func=AF.Exp, bias=nm1[:, 0:1]), r=["lg", "nm1"], w=["ex"])

            def rkm(e):
                e.matmul(ps_t[:, 5, 0:8], lhsT=tris, rhs=sel, start=True, stop=False)
                return e.matmul(ps_t[:, 5, 0:8], lhsT=ones_f, rhs=Mcum, start=False, stop=True)
            P.add("tensor", rkm, r=["sel", "Mcum", "tris", "ones_f"], w=[("ps", 5)])
            P.chain("vector", [
                lambda e: e.tensor_tensor(out=ex, in0=ex, in1=sel, op=ALU.mult),
                lambda e: e.tensor_reduce(out=den, in_=ex, axis=AX.X, op=ALU.add),
                lambda e: e.reciprocal(out=rden, in_=den),
                lambda e: e.tensor_scalar(out=gt, in0=ex, scalar1=rden[:, 0:1], scalar2=None, op0=ALU.mult),
                lambda e: e.tensor_copy(out=rk, in_=ps_t[:, 5, 0:8]),
                lambda e: e.tensor_tensor(out=Mcum, in0=Mcum, in1=sel, op=ALU.add),
                lambda e: e.tensor_tensor(out=sf, in0=rk, in1=eoff, op=ALU.add),
                lambda e: e.scalar_tensor_tensor(out=sm, in0=sf, scalar=-BIG, in1=sel, op0=ALU.add, op1=ALU.mult),
                lambda e: e.tensor_scalar(out=sm, in0=sm, scalar1=BIG, scalar2=None, op0=ALU.add),
                lambda e: e.tensor_reduce(out=sA, in_=sm, axis=AX.X, op=ALU.min),
                lambda e: e.tensor_tensor(out=t2, in0=sf, in1=sel, op=ALU.mult),
                lambda e: e.tensor_reduce(out=sB, in_=t2, axis=AX.X, op=ALU.max),
                lambda e: e.tensor_scalar(out=oh, in0=sm, scalar1=sA[:, 0:1], scalar2=None, op0=ALU.is_equal),
                lambda e: e.tensor_tensor(out=tg, in0=gt, in1=oh, op=ALU.mult),
                (lambda T: lambda e: e.tensor_reduce(out=gAB[:, T, 0:1], in_=tg, axis=AX.X, op=ALU.add))(T),
                lambda e: e.tensor_tensor(out=tg2, in0=gt, in1=tg, op=ALU.subtract),
                (lambda T: lambda e: e.tensor_reduce(out=gBB[:, T, 0:1], in_=tg2, axis=AX.X, op=ALU.add))(T),
                (lambda T: lambda e: e.tensor_copy(out=idxA[:, T, 0:1], in_=sA))(T),
                (lambda T: lambda e: e.tensor_copy(out=idxB[:, T, 0:1], in_=sB))(T),
            ], r=["ex", "sel", ("ps", 5), "eoff", "Mcum"], w=["ex", "Mcum", ("idx", T), ("gab", T)])
            for which, idx_t in ((0, idxA), (1, idxB)):
                P.add("gpsimd", (lambda idx_t, T, xsb: lambda e: e.indirect_dma_start(
                    out=Xg_d[:, :], out_offset=bass.IndirectOffsetOnAxis(ap=idx_t[:, T, 0:1], axis=0),
                    in_=xsb, in_offset=None))(idx_t, T, xsb),
                    r=[("idx", T), ("xsbf", ("D", s2))], w=[("Xg", T, which)], dkey="sc%d%d" % (s2, which))
        P.add("tensor", lambda e: e.matmul(ps_t[:, 5, 0:8], lhsT=ones_f, rhs=Mcum, start=True, stop=True), r=["Mcum", "ones_f"], w=[("ps", 5)])
        P.add("vector", lambda e: e.tensor_copy(out=cnti, in_=ps_t[:, 5, 0:8]), r=[("ps", 5)], w=["cnti"])
        P.add("sync", lambda e: e.dma_start(out=cnt_d[0:1, :], in_=cnti[0:1, :]), r=["cnti"], w=["cnt_d"], dkey=dk())
        P.barrier()

        for e_ in range(NE):
            P.regload("cnt%d" % e_, cnt_d[0:1, e_:e_ + 1])
        wg_d = w["l1_moe_w_gate"]
        wu_d = w["l1_moe_w_up"]
        wd_d = w["l1_moe_w_down"]
        NFG = FE // 512
        groups = [(e_, rnd, fg) for e_ in range(NE) for rnd in range(2) for fg in range(NFG)]

        def load_group(gi):
            e_, rnd, fg = groups[gi]
            sl = gi % 2
            P.pred_begin("cnt%d" % e_, rnd * 2048 + 1)
            P.add("gpsimd", (lambda sl, e_, fg: lambda e: e.dma_start(
                out=wg_r[sl], in_=wg_d[e_].rearrange("(k p) f -> p k f", p=128)[:, :, fg * 512:(fg + 1) * 512]))(sl, e_, fg),
                w=[("wg", sl)], dkey="wg%d" % sl)
            P.add("gpsimd", (lambda sl, e_, fg: lambda e: e.dma_start(
                out=wu_r[sl], in_=wu_d[e_].rearrange("(k p) f -> p k f", p=128)[:, :, fg * 512:(fg + 1) * 512]))(sl, e_, fg),
                w=[("wu", sl)], dkey="wu%d" % sl)
            P.add("gpsimd", (lambda sl, e_, fg: lambda e: e.dma_start(
                out=wd_r[sl], in_=wd_d[e_][fg * 512:(fg + 1) * 512, :].rearrange("(k p) n -> p k n", p=128)))(sl, e_, fg),
                w=[("wd", sl)], dkey="wd%d" % sl)
            P.pred_end()

        psM = PsRing([0, 1, 2, 3, 4, 5, 6, 7])
        gi = 0
        hi = 0
        xgi = 0
        load_group(0)
        for e_ in range(NE):
            ck = "cnt%d" % e_
            for rnd in range(2):
                for k in range(4):
                    kk = rnd * 4 + k
                    P.pred_begin(ck, kk * 512 + 1)
                    for c in range(4):
                        xs_ = xgi % 2
                        xgi += 1
                        xg = xg_r[xs_]
                        row0 = e_ * S + kk * 512 + c * 128
                        P.add("sync", (lambda xg, row0: lambda e: e.dma_start(out=xg, in_=Xg_d[row0:row0 + 128, :]))(xg, row0),
                              w=[("xg", xs_)], dkey="xg%d" % xs_)
                        b = psM.next()

                        def trx(e, xg=xg, b=b):
                            last = None
                            for kq in range(8):
                                last = e.transpose(psb(b)[:, kq * 128:(kq + 1) * 128], xg[:, kq * 128:(kq + 1) * 128], ident_bf)
                            return last
                        P.add("tensor", trx, r=[("xg", xs_), "ident_bf"], w=[("ps", b)])
                        P.add("vector", (lambda k, c, b: lambda e: e.tensor_tensor(
                            out=XT[k][:, :, c * 128:(c + 1) * 128], in0=psb(b).rearrange("p (k t) -> p k t", k=8),
                            in1=gcol.unsqueeze(2).to_broadcast([128, 8, 128]), op=ALU.mult))(k, c, b),
                            r=[("ps", b), "gcol"], w=[("XT", k, c)])
                    P.pred_end()
                for fg in range(NFG):
                    if gi + 1 < len(groups):
                        load_group(gi + 1)
                    sl = gi % 2
                    wg, wu, wd = wg_r[sl], wu_r[sl], wd_r[sl]
                    for k in range(4):
                        kk = rnd * 4 + k
                        P.pred_begin(ck, kk * 512 + 1)
                        HT = HT_r[hi % 2]
                        hk = hi % 2
                        hi += 1
                        xk = [("XT", k, c) for c in range(4)]
                        for fc in range(4):
                            bg = psM.next()
                            bu = psM.next()

                            def mmg(e, fc=fc, bg=bg, wg=wg, k=k):
                                last = None
                                for kq in range(8):
                                    last = e.matmul(psf(bg), lhsT=wg[:, kq, fc * 128:(fc + 1) * 128], rhs=XT[k][:, kq, :],
                                                    start=(kq == 0), stop=(kq == 7))
                                return last

                            def mmu(e, fc=fc, bu=bu, wu=wu, k=k):
                                last = None
                                for kq in range(8):
                                    last = e.matmul(psf(bu), lhsT=wu[:, kq, fc * 128:(fc + 1) * 128], rhs=XT[k][:, kq, :],
                                                    start=(kq == 0), stop=(kq == 7))
                                return last
                            P.add("tensor", mmg, r=[("wg", sl)] + xk, w=[("ps", bg)])
                            P.add("tensor", mmu, r=[("wu", sl)] + xk, w=[("ps", bu)])
                            tmp = tmp_r[fc % 2]
                            P.add("scalar", (lambda tmp, bg: lambda e: e.activation(out=tmp, in_=psf(bg), func=AF.Silu))(tmp, bg),
                                  r=[("ps", bg)], w=[("tmp", fc % 2)])
                            P.add("vector", (lambda tmp, bu, fc, HT: lambda e: e.tensor_tensor(out=HT[:, fc, :], in0=psf(bu), in1=tmp, op=ALU.mult))(tmp, bu, fc, HT),
                                  r=[("ps", bu), ("tmp", fc % 2)], w=[("HT", hk, fc)])
                        for c in range(4):
                            for j in range(2):
                                b = psM.next()

                                def mmd(e, c=c, j=j, b=b, HT=HT, wd=wd):
                                    last = None
                                    for kq in range(4):
                                        last = e.matmul(psf(b), lhsT=HT[:, kq, c * 128:(c + 1) * 128], rhs=wd[:, kq, j * 512:(j + 1) * 512],
                                                        start=(kq == 0), stop=(kq == 3))
                                    return last
                                P.add("tensor", mmd, r=[("HT", hk, fc) for fc in range(4)] + [("wd", sl)], w=[("ps", b)])
                                dst = acc[k][:, c, j * 512:(j + 1) * 512]
                                if fg == 0:
                                    P.add("vector", (lambda dst, b: lambda e: e.tensor_copy(out=dst, in_=psf(b)))(dst, b),
                                          r=[("ps", b)], w=[("acc", k, c)])
                                else:
                                    P.add("vector", (lambda dst, b: lambda e: e.tensor_tensor(out=dst, in0=psf(b), in1=dst, op=ALU.add))(dst, b),
                                          r=[("ps", b), ("acc", k, c)], w=[("acc", k, c)])
                        P.pred_end()
                    gi += 1
                for k in range(4):
                    kk = rnd * 4 + k
                    P.pred_begin(ck, kk * 512 + 1)
                    row0 = e_ * S + kk * 512
                    P.add("sync", (lambda k, row0: lambda e: e.dma_start(
                        out=Yg_d[row0:row0 + 512, :].rearrange("(c p) n -> p c n", p=128), in_=acc[k]))(k, row0),
                        r=[("acc", k, c) for c in range(4)], w=[("Yg", e_, kk)], dkey="ys%d" % k)
                    P.pred_end()
        P.barrier()

        ar.reset(P_END)
        ya_r = [ar.alloc([1024], F32) for _ in range(2)]
        yb_r = [ar.alloc([1024], F32) for _ in range(2)]
        xr_r = [ar.alloc([1024], F32) for _ in range(2)]
        for T in range(NT):
            s2 = T % 2
            ya, yb, xr = ya_r[s2], yb_r[s2], xr_r[s2]
            P.add("gpsimd", (lambda ya, T: lambda e: e.indirect_dma_start(
                out=ya, out_offset=None, in_=Yg_d[:, :], in_offset=bass.IndirectOffsetOnAxis(ap=idxA[:, T, 0:1], axis=0)))(ya, T),
                w=[("ya", s2)], dkey="ga%d" % s2)
            P.add("gpsimd", (lambda yb, T: lambda e: e.indirect_dma_start(
                out=yb, out_offset=None, in_=Yg_d[:, :], in_offset=bass.IndirectOffsetOnAxis(ap=idxB[:, T, 0:1], axis=0)))(yb, T),
                w=[("yb", s2)], dkey="gb%d" % s2)
            P.add("sync", (lambda xr, T: lambda e: e.dma_start(out=xr, in_=src_d[T * 128:(T + 1) * 128, :]))(xr, T),
                  w=[("xr", s2)], dkey="lr%d" % s2)
            P.add("vector", (lambda ya, xr, T: lambda e: e.scalar_tensor_tensor(out=xr, in0=ya, scalar=gAB[:, T, 0:1], in1=xr, op0=ALU.mult, op1=ALU.add))(ya, xr, T),
                  r=[("ya", s2), ("xr", s2)], w=[("xr", s2)])
            P.add("vector", (lambda yb, xr, T: lambda e: e.scalar_tensor_tensor(out=xr, in0=yb, scalar=gBB[:, T, 0:1], in1=xr, op0=ALU.mult, op1=ALU.add))(yb, xr, T),
                  r=[("yb", s2), ("xr", s2)], w=[("xr", s2)])
            P.add("sync", (lambda xr, T: lambda e: e.dma_start(out=dst_d[T * 128:(T + 1) * 128, :], in_=xr))(xr, T),
                  r=[("xr", s2)], w=[("hD", T)], dkey="st%d" % s2)


    if os.environ.get("MOE_DENSE"):
        phase_D = phase_D_dense

    order = ["A", "B", "C", "D"]
    nph = order.index(stop_after) + 1
    srcs = [x_d, h1_d, h2_d, h3_d]
    dsts = [h1_d, h2_d, h3_d, out_d]
    dsts[nph - 1] = out_d
    fns = [phase_A, phase_B, phase_C, phase_D]
    for i in range(nph):
        fns[i](srcs[i], dsts[i])
        P.barrier()

    P.emit(es)
    es.close()
    return nc, list(declared.keys())


_CACHE = {}


def _consts():
    ident = np.eye(128, dtype=np.float32)
    tri = np.triu(np.ones((128, 128), dtype=np.float32))
    invf = (1.0 / (np.float32(500000.0) ** (np.arange(0, 16, 2, dtype=np.float32) / np.float32(16)))).astype(np.float32)
    return {
        "c_ident_bf": ident.astype(ml_dtypes.bfloat16),
        "c_ident_f": ident,
        "c_tri": tri,
        "c_invf": invf,
        "c_tris": np.triu(np.ones((128, 128), dtype=np.float32), 1),
        "c_eoff": (np.arange(8, dtype=np.float32) * 4096.0).astype(np.float32),
    }


def kernel(stop_after="D", **inputs):
    key = stop_after
    if key not in _CACHE:
        _CACHE[key] = build_program(stop_after)
    nc, used = _CACHE[key]
    consts = _consts()
    in_maps = []
    for b in range(8):
        m = {}
        for k, v in inputs.items():
            if k not in used:
                continue
            a = np.asarray(v)
            if k == "x":
                m[k] = np.ascontiguousarray(a[b])
            elif k == "positions":
                m[k] = np.ascontiguousarray(a[b]).astype(np.int32, copy=False)
            else:
                m[k] = np.ascontiguousarray(a)
        m.update({k: v for k, v in consts.items() if k in used})
        in_maps.append(m)
    res = run_bass_kernel_spmd(nc, in_maps, core_ids=list(range(8)))
    global _LAST
    _LAST = res
    return np.stack([np.asarray(r["out"]) for r in res.results], axis=0).astype(np.float32, copy=False)
```

```python
import math
import os
from contextlib import ExitStack

import ml_dtypes
import numpy as np

import concourse.bass as bass
import concourse.mybir as mybir
from concourse.bass_utils import run_bass_kernel_spmd

F32 = mybir.dt.float32
BF16 = mybir.dt.bfloat16
I32 = mybir.dt.int32
U8 = mybir.dt.uint8
AF = mybir.ActivationFunctionType
ALU = mybir.AluOpType
AX = mybir.AxisListType

S = 4096
D = 1024
NT = S // 128
NST = S // 512
EPS = 1e-6
LAMBDA_INIT = 0.8 - 0.6 * math.exp(-0.3 * 1)
FFN = 2816
NF0 = FFN // 128
NE = 8
FE = 3584
SIZEOF = {F32: 4, BF16: 2, I32: 4, U8: 1}
COMPUTE = ("tensor", "vector", "scalar", "gpsimd")
ALLENG = COMPUTE + ("sync",)


class Op:
    __slots__ = ("eng", "fn", "deps", "signal", "ticket", "is_dma", "dkey", "dval", "strict", "mark")


class Prog:
    def __init__(self, nc):
        self.nc = nc
        self.ops = {e: [] for e in ALLENG}
        self.lw = {}
        self.rd = {}
        self.dcount = {}
        self.last = {e: None for e in ALLENG}
        self.last_dma = {}

    def add(self, eng, fn, r=(), w=(), dkey=None, strict=False):
        op = Op()
        op.mark = None
        op.strict = strict
        op.eng = eng
        op.fn = fn
        op.signal = False
        op.ticket = 0
        op.is_dma = dkey is not None
        op.dkey = dkey
        op.dval = 0
        deps = []
        for k in r:
            x = self.lw.get(k)
            if x is not None:
                deps.append(x)
        for k in w:
            x = self.lw.get(k)
            if x is not None:
                deps.append(x)
            rr = self.rd.get(k)
            if rr:
                deps.extend(rr[0].values())
                deps.extend(rr[1])
        self._set_deps(op, deps)
        if op.is_dma:
            c = self.dcount.get(dkey, 0) + 1
            self.dcount[dkey] = c
            op.dval = 16 * c
            self.last_dma[dkey] = op
        for k in r:
            rr = self.rd.setdefault(k, ({}, []))
            if op.is_dma:
                rr[1].append(op)
            else:
                rr[0][eng] = op
        for k in w:
            self.lw[k] = op
            self.rd[k] = ({}, [])
        self.ops[eng].append(op)
        if not op.is_dma:
            self.last[eng] = op
        return op

    def _marker(self, mark):
        for e in ALLENG:
            op = Op()
            op.mark = mark
            op.strict = False
            op.eng = e
            op.fn = None
            op.signal = False
            op.ticket = 0
            op.is_dma = False
            op.dkey = None
            op.dval = 0
            op.deps = []
            self.ops[e].append(op)

    def regload(self, key, ap):
        self._marker(("regload", key, ap))

    def pred_begin(self, key, thr):
        self._marker(("begin", key, thr))

    def pred_end(self):
        self._marker(("end",))

    def chain(self, eng, fns, r=(), w=()):
        self.nchain = getattr(self, "nchain", 0) + 1
        key = ("chain", self.nchain)
        op = None
        for i, fn in enumerate(fns):
            op = self.add(eng, fn, r=list(r) + [key], w=list(w) + [key], strict=(i > 0))
        return op

    def _set_deps(self, op, deps):
        seen = set()
        out = []
        for d in deps:
            if d is op or id(d) in seen:
                continue
            seen.add(id(d))
            if (not d.is_dma) and (not op.is_dma) and d.eng == op.eng and not op.strict:
                continue
            out.append(d)
            if not d.is_dma:
                d.signal = True
        op.deps = out

    def barrier(self):
        lasts = [self.last[e] for e in COMPUTE if self.last[e] is not None]
        dmas = list(self.last_dma.values())
        for e in ALLENG:
            op = Op()
            op.mark = None
            op.strict = False
            op.eng = e
            op.fn = None
            op.signal = False
            op.ticket = 0
            op.is_dma = False
            op.dkey = None
            op.dval = 0
            deps = [d for d in lasts if d.eng != e] + dmas
            for d in deps:
                if not d.is_dma:
                    d.signal = True
            op.deps = deps
            self.ops[e].append(op)
        self.lw = {}
        self.rd = {}

    def emit(self, es):
        nc = self.nc
        for e in ALLENG:
            c = 0
            for op in self.ops[e]:
                if op.is_dma:
                    continue
                if op.signal:
                    c += 1
                    op.ticket = c
        psem = {e: es.enter_context(nc.semaphore("pg_" + e)) for e in COMPUTE}
        dsem = {}
        for i, k in enumerate(self.dcount):
            dsem[k] = es.enter_context(nc.semaphore("dq_%d" % i))
        block = es.enter_context(nc.Block())

        def mk(engname):
            def body(eng):
                waited = {}
                regs = {}

                def emit_op(op):
                    for d in op.deps:
                        if d.is_dma:
                            key = ("d", d.dkey)
                            sem = dsem[d.dkey]
                            val = d.dval
                        else:
                            key = ("p", d.eng)
                            sem = psem[d.eng]
                            val = d.ticket
                        if waited.get(key, 0) >= val:
                            continue
                        eng.wait_ge(sem, val)
                        waited[key] = val
                    if op.fn is None:
                        return
                    ins = op.fn(eng)
                    if op.is_dma:
                        ins.then_inc(dsem[op.dkey], 16)
                    elif op.signal:
                        ins.then_inc(psem[engname], 1)

                ops = self.ops[engname]

                def find_end(i):
                    depth = 0
                    j = i
                    while True:
                        m = ops[j].mark
                        if m is not None and m[0] == "begin":
                            depth += 1
                        elif m is not None and m[0] == "end":
                            depth -= 1
                            if depth == 0:
                                return j
                        j += 1

                def emit_range(i, end):
                    nonlocal waited
                    while i < end:
                        op = ops[i]
                        if op.mark is None:
                            emit_op(op)
                            i += 1
                            continue
                        kind = op.mark[0]
                        if kind == "regload":
                            _, key, ap = op.mark
                            if key not in regs:
                                regs[key] = eng.alloc_register("rg_%s_%s" % (engname, key))
                            eng.reg_load(regs[key], ap)
                            i += 1
                            continue
                        assert kind == "begin", kind
                        _, key, thr = op.mark
                        j = find_end(i)
                        region = [o for o in ops[i + 1:j] if o.mark is None]
                        nsig = sum(1 for o in region if (not o.is_dma) and o.signal)
                        dfirst = {}
                        dcnt = {}
                        for o in region:
                            if o.is_dma:
                                dfirst.setdefault(o.dkey, o.dval)
                                dcnt[o.dkey] = dcnt.get(o.dkey, 0) + 1
                        if region:
                            snap = dict(waited)
                            with eng.If_lt(regs[key], thr):
                                for k2, c2 in dcnt.items():
                                    prior = dfirst[k2] - 16
                                    if prior > 0:
                                        eng.wait_ge(dsem[k2], prior)
                                    eng.sem_inc(dsem[k2], 16 * c2)
                                if nsig:
                                    eng.drain().then_inc(psem[engname], nsig)
                            with eng.Else():
                                emit_range(i + 1, j)
                            waited = snap
                        i = j + 1

                emit_range(0, len(ops))
                if engname == "sync":
                    for k, c in self.dcount.items():
                        if waited.get(("d", k), 0) < 16 * c:
                            eng.wait_ge(dsem[k], 16 * c)
            return body

        block.tensor(mk("tensor"))
        block.vector(mk("vector"))
        block.scalar(mk("scalar"))
        block.gpsimd(mk("gpsimd"))
        block.sync(mk("sync"))


class Arena:
    def __init__(self, base_ap, nbytes):
        self.base = base_ap
        self.nbytes = nbytes
        self.off = 0

    def reset(self, off=0):
        self.off = off

    def alloc(self, shape, dt):
        n = SIZEOF[dt]
        for s in shape:
            n *= s
        start = (self.off + 63) // 64 * 64
        assert start + n <= self.nbytes, ("SBUF arena overflow", start + n, self.nbytes)
        self.off = start + n
        ap = self.base[:, start:start + n].bitcast(dt)
        if len(shape) == 2:
            ap = ap.rearrange("p (a b) -> p a b", a=shape[0])
        elif len(shape) == 3:
            ap = ap.rearrange("p (a b c) -> p a b c", a=shape[0], b=shape[1])
        return ap


class PsRing:
    def __init__(self, banks):
        self.banks = list(banks)
        self.i = 0

    def next(self):
        b = self.banks[self.i % len(self.banks)]
        self.i += 1
        return b


def build_program(stop_after="D"):
    nc = bass.Bass("TRN2", target_bir_lowering=False)
    es = ExitStack()

    def din(name, shape, dt=F32):
        return nc.dram_tensor(name, list(shape), dt, kind="ExternalInput").ap()

    specs = {
        "x": ([S, D], F32), "positions": ([S], I32),
        "l0_mix_norm": [D], "l0_sg_w_in": [D, 4096], "l0_sg_v_norm": [2048],
        "l0_sg_w_spatial": [8, 128, 128], "l0_sg_b_spatial": [8, 128], "l0_sg_w_out": [2048, D],
        "l0_ffn_norm": [D], "l0_ffn_w_gate": [D, FFN], "l0_ffn_w_up": [D, FFN], "l0_ffn_w_down": [FFN, D],
        "l1_mix_norm": [D], "l1_da_w_qkv": [D, 3072], "l1_da_q_norm": [64], "l1_da_k_norm": [64],
        "l1_da_lambda_q1": [64], "l1_da_lambda_k1": [64], "l1_da_lambda_q2": [64], "l1_da_lambda_k2": [64],
        "l1_da_subln": [128], "l1_da_w_out": [D, D],
        "l1_moe_norm": [D], "l1_moe_w_router": [D, 8], "l1_moe_w_gate": [8, D, FE], "l1_moe_w_up": [8, D, FE],
        "l1_moe_w_down": [8, FE, D],
        "c_ident_bf": ([128, 128], BF16), "c_ident_f": ([128, 128], F32), "c_tri": ([128, 128], F32),
        "c_invf": ([8], F32), "c_tris": ([128, 128], F32), "c_eoff": ([8], F32),
    }
    declared = {}

    class _W:
        def __getitem__(self, k):
            if k not in declared:
                sp = specs[k]
                if isinstance(sp, tuple):
                    declared[k] = din(k, sp[0], sp[1])
                else:
                    declared[k] = din(k, sp)
            return declared[k]
    w = _W()
    x_d = w["x"]
    ident_bf_d = w["c_ident_bf"]
    ident_f_d = w["c_ident_f"]
    tri_d = w["c_tri"]
    out_d = nc.dram_tensor("out", [S, D], F32, kind="ExternalOutput").ap()
    h1_d = nc.dram_tensor("h1_scr", [S, D], F32).ap()
    h2_d = nc.dram_tensor("h2_scr", [S, D], F32).ap()
    h3_d = nc.dram_tensor("h3_scr", [S, D], F32).ap()
    dbg_kind = "ExternalOutput" if os.environ.get("KDBG") else "Internal"
    qT_d = nc.dram_tensor("qT_scr", [8, 128, S], BF16, kind=dbg_kind).ap()
    kT_d = nc.dram_tensor("kT_scr", [8, 128, S], BF16, kind=dbg_kind).ap()
    v_d = nc.dram_tensor("v_scr", [S, 8, 128], BF16, kind=dbg_kind).ap()

    NSLOT = 8 * S
    Xg_d = nc.dram_tensor("xg_scr", [NSLOT, D], BF16).ap()
    Yg_d = nc.dram_tensor("yg_scr", [NSLOT, D], F32).ap()
    cnt_d = nc.dram_tensor("cnt_scr", [1, 8], I32).ap()
    dbg_d = nc.dram_tensor("dbg", [128, 8, 512], F32, kind=dbg_kind).ap()
    ARENA_BYTES = 199 * 1024
    arena_t = es.enter_context(nc.sbuf_tensor("arena", [128, ARENA_BYTES], U8))
    ar = Arena(arena_t, ARENA_BYTES)
    ps_t = es.enter_context(nc.psum_tensor("psum", [128, 8, 512], F32))

    def psf(b):
        return ps_t[:, b, :]

    def psb(b):
        return ps_t[:, b, :].bitcast(BF16)

    P = Prog(nc)

    ident_bf = ar.alloc([128], BF16)
    ident_f = ar.alloc([128], F32)
    P.add("sync", lambda e: e.dma_start(out=ident_bf, in_=ident_bf_d[:, :]), w=["ident_bf"], dkey="k0")
    P.add("sync", lambda e: e.dma_start(out=ident_f, in_=ident_f_d[:, :]), w=["ident_f"], dkey="k1")
    neghalf = ar.alloc([64], F32)
    P.add("gpsimd", lambda e: e.memset(neghalf, -0.5), w=["neghalf"])
    CONST_END = ar.off

    def col_load(dst, src, n):
        def f(e):
            with nc.allow_non_contiguous_dma(reason="tiny gain vector"):
                return e.dma_start(out=dst, in_=src.rearrange("(k p) -> p k", p=128))
        return f

    def norm_p1(xt, xs, ss, rstd, tag, keys_r, xs32=None):
        def sq(e):
            return e.activation(out=xs["junk"], in_=xt, func=AF.Square, accum_out=ss)
        P.add("gpsimd", lambda e: e.memset(ss, 0.0), w=[("ss", tag)])
        P.add("scalar", sq, r=keys_r + [("ss", tag)], w=[("ss", tag), ("junk", tag)])

        P.chain("gpsimd", [
            lambda e: e.tensor_scalar(out=rstd, in0=ss, scalar1=1.0 / D, scalar2=EPS, op0=ALU.mult, op1=ALU.add),
            lambda e: e.tensor_tensor(out=rstd, in0=rstd, in1=neghalf[:, 0:1], op=ALU.pow),
        ], r=[("ss", tag)], w=[("rstd", tag)])
        if xs32 is None:
            P.add("vector", lambda e: e.tensor_scalar(out=xs["bf"], in0=xt, scalar1=rstd[:, 0:1], scalar2=None, op0=ALU.mult),
                  r=keys_r + [("rstd", tag)], w=[("xsbf", tag)])
        else:
            P.add("vector", lambda e: e.tensor_scalar(out=xs32, in0=xt, scalar1=rstd[:, 0:1], scalar2=None, op0=ALU.mult),
                  r=keys_r + [("rstd", tag)], w=[("xs32", tag)])
            P.add("gpsimd", lambda e: e.tensor_copy(out=xs["bf"], in_=xs32), r=[("xs32", tag)], w=[("xsbf", tag)])

    def norm_p2(gcol, xs, xnT_dst, tag, psT, keys_w):
        b = psT.next()

        def tr(e):
            last = None
            for k in range(8):
                last = e.transpose(psb(b)[:, k * 128:(k + 1) * 128], xs["bf"][:, k * 128:(k + 1) * 128], ident_bf)
            return last
        P.add("tensor", tr, r=[("xsbf", tag), "ident_bf"], w=[("ps", b)])

        def ev(e):
            return e.tensor_tensor(out=xnT_dst, in0=psb(b).rearrange("p (k t) -> p k t", k=8),
                                   in1=gcol.unsqueeze(2).to_broadcast([128, 8, 128]), op=ALU.mult)
        P.add("vector", ev, r=[("ps", b), "gcol"], w=keys_w)

    def norm_tile(xt, gcol, xs, ss, rstd, xnT_dst, tag, psT, keys_r, keys_w):
        norm_p1(xt, xs, ss, rstd, tag, keys_r)
        norm_p2(gcol, xs, xnT_dst, tag, psT, keys_w)

    dk_ctr = [0]

    def dk():
        dk_ctr[0] += 1
        return "c%d" % dk_ctr[0]

    def phase_A(src_d, dst_d):
        ar.reset(CONST_END)
        win = ar.alloc([8, 4096], BF16)
        wout = ar.alloc([16, 1024], BF16)
        wsp_f = ar.alloc([8, 128], F32)
        wmT = ar.alloc([8, 128], BF16)
        tri = ar.alloc([128], F32)
        biasT = ar.alloc([8, 128], F32)
        gcol = ar.alloc([8], F32)
        gv = ar.alloc([16], F32)
        xt_ring = [ar.alloc([1024], F32) for _ in range(2)]
        xr_ring = [ar.alloc([1024], F32) for _ in range(2)]
        xs_ring = [{"bf": ar.alloc([1024], BF16), "junk": None} for _ in range(2)]
        junk = ar.alloc([1024], BF16)
        for d_ in xs_ring:
            d_["junk"] = junk
        ss_r = [ar.alloc([1], F32) for _ in range(2)]
        rstd_r = [ar.alloc([1], F32) for _ in range(2)]
        xnT = ar.alloc([8, 512], BF16)
        uT = ar.alloc([16, 512], BF16)
        vbf = ar.alloc([4, 2048], BF16)
        ssv_r = [ar.alloc([4], F32) for _ in range(2)]
        rsv_r = [ar.alloc([1], F32) for _ in range(2)]
        wms_r = [ar.alloc([8, 128], BF16) for _ in range(4)]
        yT = ar.alloc([16, 512], BF16)
        tmp_r = [ar.alloc([512], F32) for _ in range(2)]

        w_in = w["l0_sg_w_in"].rearrange("(k p) n -> p k n", p=128)
        for k in range(8):
            P.add("gpsimd", (lambda k: lambda e: e.dma_start(out=win[:, k, :], in_=w_in[:, k, :]))(k),
                  w=[("win", k)], dkey="w%d" % k)
        w_o = w["l0_sg_w_out"].rearrange("(k p) n -> p k n", p=128)
        for q in range(4):
            P.add("gpsimd", (lambda q: lambda e: e.dma_start(out=wout[:, 4 * q:4 * q + 4, :], in_=w_o[:, 4 * q:4 * q + 4, :]))(q),
                  w=[("wout", q)], dkey="w%d" % (8 + q))
        P.add("sync", lambda e: e.dma_start(out=wsp_f, in_=w["l0_sg_w_spatial"].rearrange("g t s -> t g s")), w=["wsp_f"], dkey="c0")
        P.add("sync", lambda e: e.dma_start(out=tri, in_=tri_d[:, :]), w=["tri"], dkey="c1")
        P.add("sync", lambda e: e.dma_start(out=biasT.rearrange("p g t -> p (g t)"),
                                            in_=w["l0_sg_b_spatial"].rearrange("g t -> (g t)").partition_broadcast(128)),
              w=["biasT"], dkey="c2")
        P.add("sync", col_load(gcol, w["l0_mix_norm"], 8), w=["gcol"], dkey="c3")
        P.add("sync", col_load(gv, w["l0_sg_v_norm"], 16), w=["gv"], dkey="c4")

        bT = 0

        def wtr(e):
            last = None
            for g in range(8):
                last = e.transpose(ps_t[:, g // 4, (g % 4) * 128:(g % 4 + 1) * 128], wsp_f[:, g, :], ident_f)
            return last
        P.add("tensor", wtr, r=["wsp_f", "ident_f"], w=[("ps", 0), ("ps", 1)])

        def wmk(e):
            return e.tensor_tensor(out=wmT, in0=ps_t[:, 0:2, :].rearrange("p a (b t) -> p (a b) t", b=4),
                                   in1=tri.unsqueeze(1).to_broadcast([128, 8, 128]), op=ALU.mult)
        P.add("vector", wmk, r=[("ps", 0), ("ps", 1), "tri"], w=["wmT"])

        psT = PsRing([0, 1])
        psM = PsRing([2, 3, 4, 5, 6, 7])

        for st in range(NST):
            for c in range(4):
                T = st * 4 + c
                xt = xt_ring[T % 2]
                P.add("sync", (lambda xt, T: lambda e: e.dma_start(out=xt, in_=src_d[T * 128:(T + 1) * 128, :]))(xt, T),
                      w=[("xt", T % 2)], dkey="ld%d" % (T % 2))
                norm_tile(xt, gcol, xs_ring[T % 2], ss_r[T % 2], rstd_r[T % 2],
                          xnT[:, :, c * 128:(c + 1) * 128], ("A", T % 2), psT,
                          keys_r=[("xt", T % 2)], keys_w=[("xnT", c)])
            for m in range(16):
                b = psM.next()

                def mm(e, m=m, b=b):
                    last = None
                    for k in range(8):
                        last = e.matmul(psf(b), lhsT=win[:, k, m * 128:(m + 1) * 128], rhs=xnT[:, k, :],
                                        start=(k == 0), stop=(k == 7))
                    return last
                P.add("tensor", mm, r=[("win", k) for k in range(8)] + [("xnT", c) for c in range(4)], w=[("ps", b)])
                P.add("scalar", (lambda m, b: lambda e: e.activation(out=uT[:, m, :], in_=psf(b), func=AF.Gelu_apprx_tanh))(m, b),
                      r=[("ps", b)], w=[("uT", m)])
            for c in range(4):
                T = st * 4 + c
                ssv = ssv_r[T % 2]
                rsv = rsv_r[T % 2]
                P.add("gpsimd", (lambda ssv: lambda e: e.memset(ssv, 0.0))(ssv), w=[("ssv", T % 2)])
                for j in range(4):
                    b = psM.next()

                    def mmv(e, c=c, j=j, b=b):
                        last = None
                        for k in range(8):
                            last = e.matmul(psf(b), lhsT=xnT[:, k, c * 128:(c + 1) * 128],
                                            rhs=win[:, k, 2048 + j * 512:2048 + (j + 1) * 512],
                                            start=(k == 0), stop=(k == 7))
                        return last
                    P.add("tensor", mmv, r=[("win", k) for k in range(8)] + [("xnT", c)], w=[("ps", b)])
                    P.add("scalar", (lambda c, j, b: lambda e: e.activation(out=vbf[:, c, j * 512:(j + 1) * 512], in_=psf(b),
                                                                             func=AF.Gelu_apprx_tanh))(c, j, b),
                          r=[("ps", b)], w=[("vbf", c, j)])
                    P.add("scalar", (lambda c, j, ssv: lambda e: e.activation(out=junk[:, 0:512], in_=vbf[:, c, j * 512:(j + 1) * 512],
                                                                               func=AF.Square, accum_out=ssv[:, j:j + 1]))(c, j, ssv),
                          r=[("vbf", c, j), ("ssv", T % 2)], w=[("ssv", T % 2), ("junkA",)])

                P.chain("gpsimd", [
                    (lambda ssv, rsv: lambda e: e.tensor_tensor(out=rsv, in0=ssv[:, 0:1], in1=ssv[:, 1:2], op=ALU.add))(ssv, rsv),
                    (lambda ssv, rsv: lambda e: e.tensor_tensor(out=rsv, in0=rsv, in1=ssv[:, 2:3], op=ALU.add))(ssv, rsv),
                    (lambda ssv, rsv: lambda e: e.tensor_tensor(out=rsv, in0=rsv, in1=ssv[:, 3:4], op=ALU.add))(ssv, rsv),
                    (lambda ssv, rsv: lambda e: e.tensor_scalar(out=rsv, in0=rsv, scalar1=1.0 / 2048, scalar2=EPS, op0=ALU.mult, op1=ALU.add))(ssv, rsv),
                    (lambda ssv, rsv: lambda e: e.tensor_tensor(out=rsv, in0=rsv, in1=neghalf[:, 0:1], op=ALU.pow))(ssv, rsv),
                ], r=[("ssv", T % 2)], w=[("rsv", T % 2)])
                wms = wms_r[T % 4]
                P.add("gpsimd", (lambda wms, rsv: lambda e: e.tensor_scalar(out=wms, in0=wmT, scalar1=rsv[:, 0:1], scalar2=None,
                                                                             op0=ALU.mult))(wms, rsv),
                      r=["wmT", ("rsv", T % 2)], w=[("wms", T % 4)], strict=True)
            for m in range(16):
                g = m // 2
                b = psM.next()

                def mmx(e, m=m, g=g, b=b):
                    last = None
                    for c in range(4):
                        T = st * 4 + c
                        last = e.matmul(psf(b)[:, c * 128:(c + 1) * 128], lhsT=vbf[:, c, m * 128:(m + 1) * 128],
                                        rhs=wms_r[T % 4][:, g, :], start=True, stop=True)
                    return last
                P.add("tensor", mmx, r=[("vbf", c, m // 4) for c in range(4)] + [("wms", (st * 4 + c) % 4) for c in range(4)],
                      w=[("ps", b)])
                tmp = tmp_r[m % 2]

                def e1(e, m=m, g=g, b=b, tmp=tmp):
                    return e.scalar_tensor_tensor(out=tmp.rearrange("p (c t) -> p c t", c=4),
                                                  in0=psf(b).rearrange("p (c t) -> p c t", c=4),
                                                  scalar=gv[:, m:m + 1],
                                                  in1=biasT[:, g, :].unsqueeze(1).to_broadcast([128, 4, 128]),
                                                  op0=ALU.mult, op1=ALU.add)
                P.add("vector", e1, r=[("ps", b), "gv", "biasT"], w=[("tmp", m % 2)])
                P.add("vector", (lambda m, tmp: lambda e: e.tensor_tensor(out=yT[:, m, :], in0=tmp, in1=uT[:, m, :], op=ALU.mult))(m, tmp),
                      r=[("tmp", m % 2), ("uT", m)], w=[("yT", m)])
            for c in range(4):
                T = st * 4 + c
                xr = xr_ring[T % 2]
                P.add("sync", (lambda xr, T: lambda e: e.dma_start(out=xr, in_=src_d[T * 128:(T + 1) * 128, :]))(xr, T),
                      w=[("xr", T % 2)], dkey="lr%d" % (T % 2))
                for j in range(2):
                    b = psM.next()

                    def mmo(e, c=c, j=j, b=b):
                        last = None
                        for k in range(16):
                            last = e.matmul(psf(b), lhsT=yT[:, k, c * 128:(c + 1) * 128], rhs=wout[:, k, j * 512:(j + 1) * 512],
                                            start=(k == 0), stop=(k == 15))
                        return last
                    P.add("tensor", mmo, r=[("yT", m) for m in range(16)] + [("wout", q) for q in range(4)], w=[("ps", b)])
                    P.add("vector", (lambda xr, j, b: lambda e: e.tensor_tensor(out=xr[:, j * 512:(j + 1) * 512], in0=psf(b),
                                                                                 in1=xr[:, j * 512:(j + 1) * 512], op=ALU.add))(xr, j, b),
                          r=[("ps", b), ("xr", T % 2)], w=[("xr", T % 2)])
                P.add("sync", (lambda xr, T: lambda e: e.dma_start(out=dst_d[T * 128:(T + 1) * 128, :], in_=xr))(xr, T),
                      r=[("xr", T % 2)], w=[("hA", T)], dkey="st%d" % (T % 2))


    def phase_B(src_d, dst_d):
        ar.reset(CONST_END)
        dk_ctr[0] = 0
        wg = ar.alloc([8, FFN], BF16)
        wu = ar.alloc([8, FFN], BF16)
        wd = ar.alloc([NF0, 1024], BF16)
        gcol = ar.alloc([8], F32)
        xt_ring = [ar.alloc([1024], F32) for _ in range(2)]
        xr_ring = [ar.alloc([1024], F32) for _ in range(2)]
        junk = ar.alloc([1024], BF16)
        xs_ring = [{"bf": ar.alloc([1024], BF16), "junk": junk} for _ in range(4)]
        ss_r = [ar.alloc([1], F32) for _ in range(4)]
        rstd_r = [ar.alloc([1], F32) for _ in range(4)]
        xnT = ar.alloc([8, 512], BF16)
        HT = ar.alloc([NF0, 512], BF16)
        tmp_r = [ar.alloc([512], F32) for _ in range(2)]

        wgd = w["l0_ffn_w_gate"].rearrange("(k p) n -> p k n", p=128)
        wud = w["l0_ffn_w_up"].rearrange("(k p) n -> p k n", p=128)
        wdd = w["l0_ffn_w_down"].rearrange("(k p) n -> p k n", p=128)
        P.add("sync", col_load(gcol, w["l0_ffn_norm"], 8), w=["gcol"], dkey=dk())
        for k in range(8):
            P.add("gpsimd", (lambda k: lambda e: e.dma_start(out=wg[:, k, :], in_=wgd[:, k, :]))(k), w=[("wg", k)], dkey="w%d" % k)
            P.add("gpsimd", (lambda k: lambda e: e.dma_start(out=wu[:, k, :], in_=wud[:, k, :]))(k), w=[("wu", k)], dkey="w%d" % (8 + k))
        for q in range(2):
            P.add("gpsimd", (lambda q: lambda e: e.dma_start(out=wd[:, 11 * q:11 * q + 11, :], in_=wdd[:, 11 * q:11 * q + 11, :]))(q),
                  w=[("wd", q)], dkey="w%d" % (16 + q))
        psT = PsRing([0, 1])
        psM = PsRing([2, 3, 4, 5, 6, 7])

        def p1(st):
            for c in range(4):
                T = st * 4 + c
                xt = xt_ring[T % 2]
                P.add("sync", (lambda xt, T: lambda e: e.dma_start(out=xt, in_=src_d[T * 128:(T + 1) * 128, :]))(xt, T),
                      w=[("xt", T % 2)], dkey="ld%d" % (T % 2))
                norm_p1(xt, xs_ring[c], ss_r[c], rstd_r[c], ("B", c), [("xt", T % 2)])

        def p2(st):
            for c in range(4):
                norm_p2(gcol, xs_ring[c], xnT[:, :, c * 128:(c + 1) * 128], ("B", c), psT, [("xnT", c)])

        p1(0)
        for st in range(NST):
            p2(st)
            for m in range(NF0):
                bg = psM.next()
                bu = psM.next()

                def mmg(e, m=m, bg=bg):
                    last = None
                    for k in range(8):
                        last = e.matmul(psf(bg), lhsT=wg[:, k, m * 128:(m + 1) * 128], rhs=xnT[:, k, :], start=(k == 0), stop=(k == 7))
                    return last

                def mmu(e, m=m, bu=bu):
                    last = None
                    for k in range(8):
                        last = e.matmul(psf(bu), lhsT=wu[:, k, m * 128:(m + 1) * 128], rhs=xnT[:, k, :], start=(k == 0), stop=(k == 7))
                    return last
                xk = [("xnT", c) for c in range(4)]
                P.add("tensor", mmg, r=[("wg", k) for k in range(8)] + xk, w=[("ps", bg)])
                P.add("tensor", mmu, r=[("wu", k) for k in range(8)] + xk, w=[("ps", bu)])
                tmp = tmp_r[m % 2]
                P.add("scalar", (lambda tmp, bg: lambda e: e.activation(out=tmp, in_=psf(bg), func=AF.Silu))(tmp, bg),
                      r=[("ps", bg)], w=[("tmp", m % 2)])
                P.add("vector", (lambda tmp, bu, m: lambda e: e.tensor_tensor(out=HT[:, m, :], in0=psf(bu), in1=tmp, op=ALU.mult))(tmp, bu, m),
                      r=[("ps", bu), ("tmp", m % 2)], w=[("HT", m)])
            if st + 1 < NST:
                p1(st + 1)
            for c in range(4):
                T = st * 4 + c
                xr = xr_ring[T % 2]
                P.add("sync", (lambda xr, T: lambda e: e.dma_start(out=xr, in_=src_d[T * 128:(T + 1) * 128, :]))(xr, T),
                      w=[("xr", T % 2)], dkey="lr%d" % (T % 2))
                for j in range(2):
                    b = psM.next()

                    def mmd(e, c=c, j=j, b=b):
                        last = None
                        for k in range(NF0):
                            last = e.matmul(psf(b), lhsT=HT[:, k, c * 128:(c + 1) * 128], rhs=wd[:, k, j * 512:(j + 1) * 512],
                                            start=(k == 0), stop=(k == NF0 - 1))
                        return last
                    P.add("tensor", mmd, r=[("HT", m) for m in range(NF0)] + [("wd", 0), ("wd", 1)], w=[("ps", b)])
                    P.add("vector", (lambda xr, j, b: lambda e: e.tensor_tensor(out=xr[:, j * 512:(j + 1) * 512], in0=psf(b),
                                                                                 in1=xr[:, j * 512:(j + 1) * 512], op=ALU.add))(xr, j, b),
                          r=[("ps", b), ("xr", T % 2)], w=[("xr", T % 2)])
                P.add("sync", (lambda xr, T: lambda e: e.dma_start(out=dst_d[T * 128:(T + 1) * 128, :], in_=xr))(xr, T),
                      r=[("xr", T % 2)], w=[("hB", T)], dkey="st%d" % (T % 2))


    def phase_C(src_d, dst_d):
        ar.reset(CONST_END)
        dk_ctr[0] = 0
        wqkv = ar.alloc([8, 3072], BF16)
        gcol = ar.alloc([8], F32)
        gqk = ar.alloc([2, 64], F32)
        posi = ar.alloc([NT], I32)
        posf = ar.alloc([NT], F32)
        invf = ar.alloc([8], F32)
        ang = ar.alloc([NT, 8], F32)
        angk = ar.alloc([NT, 8], I32)
        angf = ar.alloc([NT, 8], F32)
        angm = ar.alloc([NT, 8], F32)
        sinT = ar.alloc([NT, 8], F32)
        cosT = ar.alloc([NT, 8], F32)
        xt_ring = [ar.alloc([1024], F32) for _ in range(2)]
        junk = ar.alloc([1024], BF16)
        xs_ring = [{"bf": ar.alloc([1024], BF16), "junk": junk} for _ in range(2)]
        ss_r = [ar.alloc([1], F32) for _ in range(2)]
        rstd_r = [ar.alloc([1], F32) for _ in range(2)]
        xnT_r = [ar.alloc([8, 128], BF16) for _ in range(2)]
        qk_r = [ar.alloc([32, 64], F32) for _ in range(2)]
        sq = ar.alloc([32, 64], F32)
        ssq_r = [ar.alloc([32], F32) for _ in range(2)]
        qkb_r = [ar.alloc([32, 64], BF16) for _ in range(2)]
        rt = [ar.alloc([32, 8], F32) for _ in range(4)]
        qT_st = [ar.alloc([8, 512], BF16) for _ in range(2)]
        kT_st = [ar.alloc([8, 512], BF16) for _ in range(2)]
        vb_r = [ar.alloc([1024], BF16) for _ in range(2)]

        wq_d = w["l1_da_w_qkv"].rearrange("(k p) n -> p k n", p=128)
        for k in range(8):
            P.add("gpsimd", (lambda k: lambda e: e.dma_start(out=wqkv[:, k, :], in_=wq_d[:, k, :]))(k), w=[("wqkv", k)], dkey="w%d" % k)
        P.add("sync", col_load(gcol, w["l1_mix_norm"], 8), w=["gcol"], dkey=dk())
        P.add("sync", lambda e: e.dma_start(out=gqk[:, 0, :], in_=w["l1_da_q_norm"].partition_broadcast(128)), w=["gqk0"], dkey=dk())
        P.add("sync", lambda e: e.dma_start(out=gqk[:, 1, :], in_=w["l1_da_k_norm"].partition_broadcast(128)), w=["gqk1"], dkey=dk())
        P.add("sync", lambda e: e.dma_start(out=invf, in_=w["c_invf"].partition_broadcast(128)), w=["invf"], dkey=dk())

        def posld(e):
            with nc.allow_non_contiguous_dma(reason="positions to token-major columns"):
                return e.dma_start(out=posi, in_=w["positions"].rearrange("(t p) -> p t", p=128))
        P.add("sync", posld, w=["posi"], dkey=dk())
        P.add("vector", lambda e: e.tensor_scalar(out=gqk[:, 0, :], in0=gqk[:, 0, :], scalar1=0.125, scalar2=None, op0=ALU.mult),
              r=["gqk0"], w=["gqk0"])

        TWO_PI = 2.0 * math.pi

        def trig(dst, shift, tagk):
            def f(e):
                a3 = ang.rearrange("p t i -> p (t i)")
                k3 = angk.rearrange("p t i -> p (t i)")
                f3 = angf.rearrange("p t i -> p (t i)")
                m3 = angm.rearrange("p t i -> p (t i)")
                e.tensor_scalar(out=f3, in0=a3, scalar1=shift, scalar2=None, op0=ALU.add)
                e.tensor_scalar(out=k3, in0=f3, scalar1=1.0 / TWO_PI, scalar2=None, op0=ALU.mult)
                e.tensor_copy(out=m3, in_=k3)
                e.scalar_tensor_tensor(out=f3, in0=m3, scalar=-TWO_PI, in1=f3, op0=ALU.mult, op1=ALU.add)
                e.tensor_scalar(out=m3, in0=f3, scalar1=math.pi, scalar2=-TWO_PI, op0=ALU.is_gt, op1=ALU.mult)
                e.tensor_tensor(out=f3, in0=f3, in1=m3, op=ALU.add)
                e.tensor_scalar(out=m3, in0=f3, scalar1=-math.pi, scalar2=TWO_PI, op0=ALU.is_lt, op1=ALU.mult)
                e.tensor_tensor(out=f3, in0=f3, in1=m3, op=ALU.add)
                return e.tensor_scalar(out=f3, in0=f3, scalar1=3.1415925, scalar2=-3.1415925, op0=ALU.min, op1=ALU.max)
            P.add("vector", f, r=["ang"], w=["angf"])
            P.add("scalar", lambda e: e.activation(out=dst.rearrange("p t i -> p (t i)"), in_=angf.rearrange("p t i -> p (t i)"), func=AF.Sin),
                  r=["angf"], w=[tagk])

        P.add("vector", lambda e: e.tensor_copy(out=posf, in_=posi), r=["posi"], w=["posf"])
        P.add("vector", lambda e: e.tensor_tensor(out=ang, in0=posf.unsqueeze(2).to_broadcast([128, NT, 8]),
                                                  in1=invf.unsqueeze(1).to_broadcast([128, NT, 8]), op=ALU.mult),
              r=["posf", "invf"], w=["ang"])
        trig(sinT, 0.0, "sinT")
        trig(cosT, 0.5 * math.pi, "cosT")

        psT = PsRing([0, 1])
        psQ = PsRing([2, 3])
        psM = PsRing([4, 5, 6, 7])
        qT_v = qT_d.rearrange("h p s -> p h s")
        kT_v = kT_d.rearrange("h p s -> p h s")
        v_v = v_d.rearrange("s h d -> s (h d)")

        for T in range(NT):
            st, c = T // 4, T % 4
            s2 = T % 2
            xt = xt_ring[s2]
            P.add("sync", (lambda xt, T: lambda e: e.dma_start(out=xt, in_=src_d[T * 128:(T + 1) * 128, :]))(xt, T),
                  w=[("xt", s2)], dkey="ld%d" % s2)
            xnT = xnT_r[s2]
            norm_tile(xt, gcol, xs_ring[s2], ss_r[s2], rstd_r[s2], xnT, ("C", s2), psT, [("xt", s2)], [("xnT", s2)])
            qk = qk_r[s2]
            qkf = qk.rearrange("p g d -> p (g d)")
            sqf = sq.rearrange("p g d -> p (g d)")
            vb = vb_r[s2]
            for jb in range(6):
                b = psM.next()

                def mm(e, jb=jb, b=b, xnT=xnT):
                    last = None
                    for k in range(8):
                        last = e.matmul(psf(b), lhsT=xnT[:, k, :], rhs=wqkv[:, k, jb * 512:(jb + 1) * 512], start=(k == 0), stop=(k == 7))
                    return last
                P.add("tensor", mm, r=[("wqkv", k) for k in range(8)] + [("xnT", s2)], w=[("ps", b)])
                if jb < 4:
                    P.add("scalar", (lambda jb, b, qkf: lambda e: e.activation(out=qkf[:, jb * 512:(jb + 1) * 512], in_=psf(b), func=AF.Copy))(jb, b, qkf),
                          r=[("ps", b)], w=[("qk", s2, jb)])
                    P.add("scalar", (lambda jb, b: lambda e: e.activation(out=sqf[:, jb * 512:(jb + 1) * 512], in_=psf(b), func=AF.Square))(jb, b),
                          r=[("ps", b)], w=[("sq", jb)])
                else:
                    P.add("scalar", (lambda jb, b, vb: lambda e: e.activation(out=vb[:, (jb - 4) * 512:(jb - 3) * 512], in_=psf(b), func=AF.Copy))(jb, b, vb),
                          r=[("ps", b)], w=[("vb", s2, jb)])
            ssq = ssq_r[s2]
            P.add("vector", (lambda ssq: lambda e: e.tensor_reduce(out=ssq, in_=sq, axis=AX.X, op=ALU.add))(ssq),
                  r=[("sq", j) for j in range(4)], w=[("ssq", s2)])

            P.chain("gpsimd", [
                (lambda ssq: lambda e: e.tensor_scalar(out=ssq, in0=ssq, scalar1=1.0 / 64, scalar2=EPS, op0=ALU.mult, op1=ALU.add))(ssq),
                (lambda ssq: lambda e: e.tensor_tensor(out=ssq, in0=ssq, in1=neghalf[:, 0:32], op=ALU.pow))(ssq),
            ], r=[("ssq", s2)], w=[("ssq", s2)])
            qkeys = [("qk", s2, j) for j in range(4)]
            P.add("vector", (lambda qk, ssq: lambda e: e.tensor_tensor(out=qk, in0=qk, in1=ssq.unsqueeze(2).to_broadcast([128, 32, 64]), op=ALU.mult))(qk, ssq),
                  r=qkeys + [("ssq", s2)], w=qkeys)

            def gmul(e, qk=qk):
                q4 = qk.rearrange("p (a g) d -> p a g d", a=2)
                return e.tensor_tensor(out=q4, in0=q4, in1=gqk.unsqueeze(2).to_broadcast([128, 2, 16, 64]), op=ALU.mult)
            P.add("gpsimd", gmul, r=qkeys + ["gqk0", "gqk1"], w=qkeys)
            qkb = qkb_r[s2]
            P.add("scalar", (lambda qkb, qkf: lambda e: e.activation(out=qkb.rearrange("p g d -> p (g d)"), in_=qkf, func=AF.Copy))(qkb, qkf),
                  r=qkeys, w=[("qkb", s2)])

            def rope(e, qk=qk, qkb=qkb, T=T):
                cs = cosT[:, T, :].unsqueeze(1).to_broadcast([128, 32, 8])
                sn = sinT[:, T, :].unsqueeze(1).to_broadcast([128, 32, 8])
                x1 = qk[:, :, 0:8]
                x2 = qk[:, :, 8:16]
                e.tensor_tensor(out=rt[0], in0=x1, in1=cs, op=ALU.mult)
                e.tensor_tensor(out=rt[1], in0=x2, in1=sn, op=ALU.mult)
                e.tensor_tensor(out=rt[2], in0=x2, in1=cs, op=ALU.mult)
                e.tensor_tensor(out=rt[3], in0=x1, in1=sn, op=ALU.mult)
                e.tensor_tensor(out=qkb[:, :, 0:8], in0=rt[0], in1=rt[1], op=ALU.subtract)
                return e.tensor_tensor(out=qkb[:, :, 8:16], in0=rt[2], in1=rt[3], op=ALU.add)
            P.add("gpsimd", rope, r=qkeys + [("qkb", s2), "sinT", "cosT"], w=[("qkb", s2)])
            bq = psQ.next()
            bk = psQ.next()

            def trq(e, qkb=qkb, bq=bq, bk=bk):
                last = None
                for h in range(8):
                    e.transpose(psb(bq)[:, h * 128:(h + 1) * 128], qkb[:, 2 * h:2 * h + 2, :].rearrange("p a d -> p (a d)"), ident_bf)
                    last = e.transpose(psb(bk)[:, h * 128:(h + 1) * 128], qkb[:, 16 + 2 * h:18 + 2 * h, :].rearrange("p a d -> p (a d)"), ident_bf)
                return last
            P.add("tensor", trq, r=[("qkb", s2), "ident_bf"], w=[("ps", bq), ("ps", bk)])
            qs = qT_st[st % 2]
            ks = kT_st[st % 2]
            P.add("scalar", (lambda qs, bq, c: lambda e: e.activation(out=qs[:, :, c * 128:(c + 1) * 128],
                                                                       in_=psb(bq).rearrange("p (h t) -> p h t", h=8), func=AF.Copy))(qs, bq, c),
                  r=[("ps", bq)], w=[("qs", st % 2, c)])
            P.add("vector", (lambda ks, bk, c: lambda e: e.tensor_copy(out=ks[:, :, c * 128:(c + 1) * 128],
                                                                        in_=psb(bk).rearrange("p (h t) -> p h t", h=8)))(ks, bk, c),
                  r=[("ps", bk)], w=[("ks", st % 2, c)])
            P.add("sync", (lambda vb, T: lambda e: e.dma_start(out=v_v[T * 128:(T + 1) * 128, :], in_=vb))(vb, T),
                  r=[("vb", s2, 4), ("vb", s2, 5)], w=[("v_d", T)], dkey="sv%d" % s2)
            if c == 3:
                P.add("sync", (lambda qs, st: lambda e: e.dma_start(out=qT_v[:, :, st * 512:(st + 1) * 512], in_=qs))(qs, st),
                      r=[("qs", st % 2, cc) for cc in range(4)], w=[("qT_d", st)], dkey="sq%d" % (st % 2))
                P.add("sync", (lambda ks, st: lambda e: e.dma_start(out=kT_v[:, :, st * 512:(st + 1) * 512], in_=ks))(ks, st),
                      r=[("ks", st % 2, cc) for cc in range(4)], w=[("kT_d", st)], dkey="sk%d" % (st % 2))
        P.barrier()

        ar.reset(CONST_END)
        dk_ctr[0] = 0
        OnT = ar.alloc([8, S], BF16)
        KT_r = [ar.alloc([S], BF16) for _ in range(2)]
        QT_r = [ar.alloc([S], BF16) for _ in range(2)]
        VA_r = [ar.alloc([NT, 132], BF16) for _ in range(2)]
        E_r = [ar.alloc([512], BF16) for _ in range(4)]
        tri_f = ar.alloc([128], F32)
        tri_b = ar.alloc([128], BF16)
        maskneg = tri_b
        lamv = ar.alloc([4, 64], F32)
        lamp = ar.alloc([2, 64], F32)
        lsum = [ar.alloc([1], F32) for _ in range(2)]
        lexp = [ar.alloc([1], F32) for _ in range(2)]
        nlam = ar.alloc([1], F32)
        gb = ar.alloc([2, 64], F32)
        mx = [ar.alloc([1], F32) for _ in range(2)]
        negB = ar.alloc([1], F32)
        sg = ar.alloc([1], F32)
        O_r = [ar.alloc([128], F32) for _ in range(4)]
        rr_r = [[ar.alloc([1], F32) for _ in range(4)] for _ in range(4)]
        Onb_r = [ar.alloc([128], BF16) for _ in range(2)]
        junkO = ar.alloc([128], BF16)
        wo = ar.alloc([8, 1024], BF16)
        xr_ring = [ar.alloc([1024], F32) for _ in range(2)]
        dbg_s = ar.alloc([8, 512], F32)
        P.add("gpsimd", lambda e: e.memset(dbg_s.rearrange("p a b -> p (a b)"), 0.0), w=["dbg%d" % z for z in range(7)])

        wo_d = w["l1_da_w_out"].rearrange("(k p) n -> p k n", p=128)
        P.add("gpsimd", lambda e: e.dma_start(out=wo, in_=wo_d), w=["wo"], dkey="w0")
        P.add("sync", lambda e: e.dma_start(out=tri_f, in_=tri_d[:, :]), w=["tri_f"], dkey=dk())
        P.add("vector", lambda e: e.tensor_scalar(out=tri_b, in0=tri_f, scalar1=-1.0, scalar2=30000.0, op0=ALU.add, op1=ALU.mult),
              r=["tri_f"], w=["maskneg"])
        for i, nm in enumerate(["l1_da_lambda_q1", "l1_da_lambda_q2", "l1_da_lambda_k1", "l1_da_lambda_k2"]):
            P.add("sync", (lambda i, nm: lambda e: e.dma_start(out=lamv[:, i, :], in_=w[nm].partition_broadcast(128)))(i, nm),
                  w=[("lamv", i)], dkey=dk())
        P.add("sync", lambda e: e.dma_start(out=gb[:, 0, :], in_=w["l1_da_q_norm"].partition_broadcast(128)), w=[("gb", 0)], dkey=dk())
        P.add("sync", lambda e: e.dma_start(out=gb[:, 1, :], in_=w["l1_da_k_norm"].partition_broadcast(128)), w=[("gb", 1)], dkey=dk())

        def sgld(e):
            with nc.allow_non_contiguous_dma(reason="tiny gain column"):
                return e.dma_start(out=sg, in_=w["l1_da_subln"].rearrange("(p o) -> p o", o=1))
        P.add("sync", sgld, w=["sg"], dkey=dk())
        P.add("vector", lambda e: e.tensor_scalar(out=sg, in0=sg, scalar1=1.0 - LAMBDA_INIT, scalar2=None, op0=ALU.mult), r=["sg"], w=["sg"])

        P.chain("vector", [
            lambda e: e.tensor_tensor(out=lamp[:, 0, :], in0=lamv[:, 0, :], in1=lamv[:, 2, :], op=ALU.mult),
            lambda e: e.tensor_tensor(out=lamp[:, 1, :], in0=lamv[:, 1, :], in1=lamv[:, 3, :], op=ALU.mult),
            lambda e: e.tensor_reduce(out=lsum[0], in_=lamp[:, 0, :], axis=AX.X, op=ALU.add),
            lambda e: e.tensor_reduce(out=lsum[1], in_=lamp[:, 1, :], axis=AX.X, op=ALU.add),
        ], r=[("lamv", i) for i in range(4)], w=["lsum"])
        def lexpf(e):
            e.activation(out=lexp[0], in_=lsum[0], func=AF.Exp)
            return e.activation(out=lexp[1], in_=lsum[1], func=AF.Exp)
        P.add("scalar", lexpf, r=["lsum"], w=["lexp"])

        P.chain("vector", [
            lambda e: e.tensor_tensor(out=nlam, in0=lexp[1], in1=lexp[0], op=ALU.subtract),
            lambda e: e.tensor_scalar(out=nlam, in0=nlam, scalar1=-LAMBDA_INIT, scalar2=None, op0=ALU.add),
        ], r=["lexp"], w=["nlam"])

        P.chain("vector", [
            lambda e: e.tensor_tensor(out=gb, in0=gb, in1=gb, op=ALU.mult),
            lambda e: e.tensor_reduce(out=mx[0], in_=gb[:, 0, :], axis=AX.X, op=ALU.max),
            lambda e: e.tensor_reduce(out=mx[1], in_=gb[:, 1, :], axis=AX.X, op=ALU.max),
            lambda e: e.tensor_tensor(out=mx[0], in0=mx[0], in1=mx[1], op=ALU.max),
            lambda e: e.tensor_scalar(out=negB, in0=mx[0], scalar1=1.0, scalar2=-8.0, op0=ALU.max, op1=ALU.mult),
        ], r=[("gb", 0), ("gb", 1)], w=["negB"])
        for s2 in range(2):
            P.add("gpsimd", (lambda s2: lambda e: e.memset(VA_r[s2].rearrange("p j d -> p (j d)"), 1.0))(s2), w=[("VA", s2)])

        psS = PsRing([4, 5, 6, 7])
        v_h = v_d.rearrange("(j p) h d -> p j h d", p=128)
        def head_loads(h):
            s2 = h % 2
            KT, QT, VA = KT_r[s2], QT_r[s2], VA_r[s2]
            P.add("sync", (lambda KT, h: lambda e: e.dma_start(out=KT, in_=kT_d[h]))(KT, h), w=[("KT", s2)], dkey="lk%d" % s2)
            P.add("sync", (lambda QT, h: lambda e: e.dma_start(out=QT, in_=qT_d[h]))(QT, h), w=[("QT", s2)], dkey="lq%d" % s2)
            P.add("sync", (lambda VA, h: lambda e: e.dma_start(out=VA[:, :, 0:128], in_=v_h[:, :, h, :]))(VA, h), w=[("VA", s2)], dkey="lv%d" % s2)

        items = [(h, Q, comp, j) for h in range(8) for Q in range(NST) for comp in range(2) for j in range(4 * Q + 4)]
        slot_of = {}
        ectr = [0]

        def front(it):
            h, Q, comp, j = it
            s2 = h % 2
            KT, QT = KT_r[s2], QT_r[s2]
            if Q == 0 and comp == 0 and j == 0:
                if h == 0:
                    head_loads(0)
                if h + 1 < 8:
                    head_loads(h + 1)
            lo_p, hi_p = comp * 64, comp * 64 + 64
            lo = max(0, j - 4 * Q)
            bS = psS.next()

            def smm(e):
                diag = j >= 4 * Q
                ins = e.matmul(psf(bS)[:, lo * 128:512], lhsT=KT[lo_p:hi_p, j * 128:(j + 1) * 128],
                               rhs=QT[lo_p:hi_p, Q * 512 + lo * 128:(Q + 1) * 512], start=True, stop=not diag)
                if diag:
                    ins = e.matmul(psf(bS)[:, lo * 128:(lo + 1) * 128], lhsT=ident_bf, rhs=maskneg, start=False, stop=True)
                return ins
            P.add("tensor", smm, r=[("KT", s2), ("QT", s2), "maskneg", "ident_bf"], w=[("ps", bS)])
            es_ = ectr[0] % 4
            ectr[0] += 1
            slot_of[it] = es_
            E = E_r[es_]
            P.add("scalar", lambda e: e.activation(out=E[:, lo * 128:512], in_=psf(bS)[:, lo * 128:512], func=AF.Exp, bias=negB[:, 0:1]),
                  r=[("ps", bS), "negB"], w=[("E", es_)])

        def back(it):
            h, Q, comp, j = it
            s2 = h % 2
            VA = VA_r[s2]
            lo = max(0, j - 4 * Q)
            es_ = slot_of.pop(it)
            E = E_r[es_]

            def av(e):
                last = None
                for i in range(lo, 4):
                    last = e.matmul(ps_t[:, i, 0:132], lhsT=E[:, i * 128:(i + 1) * 128], rhs=VA[:, j, :],
                                    start=(j == 0), stop=(j == 4 * Q + i))
                return last
            P.add("tensor", av, r=[("E", es_), ("VA", s2)], w=[("ps", i) for i in range(lo, 4)])
            if j != 4 * Q + 3:
                return
            if comp == 0:
                for i in range(4):
                    P.chain("vector", [
                        (lambda i: lambda e: e.reciprocal(out=rr_r[i][0], in_=ps_t[:, i, 128:129]))(i),
                        (lambda i: lambda e: e.tensor_scalar(out=O_r[i], in0=ps_t[:, i, 0:128], scalar1=rr_r[i][0], scalar2=None, op0=ALU.mult))(i),
                    ], r=[("ps", i)], w=[("O", i), ("rr", i)])
                return
            for i in range(4):
                T = 4 * Q + i
                o2 = i
                U2 = ps_t[:, i, 0:132]
                O, rr, Onb = O_r[o2], rr_r[o2], Onb_r[i % 2]
                P.chain("vector", [
                    (lambda U2, rr: lambda e: e.reciprocal(out=rr[1], in_=U2[:, 128:129]))(U2, rr),
                    (lambda rr: lambda e: e.tensor_tensor(out=rr[1], in0=rr[1], in1=nlam, op=ALU.mult))(rr),
                    (lambda U2, O, rr: lambda e: e.scalar_tensor_tensor(out=O, in0=U2[:, 0:128], scalar=rr[1], in1=O, op0=ALU.mult, op1=ALU.add))(U2, O, rr),
                ], r=[("ps", i), "nlam", ("O", o2), ("rr", o2)], w=[("O", o2), ("rr", o2)])
                P.add("gpsimd", (lambda rr: lambda e: e.memset(rr[2], 0.0))(rr), r=[("rr", o2)], w=[("rr2", o2)])
                P.add("scalar", (lambda O, rr: lambda e: e.activation(out=junkO, in_=O, func=AF.Square, accum_out=rr[2]))(O, rr),
                      r=[("O", o2), ("rr2", o2)], w=[("rr2", o2)])
                P.chain("gpsimd", [
                    (lambda rr: lambda e: e.tensor_scalar(out=rr[3], in0=rr[2], scalar1=1.0 / 128, scalar2=EPS, op0=ALU.mult, op1=ALU.add))(rr),
                    (lambda rr: lambda e: e.tensor_tensor(out=rr[3], in0=rr[3], in1=neghalf[:, 0:1], op=ALU.pow))(rr),
                ], r=[("rr2", o2)], w=[("rr3", o2)])
                P.add("vector", (lambda Onb, O, rr: lambda e: e.tensor_scalar(out=Onb, in0=O, scalar1=rr[3], scalar2=None, op0=ALU.mult))(Onb, O, rr),
                      r=[("O", o2), ("rr3", o2)], w=[("Onb", i % 2)])
                bt = psS.next()
                P.add("tensor", (lambda bt, Onb: lambda e: e.transpose(psb(bt)[:, 0:128], Onb, ident_bf))(bt, Onb),
                      r=[("Onb", i % 2), "ident_bf"], w=[("ps", bt)])
                P.add("vector", (lambda bt, h, T: lambda e: e.tensor_scalar(out=OnT[:, h, T * 128:(T + 1) * 128], in0=psb(bt)[:, 0:128],
                                                                             scalar1=sg[:, 0:1], scalar2=None, op0=ALU.mult))(bt, h, T),
                      r=[("ps", bt), "sg"], w=[("OnT", h, T)])

        LA = 2
        for n in range(len(items) + LA):
            if n < len(items):
                front(items[n])
            if n >= LA:
                back(items[n - LA])

        for T in range(NT):
            s2 = T % 2
            xr = xr_ring[s2]
            P.add("sync", (lambda xr, T: lambda e: e.dma_start(out=xr, in_=src_d[T * 128:(T + 1) * 128, :]))(xr, T),
                  w=[("xr", s2)], dkey="lr%d" % s2)
            for j in range(2):
                b = psS.next()

                def mmo(e, T=T, j=j, b=b):
                    last = None
                    for k in range(8):
                        last = e.matmul(psf(b), lhsT=OnT[:, k, T * 128:(T + 1) * 128], rhs=wo[:, k, j * 512:(j + 1) * 512],
                                        start=(k == 0), stop=(k == 7))
                    return last
                P.add("tensor", mmo, r=[("OnT", k, T) for k in range(8)] + ["wo"], w=[("ps", b)])
                P.add("vector", (lambda xr, j, b: lambda e: e.tensor_tensor(out=xr[:, j * 512:(j + 1) * 512], in0=psf(b),
                                                                             in1=xr[:, j * 512:(j + 1) * 512], op=ALU.add))(xr, j, b),
                      r=[("ps", b), ("xr", s2)], w=[("xr", s2)])
            P.add("sync", (lambda xr, T: lambda e: e.dma_start(out=dst_d[T * 128:(T + 1) * 128, :], in_=xr))(xr, T),
                  r=[("xr", s2)], w=[("hC", T)], dkey="st%d" % s2)


    def phase_D_dense(src_d, dst_d):
        ar.reset(CONST_END)
        dk_ctr[0] = 0
        gcol = ar.alloc([8], F32)
        wr = ar.alloc([8, 8], F32)
        acc = ar.alloc([16, 1024], F32)
        xnT = ar.alloc([8, 2048], BF16)
        gates = ar.alloc([16, 8], F32)
        xs32_r = [ar.alloc([1024], F32) for _ in range(2)]
        junk = ar.alloc([1024], BF16)
        xs_ring = [{"bf": ar.alloc([1024], BF16), "junk": junk} for _ in range(2)]
        ss_r = [ar.alloc([1], F32) for _ in range(2)]
        rstd_r = [ar.alloc([1], F32) for _ in range(2)]
        xn32T = ar.alloc([8, 128], F32)
        lg = ar.alloc([8], F32)
        eq = ar.alloc([8], F32)
        l2 = ar.alloc([8], F32)
        sel = ar.alloc([8], F32)
        ex = ar.alloc([8], F32)
        m1 = ar.alloc([1], F32)
        m2 = ar.alloc([1], F32)
        nm1 = ar.alloc([1], F32)
        den = ar.alloc([1], F32)
        rden = ar.alloc([1], F32)
        wg_r = [ar.alloc([8, 512], BF16) for _ in range(2)]
        wu_r = [ar.alloc([8, 512], BF16) for _ in range(2)]
        wd_r = [ar.alloc([4, 1024], BF16) for _ in range(2)]
        HT_r = [ar.alloc([4, 512], BF16) for _ in range(2)]
        tmp_r = [ar.alloc([512], F32) for _ in range(2)]

        P.add("sync", col_load(gcol, w["l1_moe_norm"], 8), w=["gcol"], dkey=dk())
        P.add("sync", lambda e: e.dma_start(out=wr, in_=w["l1_moe_w_router"].rearrange("(k p) e -> p k e", p=128)), w=["wr"], dkey=dk())
        wg_d = w["l1_moe_w_gate"]
        wu_d = w["l1_moe_w_up"]
        wd_d = w["l1_moe_w_down"]
        psT = PsRing([0, 1])

        groups = [(hf, e_, fg) for hf in range(2) for e_ in range(NE) for fg in range(FE // 512)]

        def load_group(gi):
            hf, e_, fg = groups[gi]
            sl = gi % 2
            P.add("gpsimd", (lambda sl, e_, fg: lambda e: e.dma_start(
                out=wg_r[sl], in_=wg_d[e_].rearrange("(k p) f -> p k f", p=128)[:, :, fg * 512:(fg + 1) * 512]))(sl, e_, fg),
                w=[("wg", sl)], dkey="wg%d" % sl)
            P.add("gpsimd", (lambda sl, e_, fg: lambda e: e.dma_start(
                out=wu_r[sl], in_=wu_d[e_].rearrange("(k p) f -> p k f", p=128)[:, :, fg * 512:(fg + 1) * 512]))(sl, e_, fg),
                w=[("wu", sl)], dkey="wu%d" % sl)
            P.add("gpsimd", (lambda sl, e_, fg: lambda e: e.dma_start(
                out=wd_r[sl], in_=wd_d[e_][fg * 512:(fg + 1) * 512, :].rearrange("(k p) n -> p k n", p=128)))(sl, e_, fg),
                w=[("wd", sl)], dkey="wd%d" % sl)

        gi = 0
        hi = 0
        for hf in range(2):
            for t in range(16):
                T = hf * 16 + t
                s2 = t % 2
                at = acc[:, t, :]
                P.add("sync", (lambda at, T: lambda e: e.dma_start(out=at, in_=src_d[T * 128:(T + 1) * 128, :]))(at, T),
                      w=[("acc", t)], dkey="la%d" % t)
                xs32 = xs32_r[s2]
                norm_p1(at, xs_ring[s2], ss_r[s2], rstd_r[s2], ("D", s2), [("acc", t)], xs32=xs32)
                norm_p2(gcol, xs_ring[s2], xnT[:, :, t * 128:(t + 1) * 128], ("D", s2), psT, [("xnT", t)])

                def trf(e, xs32=xs32):
                    last = None
                    for k in range(8):
                        last = e.transpose(ps_t[:, 2 + k // 4, (k % 4) * 128:(k % 4 + 1) * 128], xs32[:, k * 128:(k + 1) * 128], ident_f)
                    return last
                P.add("tensor", trf, r=[("xs32", ("D", s2)), "ident_f"], w=[("ps", 2), ("ps", 3)])
                P.add("vector", lambda e: e.tensor_tensor(out=xn32T, in0=ps_t[:, 2:4, :].rearrange("p a (b t) -> p (a b) t", b=4),
                                                          in1=gcol.unsqueeze(2).to_broadcast([128, 8, 128]), op=ALU.mult),
                      r=[("ps", 2), ("ps", 3), "gcol"], w=["xn32T"])

                def lgm(e):
                    last = None
                    for k in range(8):
                        last = e.matmul(ps_t[:, 4, 0:8], lhsT=xn32T[:, k, :], rhs=wr[:, k, :], start=(k == 0), stop=(k == 7))
                    return last
                P.add("tensor", lgm, r=["xn32T", "wr"], w=[("ps", 4)])
                P.chain("vector", [
                    lambda e: e.tensor_copy(out=lg, in_=ps_t[:, 4, 0:8]),
                    lambda e: e.tensor_reduce(out=m1, in_=lg, axis=AX.X, op=ALU.max),
                    lambda e: e.tensor_scalar(out=eq, in0=lg, scalar1=m1[:, 0:1], scalar2=None, op0=ALU.is_equal),
                    lambda e: e.scalar_tensor_tensor(out=l2, in0=eq, scalar=-1.0e30, in1=lg, op0=ALU.mult, op1=ALU.add),
                    lambda e: e.tensor_reduce(out=m2, in_=l2, axis=AX.X, op=ALU.max),
                    lambda e: e.tensor_scalar(out=sel, in0=lg, scalar1=m2[:, 0:1], scalar2=None, op0=ALU.is_ge),
                    lambda e: e.tensor_scalar(out=nm1, in0=m1, scalar1=-1.0, scalar2=None, op0=ALU.mult),
                ], r=[("ps", 4)], w=["lg", "sel", "nm1"])
                P.add("scalar", lambda e: e.activation(out=ex, in_=lg, func=AF.Exp, bias=nm1[:, 0:1]), r=["lg", "nm1"], w=["ex"])
                P.chain("vector", [
                    lambda e: e.tensor_tensor(out=ex, in0=ex, in1=sel, op=ALU.mult),
                    lambda e: e.tensor_reduce(out=den, in_=ex, axis=AX.X, op=ALU.add),
                    lambda e: e.reciprocal(out=rden, in_=den),
                    (lambda t: lambda e: e.tensor_scalar(out=gates[:, t, :], in0=ex, scalar1=rden[:, 0:1], scalar2=None, op0=ALU.mult))(t),
                ], r=["ex", "sel"], w=[("gates", t), "ex"])

            psM = PsRing([0, 1, 2, 3, 4, 5, 6, 7])
            if hf == 0:
                load_group(0)
            for e_ in range(NE):
                for fg in range(FE // 512):
                    if gi + 1 < len(groups):
                        load_group(gi + 1)
                    sl = gi % 2
                    wg, wu, wd = wg_r[sl], wu_r[sl], wd_r[sl]
                    for st in range(4):
                        HT = HT_r[hi % 2]
                        hk = hi % 2
                        hi += 1
                        xk = [("xnT", st * 4 + c) for c in range(4)]
                        for fc in range(4):
                            bg = psM.next()
                            bu = psM.next()

                            def mmg(e, fc=fc, bg=bg, wg=wg, st=st):
                                last = None
                                for k in range(8):
                                    last = e.matmul(psf(bg), lhsT=wg[:, k, fc * 128:(fc + 1) * 128], rhs=xnT[:, k, st * 512:(st + 1) * 512],
                                                    start=(k == 0), stop=(k == 7))
                                return last

                            def mmu(e, fc=fc, bu=bu, wu=wu, st=st):
                                last = None
                                for k in range(8):
                                    last = e.matmul(psf(bu), lhsT=wu[:, k, fc * 128:(fc + 1) * 128], rhs=xnT[:, k, st * 512:(st + 1) * 512],
                                                    start=(k == 0), stop=(k == 7))
                                return last
                            P.add("tensor", mmg, r=[("wg", sl)] + xk, w=[("ps", bg)])
                            P.add("tensor", mmu, r=[("wu", sl)] + xk, w=[("ps", bu)])
                            tmp = tmp_r[fc % 2]
                            P.add("scalar", (lambda tmp, bg: lambda e: e.activation(out=tmp, in_=psf(bg), func=AF.Silu))(tmp, bg),
                                  r=[("ps", bg)], w=[("tmp", fc % 2)])
                            P.add("vector", (lambda tmp, bu, fc, HT: lambda e: e.tensor_tensor(out=HT[:, fc, :], in0=psf(bu), in1=tmp, op=ALU.mult))(tmp, bu, fc, HT),
                                  r=[("ps", bu), ("tmp", fc % 2)], w=[("HT", hk, fc)])
                        for c in range(4):
                            t = st * 4 + c
                            for j in range(2):
                                b = psM.next()

                                def mmd(e, c=c, j=j, b=b, HT=HT, wd=wd):
                                    last = None
                                    for k in range(4):
                                        last = e.matmul(psf(b), lhsT=HT[:, k, c * 128:(c + 1) * 128], rhs=wd[:, k, j * 512:(j + 1) * 512],
                                                        start=(k == 0), stop=(k == 3))
                                    return last
                                P.add("tensor", mmd, r=[("HT", hk, fc) for fc in range(4)] + [("wd", sl)], w=[("ps", b)])
                                P.add("vector", (lambda t, j, b, e_: lambda e: e.scalar_tensor_tensor(
                                    out=acc[:, t, j * 512:(j + 1) * 512], in0=psf(b), scalar=gates[:, t, e_:e_ + 1],
                                    in1=acc[:, t, j * 512:(j + 1) * 512], op0=ALU.mult, op1=ALU.add))(t, j, b, e_),
                                    r=[("ps", b), ("gates", t), ("acc", t)], w=[("acc", t)])
                    gi += 1
            for t in range(16):
                T = hf * 16 + t
                P.add("sync", (lambda t, T: lambda e: e.dma_start(out=dst_d[T * 128:(T + 1) * 128, :], in_=acc[:, t, :]))(t, T),
                      r=[("acc", t)], w=[("hD", T)], dkey="sa%d" % t)


    def phase_D(src_d, dst_d):
        BIG = 1.0e6
        ar.reset(CONST_END)
        dk_ctr[0] = 0
        idxA = ar.alloc([NT, 2], I32)
        idxB = ar.alloc([NT, 2], I32)
        gAB = ar.alloc([NT, 2], F32)
        gBB = ar.alloc([NT, 2], F32)
        gcol = ar.alloc([8], F32)
        P_END = ar.off
        wr = ar.alloc([8, 8], F32)
        tris = ar.alloc([128], F32)
        ones_f = ar.alloc([128], F32)
        eoff = ar.alloc([8], F32)
        Mcum = ar.alloc([8], F32)
        xt_ring = [ar.alloc([1024], F32) for _ in range(2)]
        xs32_r = [ar.alloc([1024], F32) for _ in range(2)]
        junk = ar.alloc([1024], BF16)
        xs_ring = [{"bf": ar.alloc([1024], BF16), "junk": junk} for _ in range(2)]
        ss_r = [ar.alloc([1], F32) for _ in range(2)]
        rstd_r = [ar.alloc([1], F32) for _ in range(2)]
        xn32T = ar.alloc([8, 128], F32)
        lg = ar.alloc([8], F32)
        eq = ar.alloc([8], F32)
        l2 = ar.alloc([8], F32)
        sel = ar.alloc([8], F32)
        ex = ar.alloc([8], F32)
        gt = ar.alloc([8], F32)
        rk = ar.alloc([8], F32)
        sf = ar.alloc([8], F32)
        sm = ar.alloc([8], F32)
        t2 = ar.alloc([8], F32)
        oh = ar.alloc([8], F32)
        tg = ar.alloc([8], F32)
        tg2 = ar.alloc([8], F32)
        cnti = ar.alloc([8], I32)
        m1 = ar.alloc([1], F32)
        m2 = ar.alloc([1], F32)
        nm1 = ar.alloc([1], F32)
        den = ar.alloc([1], F32)
        rden = ar.alloc([1], F32)
        sA = ar.alloc([1], F32)
        sB = ar.alloc([1], F32)
        XT = [ar.alloc([8, 512], BF16) for _ in range(4)]
        acc = [ar.alloc([4, 1024], F32) for _ in range(4)]
        wg_r = [ar.alloc([8, 512], BF16) for _ in range(2)]
        wu_r = [ar.alloc([8, 512], BF16) for _ in range(2)]
        wd_r = [ar.alloc([4, 1024], BF16) for _ in range(2)]
        HT_r = [ar.alloc([4, 512], BF16) for _ in range(2)]
        tmp_r = [ar.alloc([512], F32) for _ in range(2)]
        xg_r = [ar.alloc([1024], BF16) for _ in range(2)]

        P.add("sync", col_load(gcol, w["l1_moe_norm"], 8), w=["gcol"], dkey=dk())
        P.add("sync", lambda e: e.dma_start(out=wr, in_=w["l1_moe_w_router"].rearrange("(k p) e -> p k e", p=128)), w=["wr"], dkey=dk())
        P.add("sync", lambda e: e.dma_start(out=tris, in_=w["c_tris"][:, :]), w=["tris"], dkey=dk())
        P.add("sync", lambda e: e.dma_start(out=eoff, in_=w["c_eoff"].partition_broadcast(128)), w=["eoff"], dkey=dk())
        P.add("gpsimd", lambda e: e.memset(ones_f, 1.0), w=["ones_f"])
        P.add("gpsimd", lambda e: e.memset(Mcum, 0.0), w=["Mcum"])
        psT = PsRing([0, 1])

        for T in range(NT):
            s2 = T % 2
            xt = xt_ring[s2]
            P.add("sync", (lambda xt, T: lambda e: e.dma_start(out=xt, in_=src_d[T * 128:(T + 1) * 128, :]))(xt, T),
                  w=[("xt", s2)], dkey="ld%d" % s2)
            xs32 = xs32_r[s2]
            xsb = xs_ring[s2]["bf"]
            norm_p1(xt, xs_ring[s2], ss_r[s2], rstd_r[s2], ("D", s2), [("xt", s2)], xs32=xs32)

            def trf(e, xs32=xs32):
                last = None
                for k in range(8):
                    last = e.transpose(ps_t[:, 2 + k // 4, (k % 4) * 128:(k % 4 + 1) * 128], xs32[:, k * 128:(k + 1) * 128], ident_f)
                return last
            P.add("tensor", trf, r=[("xs32", ("D", s2)), "ident_f"], w=[("ps", 2), ("ps", 3)])
            P.add("vector", lambda e: e.tensor_tensor(out=xn32T, in0=ps_t[:, 2:4, :].rearrange("p a (b t) -> p (a b) t", b=4),
                                                      in1=gcol.unsqueeze(2).to_broadcast([128, 8, 128]), op=ALU.mult),
                  r=[("ps", 2), ("ps", 3), "gcol"], w=["xn32T"])

            def lgm(e):
                last = None
                for k in range(8):
                    last = e.matmul(ps_t[:, 4, 0:8], lhsT=xn32T[:, k, :], rhs=wr[:, k, :], start=(k == 0), stop=(k == 7))
                return last
            P.add("tensor", lgm, r=["xn32T", "wr"], w=[("ps", 4)])
            P.chain("vector", [
                lambda e: e.tensor_copy(out=lg, in_=ps_t[:, 4, 0:8]),
                lambda e: e.tensor_reduce(out=m1, in_=lg, axis=AX.X, op=ALU.max),
                lambda e: e.tensor_scalar(out=eq, in0=lg, scalar1=m1[:, 0:1], scalar2=None, op0=ALU.is_equal),
                lambda e: e.scalar_tensor_tensor(out=l2, in0=eq, scalar=-1.0e30, in1=lg, op0=ALU.mult, op1=ALU.add),
                lambda e: e.tensor_reduce(out=m2, in_=l2, axis=AX.X, op=ALU.max),
                lambda e: e.tensor_scalar(out=sel, in0=lg, scalar1=m2[:, 0:1], scalar2=None, op0=ALU.is_ge),
                lambda e: e.tensor_scalar(out=nm1, in0=m1, scalar1=-1.0, scalar2=None, op0=ALU.mult),
            ], r=[("ps", 4)], w=["lg", "sel", "nm1"])
            P.add("scalar", lambda e: e.activation(out=ex, in_=lg, func=AF.Exp, bias=nm1[:, 0:1]), r=["lg", "nm1"], w=["ex"])

            def rkm(e):
                e.matmul(ps_t[:, 5, 0:8], lhsT=tris, rhs=sel, start=True, stop=False)
                return e.matmul(ps_t[:, 5, 0:8], lhsT=ones_f, rhs=Mcum, start=False, stop=True)
            P.add("tensor", rkm, r=["sel", "Mcum", "tris", "ones_f"], w=[("ps", 5)])
            P.chain("vector", [
                lambda e: e.tensor_tensor(out=ex, in0=ex, in1=sel, op=ALU.mult),
                lambda e: e.tensor_reduce(out=den, in_=ex, axis=AX.X, op=ALU.add),
                lambda e: e.reciprocal(out=rden, in_=den),
                lambda e: e.tensor_scalar(out=gt, in0=ex, scalar1=rden[:, 0:1], scalar2=None, op0=ALU.mult),
                lambda e: e.tensor_copy(out=rk, in_=ps_t[:, 5, 0:8]),
                lambda e: e.tensor_tensor(out=Mcum, in0=Mcum, in1=sel, op=ALU.add),
                lambda e: e.tensor_tensor(out=sf, in0=rk, in1=eoff, op=ALU.add),
                lambda e: e.scalar_tensor_tensor(out=sm, in0=sf, scalar=-BIG, in1=sel, op0=ALU.add, op1=ALU.mult),
                lambda e: e.tensor_scalar(out=sm, in0=sm, scalar1=BIG, scalar2=None, op0=ALU.add),
                lambda e: e.tensor_reduce(out=sA, in_=sm, axis=AX.X, op=ALU.min),
                lambda e: e.tensor_tensor(out=t2, in0=sf, in1=sel, op=ALU.mult),
                lambda e: e.tensor_reduce(out=sB, in_=t2, axis=AX.X, op=ALU.max),
                lambda e: e.tensor_scalar(out=oh, in0=sm, scalar1=sA[:, 0:1], scalar2=None, op0=ALU.is_equal),
                lambda e: e.tensor_tensor(out=tg, in0=gt, in1=oh, op=ALU.mult),
                (lambda T: lambda e: e.tensor_reduce(out=gAB[:, T, 0:1], in_=tg, axis=AX.X, op=ALU.add))(T),
                lambda e: e.tensor_tensor(out=tg2, in0=gt, in1=tg, op=ALU.subtract),
                (lambda T: lambda e: e.tensor_reduce(out=gBB[:, T, 0:1], in_=tg2, axis=AX.X, op=ALU.add))(T),
                (lambda T: lambda e: e.tensor_copy(out=idxA[:, T, 0:1], in_=sA))(T),
                (lambda T: lambda e: e.tensor_copy(out=idxB[:, T, 0:1], in_=sB))(T),
            ], r=["ex", "sel", ("ps", 5), "eoff", "Mcum"], w=["ex", "Mcum", ("idx", T), ("gab", T)])
            for which, idx_t in ((0, idxA), (1, idxB)):
                P.add("gpsimd", (lambda idx_t, T, xsb: lambda e: e.indirect_dma_start(
                    out=Xg_d[:, :], out_offset=bass.IndirectOffsetOnAxis(ap=idx_t[:, T, 0:1], axis=0),
                    in_=xsb, in_offset=None))(idx_t, T, xsb),
                    r=[("idx", T), ("xsbf", ("D", s2))], w=[("Xg", T, which)], dkey="sc%d%d" % (s2, which))
        P.add("tensor", lambda e: e.matmul(ps_t[:, 5, 0:8], lhsT=ones_f, rhs=Mcum, start=True, stop=True), r=["Mcum", "ones_f"], w=[("ps", 5)])
        P.add("vector", lambda e: e.tensor_copy(out=cnti, in_=ps_t[:, 5, 0:8]), r=[("ps", 5)], w=["cnti"])
        P.add("sync", lambda e: e.dma_start(out=cnt_d[0:1, :], in_=cnti[0:1, :]), r=["cnti"], w=["cnt_d"], dkey=dk())
        P.barrier()

        for e_ in range(NE):
            P.regload("cnt%d" % e_, cnt_d[0:1, e_:e_ + 1])
        wg_d = w["l1_moe_w_gate"]
        wu_d = w["l1_moe_w_up"]
        wd_d = w["l1_moe_w_down"]
        NFG = FE // 512
        psM = PsRing([0, 1, 2, 3, 4, 5, 6, 7])
        ctr = {"hi": 0, "xg": 0}

        def load_w(e_, fg, sl):
            P.add("gpsimd", lambda e: e.dma_start(
                out=wg_r[sl], in_=wg_d[e_].rearrange("(k p) f -> p k f", p=128)[:, :, fg * 512:(fg + 1) * 512]),
                w=[("wg", sl)], dkey="wg%d" % sl)
            P.add("gpsimd", lambda e: e.dma_start(
                out=wu_r[sl], in_=wu_d[e_].rearrange("(k p) f -> p k f", p=128)[:, :, fg * 512:(fg + 1) * 512]),
                w=[("wu", sl)], dkey="wu%d" % sl)
            P.add("gpsimd", lambda e: e.dma_start(
                out=wd_r[sl], in_=wd_d[e_][fg * 512:(fg + 1) * 512, :].rearrange("(k p) n -> p k n", p=128)),
                w=[("wd", sl)], dkey="wd%d" % sl)

        def prep(e_, rnd):
            ck = "cnt%d" % e_
            for k in range(4):
                kk = rnd * 4 + k
                P.pred_begin(ck, kk * 512 + 1)
                for c in range(4):
                    xs_ = ctr["xg"] % 2
                    ctr["xg"] += 1
                    xg = xg_r[xs_]
                    row0 = e_ * S + kk * 512 + c * 128
                    P.add("sync", (lambda xg, row0: lambda e: e.dma_start(out=xg, in_=Xg_d[row0:row0 + 128, :]))(xg, row0),
                          w=[("xg", xs_)], dkey="xg%d" % xs_)
                    b = psM.next()

                    def trx(e, xg=xg, b=b):
                        last = None
                        for kq in range(8):
                            last = e.transpose(psb(b)[:, kq * 128:(kq + 1) * 128], xg[:, kq * 128:(kq + 1) * 128], ident_bf)
                        return last
                    P.add("tensor", trx, r=[("xg", xs_), "ident_bf"], w=[("ps", b)])
                    P.add("vector", (lambda k, c, b: lambda e: e.tensor_tensor(
                        out=XT[k][:, :, c * 128:(c + 1) * 128], in0=psb(b).rearrange("p (k t) -> p k t", k=8),
                        in1=gcol.unsqueeze(2).to_broadcast([128, 8, 128]), op=ALU.mult))(k, c, b),
                        r=[("ps", b), "gcol"], w=[("XT", k, c)])
            for k in range(4):
                P.pred_end()

        def compute(e_, rnd, fg, sl):
            ck = "cnt%d" % e_
            wg, wu, wd = wg_r[sl], wu_r[sl], wd_r[sl]
            for k in range(4):
                kk = rnd * 4 + k
                P.pred_begin(ck, kk * 512 + 1)
                HT = HT_r[ctr["hi"] % 2]
                hk = ctr["hi"] % 2
                ctr["hi"] += 1
                xk = [("XT", k, c) for c in range(4)]
                for fc in range(4):
                    bg = psM.next()
                    bu = psM.next()

                    def mmg(e, fc=fc, bg=bg, wg=wg, k=k):
                        last = None
                        for kq in range(8):
                            last = e.matmul(psf(bg), lhsT=wg[:, kq, fc * 128:(fc + 1) * 128], rhs=XT[k][:, kq, :],
                                            start=(kq == 0), stop=(kq == 7))
                        return last

                    def mmu(e, fc=fc, bu=bu, wu=wu, k=k):
                        last = None
                        for kq in range(8):
                            last = e.matmul(psf(bu), lhsT=wu[:, kq, fc * 128:(fc + 1) * 128], rhs=XT[k][:, kq, :],
                                            start=(kq == 0), stop=(kq == 7))
                        return last
                    P.add("tensor", mmg, r=[("wg", sl)] + xk, w=[("ps", bg)])
                    P.add("tensor", mmu, r=[("wu", sl)] + xk, w=[("ps", bu)])
                    tmp = tmp_r[fc % 2]
                    P.add("scalar", (lambda tmp, bg: lambda e: e.activation(out=tmp, in_=psf(bg), func=AF.Silu))(tmp, bg),
                          r=[("ps", bg)], w=[("tmp", fc % 2)])
                    P.add("vector", (lambda tmp, bu, fc, HT: lambda e: e.tensor_tensor(out=HT[:, fc, :], in0=psf(bu), in1=tmp, op=ALU.mult))(tmp, bu, fc, HT),
                          r=[("ps", bu), ("tmp", fc % 2)], w=[("HT", hk, fc)])
                for c in range(4):
                    for j in range(2):
                        b = psM.next()

                        def mmd(e, c=c, j=j, b=b, HT=HT, wd=wd):
                            last = None
                            for kq in range(4):
                                last = e.matmul(psf(b), lhsT=HT[:, kq, c * 128:(c + 1) * 128], rhs=wd[:, kq, j * 512:(j + 1) * 512],
                                                start=(kq == 0), stop=(kq == 3))
                            return last
                        P.add("tensor", mmd, r=[("HT", hk, fc) for fc in range(4)] + [("wd", sl)], w=[("ps", b)])
                        dst = acc[k][:, c, j * 512:(j + 1) * 512]
                        if fg == 0:
                            P.add("vector", (lambda dst, b: lambda e: e.tensor_copy(out=dst, in_=psf(b)))(dst, b),
                                  r=[("ps", b)], w=[("acc", k, c)])
                        else:
                            P.add("vector", (lambda dst, b: lambda e: e.tensor_tensor(out=dst, in0=psf(b), in1=dst, op=ALU.add))(dst, b),
                                  r=[("ps", b), ("acc", k, c)], w=[("acc", k, c)])
            for k in range(4):
                P.pred_end()

        def stores(e_, rnd):
            ck = "cnt%d" % e_
            for k in range(4):
                kk = rnd * 4 + k
                P.pred_begin(ck, kk * 512 + 1)
                row0 = e_ * S + kk * 512
                P.add("sync", (lambda k, row0: lambda e: e.dma_start(
                    out=Yg_d[row0:row0 + 512, :].rearrange("(c p) n -> p c n", p=128), in_=acc[k]))(k, row0),
                    r=[("acc", k, c) for c in range(4)], w=[("Yg", e_, kk)], dkey="ys%d" % k)
            for k in range(4):
                P.pred_end()

        order0 = [(e_, fg) for e_ in range(NE) for fg in range(NFG)]
        load_w(order0[0][0], order0[0][1], 0)
        for n, (e_, fg) in enumerate(order0):
            if fg == 0:
                prep(e_, 0)
            if n + 1 < len(order0):
                load_w(order0[n + 1][0], order0[n + 1][1], (n + 1) % 2)
            compute(e_, 0, fg, n % 2)
            if fg == NFG - 1:
                stores(e_, 0)
        g = len(order0)
        for e_ in range(NE):
            P.pred_begin("cnt%d" % e_, 2049)
            prep(e_, 1)
            load_w(e_, 0, g % 2)
            for fg in range(NFG):
                if fg + 1 < NFG:
                    load_w(e_, fg + 1, (g + 1) % 2)
                compute(e_, 1, fg, g % 2)
                g += 1
            stores(e_, 1)
            P.pred_end()
        P.barrier()

        ar.reset(P_END)
        ya_r = [ar.alloc([1024], F32) for _ in range(2)]
        yb_r = [ar.alloc([1024], F32) for _ in range(2)]
        xr_r = [ar.alloc([1024], F32) for _ in range(2)]
        for T in range(NT):
            s2 = T % 2
            ya, yb, xr = ya_r[s2], yb_r[s2], xr_r[s2]
            P.add("gpsimd", (lambda ya, T: lambda e: e.indirect_dma_start(
                out=ya, out_offset=None, in_=Yg_d[:, :], in_offset=bass.IndirectOffsetOnAxis(ap=idxA[:, T, 0:1], axis=0)))(ya, T),
                w=[("ya", s2)], dkey="ga%d" % s2)
            P.add("gpsimd", (lambda yb, T: lambda e: e.indirect_dma_start(
                out=yb, out_offset=None, in_=Yg_d[:, :], in_offset=bass.IndirectOffsetOnAxis(ap=idxB[:, T, 0:1], axis=0)))(yb, T),
                w=[("yb", s2)], dkey="gb%d" % s2)
            P.add("sync", (lambda xr, T: lambda e: e.dma_start(out=xr, in_=src_d[T * 128:(T + 1) * 128, :]))(xr, T),
                  w=[("xr", s2)], dkey="lr%d" % s2)
            P.add("vector", (lambda ya, xr, T: lambda e: e.scalar_tensor_tensor(out=xr, in0=ya, scalar=gAB[:, T, 0:1], in1=xr, op0=ALU.mult, op1=ALU.add))(ya, xr, T),
                  r=[("ya", s2), ("xr", s2)], w=[("xr", s2)])
            P.add("vector", (lambda yb, xr, T: lambda e: e.scalar_tensor_tensor(out=xr, in0=yb, scalar=gBB[:, T, 0:1], in1=xr, op0=ALU.mult, op1=ALU.add))(yb, xr, T),
                  r=[("yb", s2), ("xr", s2)], w=[("xr", s2)])
            P.add("sync", (lambda xr, T: lambda e: e.dma_start(out=dst_d[T * 128:(T + 1) * 128, :], in_=xr))(xr, T),
                  r=[("xr", s2)], w=[("hD", T)], dkey="st%d" % s2)


    if os.environ.get("MOE_DENSE"):
        phase_D = phase_D_dense

    order = ["A", "B", "C", "D"]
    nph = order.index(stop_after) + 1
    srcs = [x_d, h1_d, h2_d, h3_d]
    dsts = [h1_d, h2_d, h3_d, out_d]
    dsts[nph - 1] = out_d
    fns = [phase_A, phase_B, phase_C, phase_D]
    for i in range(nph):
        fns[i](srcs[i], dsts[i])
        P.barrier()

    P.emit(es)
    es.close()
    return nc, list(declared.keys())


_CACHE = {}


def _consts():
    ident = np.eye(128, dtype=np.float32)
    tri = np.triu(np.ones((128, 128), dtype=np.float32))
    invf = (1.0 / (np.float32(500000.0) ** (np.arange(0, 16, 2, dtype=np.float32) / np.float32(16)))).astype(np.float32)
    return {
        "c_ident_bf": ident.astype(ml_dtypes.bfloat16),
        "c_ident_f": ident,
        "c_tri": tri,
        "c_invf": invf,
        "c_tris": np.triu(np.ones((128, 128), dtype=np.float32), 1),
        "c_eoff": (np.arange(8, dtype=np.float32) * 4096.0).astype(np.float32),
    }


def kernel(stop_after="D", **inputs):
    key = stop_after
    if key not in _CACHE:
        _CACHE[key] = build_program(stop_after)
    nc, used = _CACHE[key]
    consts = _consts()
    in_maps = []
    for b in range(8):
        m = {}
        for k, v in inputs.items():
            if k not in used:
                continue
            a = np.asarray(v)
            if k == "x":
                m[k] = np.ascontiguousarray(a[b])
            elif k == "positions":
                m[k] = np.ascontiguousarray(a[b]).astype(np.int32, copy=False)
            else:
                m[k] = np.ascontiguousarray(a)
        m.update({k: v for k, v in consts.items() if k in used})
        in_maps.append(m)
    res = run_bass_kernel_spmd(nc, in_maps, core_ids=list(range(8)))
    global _LAST
    _LAST = res
    return np.stack([np.asarray(r["out"]) for r in res.results], axis=0).astype(np.float32, copy=False)
```

```python
import math
import os
from contextlib import ExitStack

import ml_dtypes
import numpy as np

import concourse.bass as bass
import concourse.mybir as mybir
from concourse.bass_utils import run_bass_kernel_spmd

F32 = mybir.dt.float32
BF16 = mybir.dt.bfloat16
I32 = mybir.dt.int32
U8 = mybir.dt.uint8
AF = mybir.ActivationFunctionType
ALU = mybir.AluOpType
AX = mybir.AxisListType

S = 4096
D = 1024
NT = S // 128
NST = S // 512
EPS = 1e-6
LAMBDA_INIT = 0.8 - 0.6 * math.exp(-0.3 * 1)
FFN = 2816
NF0 = FFN // 128
NE = 8
FE = 3584
SIZEOF = {F32: 4, BF16: 2, I32: 4, U8: 1}
COMPUTE = ("tensor", "vector", "scalar", "gpsimd")
ALLENG = COMPUTE + ("sync",)


class Op:
    __slots__ = ("eng", "fn", "deps", "signal", "ticket", "is_dma", "dkey", "dval", "strict", "mark")


class Prog:
    def __init__(self, nc):
        self.nc = nc
        self.ops = {e: [] for e in ALLENG}
        self.lw = {}
        self.rd = {}
        self.dcount = {}
        self.last = {e: None for e in ALLENG}
        self.last_dma = {}

    def add(self, eng, fn, r=(), w=(), dkey=None, strict=False):
        op = Op()
        op.mark = None
        op.strict = strict
        op.eng = eng
        op.fn = fn
        op.signal = False
        op.ticket = 0
        op.is_dma = dkey is not None
        op.dkey = dkey
        op.dval = 0
        deps = []
        for k in r:
            x = self.lw.get(k)
            if x is not None:
                deps.append(x)
        for k in w:
            x = self.lw.get(k)
            if x is not None:
                deps.append(x)
            rr = self.rd.get(k)
            if rr:
                deps.extend(rr[0].values())
                deps.extend(rr[1])
        self._set_deps(op, deps)
        if op.is_dma:
            c = self.dcount.get(dkey, 0) + 1
            self.dcount[dkey] = c
            op.dval = 16 * c
            self.last_dma[dkey] = op
        for k in r:
            rr = self.rd.setdefault(k, ({}, []))
            if op.is_dma:
                rr[1].append(op)
            else:
                rr[0][eng] = op
        for k in w:
            self.lw[k] = op
            self.rd[k] = ({}, [])
        self.ops[eng].append(op)
        if not op.is_dma:
            self.last[eng] = op
        return op

    def _marker(self, mark):
        for e in ALLENG:
            op = Op()
            op.mark = mark
            op.strict = False
            op.eng = e
            op.fn = None
            op.signal = False
            op.ticket = 0
            op.is_dma = False
            op.dkey = None
            op.dval = 0
            op.deps = []
            self.ops[e].append(op)

    def regload(self, key, ap):
        self._marker(("regload", key, ap))

    def pred_begin(self, key, thr):
        self._marker(("begin", key, thr))

    def pred_end(self):
        self._marker(("end",))

    def chain(self, eng, fns, r=(), w=()):
        self.nchain = getattr(self, "nchain", 0) + 1
        key = ("chain", self.nchain)
        op = None
        for i, fn in enumerate(fns):
            op = self.add(eng, fn, r=list(r) + [key], w=list(w) + [key], strict=(i > 0))
        return op

    def _set_deps(self, op, deps):
        seen = set()
        out = []
        for d in deps:
            if d is op or id(d) in seen:
                continue
            seen.add(id(d))
            if (not d.is_dma) and (not op.is_dma) and d.eng == op.eng and not op.strict:
                continue
            out.append(d)
            if not d.is_dma:
                d.signal = True
        op.deps = out

    def barrier(self):
        lasts = [self.last[e] for e in COMPUTE if self.last[e] is not None]
        dmas = list(self.last_dma.values())
        for e in ALLENG:
            op = Op()
            op.mark = None
            op.strict = False
            op.eng = e
            op.fn = None
            op.signal = False
            op.ticket = 0
            op.is_dma = False
            op.dkey = None
            op.dval = 0
            deps = [d for d in lasts if d.eng != e] + dmas
            for d in deps:
                if not d.is_dma:
                    d.signal = True
            op.deps = deps
            self.ops[e].append(op)
        self.lw = {}
        self.rd = {}

    def emit(self, es):
        nc = self.nc
        for e in ALLENG:
            c = 0
            for op in self.ops[e]:
                if op.is_dma:
                    continue
                if op.signal:
                    c += 1
                    op.ticket = c
        psem = {e: es.enter_context(nc.semaphore("pg_" + e)) for e in COMPUTE}
        dsem = {}
        for i, k in enumerate(self.dcount):
            dsem[k] = es.enter_context(nc.semaphore("dq_%d" % i))
        block = es.enter_context(nc.Block())

        def mk(engname):
            def body(eng):
                waited = {}
                regs = {}

                def emit_op(op):
                    for d in op.deps:
                        if d.is_dma:
                            key = ("d", d.dkey)
                            sem = dsem[d.dkey]
                            val = d.dval
                        else:
                            key = ("p", d.eng)
                            sem = psem[d.eng]
                            val = d.ticket
                        if waited.get(key, 0) >= val:
                            continue
                        eng.wait_ge(sem, val)
                        waited[key] = val
                    if op.fn is None:
                        return
                    ins = op.fn(eng)
                    if op.is_dma:
                        ins.then_inc(dsem[op.dkey], 16)
                    elif op.signal:
                        ins.then_inc(psem[engname], 1)

                ops = self.ops[engname]

                def find_end(i):
                    depth = 0
                    j = i
                    while True:
                        m = ops[j].mark
                        if m is not None and m[0] == "begin":
                            depth += 1
                        elif m is not None and m[0] == "end":
                            depth -= 1
                            if depth == 0:
                                return j
                        j += 1

                def emit_range(i, end):
                    nonlocal waited
                    while i < end:
                        op = ops[i]
                        if op.mark is None:
                            emit_op(op)
                            i += 1
                            continue
                        kind = op.mark[0]
                        if kind == "regload":
                            _, key, ap = op.mark
                            if key not in regs:
                                regs[key] = eng.alloc_register("rg_%s_%s" % (engname, key))
                            eng.reg_load(regs[key], ap)
                            i += 1
                            continue
                        assert kind == "begin", kind
                        _, key, thr = op.mark
                        j = find_end(i)
                        region = [o for o in ops[i + 1:j] if o.mark is None]
                        nsig = sum(1 for o in region if (not o.is_dma) and o.signal)
                        dfirst = {}
                        dcnt = {}
                        for o in region:
                            if o.is_dma:
                                dfirst.setdefault(o.dkey, o.dval)
                                dcnt[o.dkey] = dcnt.get(o.dkey, 0) + 1
                        if region:
                            snap = dict(waited)
                            with eng.If_lt(regs[key], thr):
                                for k2, c2 in dcnt.items():
                                    prior = dfirst[k2] - 16
                                    if prior > 0:
                                        eng.wait_ge(dsem[k2], prior)
                                    eng.sem_inc(dsem[k2], 16 * c2)
                                if nsig:
                                    eng.drain().then_inc(psem[engname], nsig)
                            with eng.Else():
                                emit_range(i + 1, j)
                            waited = snap
                        i = j + 1

                emit_range(0, len(ops))
                if engname == "sync":
                    for k, c in self.dcount.items():
                        if waited.get(("d", k), 0) < 16 * c:
                            eng.wait_ge(dsem[k], 16 * c)
            return body

        block.tensor(mk("tensor"))
        block.vector(mk("vector"))
        block.scalar(mk("scalar"))
        block.gpsimd(mk("gpsimd"))
        block.sync(mk("sync"))


class Arena:
    def __init__(self, base_ap, nbytes):
        self.base = base_ap
        self.nbytes = nbytes
        self.off = 0

    def reset(self, off=0):
        self.off = off

    def alloc(self, shape, dt):
        n = SIZEOF[dt]
        for s in shape:
            n *= s
        start = (self.off + 63) // 64 * 64
        assert start + n <= self.nbytes, ("SBUF arena overflow", start + n, self.nbytes)
        self.off = start + n
        ap = self.base[:, start:start + n].bitcast(dt)
        if len(shape) == 2:
            ap = ap.rearrange("p (a b) -> p a b", a=shape[0])
        elif len(shape) == 3:
            ap = ap.rearrange("p (a b c) -> p a b c", a=shape[0], b=shape[1])
        return ap


class PsRing:
    def __init__(self, banks):
        self.banks = list(banks)
        self.i = 0

    def next(self):
        b = self.banks[self.i % len(self.banks)]
        self.i += 1
        return b


def build_program(stop_after="D"):
    nc = bass.Bass("TRN2", target_bir_lowering=False)
    es = ExitStack()

    def din(name, shape, dt=F32):
        return nc.dram_tensor(name, list(shape), dt, kind="ExternalInput").ap()

    specs = {
        "x": ([S, D], F32), "positions": ([S], I32),
        "l0_mix_norm": [D], "l0_sg_w_in": [D, 4096], "l0_sg_v_norm": [2048],
        "l0_sg_w_spatial": [8, 128, 128], "l0_sg_b_spatial": [8, 128], "l0_sg_w_out": [2048, D],
        "l0_ffn_norm": [D], "l0_ffn_w_gate": [D, FFN], "l0_ffn_w_up": [D, FFN], "l0_ffn_w_down": [FFN, D],
        "l1_mix_norm": [D], "l1_da_w_qkv": [D, 3072], "l1_da_q_norm": [64], "l1_da_k_norm": [64],
        "l1_da_lambda_q1": [64], "l1_da_lambda_k1": [64], "l1_da_lambda_q2": [64], "l1_da_lambda_k2": [64],
        "l1_da_subln": [128], "l1_da_w_out": [D, D],
        "l1_moe_norm": [D], "l1_moe_w_router": [D, 8], "l1_moe_w_gate": [8, D, FE], "l1_moe_w_up": [8, D, FE],
        "l1_moe_w_down": [8, FE, D],
        "c_ident_bf": ([128, 128], BF16), "c_ident_f": ([128, 128], F32), "c_tri": ([128, 128], F32),
        "c_invf": ([8], F32), "c_tris": ([128, 128], F32), "c_eoff": ([8], F32),
    }
    declared = {}

    class _W:
        def __getitem__(self, k):
            if k not in declared:
                sp = specs[k]
                if isinstance(sp, tuple):
                    declared[k] = din(k, sp[0], sp[1])
                else:
                    declared[k] = din(k, sp)
            return declared[k]
    w = _W()
    x_d = w["x"]
    ident_bf_d = w["c_ident_bf"]
    ident_f_d = w["c_ident_f"]
    tri_d = w["c_tri"]
    out_d = nc.dram_tensor("out", [S, D], F32, kind="ExternalOutput").ap()
    h1_d = nc.dram_tensor("h1_scr", [S, D], F32).ap()
    h2_d = nc.dram_tensor("h2_scr", [S, D], F32).ap()
    h3_d = nc.dram_tensor("h3_scr", [S, D], F32).ap()
    dbg_kind = "ExternalOutput" if os.environ.get("KDBG") else "Internal"
    qT_d = nc.dram_tensor("qT_scr", [8, 128, S], BF16, kind=dbg_kind).ap()
    kT_d = nc.dram_tensor("kT_scr", [8, 128, S], BF16, kind=dbg_kind).ap()
    v_d = nc.dram_tensor("v_scr", [S, 8, 128], BF16, kind=dbg_kind).ap()

    NSLOT = 8 * S
    Xg_d = nc.dram_tensor("xg_scr", [NSLOT, D], BF16).ap()
    Yg_d = nc.dram_tensor("yg_scr", [NSLOT, D], F32).ap()
    cnt_d = nc.dram_tensor("cnt_scr", [1, 8], I32).ap()
    dbg_d = nc.dram_tensor("dbg", [128, 8, 512], F32, kind=dbg_kind).ap()
    ARENA_BYTES = 199 * 1024
    arena_t = es.enter_context(nc.sbuf_tensor("arena", [128, ARENA_BYTES], U8))
    ar = Arena(arena_t, ARENA_BYTES)
    ps_t = es.enter_context(nc.psum_tensor("psum", [128, 8, 512], F32))

    def psf(b):
        return ps_t[:, b, :]

    def psb(b):
        return ps_t[:, b, :].bitcast(BF16)

    P = Prog(nc)

    ident_bf = ar.alloc([128], BF16)
    ident_f = ar.alloc([128], F32)
    P.add("sync", lambda e: e.dma_start(out=ident_bf, in_=ident_bf_d[:, :]), w=["ident_bf"], dkey="k0")
    P.add("sync", lambda e: e.dma_start(out=ident_f, in_=ident_f_d[:, :]), w=["ident_f"], dkey="k1")
    neghalf = ar.alloc([64], F32)
    P.add("gpsimd", lambda e: e.memset(neghalf, -0.5), w=["neghalf"])
    CONST_END = ar.off

    def col_load(dst, src, n):
        def f(e):
            with nc.allow_non_contiguous_dma(reason="tiny gain vector"):
                return e.dma_start(out=dst, in_=src.rearrange("(k p) -> p k", p=128))
        return f

    def norm_p1(xt, xs, ss, rstd, tag, keys_r, xs32=None):
        def sq(e):
            return e.activation(out=xs["junk"], in_=xt, func=AF.Square, accum_out=ss)
        P.add("gpsimd", lambda e: e.memset(ss, 0.0), w=[("ss", tag)])
        P.add("scalar", sq, r=keys_r + [("ss", tag)], w=[("ss", tag), ("junk", tag)])

        P.chain("gpsimd", [
            lambda e: e.tensor_scalar(out=rstd, in0=ss, scalar1=1.0 / D, scalar2=EPS, op0=ALU.mult, op1=ALU.add),
            lambda e: e.tensor_tensor(out=rstd, in0=rstd, in1=neghalf[:, 0:1], op=ALU.pow),
        ], r=[("ss", tag)], w=[("rstd", tag)])
        if xs32 is None:
            P.add("vector", lambda e: e.tensor_scalar(out=xs["bf"], in0=xt, scalar1=rstd[:, 0:1], scalar2=None, op0=ALU.mult),
                  r=keys_r + [("rstd", tag)], w=[("xsbf", tag)])
        else:
            P.add("vector", lambda e: e.tensor_scalar(out=xs32, in0=xt, scalar1=rstd[:, 0:1], scalar2=None, op0=ALU.mult),
                  r=keys_r + [("rstd", tag)], w=[("xs32", tag)])
            P.add("gpsimd", lambda e: e.tensor_copy(out=xs["bf"], in_=xs32), r=[("xs32", tag)], w=[("xsbf", tag)])

    def norm_p2(gcol, xs, xnT_dst, tag, psT, keys_w):
        b = psT.next()

        def tr(e):
            last = None
            for k in range(8):
                last = e.transpose(psb(b)[:, k * 128:(k + 1) * 128], xs["bf"][:, k * 128:(k + 1) * 128], ident_bf)
            return last
        P.add("tensor", tr, r=[("xsbf", tag), "ident_bf"], w=[("ps", b)])

        def ev(e):
            return e.tensor_tensor(out=xnT_dst, in0=psb(b).rearrange("p (k t) -> p k t", k=8),
                                   in1=gcol.unsqueeze(2).to_broadcast([128, 8, 128]), op=ALU.mult)
        P.add("vector", ev, r=[("ps", b), "gcol"], w=keys_w)

    def norm_tile(xt, gcol, xs, ss, rstd, xnT_dst, tag, psT, keys_r, keys_w):
        norm_p1(xt, xs, ss, rstd, tag, keys_r)
        norm_p2(gcol, xs, xnT_dst, tag, psT, keys_w)

    dk_ctr = [0]

    def dk():
        dk_ctr[0] += 1
        return "c%d" % dk_ctr[0]

    def phase_A(src_d, dst_d):
        ar.reset(CONST_END)
        win = ar.alloc([8, 4096], BF16)
        wout = ar.alloc([16, 1024], BF16)
        wsp_f = ar.alloc([8, 128], F32)
        wmT = ar.alloc([8, 128], BF16)
        tri = ar.alloc([128], F32)
        biasT = ar.alloc([8, 128], F32)
        gcol = ar.alloc([8], F32)
        gv = ar.alloc([16], F32)
        xt_ring = [ar.alloc([1024], F32) for _ in range(2)]
        xr_ring = [ar.alloc([1024], F32) for _ in range(2)]
        xs_ring = [{"bf": ar.alloc([1024], BF16), "junk": None} for _ in range(2)]
        junk = ar.alloc([1024], BF16)
        for d_ in xs_ring:
            d_["junk"] = junk
        ss_r = [ar.alloc([1], F32) for _ in range(2)]
        rstd_r = [ar.alloc([1], F32) for _ in range(2)]
        xnT = ar.alloc([8, 512], BF16)
        uT = ar.alloc([16, 512], BF16)
        vbf = ar.alloc([4, 2048], BF16)
        ssv_r = [ar.alloc([4], F32) for _ in range(2)]
        rsv_r = [ar.alloc([1], F32) for _ in range(2)]
        wms_r = [ar.alloc([8, 128], BF16) for _ in range(4)]
        yT = ar.alloc([16, 512], BF16)
        tmp_r = [ar.alloc([512], F32) for _ in range(2)]

        w_in = w["l0_sg_w_in"].rearrange("(k p) n -> p k n", p=128)
        for k in range(8):
            P.add("gpsimd", (lambda k: lambda e: e.dma_start(out=win[:, k, :], in_=w_in[:, k, :]))(k),
                  w=[("win", k)], dkey="w%d" % k)
        w_o = w["l0_sg_w_out"].rearrange("(k p) n -> p k n", p=128)
        for q in range(4):
            P.add("gpsimd", (lambda q: lambda e: e.dma_start(out=wout[:, 4 * q:4 * q + 4, :], in_=w_o[:, 4 * q:4 * q + 4, :]))(q),
                  w=[("wout", q)], dkey="w%d" % (8 + q))
        P.add("sync", lambda e: e.dma_start(out=wsp_f, in_=w["l0_sg_w_spatial"].rearrange("g t s -> t g s")), w=["wsp_f"], dkey="c0")
        P.add("sync", lambda e: e.dma_start(out=tri, in_=tri_d[:, :]), w=["tri"], dkey="c1")
        P.add("sync", lambda e: e.dma_start(out=biasT.rearrange("p g t -> p (g t)"),
                                            in_=w["l0_sg_b_spatial"].rearrange("g t -> (g t)").partition_broadcast(128)),
              w=["biasT"], dkey="c2")
        P.add("sync", col_load(gcol, w["l0_mix_norm"], 8), w=["gcol"], dkey="c3")
        P.add("sync", col_load(gv, w["l0_sg_v_norm"], 16), w=["gv"], dkey="c4")

        bT = 0

        def wtr(e):
            last = None
            for g in range(8):
                last = e.transpose(ps_t[:, g // 4, (g % 4) * 128:(g % 4 + 1) * 128], wsp_f[:, g, :], ident_f)
            return last
        P.add("tensor", wtr, r=["wsp_f", "ident_f"], w=[("ps", 0), ("ps", 1)])

        def wmk(e):
            return e.tensor_tensor(out=wmT, in0=ps_t[:, 0:2, :].rearrange("p a (b t) -> p (a b) t", b=4),
                                   in1=tri.unsqueeze(1).to_broadcast([128, 8, 128]), op=ALU.mult)
        P.add("vector", wmk, r=[("ps", 0), ("ps", 1), "tri"], w=["wmT"])

        psT = PsRing([0, 1])
        psM = PsRing([2, 3, 4, 5, 6, 7])

        for st in range(NST):
            for c in range(4):
                T = st * 4 + c
                xt = xt_ring[T % 2]
                P.add("sync", (lambda xt, T: lambda e: e.dma_start(out=xt, in_=src_d[T * 128:(T + 1) * 128, :]))(xt, T),
                      w=[("xt", T % 2)], dkey="ld%d" % (T % 2))
                norm_tile(xt, gcol, xs_ring[T % 2], ss_r[T % 2], rstd_r[T % 2],
                          xnT[:, :, c * 128:(c + 1) * 128], ("A", T % 2), psT,
                          keys_r=[("xt", T % 2)], keys_w=[("xnT", c)])
            for m in range(16):
                b = psM.next()

                def mm(e, m=m, b=b):
                    last = None
                    for k in range(8):
                        last = e.matmul(psf(b), lhsT=win[:, k, m * 128:(m + 1) * 128], rhs=xnT[:, k, :],
                                        start=(k == 0), stop=(k == 7))
                    return last
                P.add("tensor", mm, r=[("win", k) for k in range(8)] + [("xnT", c) for c in range(4)], w=[("ps", b)])
                P.add("scalar", (lambda m, b: lambda e: e.activation(out=uT[:, m, :], in_=psf(b), func=AF.Gelu_apprx_tanh))(m, b),
                      r=[("ps", b)], w=[("uT", m)])
            for c in range(4):
                T = st * 4 + c
                ssv = ssv_r[T % 2]
                rsv = rsv_r[T % 2]
                P.add("gpsimd", (lambda ssv: lambda e: e.memset(ssv, 0.0))(ssv), w=[("ssv", T % 2)])
                for j in range(4):
                    b = psM.next()

                    def mmv(e, c=c, j=j, b=b):
                        last = None
                        for k in range(8):
                            last = e.matmul(psf(b), lhsT=xnT[:, k, c * 128:(c + 1) * 128],
                                            rhs=win[:, k, 2048 + j * 512:2048 + (j + 1) * 512],
                                            start=(k == 0), stop=(k == 7))
                        return last
                    P.add("tensor", mmv, r=[("win", k) for k in range(8)] + [("xnT", c)], w=[("ps", b)])
                    P.add("scalar", (lambda c, j, b: lambda e: e.activation(out=vbf[:, c, j * 512:(j + 1) * 512], in_=psf(b),
                                                                             func=AF.Gelu_apprx_tanh))(c, j, b),
                          r=[("ps", b)], w=[("vbf", c, j)])
                    P.add("scalar", (lambda c, j, ssv: lambda e: e.activation(out=junk[:, 0:512], in_=vbf[:, c, j * 512:(j + 1) * 512],
                                                                               func=AF.Square, accum_out=ssv[:, j:j + 1]))(c, j, ssv),
                          r=[("vbf", c, j), ("ssv", T % 2)], w=[("ssv", T % 2), ("junkA",)])

                P.chain("gpsimd", [
                    (lambda ssv, rsv: lambda e: e.tensor_tensor(out=rsv, in0=ssv[:, 0:1], in1=ssv[:, 1:2], op=ALU.add))(ssv, rsv),
                    (lambda ssv, rsv: lambda e: e.tensor_tensor(out=rsv, in0=rsv, in1=ssv[:, 2:3], op=ALU.add))(ssv, rsv),
                    (lambda ssv, rsv: lambda e: e.tensor_tensor(out=rsv, in0=rsv, in1=ssv[:, 3:4], op=ALU.add))(ssv, rsv),
                    (lambda ssv, rsv: lambda e: e.tensor_scalar(out=rsv, in0=rsv, scalar1=1.0 / 2048, scalar2=EPS, op0=ALU.mult, op1=ALU.add))(ssv, rsv),
                    (lambda ssv, rsv: lambda e: e.tensor_tensor(out=rsv, in0=rsv, in1=neghalf[:, 0:1], op=ALU.pow))(ssv, rsv),
                ], r=[("ssv", T % 2)], w=[("rsv", T % 2)])
                wms = wms_r[T % 4]
                P.add("gpsimd", (lambda wms, rsv: lambda e: e.tensor_scalar(out=wms, in0=wmT, scalar1=rsv[:, 0:1], scalar2=None,
                                                                             op0=ALU.mult))(wms, rsv),
                      r=["wmT", ("rsv", T % 2)], w=[("wms", T % 4)], strict=True)
            for m in range(16):
                g = m // 2
                b = psM.next()

                def mmx(e, m=m, g=g, b=b):
                    last = None
                    for c in range(4):
                        T = st * 4 + c
                        last = e.matmul(psf(b)[:, c * 128:(c + 1) * 128], lhsT=vbf[:, c, m * 128:(m + 1) * 128],
                                        rhs=wms_r[T % 4][:, g, :], start=True, stop=True)
                    return last
                P.add("tensor", mmx, r=[("vbf", c, m // 4) for c in range(4)] + [("wms", (st * 4 + c) % 4) for c in range(4)],
                      w=[("ps", b)])
                tmp = tmp_r[m % 2]

                def e1(e, m=m, g=g, b=b, tmp=tmp):
                    return e.scalar_tensor_tensor(out=tmp.rearrange("p (c t) -> p c t", c=4),
                                                  in0=psf(b).rearrange("p (c t) -> p c t", c=4),
                                                  scalar=gv[:, m:m + 1],
                                                  in1=biasT[:, g, :].unsqueeze(1).to_broadcast([128, 4, 128]),
                                                  op0=ALU.mult, op1=ALU.add)
                P.add("vector", e1, r=[("ps", b), "gv", "biasT"], w=[("tmp", m % 2)])
                P.add("vector", (lambda m, tmp: lambda e: e.tensor_tensor(out=yT[:, m, :], in0=tmp, in1=uT[:, m, :], op=ALU.mult))(m, tmp),
                      r=[("tmp", m % 2), ("uT", m)], w=[("yT", m)])
            for c in range(4):
                T = st * 4 + c
                xr = xr_ring[T % 2]
                P.add("sync", (lambda xr, T: lambda e: e.dma_start(out=xr, in_=src_d[T * 128:(T + 1) * 128, :]))(xr, T),
                      w=[("xr", T % 2)], dkey="lr%d" % (T % 2))
                for j in range(2):
                    b = psM.next()

                    def mmo(e, c=c, j=j, b=b):
                        last = None
                        for k in range(16):
                            last = e.matmul(psf(b), lhsT=yT[:, k, c * 128:(c + 1) * 128], rhs=wout[:, k, j * 512:(j + 1) * 512],
                                            start=(k == 0), stop=(k == 15))
                        return last
                    P.add("tensor", mmo, r=[("yT", m) for m in range(16)] + [("wout", q) for q in range(4)], w=[("ps", b)])
                    P.add("vector", (lambda xr, j, b: lambda e: e.tensor_tensor(out=xr[:, j * 512:(j + 1) * 512], in0=psf(b),
                                                                                 in1=xr[:, j * 512:(j + 1) * 512], op=ALU.add))(xr, j, b),
                          r=[("ps", b), ("xr", T % 2)], w=[("xr", T % 2)])
                P.add("sync", (lambda xr, T: lambda e: e.dma_start(out=dst_d[T * 128:(T + 1) * 128, :], in_=xr))(xr, T),
                      r=[("xr", T % 2)], w=[("hA", T)], dkey="st%d" % (T % 2))


    def phase_B(src_d, dst_d):
        ar.reset(CONST_END)
        dk_ctr[0] = 0
        wg = ar.alloc([8, FFN], BF16)
        wu = ar.alloc([8, FFN], BF16)
        wd = ar.alloc([NF0, 1024], BF16)
        gcol = ar.alloc([8], F32)
        xt_ring = [ar.alloc([1024], F32) for _ in range(2)]
        xr_ring = [ar.alloc([1024], F32) for _ in range(2)]
        junk = ar.alloc([1024], BF16)
        xs_ring = [{"bf": ar.alloc([1024], BF16), "junk": junk} for _ in range(4)]
        ss_r = [ar.alloc([1], F32) for _ in range(4)]
        rstd_r = [ar.alloc([1], F32) for _ in range(4)]
        xnT = ar.alloc([8, 512], BF16)
        HT = ar.alloc([NF0, 512], BF16)
        tmp_r = [ar.alloc([512], F32) for _ in range(2)]

        wgd = w["l0_ffn_w_gate"].rearrange("(k p) n -> p k n", p=128)
        wud = w["l0_ffn_w_up"].rearrange("(k p) n -> p k n", p=128)
        wdd = w["l0_ffn_w_down"].rearrange("(k p) n -> p k n", p=128)
        P.add("sync", col_load(gcol, w["l0_ffn_norm"], 8), w=["gcol"], dkey=dk())
        for k in range(8):
            P.add("gpsimd", (lambda k: lambda e: e.dma_start(out=wg[:, k, :], in_=wgd[:, k, :]))(k), w=[("wg", k)], dkey="w%d" % k)
            P.add("gpsimd", (lambda k: lambda e: e.dma_start(out=wu[:, k, :], in_=wud[:, k, :]))(k), w=[("wu", k)], dkey="w%d" % (8 + k))
        for q in range(2):
            P.add("gpsimd", (lambda q: lambda e: e.dma_start(out=wd[:, 11 * q:11 * q + 11, :], in_=wdd[:, 11 * q:11 * q + 11, :]))(q),
                  w=[("wd", q)], dkey="w%d" % (16 + q))
        psT = PsRing([0, 1])
        psM = PsRing([2, 3, 4, 5, 6, 7])

        def p1(st):
            for c in range(4):
                T = st * 4 + c
                xt = xt_ring[T % 2]
                P.add("sync", (lambda xt, T: lambda e: e.dma_start(out=xt, in_=src_d[T * 128:(T + 1) * 128, :]))(xt, T),
                      w=[("xt", T % 2)], dkey="ld%d" % (T % 2))
                norm_p1(xt, xs_ring[c], ss_r[c], rstd_r[c], ("B", c), [("xt", T % 2)])

        def p2(st):
            for c in range(4):
                norm_p2(gcol, xs_ring[c], xnT[:, :, c * 128:(c + 1) * 128], ("B", c), psT, [("xnT", c)])

        p1(0)
        for st in range(NST):
            p2(st)
            for m in range(NF0):
                bg = psM.next()
                bu = psM.next()

                def mmg(e, m=m, bg=bg):
                    last = None
                    for k in range(8):
                        last = e.matmul(psf(bg), lhsT=wg[:, k, m * 128:(m + 1) * 128], rhs=xnT[:, k, :], start=(k == 0), stop=(k == 7))
                    return last

                def mmu(e, m=m, bu=bu):
                    last = None
                    for k in range(8):
                        last = e.matmul(psf(bu), lhsT=wu[:, k, m * 128:(m + 1) * 128], rhs=xnT[:, k, :], start=(k == 0), stop=(k == 7))
                    return last
                xk = [("xnT", c) for c in range(4)]
                P.add("tensor", mmg, r=[("wg", k) for k in range(8)] + xk, w=[("ps", bg)])
                P.add("tensor", mmu, r=[("wu", k) for k in range(8)] + xk, w=[("ps", bu)])
                tmp = tmp_r[m % 2]
                P.add("scalar", (lambda tmp, bg: lambda e: e.activation(out=tmp, in_=psf(bg), func=AF.Silu))(tmp, bg),
                      r=[("ps", bg)], w=[("tmp", m % 2)])
                P.add("vector", (lambda tmp, bu, m: lambda e: e.tensor_tensor(out=HT[:, m, :], in0=psf(bu), in1=tmp, op=ALU.mult))(tmp, bu, m),
                      r=[("ps", bu), ("tmp", m % 2)], w=[("HT", m)])
            if st + 1 < NST:
                p1(st + 1)
            for c in range(4):
                T = st * 4 + c
                xr = xr_ring[T % 2]
                P.add("sync", (lambda xr, T: lambda e: e.dma_start(out=xr, in_=src_d[T * 128:(T + 1) * 128, :]))(xr, T),
                      w=[("xr", T % 2)], dkey="lr%d" % (T % 2))
                for j in range(2):
                    b = psM.next()

                    def mmd(e, c=c, j=j, b=b):
                        last = None
                        for k in range(NF0):
                            last = e.matmul(psf(b), lhsT=HT[:, k, c * 128:(c + 1) * 128], rhs=wd[:, k, j * 512:(j + 1) * 512],
                                            start=(k == 0), stop=(k == NF0 - 1))
                        return last
                    P.add("tensor", mmd, r=[("HT", m) for m in range(NF0)] + [("wd", 0), ("wd", 1)], w=[("ps", b)])
                    P.add("vector", (lambda xr, j, b: lambda e: e.tensor_tensor(out=xr[:, j * 512:(j + 1) * 512], in0=psf(b),
                                                                                 in1=xr[:, j * 512:(j + 1) * 512], op=ALU.add))(xr, j, b),
                          r=[("ps", b), ("xr", T % 2)], w=[("xr", T % 2)])
                P.add("sync", (lambda xr, T: lambda e: e.dma_start(out=dst_d[T * 128:(T + 1) * 128, :], in_=xr))(xr, T),
                      r=[("xr", T % 2)], w=[("hB", T)], dkey="st%d" % (T % 2))


    def phase_C(src_d, dst_d):
        ar.reset(CONST_END)
        dk_ctr[0] = 0
        wqkv = ar.alloc([8, 3072], BF16)
        gcol = ar.alloc([8], F32)
        gqk = ar.alloc([2, 64], F32)
        posi = ar.alloc([NT], I32)
        posf = ar.alloc([NT], F32)
        invf = ar.alloc([8], F32)
        ang = ar.alloc([NT, 8], F32)
        angk = ar.alloc([NT, 8], I32)
        angf = ar.alloc([NT, 8], F32)
        angm = ar.alloc([NT, 8], F32)
        sinT = ar.alloc([NT, 8], F32)
        cosT = ar.alloc([NT, 8], F32)
        xt_ring = [ar.alloc([1024], F32) for _ in range(2)]
        junk = ar.alloc([1024], BF16)
        xs_ring = [{"bf": ar.alloc([1024], BF16), "junk": junk} for _ in range(2)]
        ss_r = [ar.alloc([1], F32) for _ in range(2)]
        rstd_r = [ar.alloc([1], F32) for _ in range(2)]
        xnT_r = [ar.alloc([8, 128], BF16) for _ in range(2)]
        qk_r = [ar.alloc([32, 64], F32) for _ in range(2)]
        sq = ar.alloc([32, 64], F32)
        ssq_r = [ar.alloc([32], F32) for _ in range(2)]
        qkb_r = [ar.alloc([32, 64], BF16) for _ in range(2)]
        rt = [ar.alloc([32, 8], F32) for _ in range(4)]
        qT_st = [ar.alloc([8, 512], BF16) for _ in range(2)]
        kT_st = [ar.alloc([8, 512], BF16) for _ in range(2)]
        vb_r = [ar.alloc([1024], BF16) for _ in range(2)]

        wq_d = w["l1_da_w_qkv"].rearrange("(k p) n -> p k n", p=128)
        for k in range(8):
            P.add("gpsimd", (lambda k: lambda e: e.dma_start(out=wqkv[:, k, :], in_=wq_d[:, k, :]))(k), w=[("wqkv", k)], dkey="w%d" % k)
        P.add("sync", col_load(gcol, w["l1_mix_norm"], 8), w=["gcol"], dkey=dk())
        P.add("sync", lambda e: e.dma_start(out=gqk[:, 0, :], in_=w["l1_da_q_norm"].partition_broadcast(128)), w=["gqk0"], dkey=dk())
        P.add("sync", lambda e: e.dma_start(out=gqk[:, 1, :], in_=w["l1_da_k_norm"].partition_broadcast(128)), w=["gqk1"], dkey=dk())
        P.add("sync", lambda e: e.dma_start(out=invf, in_=w["c_invf"].partition_broadcast(128)), w=["invf"], dkey=dk())

        def posld(e):
            with nc.allow_non_contiguous_dma(reason="positions to token-major columns"):
                return e.dma_start(out=posi, in_=w["positions"].rearrange("(t p) -> p t", p=128))
        P.add("sync", posld, w=["posi"], dkey=dk())
        P.add("vector", lambda e: e.tensor_scalar(out=gqk[:, 0, :], in0=gqk[:, 0, :], scalar1=0.125, scalar2=None, op0=ALU.mult),
              r=["gqk0"], w=["gqk0"])

        TWO_PI = 2.0 * math.pi

        def trig(dst, shift, tagk):
            def f(e):
                a3 = ang.rearrange("p t i -> p (t i)")
                k3 = angk.rearrange("p t i -> p (t i)")
                f3 = angf.rearrange("p t i -> p (t i)")
                m3 = angm.rearrange("p t i -> p (t i)")
                e.tensor_scalar(out=f3, in0=a3, scalar1=shift, scalar2=None, op0=ALU.add)
                e.tensor_scalar(out=k3, in0=f3, scalar1=1.0 / TWO_PI, scalar2=None, op0=ALU.mult)
                e.tensor_copy(out=m3, in_=k3)
                e.scalar_tensor_tensor(out=f3, in0=m3, scalar=-TWO_PI, in1=f3, op0=ALU.mult, op1=ALU.add)
                e.tensor_scalar(out=m3, in0=f3, scalar1=math.pi, scalar2=-TWO_PI, op0=ALU.is_gt, op1=ALU.mult)
                e.tensor_tensor(out=f3, in0=f3, in1=m3, op=ALU.add)
                e.tensor_scalar(out=m3, in0=f3, scalar1=-math.pi, scalar2=TWO_PI, op0=ALU.is_lt, op1=ALU.mult)
                e.tensor_tensor(out=f3, in0=f3, in1=m3, op=ALU.add)
                return e.tensor_scalar(out=f3, in0=f3, scalar1=3.1415925, scalar2=-3.1415925, op0=ALU.min, op1=ALU.max)
            P.add("vector", f, r=["ang"], w=["angf"])
            P.add("scalar", lambda e: e.activation(out=dst.rearrange("p t i -> p (t i)"), in_=angf.rearrange("p t i -> p (t i)"), func=AF.Sin),
                  r=["angf"], w=[tagk])

        P.add("vector", lambda e: e.tensor_copy(out=posf, in_=posi), r=["posi"], w=["posf"])
        P.add("vector", lambda e: e.tensor_tensor(out=ang, in0=posf.unsqueeze(2).to_broadcast([128, NT, 8]),
                                                  in1=invf.unsqueeze(1).to_broadcast([128, NT, 8]), op=ALU.mult),
              r=["posf", "invf"], w=["ang"])
        trig(sinT, 0.0, "sinT")
        trig(cosT, 0.5 * math.pi, "cosT")

        psT = PsRing([0, 1])
        psQ = PsRing([2, 3])
        psM = PsRing([4, 5, 6, 7])
        qT_v = qT_d.rearrange("h p s -> p h s")
        kT_v = kT_d.rearrange("h p s -> p h s")
        v_v = v_d.rearrange("s h d -> s (h d)")

        for T in range(NT):
            st, c = T // 4, T % 4
            s2 = T % 2
            xt = xt_ring[s2]
            P.add("sync", (lambda xt, T: lambda e: e.dma_start(out=xt, in_=src_d[T * 128:(T + 1) * 128, :]))(xt, T),
                  w=[("xt", s2)], dkey="ld%d" % s2)
            xnT = xnT_r[s2]
            norm_tile(xt, gcol, xs_ring[s2], ss_r[s2], rstd_r[s2], xnT, ("C", s2), psT, [("xt", s2)], [("xnT", s2)])
            qk = qk_r[s2]
            qkf = qk.rearrange("p g d -> p (g d)")
            sqf = sq.rearrange("p g d -> p (g d)")
            vb = vb_r[s2]
            for jb in range(6):
                b = psM.next()

                def mm(e, jb=jb, b=b, xnT=xnT):
                    last = None
                    for k in range(8):
                        last = e.matmul(psf(b), lhsT=xnT[:, k, :], rhs=wqkv[:, k, jb * 512:(jb + 1) * 512], start=(k == 0), stop=(k == 7))
                    return last
                P.add("tensor", mm, r=[("wqkv", k) for k in range(8)] + [("xnT", s2)], w=[("ps", b)])
                if jb < 4:
                    P.add("scalar", (lambda jb, b, qkf: lambda e: e.activation(out=qkf[:, jb * 512:(jb + 1) * 512], in_=psf(b), func=AF.Copy))(jb, b, qkf),
                          r=[("ps", b)], w=[("qk", s2, jb)])
                    P.add("scalar", (lambda jb, b: lambda e: e.activation(out=sqf[:, jb * 512:(jb + 1) * 512], in_=psf(b), func=AF.Square))(jb, b),
                          r=[("ps", b)], w=[("sq", jb)])
                else:
                    P.add("scalar", (lambda jb, b, vb: lambda e: e.activation(out=vb[:, (jb - 4) * 512:(jb - 3) * 512], in_=psf(b), func=AF.Copy))(jb, b, vb),
                          r=[("ps", b)], w=[("vb", s2, jb)])
            ssq = ssq_r[s2]
            P.add("vector", (lambda ssq: lambda e: e.tensor_reduce(out=ssq, in_=sq, axis=AX.X, op=ALU.add))(ssq),
                  r=[("sq", j) for j in range(4)], w=[("ssq", s2)])

            P.chain("gpsimd", [
                (lambda ssq: lambda e: e.tensor_scalar(out=ssq, in0=ssq, scalar1=1.0 / 64, scalar2=EPS, op0=ALU.mult, op1=ALU.add))(ssq),
                (lambda ssq: lambda e: e.tensor_tensor(out=ssq, in0=ssq, in1=neghalf[:, 0:32], op=ALU.pow))(ssq),
            ], r=[("ssq", s2)], w=[("ssq", s2)])
            qkeys = [("qk", s2, j) for j in range(4)]
            P.add("vector", (lambda qk, ssq: lambda e: e.tensor_tensor(out=qk, in0=qk, in1=ssq.unsqueeze(2).to_broadcast([128, 32, 64]), op=ALU.mult))(qk, ssq),
                  r=qkeys + [("ssq", s2)], w=qkeys)

            def gmul(e, qk=qk):
                q4 = qk.rearrange("p (a g) d -> p a g d", a=2)
                return e.tensor_tensor(out=q4, in0=q4, in1=gqk.unsqueeze(2).to_broadcast([128, 2, 16, 64]), op=ALU.mult)
            P.add("gpsimd", gmul, r=qkeys + ["gqk0", "gqk1"], w=qkeys)
            qkb = qkb_r[s2]
            P.add("scalar", (lambda qkb, qkf: lambda e: e.activation(out=qkb.rearrange("p g d -> p (g d)"), in_=qkf, func=AF.Copy))(qkb, qkf),
                  r=qkeys, w=[("qkb", s2)])

            def rope(e, qk=qk, qkb=qkb, T=T):
                cs = cosT[:, T, :].unsqueeze(1).to_broadcast([128, 32, 8])
                sn = sinT[:, T, :].unsqueeze(1).to_broadcast([128, 32, 8])
                x1 = qk[:, :, 0:8]
                x2 = qk[:, :, 8:16]
                e.tensor_tensor(out=rt[0], in0=x1, in1=cs, op=ALU.mult)
                e.tensor_tensor(out=rt[1], in0=x2, in1=sn, op=ALU.mult)
                e.tensor_tensor(out=rt[2], in0=x2, in1=cs, op=ALU.mult)
                e.tensor_tensor(out=rt[3], in0=x1, in1=sn, op=ALU.mult)
                e.tensor_tensor(out=qkb[:, :, 0:8], in0=rt[0], in1=rt[1], op=ALU.subtract)
                return e.tensor_tensor(out=qkb[:, :, 8:16], in0=rt[2], in1=rt[3], op=ALU.add)
            P.add("gpsimd", rope, r=qkeys + [("qkb", s2), "sinT", "cosT"], w=[("qkb", s2)])
            bq = psQ.next()
            bk = psQ.next()

            def trq(e, qkb=qkb, bq=bq, bk=bk):
                last = None
                for h in range(8):
                    e.transpose(psb(bq)[:, h * 128:(h + 1) * 128], qkb[:, 2 * h:2 * h + 2, :].rearrange("p a d -> p (a d)"), ident_bf)
                    last = e.transpose(psb(bk)[:, h * 128:(h + 1) * 128], qkb[:, 16 + 2 * h:18 + 2 * h, :].rearrange("p a d -> p (a d)"), ident_bf)
                return last
            P.add("tensor", trq, r=[("qkb", s2), "ident_bf"], w=[("ps", bq), ("ps", bk)])
            qs = qT_st[st % 2]
            ks = kT_st[st % 2]
            P.add("scalar", (lambda qs, bq, c: lambda e: e.activation(out=qs[:, :, c * 128:(c + 1) * 128],
                                                                       in_=psb(bq).rearrange("p (h t) -> p h t", h=8), func=AF.Copy))(qs, bq, c),
                  r=[("ps", bq)], w=[("qs", st % 2, c)])
            P.add("vector", (lambda ks, bk, c: lambda e: e.tensor_copy(out=ks[:, :, c * 128:(c + 1) * 128],
                                                                        in_=psb(bk).rearrange("p (h t) -> p h t", h=8)))(ks, bk, c),
                  r=[("ps", bk)], w=[("ks", st % 2, c)])
            P.add("sync", (lambda vb, T: lambda e: e.dma_start(out=v_v[T * 128:(T + 1) * 128, :], in_=vb))(vb, T),
                  r=[("vb", s2, 4), ("vb", s2, 5)], w=[("v_d", T)], dkey="sv%d" % s2)
            if c == 3:
                P.add("sync", (lambda qs, st: lambda e: e.dma_start(out=qT_v[:, :, st * 512:(st + 1) * 512], in_=qs))(qs, st),
                      r=[("qs", st % 2, cc) for cc in range(4)], w=[("qT_d", st)], dkey="sq%d" % (st % 2))
                P.add("sync", (lambda ks, st: lambda e: e.dma_start(out=kT_v[:, :, st * 512:(st + 1) * 512], in_=ks))(ks, st),
                      r=[("ks", st % 2, cc) for cc in range(4)], w=[("kT_d", st)], dkey="sk%d" % (st % 2))
        P.barrier()

        ar.reset(CONST_END)
        dk_ctr[0] = 0
        OnT = ar.alloc([8, S], BF16)
        KT_r = [ar.alloc([S], BF16) for _ in range(2)]
        Q0_r = [ar.alloc([S], BF16) for _ in range(2)]
        Q1_r = [ar.alloc([S], BF16) for _ in range(2)]
        V_r = [ar.alloc([NT, 128], BF16) for _ in range(2)]
        E_r = [ar.alloc([512], BF16) for _ in range(6)]
        tri_f = ar.alloc([128], F32)
        maskneg = ar.alloc([128], BF16)
        ones_b = ar.alloc([128], BF16)
        lamv = ar.alloc([4, 64], F32)
        lamp = ar.alloc([2, 64], F32)
        lsum = [ar.alloc([1], F32) for _ in range(2)]
        lexp = [ar.alloc([1], F32) for _ in range(2)]
        nlam = ar.alloc([1], F32)
        gb = ar.alloc([2, 64], F32)
        mx = [ar.alloc([1], F32) for _ in range(2)]
        negB = ar.alloc([1], F32)
        sg = ar.alloc([1], F32)
        rinv = [ar.alloc([512], F32) for _ in range(2)]
        tA = ar.alloc([512], F32)
        tB = ar.alloc([512], F32)
        sqb = ar.alloc([512], BF16)
        rstd5 = ar.alloc([512], F32)
        epsb = ar.alloc([1], F32)
        wo = ar.alloc([8, 1024], BF16)
        xr_ring = [ar.alloc([1024], F32) for _ in range(2)]

        wo_d = w["l1_da_w_out"].rearrange("(k p) n -> p k n", p=128)
        P.add("gpsimd", lambda e: e.dma_start(out=wo, in_=wo_d), w=["wo"], dkey="w0")
        P.add("sync", lambda e: e.dma_start(out=tri_f, in_=tri_d[:, :]), w=["tri_f"], dkey=dk())
        P.add("vector", lambda e: e.tensor_scalar(out=maskneg, in0=tri_f, scalar1=-1.0, scalar2=30000.0, op0=ALU.add, op1=ALU.mult),
              r=["tri_f"], w=["maskneg"])
        P.add("gpsimd", lambda e: e.memset(ones_b, 1.0), w=["ones_b"])
        P.add("gpsimd", lambda e: e.memset(epsb, EPS), w=["epsb"])
        for s2 in range(2):
            P.add("gpsimd", (lambda s2: lambda e: e.memset(Q0_r[s2][64:128, :], 0.0))(s2), w=[("Q0", s2)])
            P.add("gpsimd", (lambda s2: lambda e: e.memset(Q1_r[s2][0:64, :], 0.0))(s2), w=[("Q1", s2)])
        for i, nm in enumerate(["l1_da_lambda_q1", "l1_da_lambda_q2", "l1_da_lambda_k1", "l1_da_lambda_k2"]):
            P.add("sync", (lambda i, nm: lambda e: e.dma_start(out=lamv[:, i, :], in_=w[nm].partition_broadcast(128)))(i, nm),
                  w=[("lamv", i)], dkey=dk())
        P.add("sync", lambda e: e.dma_start(out=gb[:, 0, :], in_=w["l1_da_q_norm"].partition_broadcast(128)), w=[("gb", 0)], dkey=dk())
        P.add("sync", lambda e: e.dma_start(out=gb[:, 1, :], in_=w["l1_da_k_norm"].partition_broadcast(128)), w=[("gb", 1)], dkey=dk())

        def sgld(e):
            with nc.allow_non_contiguous_dma(reason="tiny gain column"):
                return e.dma_start(out=sg, in_=w["l1_da_subln"].rearrange("(p o) -> p o", o=1))
        P.add("sync", sgld, w=["sg"], dkey=dk())
        P.add("vector", lambda e: e.tensor_scalar(out=sg, in0=sg, scalar1=1.0 - LAMBDA_INIT, scalar2=None, op0=ALU.mult), r=["sg"], w=["sg"])
        P.chain("vector", [
            lambda e: e.tensor_tensor(out=lamp[:, 0, :], in0=lamv[:, 0, :], in1=lamv[:, 2, :], op=ALU.mult),
            lambda e: e.tensor_tensor(out=lamp[:, 1, :], in0=lamv[:, 1, :], in1=lamv[:, 3, :], op=ALU.mult),
            lambda e: e.tensor_reduce(out=lsum[0], in_=lamp[:, 0, :], axis=AX.X, op=ALU.add),
            lambda e: e.tensor_reduce(out=lsum[1], in_=lamp[:, 1, :], axis=AX.X, op=ALU.add),
        ], r=[("lamv", i) for i in range(4)], w=["lsum"])

        def lexpf(e):
            e.activation(out=lexp[0], in_=lsum[0], func=AF.Exp)
            return e.activation(out=lexp[1], in_=lsum[1], func=AF.Exp)
        P.add("scalar", lexpf, r=["lsum"], w=["lexp"])
        P.chain("vector", [
            lambda e: e.tensor_tensor(out=nlam, in0=lexp[1], in1=lexp[0], op=ALU.subtract),
            lambda e: e.tensor_scalar(out=nlam, in0=nlam, scalar1=-LAMBDA_INIT, scalar2=None, op0=ALU.add),
        ], r=["lexp"], w=["nlam"])
        P.chain("vector", [
            lambda e: e.tensor_tensor(out=gb, in0=gb, in1=gb, op=ALU.mult),
            lambda e: e.tensor_reduce(out=mx[0], in_=gb[:, 0, :], axis=AX.X, op=ALU.max),
            lambda e: e.tensor_reduce(out=mx[1], in_=gb[:, 1, :], axis=AX.X, op=ALU.max),
            lambda e: e.tensor_tensor(out=mx[0], in0=mx[0], in1=mx[1], op=ALU.max),
            lambda e: e.tensor_scalar(out=negB, in0=mx[0], scalar1=1.0, scalar2=-8.0, op0=ALU.max, op1=ALU.mult),
        ], r=[("gb", 0), ("gb", 1)], w=["negB"])

        psS = PsRing([4, 5, 6, 7])
        v_h = v_d.rearrange("(j p) h d -> p j h d", p=128)

        def head_loads(h):
            s2 = h % 2
            P.add("sync", lambda e: e.dma_start(out=KT_r[s2], in_=kT_d[h]), w=[("KT", s2)], dkey="lk%d" % s2)
            P.add("sync", lambda e: e.dma_start(out=Q0_r[s2][0:64, :], in_=qT_d[h][0:64, :]), w=[("Q0", s2)], dkey="lq%d" % s2)
            P.add("sync", lambda e: e.dma_start(out=Q1_r[s2][64:128, :], in_=qT_d[h][64:128, :]), w=[("Q1", s2)], dkey="lp%d" % s2)
            P.add("sync", lambda e: e.dma_start(out=V_r[s2], in_=v_h[:, :, h, :]), w=[("V", s2)], dkey="lv%d" % s2)

        items = [(h, Q, j) for h in range(8) for Q in range(NST) for j in range(4 * Q + 4)]
        slot_of = {}
        ectr = [0]

        def front(it):
            h, Q, j = it
            s2 = h % 2
            KT, Q0, Q1 = KT_r[s2], Q0_r[s2], Q1_r[s2]
            if Q == 0 and j == 0:
                if h == 0:
                    head_loads(0)
                if h + 1 < 8:
                    head_loads(h + 1)
            lo = max(0, j - 4 * Q)
            diag = j >= 4 * Q
            bA = psS.next()
            bB = psS.next()

            def smm(e):
                ins = None
                for bS, Qz in ((bA, Q0), (bB, Q1)):
                    ins = e.matmul(psf(bS)[:, lo * 128:512], lhsT=KT[:, j * 128:(j + 1) * 128],
                                   rhs=Qz[:, Q * 512 + lo * 128:(Q + 1) * 512], start=True, stop=not diag)
                    if diag:
                        ins = e.matmul(psf(bS)[:, lo * 128:(lo + 1) * 128], lhsT=ident_bf, rhs=maskneg, start=False, stop=True)
                return ins
            P.add("tensor", smm, r=[("KT", s2), ("Q0", s2), ("Q1", s2), "maskneg", "ident_bf"], w=[("ps", bA), ("ps", bB)])
            sl = []
            for bS in (bA, bB):
                es_ = ectr[0] % 6
                ectr[0] += 1
                sl.append(es_)
                E = E_r[es_]
                P.add("scalar", (lambda E, bS: lambda e: e.activation(out=E[:, lo * 128:512], in_=psf(bS)[:, lo * 128:512],
                                                                      func=AF.Exp, bias=negB[:, 0:1]))(E, bS),
                      r=[("ps", bS), "negB"], w=[("E", es_)])
            slot_of[it] = sl

        def back(it):
            h, Q, j = it
            s2 = h % 2
            V = V_r[s2]
            lo = max(0, j - 4 * Q)
            sl = slot_of.pop(it)

            def av(e):
                ins = None
                for c_, es_ in enumerate(sl):
                    E = E_r[es_]
                    e.matmul(ps_t[:, c_, lo * 128:512], lhsT=V[:, j, :], rhs=E[:, lo * 128:512], start=(j == 0), stop=(j == 4 * Q + 3))
                    ins = e.matmul(ps_t[:, 2 + c_, lo * 128:512], lhsT=ones_b, rhs=E[:, lo * 128:512], start=(j == 0), stop=(j == 4 * Q + 3))
                return ins
            P.add("tensor", av, r=[("E", sl[0]), ("E", sl[1]), ("V", s2), "ones_b"], w=[("ps", 0), ("ps", 1), ("ps", 2), ("ps", 3)])
            if j != 4 * Q + 3:
                return
            P.add("scalar", lambda e: e.activation(out=rinv[0], in_=ps_t[:, 2, :], func=AF.Copy), r=[("ps", 2)], w=["rinv0"])
            P.add("scalar", lambda e: e.activation(out=rinv[1], in_=ps_t[:, 3, :], func=AF.Copy), r=[("ps", 3)], w=["rinv1"])
            P.add("vector", lambda e: e.tensor_copy(out=tA, in_=ps_t[:, 0, :]), r=[("ps", 0)], w=["tA"])
            P.add("vector", lambda e: e.tensor_copy(out=tB, in_=ps_t[:, 1, :]), r=[("ps", 1)], w=["tB"])
            P.add("vector", lambda e: e.reciprocal(out=rinv[0], in_=rinv[0]), r=["rinv0"], w=["rinv0"])
            P.add("vector", lambda e: e.tensor_tensor(out=tA, in0=tA, in1=rinv[0], op=ALU.mult), r=["tA", "rinv0"], w=["tA"])
            P.add("vector", lambda e: e.reciprocal(out=rinv[1], in_=rinv[1]), r=["rinv1"], w=["rinv1"])
            P.add("vector", lambda e: e.tensor_tensor(out=tB, in0=tB, in1=rinv[1], op=ALU.mult), r=["tB", "rinv1"], w=["tB"])
            P.add("vector", lambda e: e.scalar_tensor_tensor(out=tA, in0=tB, scalar=nlam[:, 0:1], in1=tA, op0=ALU.mult, op1=ALU.add),
                  r=["tA", "tB", "nlam"], w=["tA"])
            P.add("scalar", lambda e: e.activation(out=sqb, in_=tA, func=AF.Square), r=["tA"], w=["sqb"])
            bt = psS.next()
            P.add("tensor", lambda e: e.matmul(psf(bt), lhsT=ones_b, rhs=sqb, start=True, stop=True), r=["sqb", "ones_b"], w=[("ps", bt)])
            P.add("scalar", lambda e: e.activation(out=rstd5, in_=psf(bt), func=AF.Ln, scale=1.0 / 128, bias=epsb[:, 0:1]),
                  r=[("ps", bt), "epsb"], w=["rstd5"])
            P.add("scalar", lambda e: e.activation(out=rstd5, in_=rstd5, func=AF.Exp, scale=-0.5), r=["rstd5"], w=["rstd5"])
            P.add("vector", lambda e: e.scalar_tensor_tensor(out=OnT[:, h, Q * 512:(Q + 1) * 512], in0=tA, scalar=sg[:, 0:1], in1=rstd5,
                                                             op0=ALU.mult, op1=ALU.mult),
                  r=["tA", "rstd5", "sg"], w=[("OnT", h, 4 * Q + i) for i in range(4)])

        LA = 1
        for n in range(len(items) + LA):
            if n < len(items):
                front(items[n])
            if n >= LA:
                back(items[n - LA])

        for T in range(NT):
            s2 = T % 2
            xr = xr_ring[s2]
            P.add("sync", (lambda xr, T: lambda e: e.dma_start(out=xr, in_=src_d[T * 128:(T + 1) * 128, :]))(xr, T),
                  w=[("xr", s2)], dkey="lr%d" % s2)
            for j in range(2):
                b = psS.next()

                def mmo(e, T=T, j=j, b=b):
                    last = None
                    for k in range(8):
                        last = e.matmul(psf(b), lhsT=OnT[:, k, T * 128:(T + 1) * 128], rhs=wo[:, k, j * 512:(j + 1) * 512],
                                        start=(k == 0), stop=(k == 7))
                    return last
                P.add("tensor", mmo, r=[("OnT", k, T) for k in range(8)] + ["wo"], w=[("ps", b)])
                P.add("vector", (lambda xr, j, b: lambda e: e.tensor_tensor(out=xr[:, j * 512:(j + 1) * 512], in0=psf(b),
                                                                             in1=xr[:, j * 512:(j + 1) * 512], op=ALU.add))(xr, j, b),
                      r=[("ps", b), ("xr", s2)], w=[("xr", s2)])
            P.add("sync", (lambda xr, T: lambda e: e.dma_start(out=dst_d[T * 128:(T + 1) * 128, :], in_=xr))(xr, T),
                  r=[("xr", s2)], w=[("hC", T)], dkey="st%d" % s2)


    def phase_D_dense(src_d, dst_d):
        ar.reset(CONST_END)
        dk_ctr[0] = 0
        gcol = ar.alloc([8], F32)
        wr = ar.alloc([8, 8], F32)
        acc = ar.alloc([16, 1024], F32)
        xnT = ar.alloc([8, 2048], BF16)
        gates = ar.alloc([16, 8], F32)
        xs32_r = [ar.alloc([1024], F32) for _ in range(2)]
        junk = ar.alloc([1024], BF16)
        xs_ring = [{"bf": ar.alloc([1024], BF16), "junk": junk} for _ in range(2)]
        ss_r = [ar.alloc([1], F32) for _ in range(2)]
        rstd_r = [ar.alloc([1], F32) for _ in range(2)]
        xn32T = ar.alloc([8, 128], F32)
        lg = ar.alloc([8], F32)
        eq = ar.alloc([8], F32)
        l2 = ar.alloc([8], F32)
        sel = ar.alloc([8], F32)
        ex = ar.alloc([8], F32)
        m1 = ar.alloc([1], F32)
        m2 = ar.alloc([1], F32)
        nm1 = ar.alloc([1], F32)
        den = ar.alloc([1], F32)
        rden = ar.alloc([1], F32)
        wg_r = [ar.alloc([8, 512], BF16) for _ in range(2)]
        wu_r = [ar.alloc([8, 512], BF16) for _ in range(2)]
        wd_r = [ar.alloc([4, 1024], BF16) for _ in range(2)]
        HT_r = [ar.alloc([4, 512], BF16) for _ in range(2)]
        tmp_r = [ar.alloc([512], F32) for _ in range(2)]

        P.add("sync", col_load(gcol, w["l1_moe_norm"], 8), w=["gcol"], dkey=dk())
        P.add("sync", lambda e: e.dma_start(out=wr, in_=w["l1_moe_w_router"].rearrange("(k p) e -> p k e", p=128)), w=["wr"], dkey=dk())
        wg_d = w["l1_moe_w_gate"]
        wu_d = w["l1_moe_w_up"]
        wd_d = w["l1_moe_w_down"]
        psT = PsRing([0, 1])

        groups = [(hf, e_, fg) for hf in range(2) for e_ in range(NE) for fg in range(FE // 512)]

        def load_group(gi):
            hf, e_, fg = groups[gi]
            sl = gi % 2
            P.add("gpsimd", (lambda sl, e_, fg: lambda e: e.dma_start(
                out=wg_r[sl], in_=wg_d[e_].rearrange("(k p) f -> p k f", p=128)[:, :, fg * 512:(fg + 1) * 512]))(sl, e_, fg),
                w=[("wg", sl)], dkey="wg%d" % sl)
            P.add("gpsimd", (lambda sl, e_, fg: lambda e: e.dma_start(
                out=wu_r[sl], in_=wu_d[e_].rearrange("(k p) f -> p k f", p=128)[:, :, fg * 512:(fg + 1) * 512]))(sl, e_, fg),
                w=[("wu", sl)], dkey="wu%d" % sl)
            P.add("gpsimd", (lambda sl, e_, fg: lambda e: e.dma_start(
                out=wd_r[sl], in_=wd_d[e_][fg * 512:(fg + 1) * 512, :].rearrange("(k p) n -> p k n", p=128)))(sl, e_, fg),
                w=[("wd", sl)], dkey="wd%d" % sl)

        gi = 0
        hi = 0
        for hf in range(2):
            for t in range(16):
                T = hf * 16 + t
                s2 = t % 2
                at = acc[:, t, :]
                P.add("sync", (lambda at, T: lambda e: e.dma_start(out=at, in_=src_d[T * 128:(T + 1) * 128, :]))(at, T),
                      w=[("acc", t)], dkey="la%d" % t)
                xs32 = xs32_r[s2]
                norm_p1(at, xs_ring[s2], ss_r[s2], rstd_r[s2], ("D", s2), [("acc", t)], xs32=xs32)
                norm_p2(gcol, xs_ring[s2], xnT[:, :, t * 128:(t + 1) * 128], ("D", s2), psT, [("xnT", t)])

                def trf(e, xs32=xs32):
                    last = None
                    for k in range(8):
                        last = e.transpose(ps_t[:, 2 + k // 4, (k % 4) * 128:(k % 4 + 1) * 128], xs32[:, k * 128:(k + 1) * 128], ident_f)
                    return last
                P.add("tensor", trf, r=[("xs32", ("D", s2)), "ident_f"], w=[("ps", 2), ("ps", 3)])
                P.add("vector", lambda e: e.tensor_tensor(out=xn32T, in0=ps_t[:, 2:4, :].rearrange("p a (b t) -> p (a b) t", b=4),
                                                          in1=gcol.unsqueeze(2).to_broadcast([128, 8, 128]), op=ALU.mult),
                      r=[("ps", 2), ("ps", 3), "gcol"], w=["xn32T"])

                def lgm(e):
                    last = None
                    for k in range(8):
                        last = e.matmul(ps_t[:, 4, 0:8], lhsT=xn32T[:, k, :], rhs=wr[:, k, :], start=(k == 0), stop=(k == 7))
                    return last
                P.add("tensor", lgm, r=["xn32T", "wr"], w=[("ps", 4)])
                P.chain("vector", [
                    lambda e: e.tensor_copy(out=lg, in_=ps_t[:, 4, 0:8]),
                    lambda e: e.tensor_reduce(out=m1, in_=lg, axis=AX.X, op=ALU.max),
                    lambda e: e.tensor_scalar(out=eq, in0=lg, scalar1=m1[:, 0:1], scalar2=None, op0=ALU.is_equal),
                    lambda e: e.scalar_tensor_tensor(out=l2, in0=eq, scalar=-1.0e30, in1=lg, op0=ALU.mult, op1=ALU.add),
                    lambda e: e.tensor_reduce(out=m2, in_=l2, axis=AX.X, op=ALU.max),
                    lambda e: e.tensor_scalar(out=sel, in0=lg, scalar1=m2[:, 0:1], scalar2=None, op0=ALU.is_ge),
                    lambda e: e.tensor_scalar(out=nm1, in0=m1, scalar1=-1.0, scalar2=None, op0=ALU.mult),
                ], r=[("ps", 4)], w=["lg", "sel", "nm1"])
                P.add("scalar", lambda e: e.activation(out=ex, in_=lg, func=AF.Exp, bias=nm1[:, 0:1]), r=["lg", "nm1"], w=["ex"])
                P.chain("vector", [
                    lambda e: e.tensor_tensor(out=ex, in0=ex, in1=sel, op=ALU.mult),
                    lambda e: e.tensor_reduce(out=den, in_=ex, axis=AX.X, op=ALU.add),
                    lambda e: e.reciprocal(out=rden, in_=den),
                    (lambda t: lambda e: e.tensor_scalar(out=gates[:, t, :], in0=ex, scalar1=rden[:, 0:1], scalar2=None, op0=ALU.mult))(t),
                ], r=["ex", "sel"], w=[("gates", t), "ex"])

            psM = PsRing([0, 1, 2, 3, 4, 5, 6, 7])
            if hf == 0:
                load_group(0)
            for e_ in range(NE):
                for fg in range(FE // 512):
                    if gi + 1 < len(groups):
                        load_group(gi + 1)
                    sl = gi % 2
                    wg, wu, wd = wg_r[sl], wu_r[sl], wd_r[sl]
                    for st in range(4):
                        HT = HT_r[hi % 2]
                        hk = hi % 2
                        hi += 1
                        xk = [("xnT", st * 4 + c) for c in range(4)]
                        for fc in range(4):
                            bg = psM.next()
                            bu = psM.next()

                            def mmg(e, fc=fc, bg=bg, wg=wg, st=st):
                                last = None
                                for k in range(8):
                                    last = e.matmul(psf(bg), lhsT=wg[:, k, fc * 128:(fc + 1) * 128], rhs=xnT[:, k, st * 512:(st + 1) * 512],
                                                    start=(k == 0), stop=(k == 7))
                                return last

                            def mmu(e, fc=fc, bu=bu, wu=wu, st=st):
                                last = None
                                for k in range(8):
                                    last = e.matmul(psf(bu), lhsT=wu[:, k, fc * 128:(fc + 1) * 128], rhs=xnT[:, k, st * 512:(st + 1) * 512],
                                                    start=(k == 0), stop=(k == 7))
                                return last
                            P.add("tensor", mmg, r=[("wg", sl)] + xk, w=[("ps", bg)])
                            P.add("tensor", mmu, r=[("wu", sl)] + xk, w=[("ps", bu)])
                            tmp = tmp_r[fc % 2]
                            P.add("scalar", (lambda tmp, bg: lambda e: e.activation(out=tmp, in_=psf(bg), func=AF.Silu))(tmp, bg),
                                  r=[("ps", bg)], w=[("tmp", fc % 2)])
                            P.add("vector", (lambda tmp, bu, fc, HT: lambda e: e.tensor_tensor(out=HT[:, fc, :], in0=psf(bu), in1=tmp, op=ALU.mult))(tmp, bu, fc, HT),
                                  r=[("ps", bu), ("tmp", fc % 2)], w=[("HT", hk, fc)])
                        for c in range(4):
                            t = st * 4 + c
                            for j in range(2):
                                b = psM.next()

                                def mmd(e, c=c, j=j, b=b, HT=HT, wd=wd):
                                    last = None
                                    for k in range(4):
                                        last = e.matmul(psf(b), lhsT=HT[:, k, c * 128:(c + 1) * 128], rhs=wd[:, k, j * 512:(j + 1) * 512],
                                                        start=(k == 0), stop=(k == 3))
                                    return last
                                P.add("tensor", mmd, r=[("HT", hk, fc) for fc in range(4)] + [("wd", sl)], w=[("ps", b)])
                                P.add("vector", (lambda t, j, b, e_: lambda e: e.scalar_tensor_tensor(
                                    out=acc[:, t, j * 512:(j + 1) * 512], in0=psf(b), scalar=gates[:, t, e_:e_ + 1],
                                    in1=acc[:, t, j * 512:(j + 1) * 512], op0=ALU.mult, op1=ALU.add))(t, j, b, e_),
                                    r=[("ps", b), ("gates", t), ("acc", t)], w=[("acc", t)])
                    gi += 1
            for t in range(16):
                T = hf * 16 + t
                P.add("sync", (lambda t, T: lambda e: e.dma_start(out=dst_d[T * 128:(T + 1) * 128, :], in_=acc[:, t, :]))(t, T),
                      r=[("acc", t)], w=[("hD", T)], dkey="sa%d" % t)


    def phase_D(src_d, dst_d):
        BIG = 1.0e6
        ar.reset(CONST_END)
        dk_ctr[0] = 0
        idxA = ar.alloc([NT, 2], I32)
        idxB = ar.alloc([NT, 2], I32)
        gAB = ar.alloc([NT, 2], F32)
        gBB = ar.alloc([NT, 2], F32)
        gcol = ar.alloc([8], F32)
        P_END = ar.off
        wr = ar.alloc([8, 8], F32)
        tris = ar.alloc([128], F32)
        ones_f = ar.alloc([128], F32)
        eoff = ar.alloc([8], F32)
        Mcum = ar.alloc([8], F32)
        xt_ring = [ar.alloc([1024], F32) for _ in range(2)]
        xs32_r = [ar.alloc([1024], F32) for _ in range(2)]
        junk = ar.alloc([1024], BF16)
        xs_ring = [{"bf": ar.alloc([1024], BF16), "junk": junk} for _ in range(2)]
        ss_r = [ar.alloc([1], F32) for _ in range(2)]
        rstd_r = [ar.alloc([1], F32) for _ in range(2)]
        xn32T = ar.alloc([8, 128], F32)
        lg = ar.alloc([8], F32)
        eq = ar.alloc([8], F32)
        l2 = ar.alloc([8], F32)
        sel = ar.alloc([8], F32)
        ex = ar.alloc([8], F32)
        gt = ar.alloc([8], F32)
        rk = ar.alloc([8], F32)
        sf = ar.alloc([8], F32)
        sm = ar.alloc([8], F32)
        t2 = ar.alloc([8], F32)
        oh = ar.alloc([8], F32)
        tg = ar.alloc([8], F32)
        tg2 = ar.alloc([8], F32)
        cnti = ar.alloc([8], I32)
        m1 = ar.alloc([1], F32)
        m2 = ar.alloc([1], F32)
        nm1 = ar.alloc([1], F32)
        den = ar.alloc([1], F32)
        rden = ar.alloc([1], F32)
        sA = ar.alloc([1], F32)
        sB = ar.alloc([1], F32)
        XT = [ar.alloc([8, 512], BF16) for _ in range(4)]
        acc = [ar.alloc([4, 1024], F32) for _ in range(4)]
        wg_r = [ar.alloc([8, 512], BF16) for _ in range(2)]
        wu_r = [ar.alloc([8, 512], BF16) for _ in range(2)]
        wd_r = [ar.alloc([4, 1024], BF16) for _ in range(2)]
        HT_r = [ar.alloc([4, 512], BF16) for _ in range(2)]
        tmp_r = [ar.alloc([512], F32) for _ in range(2)]
        xg_r = [ar.alloc([1024], BF16) for _ in range(2)]

        P.add("sync", col_load(gcol, w["l1_moe_norm"], 8), w=["gcol"], dkey=dk())
        P.add("sync", lambda e: e.dma_start(out=wr, in_=w["l1_moe_w_router"].rearrange("(k p) e -> p k e", p=128)), w=["wr"], dkey=dk())
        P.add("sync", lambda e: e.dma_start(out=tris, in_=w["c_tris"][:, :]), w=["tris"], dkey=dk())
        P.add("sync", lambda e: e.dma_start(out=eoff, in_=w["c_eoff"].partition_broadcast(128)), w=["eoff"], dkey=dk())
        P.add("gpsimd", lambda e: e.memset(ones_f, 1.0), w=["ones_f"])
        P.add("gpsimd", lambda e: e.memset(Mcum, 0.0), w=["Mcum"])
        psT = PsRing([0, 1])

        for T in range(NT):
            s2 = T % 2
            xt = xt_ring[s2]
            P.add("sync", (lambda xt, T: lambda e: e.dma_start(out=xt, in_=src_d[T * 128:(T + 1) * 128, :]))(xt, T),
                  w=[("xt", s2)], dkey="ld%d" % s2)
            xs32 = xs32_r[s2]
            xsb = xs_ring[s2]["bf"]
            norm_p1(xt, xs_ring[s2], ss_r[s2], rstd_r[s2], ("D", s2), [("xt", s2)], xs32=xs32)

            def trf(e, xs32=xs32):
                last = None
                for k in range(8):
                    last = e.transpose(ps_t[:, 2 + k // 4, (k % 4) * 128:(k % 4 + 1) * 128], xs32[:, k * 128:(k + 1) * 128], ident_f)
                return last
            P.add("tensor", trf, r=[("xs32", ("D", s2)), "ident_f"], w=[("ps", 2), ("ps", 3)])
            P.add("vector", lambda e: e.tensor_tensor(out=xn32T, in0=ps_t[:, 2:4, :].rearrange("p a (b t) -> p (a b) t", b=4),
                                                      in1=gcol.unsqueeze(2).to_broadcast([128, 8, 128]), op=ALU.mult),
                  r=[("ps", 2), ("ps", 3), "gcol"], w=["xn32T"])

            def lgm(e):
                last = None
                for k in range(8):
                    last = e.matmul(ps_t[:, 4, 0:8], lhsT=xn32T[:, k, :], rhs=wr[:, k, :], start=(k == 0), stop=(k == 7))
                return last
            P.add("tensor", lgm, r=["xn32T", "wr"], w=[("ps", 4)])
            P.chain("vector", [
                lambda e: e.tensor_copy(out=lg, in_=ps_t[:, 4, 0:8]),
                lambda e: e.tensor_reduce(out=m1, in_=lg, axis=AX.X, op=ALU.max),
                lambda e: e.tensor_scalar(out=eq, in0=lg, scalar1=m1[:, 0:1], scalar2=None, op0=ALU.is_equal),
                lambda e: e.scalar_tensor_tensor(out=l2, in0=eq, scalar=-1.0e30, in1=lg, op0=ALU.mult, op1=ALU.add),
                lambda e: e.tensor_reduce(out=m2, in_=l2, axis=AX.X, op=ALU.max),
                lambda e: e.tensor_scalar(out=sel, in0=lg, scalar1=m2[:, 0:1], scalar2=None, op0=ALU.is_ge),
                lambda e: e.tensor_scalar(out=nm1, in0=m1, scalar1=-1.0, scalar2=None, op0=ALU.mult),
            ], r=[("ps", 4)], w=["lg", "sel", "nm1"])
            P.add("scalar", lambda e: e.activation(out=ex, in_=lg, func=AF.Exp, bias=nm1[:, 0:1]), r=["lg", "nm1"], w=["ex"])

            def rkm(e):
                e.matmul(ps_t[:, 5, 0:8], lhsT=tris, rhs=sel, start=True, stop=False)
                return e.matmul(ps_t[:, 5, 0:8], lhsT=ones_f, rhs=Mcum, start=False, stop=True)
            P.add("tensor", rkm, r=["sel", "Mcum", "tris", "ones_f"], w=[("ps", 5)])
            P.chain("vector", [
                lambda e: e.tensor_tensor(out=ex, in0=ex, in1=sel, op=ALU.mult),
                lambda e: e.tensor_reduce(out=den, in_=ex, axis=AX.X, op=ALU.add),
                lambda e: e.reciprocal(out=rden, in_=den),
                lambda e: e.tensor_scalar(out=gt, in0=ex, scalar1=rden[:, 0:1], scalar2=None, op0=ALU.mult),
                lambda e: e.tensor_copy(out=rk, in_=ps_t[:, 5, 0:8]),
                lambda e: e.tensor_tensor(out=Mcum, in0=Mcum, in1=sel, op=ALU.add),
                lambda e: e.tensor_tensor(out=sf, in0=rk, in1=eoff, op=ALU.add),
                lambda e: e.scalar_tensor_tensor(out=sm, in0=sf, scalar=-BIG, in1=sel, op0=ALU.add, op1=ALU.mult),
                lambda e: e.tensor_scalar(out=sm, in0=sm, scalar1=BIG, scalar2=None, op0=ALU.add),
                lambda e: e.tensor_reduce(out=sA, in_=sm, axis=AX.X, op=ALU.min),
                lambda e: e.tensor_tensor(out=t2, in0=sf, in1=sel, op=ALU.mult),
                lambda e: e.tensor_reduce(out=sB, in_=t2, axis=AX.X, op=ALU.max),
                lambda e: e.tensor_scalar(out=oh, in0=sm, scalar1=sA[:, 0:1], scalar2=None, op0=ALU.is_equal),
                lambda e: e.tensor_tensor(out=tg, in0=gt, in1=oh, op=ALU.mult),
                (lambda T: lambda e: e.tensor_reduce(out=gAB[:, T, 0:1], in_=tg, axis=AX.X, op=ALU.add))(T),
                lambda e: e.tensor_tensor(out=tg2, in0=gt, in1=tg, op=ALU.subtract),
                (lambda T: lambda e: e.tensor_reduce(out=gBB[:, T, 0:1], in_=tg2, axis=AX.X, op=ALU.add))(T),
                (lambda T: lambda e: e.tensor_copy(out=idxA[:, T, 0:1], in_=sA))(T),
                (lambda T: lambda e: e.tensor_copy(out=idxB[:, T, 0:1], in_=sB))(T),
            ], r=["ex", "sel", ("ps", 5), "eoff", "Mcum"], w=["ex", "Mcum", ("idx", T), ("gab", T)])
            for which, idx_t in ((0, idxA), (1, idxB)):
                P.add("gpsimd", (lambda idx_t, T, xsb: lambda e: e.indirect_dma_start(
                    out=Xg_d[:, :], out_offset=bass.IndirectOffsetOnAxis(ap=idx_t[:, T, 0:1], axis=0),
                    in_=xsb, in_offset=None))(idx_t, T, xsb),
                    r=[("idx", T), ("xsbf", ("D", s2))], w=[("Xg", T, which)], dkey="sc%d%d" % (s2, which))
        P.add("tensor", lambda e: e.matmul(ps_t[:, 5, 0:8], lhsT=ones_f, rhs=Mcum, start=True, stop=True), r=["Mcum", "ones_f"], w=[("ps", 5)])
        P.add("vector", lambda e: e.tensor_copy(out=cnti, in_=ps_t[:, 5, 0:8]), r=[("ps", 5)], w=["cnti"])
        P.add("sync", lambda e: e.dma_start(out=cnt_d[0:1, :], in_=cnti[0:1, :]), r=["cnti"], w=["cnt_d"], dkey=dk())
        P.barrier()

        for e_ in range(NE):
            P.regload("cnt%d" % e_, cnt_d[0:1, e_:e_ + 1])
        wg_d = w["l1_moe_w_gate"]
        wu_d = w["l1_moe_w_up"]
        wd_d = w["l1_moe_w_down"]
        NFG = FE // 512
        psM = PsRing([0, 1, 2, 3, 4, 5, 6, 7])
        ctr = {"hi": 0, "xg": 0}

        def load_w(e_, fg, sl):
            P.add("gpsimd", lambda e: e.dma_start(
                out=wg_r[sl], in_=wg_d[e_].rearrange("(k p) f -> p k f", p=128)[:, :, fg * 512:(fg + 1) * 512]),
                w=[("wg", sl)], dkey="wg%d" % sl)
            P.add("gpsimd", lambda e: e.dma_start(
                out=wu_r[sl], in_=wu_d[e_].rearrange("(k p) f -> p k f", p=128)[:, :, fg * 512:(fg + 1) * 512]),
                w=[("wu", sl)], dkey="wu%d" % sl)
            P.add("gpsimd", lambda e: e.dma_start(
                out=wd_r[sl], in_=wd_d[e_][fg * 512:(fg + 1) * 512, :].rearrange("(k p) n -> p k n", p=128)),
                w=[("wd", sl)], dkey="wd%d" % sl)

        def prep(e_, rnd):
            ck = "cnt%d" % e_
            for k in range(4):
                kk = rnd * 4 + k
                P.pred_begin(ck, kk * 512 + 1)
                for c in range(4):
                    xs_ = ctr["xg"] % 2
                    ctr["xg"] += 1
                    xg = xg_r[xs_]
                    row0 = e_ * S + kk * 512 + c * 128
                    P.add("sync", (lambda xg, row0: lambda e: e.dma_start(out=xg, in_=Xg_d[row0:row0 + 128, :]))(xg, row0),
                          w=[("xg", xs_)], dkey="xg%d" % xs_)
                    b = psM.next()

                    def trx(e, xg=xg, b=b):
                        last = None
                        for kq in range(8):
                            last = e.transpose(psb(b)[:, kq * 128:(kq + 1) * 128], xg[:, kq * 128:(kq + 1) * 128], ident_bf)
                        return last
                    P.add("tensor", trx, r=[("xg", xs_), "ident_bf"], w=[("ps", b)])
                    P.add("vector", (lambda k, c, b: lambda e: e.tensor_tensor(
                        out=XT[k][:, :, c * 128:(c + 1) * 128], in0=psb(b).rearrange("p (k t) -> p k t", k=8),
                        in1=gcol.unsqueeze(2).to_broadcast([128, 8, 128]), op=ALU.mult))(k, c, b),
                        r=[("ps", b), "gcol"], w=[("XT", k, c)])
            for k in range(4):
                P.pred_end()

        def compute(e_, rnd, fg, sl):
            ck = "cnt%d" % e_
            wg, wu, wd = wg_r[sl], wu_r[sl], wd_r[sl]
            for k in range(4):
                kk = rnd * 4 + k
                P.pred_begin(ck, kk * 512 + 1)
                HT = HT_r[ctr["hi"] % 2]
                hk = ctr["hi"] % 2
                ctr["hi"] += 1
                xk = [("XT", k, c) for c in range(4)]
                for fc in range(4):
                    bg = psM.next()
                    bu = psM.next()

                    def mmg(e, fc=fc, bg=bg, wg=wg, k=k):
                        last = None
                        for kq in range(8):
                            last = e.matmul(psf(bg), lhsT=wg[:, kq, fc * 128:(fc + 1) * 128], rhs=XT[k][:, kq, :],
                                            start=(kq == 0), stop=(kq == 7))
                        return last

                    def mmu(e, fc=fc, bu=bu, wu=wu, k=k):
                        last = None
                        for kq in range(8):
                            last = e.matmul(psf(bu), lhsT=wu[:, kq, fc * 128:(fc + 1) * 128], rhs=XT[k][:, kq, :],
                                            start=(kq == 0), stop=(kq == 7))
                        return last
                    P.add("tensor", mmg, r=[("wg", sl)] + xk, w=[("ps", bg)])
                    P.add("tensor", mmu, r=[("wu", sl)] + xk, w=[("ps", bu)])
                    tmp = tmp_r[fc % 2]
                    P.add("scalar", (lambda tmp, bg: lambda e: e.activation(out=tmp, in_=psf(bg), func=AF.Silu))(tmp, bg),
                          r=[("ps", bg)], w=[("tmp", fc % 2)])
                    P.add("vector", (lambda tmp, bu, fc, HT: lambda e: e.tensor_tensor(out=HT[:, fc, :], in0=psf(bu), in1=tmp, op=ALU.mult))(tmp, bu, fc, HT),
                          r=[("ps", bu), ("tmp", fc % 2)], w=[("HT", hk, fc)])
                for c in range(4):
                    for j in range(2):
                        b = psM.next()

                        def mmd(e, c=c, j=j, b=b, HT=HT, wd=wd):
                            last = None
                            for kq in range(4):
                                last = e.matmul(psf(b), lhsT=HT[:, kq, c * 128:(c + 1) * 128], rhs=wd[:, kq, j * 512:(j + 1) * 512],
                                                start=(kq == 0), stop=(kq == 3))
                            return last
                        P.add("tensor", mmd, r=[("HT", hk, fc) for fc in range(4)] + [("wd", sl)], w=[("ps", b)])
                        dst = acc[k][:, c, j * 512:(j + 1) * 512]
                        if fg == 0:
                            P.add("vector", (lambda dst, b: lambda e: e.tensor_copy(out=dst, in_=psf(b)))(dst, b),
                                  r=[("ps", b)], w=[("acc", k, c)])
                        else:
                            P.add("vector", (lambda dst, b: lambda e: e.tensor_tensor(out=dst, in0=psf(b), in1=dst, op=ALU.add))(dst, b),
                                  r=[("ps", b), ("acc", k, c)], w=[("acc", k, c)])
            for k in range(4):
                P.pred_end()

        def stores(e_, rnd):
            ck = "cnt%d" % e_
            for k in range(4):
                kk = rnd * 4 + k
                P.pred_begin(ck, kk * 512 + 1)
                row0 = e_ * S + kk * 512
                P.add("sync", (lambda k, row0: lambda e: e.dma_start(
                    out=Yg_d[row0:row0 + 512, :].rearrange("(c p) n -> p c n", p=128), in_=acc[k]))(k, row0),
                    r=[("acc", k, c) for c in range(4)], w=[("Yg", e_, kk)], dkey="ys%d" % k)
            for k in range(4):
                P.pred_end()

        order0 = [(e_, fg) for e_ in range(NE) for fg in range(NFG)]
        load_w(order0[0][0], order0[0][1], 0)
        for n, (e_, fg) in enumerate(order0):
            if fg == 0:
                prep(e_, 0)
            if n + 1 < len(order0):
                load_w(order0[n + 1][0], order0[n + 1][1], (n + 1) % 2)
            compute(e_, 0, fg, n % 2)
            if fg == NFG - 1:
                stores(e_, 0)
        g = len(order0)
        for e_ in range(NE):
            P.pred_begin("cnt%d" % e_, 2049)
            prep(e_, 1)
            load_w(e_, 0, g % 2)
            for fg in range(NFG):
                if fg + 1 < NFG:
                    load_w(e_, fg + 1, (g + 1) % 2)
                compute(e_, 1, fg, g % 2)
                g += 1
            stores(e_, 1)
            P.pred_end()
        P.barrier()

        ar.reset(P_END)
        ya_r = [ar.alloc([1024], F32) for _ in range(2)]
        yb_r = [ar.alloc([1024], F32) for _ in range(2)]
        xr_r = [ar.alloc([1024], F32) for _ in range(2)]
        for T in range(NT):
            s2 = T % 2
            ya, yb, xr = ya_r[s2], yb_r[s2], xr_r[s2]
            P.add("gpsimd", (lambda ya, T: lambda e: e.indirect_dma_start(
                out=ya, out_offset=None, in_=Yg_d[:, :], in_offset=bass.IndirectOffsetOnAxis(ap=idxA[:, T, 0:1], axis=0)))(ya, T),
                w=[("ya", s2)], dkey="ga%d" % s2)
            P.add("gpsimd", (lambda yb, T: lambda e: e.indirect_dma_start(
                out=yb, out_offset=None, in_=Yg_d[:, :], in_offset=bass.IndirectOffsetOnAxis(ap=idxB[:, T, 0:1], axis=0)))(yb, T),
                w=[("yb", s2)], dkey="gb%d" % s2)
            P.add("sync", (lambda xr, T: lambda e: e.dma_start(out=xr, in_=src_d[T * 128:(T + 1) * 128, :]))(xr, T),
                  w=[("xr", s2)], dkey="lr%d" % s2)
            P.add("vector", (lambda ya, xr, T: lambda e: e.scalar_tensor_tensor(out=xr, in0=ya, scalar=gAB[:, T, 0:1], in1=xr, op0=ALU.mult, op1=ALU.add))(ya, xr, T),
                  r=[("ya", s2), ("xr", s2)], w=[("xr", s2)])
            P.add("vector", (lambda yb, xr, T: lambda e: e.scalar_tensor_tensor(out=xr, in0=yb, scalar=gBB[:, T, 0:1], in1=xr, op0=ALU.mult, op1=ALU.add))(yb, xr, T),
                  r=[("yb", s2), ("xr", s2)], w=[("xr", s2)])
            P.add("sync", (lambda xr, T: lambda e: e.dma_start(out=dst_d[T * 128:(T + 1) * 128, :], in_=xr))(xr, T),
                  r=[("xr", s2)], w=[("hD", T)], dkey="st%d" % s2)


    if os.environ.get("MOE_DENSE"):
        phase_D = phase_D_dense

    order = ["A", "B", "C", "D"]
    nph = order.index(stop_after) + 1
    srcs = [x_d, h1_d, h2_d, h3_d]
    dsts = [h1_d, h2_d, h3_d, out_d]
    dsts[nph - 1] = out_d
    fns = [phase_A, phase_B, phase_C, phase_D]
    for i in range(nph):
        fns[i](srcs[i], dsts[i])
        P.barrier()

    P.emit(es)
    es.close()
    return nc, list(declared.keys())


_CACHE = {}


def _consts():
    ident = np.eye(128, dtype=np.float32)
    tri = np.triu(np.ones((128, 128), dtype=np.float32))
    invf = (1.0 / (np.float32(500000.0) ** (np.arange(0, 16, 2, dtype=np.float32) / np.float32(16)))).astype(np.float32)
    return {
        "c_ident_bf": ident.astype(ml_dtypes.bfloat16),
        "c_ident_f": ident,
        "c_tri": tri,
        "c_invf": invf,
        "c_tris": np.triu(np.ones((128, 128), dtype=np.float32), 1),
        "c_eoff": (np.arange(8, dtype=np.float32) * 4096.0).astype(np.float32),
    }


def kernel(stop_after="D", **inputs):
    key = stop_after
    if key not in _CACHE:
        _CACHE[key] = build_program(stop_after)
    nc, used = _CACHE[key]
    consts = _consts()
    in_maps = []
    for b in range(8):
        m = {}
        for k, v in inputs.items():
            if k not in used:
                continue
            a = np.asarray(v)
            if k == "x":
                m[k] = np.ascontiguousarray(a[b])
            elif k == "positions":
                m[k] = np.ascontiguousarray(a[b]).astype(np.int32, copy=False)
            else:
                m[k] = np.ascontiguousarray(a)
        m.update({k: v for k, v in consts.items() if k in used})
        in_maps.append(m)
    res = run_bass_kernel_spmd(nc, in_maps, core_ids=list(range(8)))
    global _LAST
    _LAST = res
    return np.stack([np.asarray(r["out"]) for r in res.results], axis=0).astype(np.float32, copy=False)
```

```python
import math
import os
from contextlib import ExitStack

import ml_dtypes
import numpy as np

import concourse.bass as bass
import concourse.mybir as mybir
from concourse.bass_utils import run_bass_kernel_spmd

F32 = mybir.dt.float32
BF16 = mybir.dt.bfloat16
I32 = mybir.dt.int32
U8 = mybir.dt.uint8
AF = mybir.ActivationFunctionType
ALU = mybir.AluOpType
AX = mybir.AxisListType

S = 4096
D = 1024
NT = S // 128
NST = S // 512
EPS = 1e-6
LAMBDA_INIT = 0.8 - 0.6 * math.exp(-0.3 * 1)
FFN = 2816
NF0 = FFN // 128
NE = 8
FE = 3584
SIZEOF = {F32: 4, BF16: 2, I32: 4, U8: 1}
COMPUTE = ("tensor", "vector", "scalar", "gpsimd")
ALLENG = COMPUTE + ("sync",)


class Op:
    __slots__ = ("eng", "fn", "deps", "signal", "ticket", "is_dma", "dkey", "dval", "strict", "mark")


class Prog:
    def __init__(self, nc):
        self.nc = nc
        self.ops = {e: [] for e in ALLENG}
        self.lw = {}
        self.rd = {}
        self.dcount = {}
        self.last = {e: None for e in ALLENG}
        self.last_dma = {}

    def add(self, eng, fn, r=(), w=(), dkey=None, strict=False):
        op = Op()
        op.mark = None
        op.strict = strict
        op.eng = eng
        op.fn = fn
        op.signal = False
        op.ticket = 0
        op.is_dma = dkey is not None
        op.dkey = dkey
        op.dval = 0
        deps = []
        for k in r:
            x = self.lw.get(k)
            if x is not None:
                deps.append(x)
        for k in w:
            x = self.lw.get(k)
            if x is not None:
                deps.append(x)
            rr = self.rd.get(k)
            if rr:
                deps.extend(rr[0].values())
                deps.extend(rr[1])
        self._set_deps(op, deps)
        if op.is_dma:
            c = self.dcount.get(dkey, 0) + 1
            self.dcount[dkey] = c
            op.dval = 16 * c
            self.last_dma[dkey] = op
        for k in r:
            rr = self.rd.setdefault(k, ({}, []))
            if op.is_dma:
                rr[1].append(op)
            else:
                rr[0][eng] = op
        for k in w:
            self.lw[k] = op
            self.rd[k] = ({}, [])
        self.ops[eng].append(op)
        if not op.is_dma:
            self.last[eng] = op
        return op

    def _marker(self, mark):
        for e in ALLENG:
            op = Op()
            op.mark = mark
            op.strict = False
            op.eng = e
            op.fn = None
            op.signal = False
            op.ticket = 0
            op.is_dma = False
            op.dkey = None
            op.dval = 0
            op.deps = []
            self.ops[e].append(op)

    def regload(self, key, ap):
        self._marker(("regload", key, ap))

    def pred_begin(self, key, thr):
        self._marker(("begin", key, thr))

    def pred_end(self):
        self._marker(("end",))

    def chain(self, eng, fns, r=(), w=()):
        self.nchain = getattr(self, "nchain", 0) + 1
        key = ("chain", self.nchain)
        op = None
        for i, fn in enumerate(fns):
            op = self.add(eng, fn, r=list(r) + [key], w=list(w) + [key], strict=(i > 0))
        return op

    def _set_deps(self, op, deps):
        seen = set()
        out = []
        for d in deps:
            if d is op or id(d) in seen:
                continue
            seen.add(id(d))
            if (not d.is_dma) and (not op.is_dma) and d.eng == op.eng and not op.strict:
                continue
            out.append(d)
            if not d.is_dma:
                d.signal = True
        op.deps = out

    def barrier(self):
        lasts = [self.last[e] for e in COMPUTE if self.last[e] is not None]
        dmas = list(self.last_dma.values())
        for e in ALLENG:
            op = Op()
            op.mark = None
            op.strict = False
            op.eng = e
            op.fn = None
            op.signal = False
            op.ticket = 0
            op.is_dma = False
            op.dkey = None
            op.dval = 0
            deps = [d for d in lasts if d.eng != e] + dmas
            for d in deps:
                if not d.is_dma:
                    d.signal = True
            op.deps = deps
            self.ops[e].append(op)
        self.lw = {}
        self.rd = {}

    def emit(self, es):
        nc = self.nc
        for e in ALLENG:
            c = 0
            for op in self.ops[e]:
                if op.is_dma:
                    continue
                if op.signal:
                    c += 1
                    op.ticket = c
        psem = {e: es.enter_context(nc.semaphore("pg_" + e)) for e in COMPUTE}
        dsem = {}
        for i, k in enumerate(self.dcount):
            dsem[k] = es.enter_context(nc.semaphore("dq_%d" % i))
        block = es.enter_context(nc.Block())

        def mk(engname):
            def body(eng):
                waited = {}
                regs = {}

                def emit_op(op):
                    for d in op.deps:
                        if d.is_dma:
                            key = ("d", d.dkey)
                            sem = dsem[d.dkey]
                            val = d.dval
                        else:
                            key = ("p", d.eng)
                            sem = psem[d.eng]
                            val = d.ticket
                        if waited.get(key, 0) >= val:
                            continue
                        eng.wait_ge(sem, val)
                        waited[key] = val
                    if op.fn is None:
                        return
                    ins = op.fn(eng)
                    if op.is_dma:
                        ins.then_inc(dsem[op.dkey], 16)
                    elif op.signal:
                        ins.then_inc(psem[engname], 1)

                ops = self.ops[engname]

                def find_end(i):
                    depth = 0
                    j = i
                    while True:
                        m = ops[j].mark
                        if m is not None and m[0] == "begin":
                            depth += 1
                        elif m is not None and m[0] == "end":
                            depth -= 1
                            if depth == 0:
                                return j
                        j += 1

                def emit_range(i, end):
                    nonlocal waited
                    while i < end:
                        op = ops[i]
                        if op.mark is None:
                            emit_op(op)
                            i += 1
                            continue
                        kind = op.mark[0]
                        if kind == "regload":
                            _, key, ap = op.mark
                            if key not in regs:
                                regs[key] = eng.alloc_register("rg_%s_%s" % (engname, key))
                            eng.reg_load(regs[key], ap)
                            i += 1
                            continue
                        assert kind == "begin", kind
                        _, key, thr = op.mark
                        j = find_end(i)
                        region = [o for o in ops[i + 1:j] if o.mark is None]
                        nsig = sum(1 for o in region if (not o.is_dma) and o.signal)
                        dfirst = {}
                        dcnt = {}
                        for o in region:
                            if o.is_dma:
                                dfirst.setdefault(o.dkey, o.dval)
                                dcnt[o.dkey] = dcnt.get(o.dkey, 0) + 1
                        if region:
                            snap = dict(waited)
                            with eng.If_lt(regs[key], thr):
                                for k2, c2 in dcnt.items():
                                    prior = dfirst[k2] - 16
                                    if prior > 0:
                                        eng.wait_ge(dsem[k2], prior)
                                    eng.sem_inc(dsem[k2], 16 * c2)
                                if nsig:
                                    eng.drain().then_inc(psem[engname], nsig)
                            with eng.Else():
                                emit_range(i + 1, j)
                            waited = snap
                        i = j + 1

                emit_range(0, len(ops))
                if engname == "sync":
                    for k, c in self.dcount.items():
                        if waited.get(("d", k), 0) < 16 * c:
                            eng.wait_ge(dsem[k], 16 * c)
            return body

        block.tensor(mk("tensor"))
        block.vector(mk("vector"))
        block.scalar(mk("scalar"))
        block.gpsimd(mk("gpsimd"))
        block.sync(mk("sync"))


class Arena:
    def __init__(self, base_ap, nbytes):
        self.base = base_ap
        self.nbytes = nbytes
        self.off = 0

    def reset(self, off=0):
        self.off = off

    def alloc(self, shape, dt):
        n = SIZEOF[dt]
        for s in shape:
            n *= s
        start = (self.off + 63) // 64 * 64
        assert start + n <= self.nbytes, ("SBUF arena overflow", start + n, self.nbytes)
        self.off = start + n
        ap = self.base[:, start:start + n].bitcast(dt)
        if len(shape) == 2:
            ap = ap.rearrange("p (a b) -> p a b", a=shape[0])
        elif len(shape) == 3:
            ap = ap.rearrange("p (a b c) -> p a b c", a=shape[0], b=shape[1])
        return ap


class PsRing:
    def __init__(self, banks):
        self.banks = list(banks)
        self.i = 0

    def next(self):
        b = self.banks[self.i % len(self.banks)]
        self.i += 1
        return b


def build_program(stop_after="D"):
    nc = bass.Bass("TRN2", target_bir_lowering=False)
    es = ExitStack()

    def din(name, shape, dt=F32):
        return nc.dram_tensor(name, list(shape), dt, kind="ExternalInput").ap()

    specs = {
        "x": ([S, D], F32), "positions": ([S], I32),
        "l0_mix_norm": [D], "l0_sg_w_in": [D, 4096], "l0_sg_v_norm": [2048],
        "l0_sg_w_spatial": [8, 128, 128], "l0_sg_b_spatial": [8, 128], "l0_sg_w_out": [2048, D],
        "l0_ffn_norm": [D], "l0_ffn_w_gate": [D, FFN], "l0_ffn_w_up": [D, FFN], "l0_ffn_w_down": [FFN, D],
        "l1_mix_norm": [D], "l1_da_w_qkv": [D, 3072], "l1_da_q_norm": [64], "l1_da_k_norm": [64],
        "l1_da_lambda_q1": [64], "l1_da_lambda_k1": [64], "l1_da_lambda_q2": [64], "l1_da_lambda_k2": [64],
        "l1_da_subln": [128], "l1_da_w_out": [D, D],
        "l1_moe_norm": [D], "l1_moe_w_router": [D, 8], "l1_moe_w_gate": [8, D, FE], "l1_moe_w_up": [8, D, FE],
        "l1_moe_w_down": [8, FE, D],
        "c_ident_bf": ([128, 128], BF16), "c_ident_f": ([128, 128], F32), "c_tri": ([128, 128], F32),
        "c_invf": ([8], F32), "c_tris": ([128, 128], F32), "c_eoff": ([8], F32),
    }
    declared = {}

    class _W:
        def __getitem__(self, k):
            if k not in declared:
                sp = specs[k]
                if isinstance(sp, tuple):
                    declared[k] = din(k, sp[0], sp[1])
                else:
                    declared[k] = din(k, sp)
            return declared[k]
    w = _W()
    x_d = w["x"]
    ident_bf_d = w["c_ident_bf"]
    ident_f_d = w["c_ident_f"]
    tri_d = w["c_tri"]
    out_d = nc.dram_tensor("out", [S, D], F32, kind="ExternalOutput").ap()
    h1_d = nc.dram_tensor("h1_scr", [S, D], F32).ap()
    h2_d = nc.dram_tensor("h2_scr", [S, D], F32).ap()
    h3_d = nc.dram_tensor("h3_scr", [S, D], F32).ap()
    dbg_kind = "ExternalOutput" if os.environ.get("KDBG") else "Internal"
    qT_d = nc.dram_tensor("qT_scr", [8, 128, S], BF16, kind=dbg_kind).ap()
    kT_d = nc.dram_tensor("kT_scr", [8, 128, S], BF16, kind=dbg_kind).ap()
    v_d = nc.dram_tensor("v_scr", [S, 8, 128], BF16, kind=dbg_kind).ap()

    NSLOT = 8 * S
    Xg_d = nc.dram_tensor("xg_scr", [NSLOT, D], BF16).ap()
    Yg_d = nc.dram_tensor("yg_scr", [NSLOT, D], F32).ap()
    cnt_d = nc.dram_tensor("cnt_scr", [1, 8], I32).ap()
    dbg_d = nc.dram_tensor("dbg", [128, 8, 512], F32, kind=dbg_kind).ap()
    ARENA_BYTES = 199 * 1024
    arena_t = es.enter_context(nc.sbuf_tensor("arena", [128, ARENA_BYTES], U8))
    ar = Arena(arena_t, ARENA_BYTES)
    ps_t = es.enter_context(nc.psum_tensor("psum", [128, 8, 512], F32))

    def psf(b):
        return ps_t[:, b, :]

    def psb(b):
        return ps_t[:, b, :].bitcast(BF16)

    P = Prog(nc)

    ident_bf = ar.alloc([128], BF16)
    ident_f = ar.alloc([128], F32)
    P.add("sync", lambda e: e.dma_start(out=ident_bf, in_=ident_bf_d[:, :]), w=["ident_bf"], dkey="k0")
    P.add("sync", lambda e: e.dma_start(out=ident_f, in_=ident_f_d[:, :]), w=["ident_f"], dkey="k1")
    neghalf = ar.alloc([64], F32)
    P.add("gpsimd", lambda e: e.memset(neghalf, -0.5), w=["neghalf"])
    CONST_END = ar.off

    def col_load(dst, src, n):
        def f(e):
            with nc.allow_non_contiguous_dma(reason="tiny gain vector"):
                return e.dma_start(out=dst, in_=src.rearrange("(k p) -> p k", p=128))
        return f

    def norm_p1(xt, xs, ss, rstd, tag, keys_r, xs32=None):
        def sq(e):
            return e.activation(out=xs["junk"], in_=xt, func=AF.Square, accum_out=ss)
        P.add("gpsimd", lambda e: e.memset(ss, 0.0), w=[("ss", tag)])
        P.add("scalar", sq, r=keys_r + [("ss", tag)], w=[("ss", tag), ("junk", tag)])

        P.chain("gpsimd", [
            lambda e: e.tensor_scalar(out=rstd, in0=ss, scalar1=1.0 / D, scalar2=EPS, op0=ALU.mult, op1=ALU.add),
            lambda e: e.tensor_tensor(out=rstd, in0=rstd, in1=neghalf[:, 0:1], op=ALU.pow),
        ], r=[("ss", tag)], w=[("rstd", tag)])
        if xs32 is None:
            P.add("vector", lambda e: e.tensor_scalar(out=xs["bf"], in0=xt, scalar1=rstd[:, 0:1], scalar2=None, op0=ALU.mult),
                  r=keys_r + [("rstd", tag)], w=[("xsbf", tag)])
        else:
            P.add("vector", lambda e: e.tensor_scalar(out=xs32, in0=xt, scalar1=rstd[:, 0:1], scalar2=None, op0=ALU.mult),
                  r=keys_r + [("rstd", tag)], w=[("xs32", tag)])
            P.add("gpsimd", lambda e: e.tensor_copy(out=xs["bf"], in_=xs32), r=[("xs32", tag)], w=[("xsbf", tag)])

    def norm_p2(gcol, xs, xnT_dst, tag, psT, keys_w):
        b = psT.next()

        def tr(e):
            last = None
            for k in range(8):
                last = e.transpose(psb(b)[:, k * 128:(k + 1) * 128], xs["bf"][:, k * 128:(k + 1) * 128], ident_bf)
            return last
        P.add("tensor", tr, r=[("xsbf", tag), "ident_bf"], w=[("ps", b)])

        def ev(e):
            return e.tensor_tensor(out=xnT_dst, in0=psb(b).rearrange("p (k t) -> p k t", k=8),
                                   in1=gcol.unsqueeze(2).to_broadcast([128, 8, 128]), op=ALU.mult)
        P.add("vector", ev, r=[("ps", b), "gcol"], w=keys_w)

    def norm_tile(xt, gcol, xs, ss, rstd, xnT_dst, tag, psT, keys_r, keys_w):
        norm_p1(xt, xs, ss, rstd, tag, keys_r)
        norm_p2(gcol, xs, xnT_dst, tag, psT, keys_w)

    dk_ctr = [0]

    def dk():
        dk_ctr[0] += 1
        return "c%d" % dk_ctr[0]

    def phase_A(src_d, dst_d):
        ar.reset(CONST_END)
        win = ar.alloc([8, 4096], BF16)
        wout = ar.alloc([16, 1024], BF16)
        wsp_f = ar.alloc([8, 128], F32)
        wmT = ar.alloc([8, 128], BF16)
        tri = ar.alloc([128], F32)
        biasT = ar.alloc([8, 128], F32)
        gcol = ar.alloc([8], F32)
        gv = ar.alloc([16], F32)
        xt_ring = [ar.alloc([1024], F32) for _ in range(2)]
        xr_ring = [ar.alloc([1024], F32) for _ in range(2)]
        xs_ring = [{"bf": ar.alloc([1024], BF16), "junk": None} for _ in range(2)]
        junk = ar.alloc([1024], BF16)
        for d_ in xs_ring:
            d_["junk"] = junk
        ss_r = [ar.alloc([1], F32) for _ in range(2)]
        rstd_r = [ar.alloc([1], F32) for _ in range(2)]
        xnT = ar.alloc([8, 512], BF16)
        uT = ar.alloc([16, 512], BF16)
        vbf = ar.alloc([4, 2048], BF16)
        ssv_r = [ar.alloc([4], F32) for _ in range(2)]
        rsv_r = [ar.alloc([1], F32) for _ in range(2)]
        wms_r = [ar.alloc([8, 128], BF16) for _ in range(4)]
        yT = ar.alloc([16, 512], BF16)
        tmp_r = [ar.alloc([512], F32) for _ in range(2)]

        w_in = w["l0_sg_w_in"].rearrange("(k p) n -> p k n", p=128)
        for k in range(8):
            P.add("gpsimd", (lambda k: lambda e: e.dma_start(out=win[:, k, :], in_=w_in[:, k, :]))(k),
                  w=[("win", k)], dkey="w%d" % k)
        w_o = w["l0_sg_w_out"].rearrange("(k p) n -> p k n", p=128)
        for q in range(4):
            P.add("gpsimd", (lambda q: lambda e: e.dma_start(out=wout[:, 4 * q:4 * q + 4, :], in_=w_o[:, 4 * q:4 * q + 4, :]))(q),
                  w=[("wout", q)], dkey="w%d" % (8 + q))
        P.add("sync", lambda e: e.dma_start(out=wsp_f, in_=w["l0_sg_w_spatial"].rearrange("g t s -> t g s")), w=["wsp_f"], dkey="c0")
        P.add("sync", lambda e: e.dma_start(out=tri, in_=tri_d[:, :]), w=["tri"], dkey="c1")
        P.add("sync", lambda e: e.dma_start(out=biasT.rearrange("p g t -> p (g t)"),
                                            in_=w["l0_sg_b_spatial"].rearrange("g t -> (g t)").partition_broadcast(128)),
              w=["biasT"], dkey="c2")
        P.add("sync", col_load(gcol, w["l0_mix_norm"], 8), w=["gcol"], dkey="c3")
        P.add("sync", col_load(gv, w["l0_sg_v_norm"], 16), w=["gv"], dkey="c4")

        bT = 0

        def wtr(e):
            last = None
            for g in range(8):
                last = e.transpose(ps_t[:, g // 4, (g % 4) * 128:(g % 4 + 1) * 128], wsp_f[:, g, :], ident_f)
            return last
        P.add("tensor", wtr, r=["wsp_f", "ident_f"], w=[("ps", 0), ("ps", 1)])

        def wmk(e):
            return e.tensor_tensor(out=wmT, in0=ps_t[:, 0:2, :].rearrange("p a (b t) -> p (a b) t", b=4),
                                   in1=tri.unsqueeze(1).to_broadcast([128, 8, 128]), op=ALU.mult)
        P.add("vector", wmk, r=[("ps", 0), ("ps", 1), "tri"], w=["wmT"])

        psT = PsRing([0, 1])
        psM = PsRing([2, 3, 4, 5, 6, 7])

        for st in range(NST):
            for c in range(4):
                T = st * 4 + c
                xt = xt_ring[T % 2]
                P.add("sync", (lambda xt, T: lambda e: e.dma_start(out=xt, in_=src_d[T * 128:(T + 1) * 128, :]))(xt, T),
                      w=[("xt", T % 2)], dkey="ld%d" % (T % 2))
                norm_tile(xt, gcol, xs_ring[T % 2], ss_r[T % 2], rstd_r[T % 2],
                          xnT[:, :, c * 128:(c + 1) * 128], ("A", T % 2), psT,
                          keys_r=[("xt", T % 2)], keys_w=[("xnT", c)])
            for m in range(16):
                b = psM.next()

                def mm(e, m=m, b=b):
                    last = None
                    for k in range(8):
                        last = e.matmul(psf(b), lhsT=win[:, k, m * 128:(m + 1) * 128], rhs=xnT[:, k, :],
                                        start=(k == 0), stop=(k == 7))
                    return last
                P.add("tensor", mm, r=[("win", k) for k in range(8)] + [("xnT", c) for c in range(4)], w=[("ps", b)])
                P.add("scalar", (lambda m, b: lambda e: e.activation(out=uT[:, m, :], in_=psf(b), func=AF.Gelu_apprx_tanh))(m, b),
                      r=[("ps", b)], w=[("uT", m)])
            for c in range(4):
                T = st * 4 + c
                ssv = ssv_r[T % 2]
                rsv = rsv_r[T % 2]
                P.add("gpsimd", (lambda ssv: lambda e: e.memset(ssv, 0.0))(ssv), w=[("ssv", T % 2)])
                for j in range(4):
                    b = psM.next()

                    def mmv(e, c=c, j=j, b=b):
                        last = None
                        for k in range(8):
                            last = e.matmul(psf(b), lhsT=xnT[:, k, c * 128:(c + 1) * 128],
                                            rhs=win[:, k, 2048 + j * 512:2048 + (j + 1) * 512],
                                            start=(k == 0), stop=(k == 7))
                        return last
                    P.add("tensor", mmv, r=[("win", k) for k in range(8)] + [("xnT", c)], w=[("ps", b)])
                    P.add("scalar", (lambda c, j, b: lambda e: e.activation(out=vbf[:, c, j * 512:(j + 1) * 512], in_=psf(b),
                                                                             func=AF.Gelu_apprx_tanh))(c, j, b),
                          r=[("ps", b)], w=[("vbf", c, j)])
                    P.add("scalar", (lambda c, j, ssv: lambda e: e.activation(out=junk[:, 0:512], in_=vbf[:, c, j * 512:(j + 1) * 512],
                                                                               func=AF.Square, accum_out=ssv[:, j:j + 1]))(c, j, ssv),
                          r=[("vbf", c, j), ("ssv", T % 2)], w=[("ssv", T % 2), ("junkA",)])

                P.chain("gpsimd", [
                    (lambda ssv, rsv: lambda e: e.tensor_tensor(out=rsv, in0=ssv[:, 0:1], in1=ssv[:, 1:2], op=ALU.add))(ssv, rsv),
                    (lambda ssv, rsv: lambda e: e.tensor_tensor(out=rsv, in0=rsv, in1=ssv[:, 2:3], op=ALU.add))(ssv, rsv),
                    (lambda ssv, rsv: lambda e: e.tensor_tensor(out=rsv, in0=rsv, in1=ssv[:, 3:4], op=ALU.add))(ssv, rsv),
                    (lambda ssv, rsv: lambda e: e.tensor_scalar(out=rsv, in0=rsv, scalar1=1.0 / 2048, scalar2=EPS, op0=ALU.mult, op1=ALU.add))(ssv, rsv),
                    (lambda ssv, rsv: lambda e: e.tensor_tensor(out=rsv, in0=rsv, in1=neghalf[:, 0:1], op=ALU.pow))(ssv, rsv),
                ], r=[("ssv", T % 2)], w=[("rsv", T % 2)])
                wms = wms_r[T % 4]
                P.add("gpsimd", (lambda wms, rsv: lambda e: e.tensor_scalar(out=wms, in0=wmT, scalar1=rsv[:, 0:1], scalar2=None,
                                                                             op0=ALU.mult))(wms, rsv),
                      r=["wmT", ("rsv", T % 2)], w=[("wms", T % 4)], strict=True)
            for m in range(16):
                g = m // 2
                b = psM.next()

                def mmx(e, m=m, g=g, b=b):
                    last = None
                    for c in range(4):
                        T = st * 4 + c
                        last = e.matmul(psf(b)[:, c * 128:(c + 1) * 128], lhsT=vbf[:, c, m * 128:(m + 1) * 128],
                                        rhs=wms_r[T % 4][:, g, :], start=True, stop=True)
                    return last
                P.add("tensor", mmx, r=[("vbf", c, m // 4) for c in range(4)] + [("wms", (st * 4 + c) % 4) for c in range(4)],
                      w=[("ps", b)])
                tmp = tmp_r[m % 2]

                def e1(e, m=m, g=g, b=b, tmp=tmp):
                    return e.scalar_tensor_tensor(out=tmp.rearrange("p (c t) -> p c t", c=4),
                                                  in0=psf(b).rearrange("p (c t) -> p c t", c=4),
                                                  scalar=gv[:, m:m + 1],
                                                  in1=biasT[:, g, :].unsqueeze(1).to_broadcast([128, 4, 128]),
                                                  op0=ALU.mult, op1=ALU.add)
                P.add("vector", e1, r=[("ps", b), "gv", "biasT"], w=[("tmp", m % 2)])
                P.add("vector", (lambda m, tmp: lambda e: e.tensor_tensor(out=yT[:, m, :], in0=tmp, in1=uT[:, m, :], op=ALU.mult))(m, tmp),
                      r=[("tmp", m % 2), ("uT", m)], w=[("yT", m)])
            for c in range(4):
                T = st * 4 + c
                xr = xr_ring[T % 2]
                P.add("sync", (lambda xr, T: lambda e: e.dma_start(out=xr, in_=src_d[T * 128:(T + 1) * 128, :]))(xr, T),
                      w=[("xr", T % 2)], dkey="lr%d" % (T % 2))
                for j in range(2):
                    b = psM.next()

                    def mmo(e, c=c, j=j, b=b):
                        last = None
                        for k in range(16):
                            last = e.matmul(psf(b), lhsT=yT[:, k, c * 128:(c + 1) * 128], rhs=wout[:, k, j * 512:(j + 1) * 512],
                                            start=(k == 0), stop=(k == 15))
                        return last
                    P.add("tensor", mmo, r=[("yT", m) for m in range(16)] + [("wout", q) for q in range(4)], w=[("ps", b)])
                    P.add("vector", (lambda xr, j, b: lambda e: e.tensor_tensor(out=xr[:, j * 512:(j + 1) * 512], in0=psf(b),
                                                                                 in1=xr[:, j * 512:(j + 1) * 512], op=ALU.add))(xr, j, b),
                          r=[("ps", b), ("xr", T % 2)], w=[("xr", T % 2)])
                P.add("sync", (lambda xr, T: lambda e: e.dma_start(out=dst_d[T * 128:(T + 1) * 128, :], in_=xr))(xr, T),
                      r=[("xr", T % 2)], w=[("hA", T)], dkey="st%d" % (T % 2))


    def phase_B(src_d, dst_d):
        ar.reset(CONST_END)
        dk_ctr[0] = 0
        wg = ar.alloc([8, FFN], BF16)
        wu = ar.alloc([8, FFN], BF16)
        wd = ar.alloc([NF0, 1024], BF16)
        gcol = ar.alloc([8], F32)
        xt_ring = [ar.alloc([1024], F32) for _ in range(2)]
        xr_ring = [ar.alloc([1024], F32) for _ in range(2)]
        junk = ar.alloc([1024], BF16)
        xs_ring = [{"bf": ar.alloc([1024], BF16), "junk": junk} for _ in range(4)]
        ss_r = [ar.alloc([1], F32) for _ in range(4)]
        rstd_r = [ar.alloc([1], F32) for _ in range(4)]
        xnT = ar.alloc([8, 512], BF16)
        HT = ar.alloc([NF0, 512], BF16)
        tmp_r = [ar.alloc([512], F32) for _ in range(2)]

        wgd = w["l0_ffn_w_gate"].rearrange("(k p) n -> p k n", p=128)
        wud = w["l0_ffn_w_up"].rearrange("(k p) n -> p k n", p=128)
        wdd = w["l0_ffn_w_down"].rearrange("(k p) n -> p k n", p=128)
        P.add("sync", col_load(gcol, w["l0_ffn_norm"], 8), w=["gcol"], dkey=dk())
        for k in range(8):
            P.add("gpsimd", (lambda k: lambda e: e.dma_start(out=wg[:, k, :], in_=wgd[:, k, :]))(k), w=[("wg", k)], dkey="w%d" % k)
            P.add("gpsimd", (lambda k: lambda e: e.dma_start(out=wu[:, k, :], in_=wud[:, k, :]))(k), w=[("wu", k)], dkey="w%d" % (8 + k))
        for q in range(2):
            P.add("gpsimd", (lambda q: lambda e: e.dma_start(out=wd[:, 11 * q:11 * q + 11, :], in_=wdd[:, 11 * q:11 * q + 11, :]))(q),
                  w=[("wd", q)], dkey="w%d" % (16 + q))
        psT = PsRing([0, 1])
        psM = PsRing([2, 3, 4, 5, 6, 7])

        def p1(st):
            for c in range(4):
                T = st * 4 + c
                xt = xt_ring[T % 2]
                P.add("sync", (lambda xt, T: lambda e: e.dma_start(out=xt, in_=src_d[T * 128:(T + 1) * 128, :]))(xt, T),
                      w=[("xt", T % 2)], dkey="ld%d" % (T % 2))
                norm_p1(xt, xs_ring[c], ss_r[c], rstd_r[c], ("B", c), [("xt", T % 2)])

        def p2(st):
            for c in range(4):
                norm_p2(gcol, xs_ring[c], xnT[:, :, c * 128:(c + 1) * 128], ("B", c), psT, [("xnT", c)])

        p1(0)
        for st in range(NST):
            p2(st)
            for m in range(NF0):
                bg = psM.next()
                bu = psM.next()

                def mmg(e, m=m, bg=bg):
                    last = None
                    for k in range(8):
                        last = e.matmul(psf(bg), lhsT=wg[:, k, m * 128:(m + 1) * 128], rhs=xnT[:, k, :], start=(k == 0), stop=(k == 7))
                    return last

                def mmu(e, m=m, bu=bu):
                    last = None
                    for k in range(8):
                        last = e.matmul(psf(bu), lhsT=wu[:, k, m * 128:(m + 1) * 128], rhs=xnT[:, k, :], start=(k == 0), stop=(k == 7))
                    return last
                xk = [("xnT", c) for c in range(4)]
                P.add("tensor", mmg, r=[("wg", k) for k in range(8)] + xk, w=[("ps", bg)])
                P.add("tensor", mmu, r=[("wu", k) for k in range(8)] + xk, w=[("ps", bu)])
                tmp = tmp_r[m % 2]
                P.add("scalar", (lambda tmp, bg: lambda e: e.activation(out=tmp, in_=psf(bg), func=AF.Silu))(tmp, bg),
                      r=[("ps", bg)], w=[("tmp", m % 2)])
                P.add("vector", (lambda tmp, bu, m: lambda e: e.tensor_tensor(out=HT[:, m, :], in0=psf(bu), in1=tmp, op=ALU.mult))(tmp, bu, m),
                      r=[("ps", bu), ("tmp", m % 2)], w=[("HT", m)])
            if st + 1 < NST:
                p1(st + 1)
            for c in range(4):
                T = st * 4 + c
                xr = xr_ring[T % 2]
                P.add("sync", (lambda xr, T: lambda e: e.dma_start(out=xr, in_=src_d[T * 128:(T + 1) * 128, :]))(xr, T),
                      w=[("xr", T % 2)], dkey="lr%d" % (T % 2))
                for j in range(2):
                    b = psM.next()

                    def mmd(e, c=c, j=j, b=b):
                        last = None
                        for k in range(NF0):
                            last = e.matmul(psf(b), lhsT=HT[:, k, c * 128:(c + 1) * 128], rhs=wd[:, k, j * 512:(j + 1) * 512],
                                            start=(k == 0), stop=(k == NF0 - 1))
                        return last
                    P.add("tensor", mmd, r=[("HT", m) for m in range(NF0)] + [("wd", 0), ("wd", 1)], w=[("ps", b)])
                    P.add("vector", (lambda xr, j, b: lambda e: e.tensor_tensor(out=xr[:, j * 512:(j + 1) * 512], in0=psf(b),
                                                                                 in1=xr[:, j * 512:(j + 1) * 512], op=ALU.add))(xr, j, b),
                          r=[("ps", b), ("xr", T % 2)], w=[("xr", T % 2)])
                P.add("sync", (lambda xr, T: lambda e: e.dma_start(out=dst_d[T * 128:(T + 1) * 128, :], in_=xr))(xr, T),
                      r=[("xr", T % 2)], w=[("hB", T)], dkey="st%d" % (T % 2))


    def phase_C(src_d, dst_d):
        ar.reset(CONST_END)
        dk_ctr[0] = 0
        wqkv = ar.alloc([8, 3072], BF16)
        gcol = ar.alloc([8], F32)
        gqk = ar.alloc([2, 64], F32)
        posi = ar.alloc([NT], I32)
        posf = ar.alloc([NT], F32)
        invf = ar.alloc([8], F32)
        ang = ar.alloc([NT, 8], F32)
        angk = ar.alloc([NT, 8], I32)
        angf = ar.alloc([NT, 8], F32)
        angm = ar.alloc([NT, 8], F32)
        sinT = ar.alloc([NT, 8], F32)
        cosT = ar.alloc([NT, 8], F32)
        xt_ring = [ar.alloc([1024], F32) for _ in range(2)]
        junk = ar.alloc([1024], BF16)
        xs_ring = [{"bf": ar.alloc([1024], BF16), "junk": junk} for _ in range(2)]
        ss_r = [ar.alloc([1], F32) for _ in range(2)]
        rstd_r = [ar.alloc([1], F32) for _ in range(2)]
        xnT_r = [ar.alloc([8, 128], BF16) for _ in range(2)]
        qk_r = [ar.alloc([32, 64], F32) for _ in range(2)]
        sq = ar.alloc([32, 64], F32)
        ssq_r = [ar.alloc([32], F32) for _ in range(2)]
        qkb_r = [ar.alloc([32, 64], BF16) for _ in range(2)]
        rt = [ar.alloc([32, 8], F32) for _ in range(4)]
        qT_st = [ar.alloc([8, 512], BF16) for _ in range(2)]
        kT_st = [ar.alloc([8, 512], BF16) for _ in range(2)]
        vb_r = [ar.alloc([1024], BF16) for _ in range(2)]

        wq_d = w["l1_da_w_qkv"].rearrange("(k p) n -> p k n", p=128)
        for k in range(8):
            P.add("gpsimd", (lambda k: lambda e: e.dma_start(out=wqkv[:, k, :], in_=wq_d[:, k, :]))(k), w=[("wqkv", k)], dkey="w%d" % k)
        P.add("sync", col_load(gcol, w["l1_mix_norm"], 8), w=["gcol"], dkey=dk())
        P.add("sync", lambda e: e.dma_start(out=gqk[:, 0, :], in_=w["l1_da_q_norm"].partition_broadcast(128)), w=["gqk0"], dkey=dk())
        P.add("sync", lambda e: e.dma_start(out=gqk[:, 1, :], in_=w["l1_da_k_norm"].partition_broadcast(128)), w=["gqk1"], dkey=dk())
        P.add("sync", lambda e: e.dma_start(out=invf, in_=w["c_invf"].partition_broadcast(128)), w=["invf"], dkey=dk())

        def posld(e):
            with nc.allow_non_contiguous_dma(reason="positions to token-major columns"):
                return e.dma_start(out=posi, in_=w["positions"].rearrange("(t p) -> p t", p=128))
        P.add("sync", posld, w=["posi"], dkey=dk())
        P.add("vector", lambda e: e.tensor_scalar(out=gqk[:, 0, :], in0=gqk[:, 0, :], scalar1=0.125, scalar2=None, op0=ALU.mult),
              r=["gqk0"], w=["gqk0"])

        TWO_PI = 2.0 * math.pi

        def trig(dst, shift, tagk):
            def f(e):
                a3 = ang.rearrange("p t i -> p (t i)")
                k3 = angk.rearrange("p t i -> p (t i)")
                f3 = angf.rearrange("p t i -> p (t i)")
                m3 = angm.rearrange("p t i -> p (t i)")
                e.tensor_scalar(out=f3, in0=a3, scalar1=shift, scalar2=None, op0=ALU.add)
                e.tensor_scalar(out=k3, in0=f3, scalar1=1.0 / TWO_PI, scalar2=None, op0=ALU.mult)
                e.tensor_copy(out=m3, in_=k3)
                e.scalar_tensor_tensor(out=f3, in0=m3, scalar=-TWO_PI, in1=f3, op0=ALU.mult, op1=ALU.add)
                e.tensor_scalar(out=m3, in0=f3, scalar1=math.pi, scalar2=-TWO_PI, op0=ALU.is_gt, op1=ALU.mult)
                e.tensor_tensor(out=f3, in0=f3, in1=m3, op=ALU.add)
                e.tensor_scalar(out=m3, in0=f3, scalar1=-math.pi, scalar2=TWO_PI, op0=ALU.is_lt, op1=ALU.mult)
                e.tensor_tensor(out=f3, in0=f3, in1=m3, op=ALU.add)
                return e.tensor_scalar(out=f3, in0=f3, scalar1=3.1415925, scalar2=-3.1415925, op0=ALU.min, op1=ALU.max)
            P.add("vector", f, r=["ang"], w=["angf"])
            P.add("scalar", lambda e: e.activation(out=dst.rearrange("p t i -> p (t i)"), in_=angf.rearrange("p t i -> p (t i)"), func=AF.Sin),
                  r=["angf"], w=[tagk])

        P.add("vector", lambda e: e.tensor_copy(out=posf, in_=posi), r=["posi"], w=["posf"])
        P.add("vector", lambda e: e.tensor_tensor(out=ang, in0=posf.unsqueeze(2).to_broadcast([128, NT, 8]),
                                                  in1=invf.unsqueeze(1).to_broadcast([128, NT, 8]), op=ALU.mult),
              r=["posf", "invf"], w=["ang"])
        trig(sinT, 0.0, "sinT")
        trig(cosT, 0.5 * math.pi, "cosT")

        psT = PsRing([0, 1])
        psQ = PsRing([2, 3])
        psM = PsRing([4, 5, 6, 7])
        qT_v = qT_d.rearrange("h p s -> p h s")
        kT_v = kT_d.rearrange("h p s -> p h s")
        v_v = v_d.rearrange("s h d -> s (h d)")

        for T in range(NT):
            st, c = T // 4, T % 4
            s2 = T % 2
            xt = xt_ring[s2]
            P.add("sync", (lambda xt, T: lambda e: e.dma_start(out=xt, in_=src_d[T * 128:(T + 1) * 128, :]))(xt, T),
                  w=[("xt", s2)], dkey="ld%d" % s2)
            xnT = xnT_r[s2]
            norm_tile(xt, gcol, xs_ring[s2], ss_r[s2], rstd_r[s2], xnT, ("C", s2), psT, [("xt", s2)], [("xnT", s2)])
            qk = qk_r[s2]
            qkf = qk.rearrange("p g d -> p (g d)")
            sqf = sq.rearrange("p g d -> p (g d)")
            vb = vb_r[s2]
            for jb in range(6):
                b = psM.next()

                def mm(e, jb=jb, b=b, xnT=xnT):
                    last = None
                    for k in range(8):
                        last = e.matmul(psf(b), lhsT=xnT[:, k, :], rhs=wqkv[:, k, jb * 512:(jb + 1) * 512], start=(k == 0), stop=(k == 7))
                    return last
                P.add("tensor", mm, r=[("wqkv", k) for k in range(8)] + [("xnT", s2)], w=[("ps", b)])
                if jb < 4:
                    P.add("scalar", (lambda jb, b, qkf: lambda e: e.activation(out=qkf[:, jb * 512:(jb + 1) * 512], in_=psf(b), func=AF.Copy))(jb, b, qkf),
                          r=[("ps", b)], w=[("qk", s2, jb)])
                    P.add("scalar", (lambda jb, b: lambda e: e.activation(out=sqf[:, jb * 512:(jb + 1) * 512], in_=psf(b), func=AF.Square))(jb, b),
                          r=[("ps", b)], w=[("sq", jb)])
                else:
                    P.add("scalar", (lambda jb, b, vb: lambda e: e.activation(out=vb[:, (jb - 4) * 512:(jb - 3) * 512], in_=psf(b), func=AF.Copy))(jb, b, vb),
                          r=[("ps", b)], w=[("vb", s2, jb)])
            ssq = ssq_r[s2]
            P.add("vector", (lambda ssq: lambda e: e.tensor_reduce(out=ssq, in_=sq, axis=AX.X, op=ALU.add))(ssq),
                  r=[("sq", j) for j in range(4)], w=[("ssq", s2)])

            P.chain("gpsimd", [
                (lambda ssq: lambda e: e.tensor_scalar(out=ssq, in0=ssq, scalar1=1.0 / 64, scalar2=EPS, op0=ALU.mult, op1=ALU.add))(ssq),
                (lambda ssq: lambda e: e.tensor_tensor(out=ssq, in0=ssq, in1=neghalf[:, 0:32], op=ALU.pow))(ssq),
            ], r=[("ssq", s2)], w=[("ssq", s2)])
            qkeys = [("qk", s2, j) for j in range(4)]
            P.add("vector", (lambda qk, ssq: lambda e: e.tensor_tensor(out=qk, in0=qk, in1=ssq.unsqueeze(2).to_broadcast([128, 32, 64]), op=ALU.mult))(qk, ssq),
                  r=qkeys + [("ssq", s2)], w=qkeys)

            def gmul(e, qk=qk):
                q4 = qk.rearrange("p (a g) d -> p a g d", a=2)
                return e.tensor_tensor(out=q4, in0=q4, in1=gqk.unsqueeze(2).to_broadcast([128, 2, 16, 64]), op=ALU.mult)
            P.add("gpsimd", gmul, r=qkeys + ["gqk0", "gqk1"], w=qkeys)
            qkb = qkb_r[s2]
            P.add("scalar", (lambda qkb, qkf: lambda e: e.activation(out=qkb.rearrange("p g d -> p (g d)"), in_=qkf, func=AF.Copy))(qkb, qkf),
                  r=qkeys, w=[("qkb", s2)])

            def rope(e, qk=qk, qkb=qkb, T=T):
                cs = cosT[:, T, :].unsqueeze(1).to_broadcast([128, 32, 8])
                sn = sinT[:, T, :].unsqueeze(1).to_broadcast([128, 32, 8])
                x1 = qk[:, :, 0:8]
                x2 = qk[:, :, 8:16]
                e.tensor_tensor(out=rt[0], in0=x1, in1=cs, op=ALU.mult)
                e.tensor_tensor(out=rt[1], in0=x2, in1=sn, op=ALU.mult)
                e.tensor_tensor(out=rt[2], in0=x2, in1=cs, op=ALU.mult)
                e.tensor_tensor(out=rt[3], in0=x1, in1=sn, op=ALU.mult)
                e.tensor_tensor(out=qkb[:, :, 0:8], in0=rt[0], in1=rt[1], op=ALU.subtract)
                return e.tensor_tensor(out=qkb[:, :, 8:16], in0=rt[2], in1=rt[3], op=ALU.add)
            P.add("gpsimd", rope, r=qkeys + [("qkb", s2), "sinT", "cosT"], w=[("qkb", s2)])
            bq = psQ.next()
            bk = psQ.next()

            def trq(e, qkb=qkb, bq=bq, bk=bk):
                last = None
                for h in range(8):
                    e.transpose(psb(bq)[:, h * 128:(h + 1) * 128], qkb[:, 2 * h:2 * h + 2, :].rearrange("p a d -> p (a d)"), ident_bf)
                    last = e.transpose(psb(bk)[:, h * 128:(h + 1) * 128], qkb[:, 16 + 2 * h:18 + 2 * h, :].rearrange("p a d -> p (a d)"), ident_bf)
                return last
            P.add("tensor", trq, r=[("qkb", s2), "ident_bf"], w=[("ps", bq), ("ps", bk)])
            qs = qT_st[st % 2]
            ks = kT_st[st % 2]
            P.add("scalar", (lambda qs, bq, c: lambda e: e.activation(out=qs[:, :, c * 128:(c + 1) * 128],
                                                                       in_=psb(bq).rearrange("p (h t) -> p h t", h=8), func=AF.Copy))(qs, bq, c),
                  r=[("ps", bq)], w=[("qs", st % 2, c)])
            P.add("vector", (lambda ks, bk, c: lambda e: e.tensor_copy(out=ks[:, :, c * 128:(c + 1) * 128],
                                                                        in_=psb(bk).rearrange("p (h t) -> p h t", h=8)))(ks, bk, c),
                  r=[("ps", bk)], w=[("ks", st % 2, c)])
            P.add("sync", (lambda vb, T: lambda e: e.dma_start(out=v_v[T * 128:(T + 1) * 128, :], in_=vb))(vb, T),
                  r=[("vb", s2, 4), ("vb", s2, 5)], w=[("v_d", T)], dkey="sv%d" % s2)
            if c == 3:
                P.add("sync", (lambda qs, st: lambda e: e.dma_start(out=qT_v[:, :, st * 512:(st + 1) * 512], in_=qs))(qs, st),
                      r=[("qs", st % 2, cc) for cc in range(4)], w=[("qT_d", st)], dkey="sq%d" % (st % 2))
                P.add("sync", (lambda ks, st: lambda e: e.dma_start(out=kT_v[:, :, st * 512:(st + 1) * 512], in_=ks))(ks, st),
                      r=[("ks", st % 2, cc) for cc in range(4)], w=[("kT_d", st)], dkey="sk%d" % (st % 2))
        P.barrier()

        ar.reset(CONST_END)
        dk_ctr[0] = 0
        OnT = ar.alloc([8, S], BF16)
        KT_r = [ar.alloc([S], BF16) for _ in range(2)]
        Q0_r = [ar.alloc([S], BF16) for _ in range(2)]
        Q1_r = [ar.alloc([S], BF16) for _ in range(2)]
        V_r = [ar.alloc([NT, 128], BF16) for _ in range(2)]
        E_r = [ar.alloc([512], BF16) for _ in range(8)]
        Es = [ar.alloc([512], F32) for _ in range(2)]
        tri_f = ar.alloc([128], F32)
        maskneg = ar.alloc([128], BF16)
        ones_b = ar.alloc([128], BF16)
        ones_f = ar.alloc([128], F32)
        lamv = ar.alloc([4, 64], F32)
        lamp = ar.alloc([2, 64], F32)
        lsum = [ar.alloc([1], F32) for _ in range(2)]
        lexp = [ar.alloc([1], F32) for _ in range(2)]
        nlam = ar.alloc([1], F32)
        gb = ar.alloc([2, 64], F32)
        mx = [ar.alloc([1], F32) for _ in range(2)]
        negB = ar.alloc([1], F32)
        sg = ar.alloc([1], F32)
        rinv = [ar.alloc([512], F32) for _ in range(2)]
        tA = ar.alloc([512], F32)
        tB = ar.alloc([512], F32)
        sqb = ar.alloc([512], BF16)
        rstd5 = ar.alloc([512], F32)
        epsb = ar.alloc([1], F32)
        wo = ar.alloc([8, 1024], BF16)
        xr_ring = [ar.alloc([1024], F32) for _ in range(2)]

        wo_d = w["l1_da_w_out"].rearrange("(k p) n -> p k n", p=128)
        P.add("gpsimd", lambda e: e.dma_start(out=wo, in_=wo_d), w=["wo"], dkey="w0")
        P.add("sync", lambda e: e.dma_start(out=tri_f, in_=tri_d[:, :]), w=["tri_f"], dkey=dk())
        P.add("vector", lambda e: e.tensor_scalar(out=maskneg, in0=tri_f, scalar1=-1.0, scalar2=30000.0, op0=ALU.add, op1=ALU.mult),
              r=["tri_f"], w=["maskneg"])
        P.add("gpsimd", lambda e: e.memset(ones_b, 1.0), w=["ones_b"])
        P.add("gpsimd", lambda e: e.memset(ones_f, 1.0), w=["ones_f"])
        P.add("gpsimd", lambda e: e.memset(epsb, EPS), w=["epsb"])
        for s2 in range(2):
            P.add("gpsimd", (lambda s2: lambda e: e.memset(Q0_r[s2][64:128, :], 0.0))(s2), w=[("Q0", s2)])
            P.add("gpsimd", (lambda s2: lambda e: e.memset(Q1_r[s2][0:64, :], 0.0))(s2), w=[("Q1", s2)])
        for i, nm in enumerate(["l1_da_lambda_q1", "l1_da_lambda_q2", "l1_da_lambda_k1", "l1_da_lambda_k2"]):
            P.add("sync", (lambda i, nm: lambda e: e.dma_start(out=lamv[:, i, :], in_=w[nm].partition_broadcast(128)))(i, nm),
                  w=[("lamv", i)], dkey=dk())
        P.add("sync", lambda e: e.dma_start(out=gb[:, 0, :], in_=w["l1_da_q_norm"].partition_broadcast(128)), w=[("gb", 0)], dkey=dk())
        P.add("sync", lambda e: e.dma_start(out=gb[:, 1, :], in_=w["l1_da_k_norm"].partition_broadcast(128)), w=[("gb", 1)], dkey=dk())

        def sgld(e):
            with nc.allow_non_contiguous_dma(reason="tiny gain column"):
                return e.dma_start(out=sg, in_=w["l1_da_subln"].rearrange("(p o) -> p o", o=1))
        P.add("sync", sgld, w=["sg"], dkey=dk())
        P.add("vector", lambda e: e.tensor_scalar(out=sg, in0=sg, scalar1=1.0 - LAMBDA_INIT, scalar2=None, op0=ALU.mult), r=["sg"], w=["sg"])
        P.chain("vector", [
            lambda e: e.tensor_tensor(out=lamp[:, 0, :], in0=lamv[:, 0, :], in1=lamv[:, 2, :], op=ALU.mult),
            lambda e: e.tensor_tensor(out=lamp[:, 1, :], in0=lamv[:, 1, :], in1=lamv[:, 3, :], op=ALU.mult),
            lambda e: e.tensor_reduce(out=lsum[0], in_=lamp[:, 0, :], axis=AX.X, op=ALU.add),
            lambda e: e.tensor_reduce(out=lsum[1], in_=lamp[:, 1, :], axis=AX.X, op=ALU.add),
        ], r=[("lamv", i) for i in range(4)], w=["lsum"])

        def lexpf(e):
            e.activation(out=lexp[0], in_=lsum[0], func=AF.Exp)
            return e.activation(out=lexp[1], in_=lsum[1], func=AF.Exp)
        P.add("scalar", lexpf, r=["lsum"], w=["lexp"])
        P.chain("vector", [
            lambda e: e.tensor_tensor(out=nlam, in0=lexp[1], in1=lexp[0], op=ALU.subtract),
            lambda e: e.tensor_scalar(out=nlam, in0=nlam, scalar1=-LAMBDA_INIT, scalar2=None, op0=ALU.add),
        ], r=["lexp"], w=["nlam"])
        P.chain("vector", [
            lambda e: e.tensor_tensor(out=gb, in0=gb, in1=gb, op=ALU.mult),
            lambda e: e.tensor_reduce(out=mx[0], in_=gb[:, 0, :], axis=AX.X, op=ALU.max),
            lambda e: e.tensor_reduce(out=mx[1], in_=gb[:, 1, :], axis=AX.X, op=ALU.max),
            lambda e: e.tensor_tensor(out=mx[0], in0=mx[0], in1=mx[1], op=ALU.max),
            lambda e: e.tensor_scalar(out=negB, in0=mx[0], scalar1=1.0, scalar2=-8.0, op0=ALU.max, op1=ALU.mult),
        ], r=[("gb", 0), ("gb", 1)], w=["negB"])

        psS = PsRing([2, 3, 4, 5, 6, 7])
        v_h = v_d.rearrange("(j p) h d -> p j h d", p=128)

        def head_loads(h):
            s2 = h % 2
            P.add("sync", lambda e: e.dma_start(out=KT_r[s2], in_=kT_d[h]), w=[("KT", s2)], dkey="lk%d" % s2)
            P.add("sync", lambda e: e.dma_start(out=Q0_r[s2][0:64, :], in_=qT_d[h][0:64, :]), w=[("Q0", s2)], dkey="lq%d" % s2)
            P.add("sync", lambda e: e.dma_start(out=Q1_r[s2][64:128, :], in_=qT_d[h][64:128, :]), w=[("Q1", s2)], dkey="lp%d" % s2)
            P.add("sync", lambda e: e.dma_start(out=V_r[s2], in_=v_h[:, :, h, :]), w=[("V", s2)], dkey="lv%d" % s2)

        items = [(h, Q, j) for h in range(8) for Q in range(NST) for j in range(4 * Q + 4)]
        slot_of = {}
        ectr = [0]

        def front(it):
            h, Q, j = it
            s2 = h % 2
            KT, Q0, Q1 = KT_r[s2], Q0_r[s2], Q1_r[s2]
            if h == 0 and Q == 0 and j == 0:
                head_loads(0)
            lo = max(0, j - 4 * Q)
            diag = j >= 4 * Q
            bA = psS.next()
            bB = psS.next()

            def smm(e):
                ins = None
                for bS, Qz in ((bA, Q0), (bB, Q1)):
                    ins = e.matmul(psf(bS)[:, lo * 128:512], lhsT=KT[:, j * 128:(j + 1) * 128],
                                   rhs=Qz[:, Q * 512 + lo * 128:(Q + 1) * 512], start=True, stop=not diag)
                    if diag:
                        ins = e.matmul(psf(bS)[:, lo * 128:(lo + 1) * 128], lhsT=ident_bf, rhs=maskneg, start=False, stop=True)
                return ins
            P.add("tensor", smm, r=[("KT", s2), ("Q0", s2), ("Q1", s2), "maskneg", "ident_bf"], w=[("ps", bA), ("ps", bB)])
            sl = []
            for bS in (bA, bB):
                es_ = ectr[0] % 8
                ectr[0] += 1
                sl.append(es_)
                E = E_r[es_]
                P.add("scalar", (lambda E, bS: lambda e: e.activation(out=E[:, lo * 128:512], in_=psf(bS)[:, lo * 128:512],
                                                                      func=AF.Exp, bias=negB[:, 0:1]))(E, bS),
                      r=[("ps", bS), "negB"], w=[("E", es_)])
            slot_of[it] = sl

        def back(it):
            h, Q, j = it
            s2 = h % 2
            V = V_r[s2]
            lo = max(0, j - 4 * Q)
            sl = slot_of.pop(it)
            if Q == 0 and j == 0 and h + 1 < 8:
                head_loads(h + 1)

            def av(e):
                ins = None
                for c_, es_ in enumerate(sl):
                    E = E_r[es_]
                    ins = e.matmul(ps_t[:, c_, lo * 128:512], lhsT=V[:, j, :], rhs=E[:, lo * 128:512], start=(j == 0), stop=(j == 4 * Q + 3))
                return ins
            P.add("tensor", av, r=[("E", sl[0]), ("E", sl[1]), ("V", s2)], w=[("ps", 0), ("ps", 1)])
            for c_, es_ in enumerate(sl):
                E = E_r[es_]
                eng_ = "vector"
                if j == 0:
                    P.add(eng_, (lambda c_, E: lambda e: e.tensor_copy(out=Es[c_], in_=E))(c_, E), r=[("E", es_)], w=[("Es", c_)])
                else:
                    P.add(eng_, (lambda c_, E: lambda e: e.tensor_tensor(out=Es[c_][:, lo * 128:512], in0=Es[c_][:, lo * 128:512],
                                                                         in1=E[:, lo * 128:512], op=ALU.add))(c_, E),
                          r=[("E", es_), ("Es", c_)], w=[("Es", c_)])
            if j != 4 * Q + 3:
                return
            b1 = psS.next()
            b2 = psS.next()
            P.add("tensor", lambda e: e.matmul(psf(b1), lhsT=ones_f, rhs=Es[0], start=True, stop=True), r=[("Es", 0), "ones_f"], w=[("ps", b1)])
            P.add("tensor", lambda e: e.matmul(psf(b2), lhsT=ones_f, rhs=Es[1], start=True, stop=True), r=[("Es", 1), "ones_f"], w=[("ps", b2)])
            P.add("scalar", lambda e: e.activation(out=rinv[0], in_=psf(b1), func=AF.Ln), r=[("ps", b1)], w=["rinv0"])
            P.add("scalar", lambda e: e.activation(out=rinv[1], in_=psf(b2), func=AF.Ln), r=[("ps", b2)], w=["rinv1"])
            P.add("scalar", lambda e: e.activation(out=rinv[0], in_=rinv[0], func=AF.Exp, scale=-1.0), r=["rinv0"], w=["rinv0"])
            P.add("scalar", lambda e: e.activation(out=rinv[1], in_=rinv[1], func=AF.Exp, scale=-1.0), r=["rinv1"], w=["rinv1"])
            P.add("vector", lambda e: e.tensor_copy(out=tA, in_=ps_t[:, 0, :]), r=[("ps", 0)], w=["tA"])
            P.add("vector", lambda e: e.tensor_copy(out=tB, in_=ps_t[:, 1, :]), r=[("ps", 1)], w=["tB"])
            P.add("vector", lambda e: e.tensor_tensor(out=tA, in0=tA, in1=rinv[0], op=ALU.mult), r=["tA", "rinv0"], w=["tA"])
            P.add("vector", lambda e: e.tensor_tensor(out=tB, in0=tB, in1=rinv[1], op=ALU.mult), r=["tB", "rinv1"], w=["tB"])
            P.add("vector", lambda e: e.scalar_tensor_tensor(out=tA, in0=tB, scalar=nlam[:, 0:1], in1=tA, op0=ALU.mult, op1=ALU.add),
                  r=["tA", "tB", "nlam"], w=["tA"])
            P.add("scalar", lambda e: e.activation(out=sqb, in_=tA, func=AF.Square), r=["tA"], w=["sqb"])
            bt = psS.next()
            P.add("tensor", lambda e: e.matmul(psf(bt), lhsT=ones_b, rhs=sqb, start=True, stop=True), r=["sqb", "ones_b"], w=[("ps", bt)])
            P.add("scalar", lambda e: e.activation(out=rstd5, in_=psf(bt), func=AF.Ln, scale=1.0 / 128, bias=epsb[:, 0:1]),
                  r=[("ps", bt), "epsb"], w=["rstd5"])
            P.add("scalar", lambda e: e.activation(out=rstd5, in_=rstd5, func=AF.Exp, scale=-0.5), r=["rstd5"], w=["rstd5"])
            P.add("vector", lambda e: e.scalar_tensor_tensor(out=OnT[:, h, Q * 512:(Q + 1) * 512], in0=tA, scalar=sg[:, 0:1], in1=rstd5,
                                                             op0=ALU.mult, op1=ALU.mult),
                  r=["tA", "rstd5", "sg"], w=[("OnT", h, 4 * Q + i) for i in range(4)])

        LA = 2
        for n in range(len(items) + LA):
            if n < len(items):
                front(items[n])
            if n >= LA:
                back(items[n - LA])

        for T in range(NT):
            s2 = T % 2
            xr = xr_ring[s2]
            P.add("sync", (lambda xr, T: lambda e: e.dma_start(out=xr, in_=src_d[T * 128:(T + 1) * 128, :]))(xr, T),
                  w=[("xr", s2)], dkey="lr%d" % s2)
            for j in range(2):
                b = psS.next()

                def mmo(e, T=T, j=j, b=b):
                    last = None
                    for k in range(8):
                        last = e.matmul(psf(b), lhsT=OnT[:, k, T * 128:(T + 1) * 128], rhs=wo[:, k, j * 512:(j + 1) * 512],
                                        start=(k == 0), stop=(k == 7))
                    return last
                P.add("tensor", mmo, r=[("OnT", k, T) for k in range(8)] + ["wo"], w=[("ps", b)])
                P.add("vector", (lambda xr, j, b: lambda e: e.tensor_tensor(out=xr[:, j * 512:(j + 1) * 512], in0=psf(b),
                                                                             in1=xr[:, j * 512:(j + 1) * 512], op=ALU.add))(xr, j, b),
                      r=[("ps", b), ("xr", s2)], w=[("xr", s2)])
            P.add("sync", (lambda xr, T: lambda e: e.dma_start(out=dst_d[T * 128:(T + 1) * 128, :], in_=xr))(xr, T),
                  r=[("xr", s2)], w=[("hC", T)], dkey="st%d" % s2)


    def phase_D_dense(src_d, dst_d):
        ar.reset(CONST_END)
        dk_ctr[0] = 0
        gcol = ar.alloc([8], F32)
        wr = ar.alloc([8, 8], F32)
        acc = ar.alloc([16, 1024], F32)
        xnT = ar.alloc([8, 2048], BF16)
        gates = ar.alloc([16, 8], F32)
        xs32_r = [ar.alloc([1024], F32) for _ in range(2)]
        junk = ar.alloc([1024], BF16)
        xs_ring = [{"bf": ar.alloc([1024], BF16), "junk": junk} for _ in range(2)]
        ss_r = [ar.alloc([1], F32) for _ in range(2)]
        rstd_r = [ar.alloc([1], F32) for _ in range(2)]
        xn32T = ar.alloc([8, 128], F32)
        lg = ar.alloc([8], F32)
        eq = ar.alloc([8], F32)
        l2 = ar.alloc([8], F32)
        sel = ar.alloc([8], F32)
        ex = ar.alloc([8], F32)
        m1 = ar.alloc([1], F32)
        m2 = ar.alloc([1], F32)
        nm1 = ar.alloc([1], F32)
        den = ar.alloc([1], F32)
        rden = ar.alloc([1], F32)
        wg_r = [ar.alloc([8, 512], BF16) for _ in range(2)]
        wu_r = [ar.alloc([8, 512], BF16) for _ in range(2)]
        wd_r = [ar.alloc([4, 1024], BF16) for _ in range(2)]
        HT_r = [ar.alloc([4, 512], BF16) for _ in range(2)]
        tmp_r = [ar.alloc([512], F32) for _ in range(2)]

        P.add("sync", col_load(gcol, w["l1_moe_norm"], 8), w=["gcol"], dkey=dk())
        P.add("sync", lambda e: e.dma_start(out=wr, in_=w["l1_moe_w_router"].rearrange("(k p) e -> p k e", p=128)), w=["wr"], dkey=dk())
        wg_d = w["l1_moe_w_gate"]
        wu_d = w["l1_moe_w_up"]
        wd_d = w["l1_moe_w_down"]
        psT = PsRing([0, 1])

        groups = [(hf, e_, fg) for hf in range(2) for e_ in range(NE) for fg in range(FE // 512)]

        def load_group(gi):
            hf, e_, fg = groups[gi]
            sl = gi % 2
            P.add("gpsimd", (lambda sl, e_, fg: lambda e: e.dma_start(
                out=wg_r[sl], in_=wg_d[e_].rearrange("(k p) f -> p k f", p=128)[:, :, fg * 512:(fg + 1) * 512]))(sl, e_, fg),
                w=[("wg", sl)], dkey="wg%d" % sl)
            P.add("gpsimd", (lambda sl, e_, fg: lambda e: e.dma_start(
                out=wu_r[sl], in_=wu_d[e_].rearrange("(k p) f -> p k f", p=128)[:, :, fg * 512:(fg + 1) * 512]))(sl, e_, fg),
                w=[("wu", sl)], dkey="wu%d" % sl)
            P.add("gpsimd", (lambda sl, e_, fg: lambda e: e.dma_start(
                out=wd_r[sl], in_=wd_d[e_][fg * 512:(fg + 1) * 512, :].rearrange("(k p) n -> p k n", p=128)))(sl, e_, fg),
                w=[("wd", sl)], dkey="wd%d" % sl)

        gi = 0
        hi = 0
        for hf in range(2):
            for t in range(16):
                T = hf * 16 + t
                s2 = t % 2
                at = acc[:, t, :]
                P.add("sync", (lambda at, T: lambda e: e.dma_start(out=at, in_=src_d[T * 128:(T + 1) * 128, :]))(at, T),
                      w=[("acc", t)], dkey="la%d" % t)
                xs32 = xs32_r[s2]
                norm_p1(at, xs_ring[s2], ss_r[s2], rstd_r[s2], ("D", s2), [("acc", t)], xs32=xs32)
                norm_p2(gcol, xs_ring[s2], xnT[:, :, t * 128:(t + 1) * 128], ("D", s2), psT, [("xnT", t)])

                def trf(e, xs32=xs32):
                    last = None
                    for k in range(8):
                        last = e.transpose(ps_t[:, 2 + k // 4, (k % 4) * 128:(k % 4 + 1) * 128], xs32[:, k * 128:(k + 1) * 128], ident_f)
                    return last
                P.add("tensor", trf, r=[("xs32", ("D", s2)), "ident_f"], w=[("ps", 2), ("ps", 3)])
                P.add("vector", lambda e: e.tensor_tensor(out=xn32T, in0=ps_t[:, 2:4, :].rearrange("p a (b t) -> p (a b) t", b=4),
                                                          in1=gcol.unsqueeze(2).to_broadcast([128, 8, 128]), op=ALU.mult),
                      r=[("ps", 2), ("ps", 3), "gcol"], w=["xn32T"])

                def lgm(e):
                    last = None
                    for k in range(8):
                        last = e.matmul(ps_t[:, 4, 0:8], lhsT=xn32T[:, k, :], rhs=wr[:, k, :], start=(k == 0), stop=(k == 7))
                    return last
                P.add("tensor", lgm, r=["xn32T", "wr"], w=[("ps", 4)])
                P.chain("vector", [
                    lambda e: e.tensor_copy(out=lg, in_=ps_t[:, 4, 0:8]),
                    lambda e: e.tensor_reduce(out=m1, in_=lg, axis=AX.X, op=ALU.max),
                    lambda e: e.tensor_scalar(out=eq, in0=lg, scalar1=m1[:, 0:1], scalar2=None, op0=ALU.is_equal),
                    lambda e: e.scalar_tensor_tensor(out=l2, in0=eq, scalar=-1.0e30, in1=lg, op0=ALU.mult, op1=ALU.add),
                    lambda e: e.tensor_reduce(out=m2, in_=l2, axis=AX.X, op=ALU.max),
                    lambda e: e.tensor_scalar(out=sel, in0=lg, scalar1=m2[:, 0:1], scalar2=None, op0=ALU.is_ge),
                    lambda e: e.tensor_scalar(out=nm1, in0=m1, scalar1=-1.0, scalar2=None, op0=ALU.mult),
                ], r=[("ps", 4)], w=["lg", "sel", "nm1"])
                P.add("scalar", lambda e: e.activation(out=ex, in_=lg, func=AF.Exp, bias=nm1[:, 0:1]), r=["lg", "nm1"], w=["ex"])
                P.chain("vector", [
                    lambda e: e.tensor_tensor(out=ex, in0=ex, in1=sel, op=ALU.mult),
                    lambda e: e.tensor_reduce(out=den, in_=ex, axis=AX.X, op=ALU.add),
                    lambda e: e.reciprocal(out=rden, in_=den),
                    (lambda t: lambda e: e.tensor_scalar(out=gates[:, t, :], in0=ex, scalar1=rden[:, 0:1], scalar2=None, op0=ALU.mult))(t),
                ], r=["ex", "sel"], w=[("gates", t), "ex"])

            psM = PsRing([0, 1, 2, 3, 4, 5, 6, 7])
            if hf == 0:
                load_group(0)
            for e_ in range(NE):
                for fg in range(FE // 512):
                    if gi + 1 < len(groups):
                        load_group(gi + 1)
                    sl = gi % 2
                    wg, wu, wd = wg_r[sl], wu_r[sl], wd_r[sl]
                    for st in range(4):
                        HT = HT_r[hi % 2]
                        hk = hi % 2
                        hi += 1
                        xk = [("xnT", st * 4 + c) for c in range(4)]
                        for fc in range(4):
                            bg = psM.next()
                            bu = psM.next()

                            def mmg(e, fc=fc, bg=bg, wg=wg, st=st):
                                last = None
                                for k in range(8):
                                    last = e.matmul(psf(bg), lhsT=wg[:, k, fc * 128:(fc + 1) * 128], rhs=xnT[:, k, st * 512:(st + 1) * 512],
                                                    start=(k == 0), stop=(k == 7))
                                return last

                            def mmu(e, fc=fc, bu=bu, wu=wu, st=st):
                                last = None
                                for k in range(8):
                                    last = e.matmul(psf(bu), lhsT=wu[:, k, fc * 128:(fc + 1) * 128], rhs=xnT[:, k, st * 512:(st + 1) * 512],
                                                    start=(k == 0), stop=(k == 7))
                                return last
                            P.add("tensor", mmg, r=[("wg", sl)] + xk, w=[("ps", bg)])
                            P.add("tensor", mmu, r=[("wu", sl)] + xk, w=[("ps", bu)])
                            tmp = tmp_r[fc % 2]
                            P.add("scalar", (lambda tmp, bg: lambda e: e.activation(out=tmp, in_=psf(bg), func=AF.Silu))(tmp, bg),
                                  r=[("ps", bg)], w=[("tmp", fc % 2)])
                            P.add("vector", (lambda tmp, bu, fc, HT: lambda e: e.tensor_tensor(out=HT[:, fc, :], in0=psf(bu), in1=tmp, op=ALU.mult))(tmp, bu, fc, HT),
                                  r=[("ps", bu), ("tmp", fc % 2)], w=[("HT", hk, fc)])
                        for c in range(4):
                            t = st * 4 + c
                            for j in range(2):
                                b = psM.next()

                                def mmd(e, c=c, j=j, b=b, HT=HT, wd=wd):
                                    last = None
                                    for k in range(4):
                                        last = e.matmul(psf(b), lhsT=HT[:, k, c * 128:(c + 1) * 128], rhs=wd[:, k, j * 512:(j + 1) * 512],
                                                        start=(k == 0), stop=(k == 3))
                                    return last
                                P.add("tensor", mmd, r=[("HT", hk, fc) for fc in range(4)] + [("wd", sl)], w=[("ps", b)])
                                P.add("vector", (lambda t, j, b, e_: lambda e: e.scalar_tensor_tensor(
                                    out=acc[:, t, j * 512:(j + 1) * 512], in0=psf(b), scalar=gates[:, t, e_:e_ + 1],
                                    in1=acc[:, t, j * 512:(j + 1) * 512], op0=ALU.mult, op1=ALU.add))(t, j, b, e_),
                                    r=[("ps", b), ("gates", t), ("acc", t)], w=[("acc", t)])
                    gi += 1
            for t in range(16):
                T = hf * 16 + t
                P.add("sync", (lambda t, T: lambda e: e.dma_start(out=dst_d[T * 128:(T + 1) * 128, :], in_=acc[:, t, :]))(t, T),
                      r=[("acc", t)], w=[("hD", T)], dkey="sa%d" % t)


    def phase_D(src_d, dst_d):
        BIG = 1.0e6
        ar.reset(CONST_END)
        dk_ctr[0] = 0
        idxA = ar.alloc([NT, 2], I32)
        idxB = ar.alloc([NT, 2], I32)
        gAB = ar.alloc([NT, 2], F32)
        gBB = ar.alloc([NT, 2], F32)
        gcol = ar.alloc([8], F32)
        P_END = ar.off
        wr = ar.alloc([8, 8], F32)
        tris = ar.alloc([128], F32)
        ones_f = ar.alloc([128], F32)
        eoff = ar.alloc([8], F32)
        Mcum = ar.alloc([8], F32)
        xt_ring = [ar.alloc([1024], F32) for _ in range(2)]
        xs32_r = [ar.alloc([1024], F32) for _ in range(2)]
        junk = ar.alloc([1024], BF16)
        xs_ring = [{"bf": ar.alloc([1024], BF16), "junk": junk} for _ in range(2)]
        ss_r = [ar.alloc([1], F32) for _ in range(2)]
        rstd_r = [ar.alloc([1], F32) for _ in range(2)]
        xn32T = ar.alloc([8, 128], F32)
        lg = ar.alloc([8], F32)
        eq = ar.alloc([8], F32)
        l2 = ar.alloc([8], F32)
        sel = ar.alloc([8], F32)
        ex = ar.alloc([8], F32)
        gt = ar.alloc([8], F32)
        rk = ar.alloc([8], F32)
        sf = ar.alloc([8], F32)
        sm = ar.alloc([8], F32)
        t2 = ar.alloc([8], F32)
        oh = ar.alloc([8], F32)
        tg = ar.alloc([8], F32)
        tg2 = ar.alloc([8], F32)
        cnti = ar.alloc([8], I32)
        m1 = ar.alloc([1], F32)
        m2 = ar.alloc([1], F32)
        nm1 = ar.alloc([1], F32)
        den = ar.alloc([1], F32)
        rden = ar.alloc([1], F32)
        sA = ar.alloc([1], F32)
        sB = ar.alloc([1], F32)
        XT = [ar.alloc([8, 512], BF16) for _ in range(4)]
        acc = [ar.alloc([4, 1024], F32) for _ in range(4)]
        wg_r = [ar.alloc([8, 512], BF16) for _ in range(2)]
        wu_r = [ar.alloc([8, 512], BF16) for _ in range(2)]
        wd_r = [ar.alloc([4, 1024], BF16) for _ in range(2)]
        HT_r = [ar.alloc([4, 512], BF16) for _ in range(2)]
        tmp_r = [ar.alloc([512], F32) for _ in range(2)]
        xg_r = [ar.alloc([1024], BF16) for _ in range(2)]

        P.add("sync", col_load(gcol, w["l1_moe_norm"], 8), w=["gcol"], dkey=dk())
        P.add("sync", lambda e: e.dma_start(out=wr, in_=w["l1_moe_w_router"].rearrange("(k p) e -> p k e", p=128)), w=["wr"], dkey=dk())
        P.add("sync", lambda e: e.dma_start(out=tris, in_=w["c_tris"][:, :]), w=["tris"], dkey=dk())
        P.add("sync", lambda e: e.dma_start(out=eoff, in_=w["c_eoff"].partition_broadcast(128)), w=["eoff"], dkey=dk())
        P.add("gpsimd", lambda e: e.memset(ones_f, 1.0), w=["ones_f"])
        P.add("gpsimd", lambda e: e.memset(Mcum, 0.0), w=["Mcum"])
        psT = PsRing([0, 1])

        for T in range(NT):
            s2 = T % 2
            xt = xt_ring[s2]
            P.add("sync", (lambda xt, T: lambda e: e.dma_start(out=xt, in_=src_d[T * 128:(T + 1) * 128, :]))(xt, T),
                  w=[("xt", s2)], dkey="ld%d" % s2)
            xs32 = xs32_r[s2]
            xsb = xs_ring[s2]["bf"]
            norm_p1(xt, xs_ring[s2], ss_r[s2], rstd_r[s2], ("D", s2), [("xt", s2)], xs32=xs32)

            def trf(e, xs32=xs32):
                last = None
                for k in range(8):
                    last = e.transpose(ps_t[:, 2 + k // 4, (k % 4) * 128:(k % 4 + 1) * 128], xs32[:, k * 128:(k + 1) * 128], ident_f)
                return last
            P.add("tensor", trf, r=[("xs32", ("D", s2)), "ident_f"], w=[("ps", 2), ("ps", 3)])
            P.add("vector", lambda e: e.tensor_tensor(out=xn32T, in0=ps_t[:, 2:4, :].rearrange("p a (b t) -> p (a b) t", b=4),
                                                      in1=gcol.unsqueeze(2).to_broadcast([128, 8, 128]), op=ALU.mult),
                  r=[("ps", 2), ("ps", 3), "gcol"], w=["xn32T"])

            def lgm(e):
                last = None
                for k in range(8):
                    last = e.matmul(ps_t[:, 4, 0:8], lhsT=xn32T[:, k, :], rhs=wr[:, k, :], start=(k == 0), stop=(k == 7))
                return last
            P.add("tensor", lgm, r=["xn32T", "wr"], w=[("ps", 4)])
            P.chain("vector", [
                lambda e: e.tensor_copy(out=lg, in_=ps_t[:, 4, 0:8]),
                lambda e: e.tensor_reduce(out=m1, in_=lg, axis=AX.X, op=ALU.max),
                lambda e: e.tensor_scalar(out=eq, in0=lg, scalar1=m1[:, 0:1], scalar2=None, op0=ALU.is_equal),
                lambda e: e.scalar_tensor_tensor(out=l2, in0=eq, scalar=-1.0e30, in1=lg, op0=ALU.mult, op1=ALU.add),
                lambda e: e.tensor_reduce(out=m2, in_=l2, axis=AX.X, op=ALU.max),
                lambda e: e.tensor_scalar(out=sel, in0=lg, scalar1=m2[:, 0:1], scalar2=None, op0=ALU.is_ge),
                lambda e: e.tensor_scalar(out=nm1, in0=m1, scalar1=-1.0, scalar2=None, op0=ALU.mult),
            ], r=[("ps", 4)], w=["lg", "sel", "nm1"])
            P.add("scalar", lambda e: e.activation(out=ex, in_=lg, func=AF.Exp, bias=nm1[:, 0:1]), r=["lg", "nm1"], w=["ex"])

            def rkm(e):
                e.matmul(ps_t[:, 5, 0:8], lhsT=tris, rhs=sel, start=True, stop=False)
                return e.matmul(ps_t[:, 5, 0:8], lhsT=ones_f, rhs=Mcum, start=False, stop=True)
            P.add("tensor", rkm, r=["sel", "Mcum", "tris", "ones_f"], w=[("ps", 5)])
            P.chain("vector", [
                lambda e: e.tensor_tensor(out=ex, in0=ex, in1=sel, op=ALU.mult),
                lambda e: e.tensor_reduce(out=den, in_=ex, axis=AX.X, op=ALU.add),
                lambda e: e.reciprocal(out=rden, in_=den),
                lambda e: e.tensor_scalar(out=gt, in0=ex, scalar1=rden[:, 0:1], scalar2=None, op0=ALU.mult),
                lambda e: e.tensor_copy(out=rk, in_=ps_t[:, 5, 0:8]),
                lambda e: e.tensor_tensor(out=Mcum, in0=Mcum, in1=sel, op=ALU.add),
                lambda e: e.tensor_tensor(out=sf, in0=rk, in1=eoff, op=ALU.add),
                lambda e: e.scalar_tensor_tensor(out=sm, in0=sf, scalar=-BIG, in1=sel, op0=ALU.add, op1=ALU.mult),
                lambda e: e.tensor_scalar(out=sm, in0=sm, scalar1=BIG, scalar2=None, op0=ALU.add),
                lambda e: e.tensor_reduce(out=sA, in_=sm, axis=AX.X, op=ALU.min),
                lambda e: e.tensor_tensor(out=t2, in0=sf, in1=sel, op=ALU.mult),
                lambda e: e.tensor_reduce(out=sB, in_=t2, axis=AX.X, op=ALU.max),
                lambda e: e.tensor_scalar(out=oh, in0=sm, scalar1=sA[:, 0:1], scalar2=None, op0=ALU.is_equal),
                lambda e: e.tensor_tensor(out=tg, in0=gt, in1=oh, op=ALU.mult),
                (lambda T: lambda e: e.tensor_reduce(out=gAB[:, T, 0:1], in_=tg, axis=AX.X, op=ALU.add))(T),
                lambda e: e.tensor_tensor(out=tg2, in0=gt, in1=tg, op=ALU.subtract),
                (lambda T: lambda e: e.tensor_reduce(out=gBB[:, T, 0:1], in_=tg2, axis=AX.X, op=ALU.add))(T),
                (lambda T: lambda e: e.tensor_copy(out=idxA[:, T, 0:1], in_=sA))(T),
                (lambda T: lambda e: e.tensor_copy(out=idxB[:, T, 0:1], in_=sB))(T),
            ], r=["ex", "sel", ("ps", 5), "eoff", "Mcum"], w=["ex", "Mcum", ("idx", T), ("gab", T)])
            for which, idx_t in ((0, idxA), (1, idxB)):
                P.add("gpsimd", (lambda idx_t, T, xsb: lambda e: e.indirect_dma_start(
                    out=Xg_d[:, :], out_offset=bass.IndirectOffsetOnAxis(ap=idx_t[:, T, 0:1], axis=0),
                    in_=xsb, in_offset=None))(idx_t, T, xsb),
                    r=[("idx", T), ("xsbf", ("D", s2))], w=[("Xg", T, which)], dkey="sc%d%d" % (s2, which))
        P.add("tensor", lambda e: e.matmul(ps_t[:, 5, 0:8], lhsT=ones_f, rhs=Mcum, start=True, stop=True), r=["Mcum", "ones_f"], w=[("ps", 5)])
        P.add("vector", lambda e: e.tensor_copy(out=cnti, in_=ps_t[:, 5, 0:8]), r=[("ps", 5)], w=["cnti"])
        P.add("sync", lambda e: e.dma_start(out=cnt_d[0:1, :], in_=cnti[0:1, :]), r=["cnti"], w=["cnt_d"], dkey=dk())
        P.barrier()

        for e_ in range(NE):
            P.regload("cnt%d" % e_, cnt_d[0:1, e_:e_ + 1])
        wg_d = w["l1_moe_w_gate"]
        wu_d = w["l1_moe_w_up"]
        wd_d = w["l1_moe_w_down"]
        NFG = FE // 512
        psM = PsRing([0, 1, 2, 3, 4, 5, 6, 7])
        ctr = {"hi": 0, "xg": 0}

        def load_w(e_, fg, sl):
            P.add("gpsimd", lambda e: e.dma_start(
                out=wg_r[sl], in_=wg_d[e_].rearrange("(k p) f -> p k f", p=128)[:, :, fg * 512:(fg + 1) * 512]),
                w=[("wg", sl)], dkey="wg%d" % sl)
            P.add("gpsimd", lambda e: e.dma_start(
                out=wu_r[sl], in_=wu_d[e_].rearrange("(k p) f -> p k f", p=128)[:, :, fg * 512:(fg + 1) * 512]),
                w=[("wu", sl)], dkey="wu%d" % sl)
            P.add("gpsimd", lambda e: e.dma_start(
                out=wd_r[sl], in_=wd_d[e_][fg * 512:(fg + 1) * 512, :].rearrange("(k p) n -> p k n", p=128)),
                w=[("wd", sl)], dkey="wd%d" % sl)

        def prep(e_, rnd):
            ck = "cnt%d" % e_
            for k in range(4):
                kk = rnd * 4 + k
                P.pred_begin(ck, kk * 512 + 1)
                for c in range(4):
                    xs_ = ctr["xg"] % 2
                    ctr["xg"] += 1
                    xg = xg_r[xs_]
                    row0 = e_ * S + kk * 512 + c * 128
                    P.add("sync", (lambda xg, row0: lambda e: e.dma_start(out=xg, in_=Xg_d[row0:row0 + 128, :]))(xg, row0),
                          w=[("xg", xs_)], dkey="xg%d" % xs_)
                    b = psM.next()

                    def trx(e, xg=xg, b=b):
                        last = None
                        for kq in range(8):
                            last = e.transpose(psb(b)[:, kq * 128:(kq + 1) * 128], xg[:, kq * 128:(kq + 1) * 128], ident_bf)
                        return last
                    P.add("tensor", trx, r=[("xg", xs_), "ident_bf"], w=[("ps", b)])
                    P.add("vector", (lambda k, c, b: lambda e: e.tensor_tensor(
                        out=XT[k][:, :, c * 128:(c + 1) * 128], in0=psb(b).rearrange("p (k t) -> p k t", k=8),
                        in1=gcol.unsqueeze(2).to_broadcast([128, 8, 128]), op=ALU.mult))(k, c, b),
                        r=[("ps", b), "gcol"], w=[("XT", k, c)])
            for k in range(4):
                P.pred_end()

        def compute(e_, rnd, fg, sl):
            ck = "cnt%d" % e_
            wg, wu, wd = wg_r[sl], wu_r[sl], wd_r[sl]
            for k in range(4):
                kk = rnd * 4 + k
                P.pred_begin(ck, kk * 512 + 1)
                HT = HT_r[ctr["hi"] % 2]
                hk = ctr["hi"] % 2
                ctr["hi"] += 1
                xk = [("XT", k, c) for c in range(4)]
                for fc in range(4):
                    bg = psM.next()
                    bu = psM.next()

                    def mmg(e, fc=fc, bg=bg, wg=wg, k=k):
                        last = None
                        for kq in range(8):
                            last = e.matmul(psf(bg), lhsT=wg[:, kq, fc * 128:(fc + 1) * 128], rhs=XT[k][:, kq, :],
                                            start=(kq == 0), stop=(kq == 7))
                        return last

                    def mmu(e, fc=fc, bu=bu, wu=wu, k=k):
                        last = None
                        for kq in range(8):
                            last = e.matmul(psf(bu), lhsT=wu[:, kq, fc * 128:(fc + 1) * 128], rhs=XT[k][:, kq, :],
                                            start=(kq == 0), stop=(kq == 7))
                        return last
                    P.add("tensor", mmg, r=[("wg", sl)] + xk, w=[("ps", bg)])
                    P.add("tensor", mmu, r=[("wu", sl)] + xk, w=[("ps", bu)])
                    tmp = tmp_r[fc % 2]
                    P.add("scalar", (lambda tmp, bg: lambda e: e.activation(out=tmp, in_=psf(bg), func=AF.Silu))(tmp, bg),
                          r=[("ps", bg)], w=[("tmp", fc % 2)])
                    P.add("vector", (lambda tmp, bu, fc, HT: lambda e: e.tensor_tensor(out=HT[:, fc, :], in0=psf(bu), in1=tmp, op=ALU.mult))(tmp, bu, fc, HT),
                          r=[("ps", bu), ("tmp", fc % 2)], w=[("HT", hk, fc)])
                for c in range(4):
                    for j in range(2):
                        b = psM.next()

                        def mmd(e, c=c, j=j, b=b, HT=HT, wd=wd):
                            last = None
                            for kq in range(4):
                                last = e.matmul(psf(b), lhsT=HT[:, kq, c * 128:(c + 1) * 128], rhs=wd[:, kq, j * 512:(j + 1) * 512],
                                                start=(kq == 0), stop=(kq == 3))
                            return last
                        P.add("tensor", mmd, r=[("HT", hk, fc) for fc in range(4)] + [("wd", sl)], w=[("ps", b)])
                        dst = acc[k][:, c, j * 512:(j + 1) * 512]
                        if fg == 0:
                            P.add("vector", (lambda dst, b: lambda e: e.tensor_copy(out=dst, in_=psf(b)))(dst, b),
                                  r=[("ps", b)], w=[("acc", k, c)])
                        else:
                            P.add("vector", (lambda dst, b: lambda e: e.tensor_tensor(out=dst, in0=psf(b), in1=dst, op=ALU.add))(dst, b),
                                  r=[("ps", b), ("acc", k, c)], w=[("acc", k, c)])
            for k in range(4):
                P.pred_end()

        def stores(e_, rnd):
            ck = "cnt%d" % e_
            for k in range(4):
                kk = rnd * 4 + k
                P.pred_begin(ck, kk * 512 + 1)
                row0 = e_ * S + kk * 512
                P.add("sync", (lambda k, row0: lambda e: e.dma_start(
                    out=Yg_d[row0:row0 + 512, :].rearrange("(c p) n -> p c n", p=128), in_=acc[k]))(k, row0),
                    r=[("acc", k, c) for c in range(4)], w=[("Yg", e_, kk)], dkey="ys%d" % k)
            for k in range(4):
                P.pred_end()

        order0 = [(e_, fg) for e_ in range(NE) for fg in range(NFG)]
        load_w(order0[0][0], order0[0][1], 0)
        for n, (e_, fg) in enumerate(order0):
            if fg == 0:
                prep(e_, 0)
            if n + 1 < len(order0):
                load_w(order0[n + 1][0], order0[n + 1][1], (n + 1) % 2)
            compute(e_, 0, fg, n % 2)
            if fg == NFG - 1:
                stores(e_, 0)
        g = len(order0)
        for e_ in range(NE):
            P.pred_begin("cnt%d" % e_, 2049)
            prep(e_, 1)
            load_w(e_, 0, g % 2)
            for fg in range(NFG):
                if fg + 1 < NFG:
                    load_w(e_, fg + 1, (g + 1) % 2)
                compute(e_, 1, fg, g % 2)
                g += 1
            stores(e_, 1)
            P.pred_end()
        P.barrier()

        ar.reset(P_END)
        ya_r = [ar.alloc([1024], F32) for _ in range(2)]
        yb_r = [ar.alloc([1024], F32) for _ in range(2)]
        xr_r = [ar.alloc([1024], F32) for _ in range(2)]
        for T in range(NT):
            s2 = T % 2
            ya, yb, xr = ya_r[s2], yb_r[s2], xr_r[s2]
            P.add("gpsimd", (lambda ya, T: lambda e: e.indirect_dma_start(
                out=ya, out_offset=None, in_=Yg_d[:, :], in_offset=bass.IndirectOffsetOnAxis(ap=idxA[:, T, 0:1], axis=0)))(ya, T),
                w=[("ya", s2)], dkey="ga%d" % s2)
            P.add("gpsimd", (lambda yb, T: lambda e: e.indirect_dma_start(
                out=yb, out_offset=None, in_=Yg_d[:, :], in_offset=bass.IndirectOffsetOnAxis(ap=idxB[:, T, 0:1], axis=0)))(yb, T),
                w=[("yb", s2)], dkey="gb%d" % s2)
            P.add("sync", (lambda xr, T: lambda e: e.dma_start(out=xr, in_=src_d[T * 128:(T + 1) * 128, :]))(xr, T),
                  w=[("xr", s2)], dkey="lr%d" % s2)
            P.add("vector", (lambda ya, xr, T: lambda e: e.scalar_tensor_tensor(out=xr, in0=ya, scalar=gAB[:, T, 0:1], in1=xr, op0=ALU.mult, op1=ALU.add))(ya, xr, T),
                  r=[("ya", s2), ("xr", s2)], w=[("xr", s2)])
            P.add("vector", (lambda yb, xr, T: lambda e: e.scalar_tensor_tensor(out=xr, in0=yb, scalar=gBB[:, T, 0:1], in1=xr, op0=ALU.mult, op1=ALU.add))(yb, xr, T),
                  r=[("yb", s2), ("xr", s2)], w=[("xr", s2)])
            P.add("sync", (lambda xr, T: lambda e: e.dma_start(out=dst_d[T * 128:(T + 1) * 128, :], in_=xr))(xr, T),
                  r=[("xr", s2)], w=[("hD", T)], dkey="st%d" % s2)


    if os.environ.get("MOE_DENSE"):
        phase_D = phase_D_dense

    order = ["A", "B", "C", "D"]
    nph = order.index(stop_after) + 1
    srcs = [x_d, h1_d, h2_d, h3_d]
    dsts = [h1_d, h2_d, h3_d, out_d]
    dsts[nph - 1] = out_d
    fns = [phase_A, phase_B, phase_C, phase_D]
    for i in range(nph):
        fns[i](srcs[i], dsts[i])
        P.barrier()

    P.emit(es)
    es.close()
    return nc, list(declared.keys())


_CACHE = {}


def _consts():
    ident = np.eye(128, dtype=np.float32)
    tri = np.triu(np.ones((128, 128), dtype=np.float32))
    invf = (1.0 / (np.float32(500000.0) ** (np.arange(0, 16, 2, dtype=np.float32) / np.float32(16)))).astype(np.float32)
    return {
        "c_ident_bf": ident.astype(ml_dtypes.bfloat16),
        "c_ident_f": ident,
        "c_tri": tri,
        "c_invf": invf,
        "c_tris": np.triu(np.ones((128, 128), dtype=np.float32), 1),
        "c_eoff": (np.arange(8, dtype=np.float32) * 4096.0).astype(np.float32),
    }


def kernel(stop_after="D", **inputs):
    key = stop_after
    if key not in _CACHE:
        _CACHE[key] = build_program(stop_after)
    nc, used = _CACHE[key]
    consts = _consts()
    in_maps = []
    for b in range(8):
        m = {}
        for k, v in inputs.items():
            if k not in used:
                continue
            a = np.asarray(v)
            if k == "x":
                m[k] = np.ascontiguousarray(a[b])
            elif k == "positions":
                m[k] = np.ascontiguousarray(a[b]).astype(np.int32, copy=False)
            else:
                m[k] = np.ascontiguousarray(a)
        m.update({k: v for k, v in consts.items() if k in used})
        in_maps.append(m)
    res = run_bass_kernel_spmd(nc, in_maps, core_ids=list(range(8)))
    global _LAST
    _LAST = res
    return np.stack([np.asarray(r["out"]) for r in res.results], axis=0).astype(np.float32, copy=False)
```

```python
import math
import os
from contextlib import ExitStack

import ml_dtypes
import numpy as np

import concourse.bass as bass
import concourse.mybir as mybir
from concourse.bass_utils import run_bass_kernel_spmd

F32 = mybir.dt.float32
BF16 = mybir.dt.bfloat16
I32 = mybir.dt.int32
U8 = mybir.dt.uint8
AF = mybir.ActivationFunctionType
ALU = mybir.AluOpType
AX = mybir.AxisListType

S = 4096
D = 1024
NT = S // 128
NST = S // 512
EPS = 1e-6
LAMBDA_INIT = 0.8 - 0.6 * math.exp(-0.3 * 1)
FFN = 2816
NF0 = FFN // 128
NE = 8
FE = 3584
SIZEOF = {F32: 4, BF16: 2, I32: 4, U8: 1}
COMPUTE = ("tensor", "vector", "scalar", "gpsimd")
ALLENG = COMPUTE + ("sync",)


class Op:
    __slots__ = ("eng", "fn", "deps", "signal", "ticket", "is_dma", "dkey", "dval", "strict", "mark")


class Prog:
    def __init__(self, nc):
        self.nc = nc
        self.ops = {e: [] for e in ALLENG}
        self.lw = {}
        self.rd = {}
        self.dcount = {}
        self.last = {e: None for e in ALLENG}
        self.last_dma = {}

    def add(self, eng, fn, r=(), w=(), dkey=None, strict=False):
        op = Op()
        op.mark = None
        op.strict = strict
        op.eng = eng
        op.fn = fn
        op.signal = False
        op.ticket = 0
        op.is_dma = dkey is not None
        op.dkey = dkey
        op.dval = 0
        deps = []
        for k in r:
            x = self.lw.get(k)
            if x is not None:
                deps.append(x)
        for k in w:
            x = self.lw.get(k)
            if x is not None:
                deps.append(x)
            rr = self.rd.get(k)
            if rr:
                deps.extend(rr[0].values())
                deps.extend(rr[1])
        self._set_deps(op, deps)
        if op.is_dma:
            c = self.dcount.get(dkey, 0) + 1
            self.dcount[dkey] = c
            op.dval = 16 * c
            self.last_dma[dkey] = op
        for k in r:
            rr = self.rd.setdefault(k, ({}, []))
            if op.is_dma:
                rr[1].append(op)
            else:
                rr[0][eng] = op
        for k in w:
            self.lw[k] = op
            self.rd[k] = ({}, [])
        self.ops[eng].append(op)
        if not op.is_dma:
            self.last[eng] = op
        return op

    def _marker(self, mark):
        for e in ALLENG:
            op = Op()
            op.mark = mark
            op.strict = False
            op.eng = e
            op.fn = None
            op.signal = False
            op.ticket = 0
            op.is_dma = False
            op.dkey = None
            op.dval = 0
            op.deps = []
            self.ops[e].append(op)

    def regload(self, key, ap):
        self._marker(("regload", key, ap))

    def pred_begin(self, key, thr):
        self._marker(("begin", key, thr))

    def pred_end(self):
        self._marker(("end",))

    def chain(self, eng, fns, r=(), w=()):
        self.nchain = getattr(self, "nchain", 0) + 1
        key = ("chain", self.nchain)
        op = None
        for i, fn in enumerate(fns):
            op = self.add(eng, fn, r=list(r) + [key], w=list(w) + [key], strict=(i > 0))
        return op

    def _set_deps(self, op, deps):
        seen = set()
        out = []
        for d in deps:
            if d is op or id(d) in seen:
                continue
            seen.add(id(d))
            if (not d.is_dma) and (not op.is_dma) and d.eng == op.eng and not op.strict:
                continue
            out.append(d)
            if not d.is_dma:
                d.signal = True
        op.deps = out

    def barrier(self):
        lasts = [self.last[e] for e in COMPUTE if self.last[e] is not None]
        dmas = list(self.last_dma.values())
        for e in ALLENG:
            op = Op()
            op.mark = None
            op.strict = False
            op.eng = e
            op.fn = None
            op.signal = False
            op.ticket = 0
            op.is_dma = False
            op.dkey = None
            op.dval = 0
            deps = [d for d in lasts if d.eng != e] + dmas
            for d in deps:
                if not d.is_dma:
                    d.signal = True
            op.deps = deps
            self.ops[e].append(op)
        self.lw = {}
        self.rd = {}

    def emit(self, es):
        nc = self.nc
        for e in ALLENG:
            c = 0
            for op in self.ops[e]:
                if op.is_dma:
                    continue
                if op.signal:
                    c += 1
                    op.ticket = c
        psem = {e: es.enter_context(nc.semaphore("pg_" + e)) for e in COMPUTE}
        dsem = {}
        for i, k in enumerate(self.dcount):
            dsem[k] = es.enter_context(nc.semaphore("dq_%d" % i))
        block = es.enter_context(nc.Block())

        def mk(engname):
            def body(eng):
                waited = {}
                regs = {}

                def emit_op(op):
                    for d in op.deps:
                        if d.is_dma:
                            key = ("d", d.dkey)
                            sem = dsem[d.dkey]
                            val = d.dval
                        else:
                            key = ("p", d.eng)
                            sem = psem[d.eng]
                            val = d.ticket
                        if waited.get(key, 0) >= val:
                            continue
                        eng.wait_ge(sem, val)
                        waited[key] = val
                    if op.fn is None:
                        return
                    ins = op.fn(eng)
                    if op.is_dma:
                        ins.then_inc(dsem[op.dkey], 16)
                    elif op.signal:
                        ins.then_inc(psem[engname], 1)

                ops = self.ops[engname]

                def find_end(i):
                    depth = 0
                    j = i
                    while True:
                        m = ops[j].mark
                        if m is not None and m[0] == "begin":
                            depth += 1
                        elif m is not None and m[0] == "end":
                            depth -= 1
                            if depth == 0:
                                return j
                        j += 1

                def emit_range(i, end):
                    nonlocal waited
                    while i < end:
                        op = ops[i]
                        if op.mark is None:
                            emit_op(op)
                            i += 1
                            continue
                        kind = op.mark[0]
                        if kind == "regload":
                            _, key, ap = op.mark
                            if key not in regs:
                                regs[key] = eng.alloc_register("rg_%s_%s" % (engname, key))
                            eng.reg_load(regs[key], ap)
                            i += 1
                            continue
                        assert kind == "begin", kind
                        _, key, thr = op.mark
                        j = find_end(i)
                        region = [o for o in ops[i + 1:j] if o.mark is None]
                        nsig = sum(1 for o in region if (not o.is_dma) and o.signal)
                        dfirst = {}
                        dcnt = {}
                        for o in region:
                            if o.is_dma:
                                dfirst.setdefault(o.dkey, o.dval)
                                dcnt[o.dkey] = dcnt.get(o.dkey, 0) + 1
                        if region:
                            snap = dict(waited)
                            with eng.If_lt(regs[key], thr):
                                for k2, c2 in dcnt.items():
                                    prior = dfirst[k2] - 16
                                    if prior > 0:
                                        eng.wait_ge(dsem[k2], prior)
                                    eng.sem_inc(dsem[k2], 16 * c2)
                                if nsig:
                                    eng.drain().then_inc(psem[engname], nsig)
                            with eng.Else():
                                emit_range(i + 1, j)
                            waited = snap
                        i = j + 1

                emit_range(0, len(ops))
                if engname == "sync":
                    for k, c in self.dcount.items():
                        if waited.get(("d", k), 0) < 16 * c:
                            eng.wait_ge(dsem[k], 16 * c)
            return body

        block.tensor(mk("tensor"))
        block.vector(mk("vector"))
        block.scalar(mk("scalar"))
        block.gpsimd(mk("gpsimd"))
        block.sync(mk("sync"))


class Arena:
    def __init__(self, base_ap, nbytes):
        self.base = base_ap
        self.nbytes = nbytes
        self.off = 0

    def reset(self, off=0):
        self.off = off

    def alloc(self, shape, dt):
        n = SIZEOF[dt]
        for s in shape:
            n *= s
        start = (self.off + 63) // 64 * 64
        assert start + n <= self.nbytes, ("SBUF arena overflow", start + n, self.nbytes)
        self.off = start + n
        ap = self.base[:, start:start + n].bitcast(dt)
        if len(shape) == 2:
            ap = ap.rearrange("p (a b) -> p a b", a=shape[0])
        elif len(shape) == 3:
            ap = ap.rearrange("p (a b c) -> p a b c", a=shape[0], b=shape[1])
        return ap


class PsRing:
    def __init__(self, banks):
        self.banks = list(banks)
        self.i = 0

    def next(self):
        b = self.banks[self.i % len(self.banks)]
        self.i += 1
        return b


def build_program(stop_after="D"):
    nc = bass.Bass("TRN2", target_bir_lowering=False)
    es = ExitStack()

    def din(name, shape, dt=F32):
        return nc.dram_tensor(name, list(shape), dt, kind="ExternalInput").ap()

    specs = {
        "x": ([S, D], F32), "positions": ([S], I32),
        "l0_mix_norm": [D], "l0_sg_w_in": [D, 4096], "l0_sg_v_norm": [2048],
        "l0_sg_w_spatial": [8, 128, 128], "l0_sg_b_spatial": [8, 128], "l0_sg_w_out": [2048, D],
        "l0_ffn_norm": [D], "l0_ffn_w_gate": [D, FFN], "l0_ffn_w_up": [D, FFN], "l0_ffn_w_down": [FFN, D],
        "l1_mix_norm": [D], "l1_da_w_qkv": [D, 3072], "l1_da_q_norm": [64], "l1_da_k_norm": [64],
        "l1_da_lambda_q1": [64], "l1_da_lambda_k1": [64], "l1_da_lambda_q2": [64], "l1_da_lambda_k2": [64],
        "l1_da_subln": [128], "l1_da_w_out": [D, D],
        "l1_moe_norm": [D], "l1_moe_w_router": [D, 8], "l1_moe_w_gate": [8, D, FE], "l1_moe_w_up": [8, D, FE],
        "l1_moe_w_down": [8, FE, D],
        "c_ident_bf": ([128, 128], BF16), "c_ident_f": ([128, 128], F32), "c_tri": ([128, 128], F32),
        "c_invf": ([8], F32), "c_tris": ([128, 128], F32), "c_eoff": ([8], F32),
    }
    declared = {}

    class _W:
        def __getitem__(self, k):
            if k not in declared:
                sp = specs[k]
                if isinstance(sp, tuple):
                    declared[k] = din(k, sp[0], sp[1])
                else:
                    declared[k] = din(k, sp)
            return declared[k]
    w = _W()
    x_d = w["x"]
    ident_bf_d = w["c_ident_bf"]
    ident_f_d = w["c_ident_f"]
    tri_d = w["c_tri"]
    out_d = nc.dram_tensor("out", [S, D], F32, kind="ExternalOutput").ap()
    h1_d = nc.dram_tensor("h1_scr", [S, D], F32).ap()
    h2_d = nc.dram_tensor("h2_scr", [S, D], F32).ap()
    h3_d = nc.dram_tensor("h3_scr", [S, D], F32).ap()
    dbg_kind = "ExternalOutput" if os.environ.get("KDBG") else "Internal"
    qT_d = nc.dram_tensor("qT_scr", [8, 128, S], BF16, kind=dbg_kind).ap()
    kT_d = nc.dram_tensor("kT_scr", [8, 128, S], BF16, kind=dbg_kind).ap()
    v_d = nc.dram_tensor("v_scr", [S, 8, 128], BF16, kind=dbg_kind).ap()

    NSLOT = 8 * S
    Xg_d = nc.dram_tensor("xg_scr", [NSLOT, D], BF16).ap()
    Yg_d = nc.dram_tensor("yg_scr", [NSLOT, D], F32).ap()
    cnt_d = nc.dram_tensor("cnt_scr", [1, 8], I32).ap()
    dbg_d = nc.dram_tensor("dbg", [128, 8, 512], F32, kind=dbg_kind).ap()
    ARENA_BYTES = 199 * 1024
    arena_t = es.enter_context(nc.sbuf_tensor("arena", [128, ARENA_BYTES], U8))
    ar = Arena(arena_t, ARENA_BYTES)
    ps_t = es.enter_context(nc.psum_tensor("psum", [128, 8, 512], F32))

    def psf(b):
        return ps_t[:, b, :]

    def psb(b):
        return ps_t[:, b, :].bitcast(BF16)

    P = Prog(nc)

    ident_bf = ar.alloc([128], BF16)
    ident_f = ar.alloc([128], F32)
    P.add("sync", lambda e: e.dma_start(out=ident_bf, in_=ident_bf_d[:, :]), w=["ident_bf"], dkey="k0")
    P.add("sync", lambda e: e.dma_start(out=ident_f, in_=ident_f_d[:, :]), w=["ident_f"], dkey="k1")
    neghalf = ar.alloc([64], F32)
    P.add("gpsimd", lambda e: e.memset(neghalf, -0.5), w=["neghalf"])
    CONST_END = ar.off

    def col_load(dst, src, n):
        def f(e):
            with nc.allow_non_contiguous_dma(reason="tiny gain vector"):
                return e.dma_start(out=dst, in_=src.rearrange("(k p) -> p k", p=128))
        return f

    def norm_p1(xt, xs, ss, rstd, tag, keys_r, xs32=None):
        def sq(e):
            return e.activation(out=xs["junk"], in_=xt, func=AF.Square, accum_out=ss)
        P.add("gpsimd", lambda e: e.memset(ss, 0.0), w=[("ss", tag)])
        P.add("scalar", sq, r=keys_r + [("ss", tag)], w=[("ss", tag), ("junk", tag)])

        P.chain("gpsimd", [
            lambda e: e.tensor_scalar(out=rstd, in0=ss, scalar1=1.0 / D, scalar2=EPS, op0=ALU.mult, op1=ALU.add),
            lambda e: e.tensor_tensor(out=rstd, in0=rstd, in1=neghalf[:, 0:1], op=ALU.pow),
        ], r=[("ss", tag)], w=[("rstd", tag)])
        if xs32 is None:
            P.add("vector", lambda e: e.tensor_scalar(out=xs["bf"], in0=xt, scalar1=rstd[:, 0:1], scalar2=None, op0=ALU.mult),
                  r=keys_r + [("rstd", tag)], w=[("xsbf", tag)])
        else:
            P.add("vector", lambda e: e.tensor_scalar(out=xs32, in0=xt, scalar1=rstd[:, 0:1], scalar2=None, op0=ALU.mult),
                  r=keys_r + [("rstd", tag)], w=[("xs32", tag)])
            P.add("gpsimd", lambda e: e.tensor_copy(out=xs["bf"], in_=xs32), r=[("xs32", tag)], w=[("xsbf", tag)])

    def norm_p2(gcol, xs, xnT_dst, tag, psT, keys_w):
        b = psT.next()

        def tr(e):
            last = None
            for k in range(8):
                last = e.transpose(psb(b)[:, k * 128:(k + 1) * 128], xs["bf"][:, k * 128:(k + 1) * 128], ident_bf)
            return last
        P.add("tensor", tr, r=[("xsbf", tag), "ident_bf"], w=[("ps", b)])

        def ev(e):
            return e.tensor_tensor(out=xnT_dst, in0=psb(b).rearrange("p (k t) -> p k t", k=8),
                                   in1=gcol.unsqueeze(2).to_broadcast([128, 8, 128]), op=ALU.mult)
        P.add("vector", ev, r=[("ps", b), "gcol"], w=keys_w)

    def norm_tile(xt, gcol, xs, ss, rstd, xnT_dst, tag, psT, keys_r, keys_w):
        norm_p1(xt, xs, ss, rstd, tag, keys_r)
        norm_p2(gcol, xs, xnT_dst, tag, psT, keys_w)

    dk_ctr = [0]

    def dk():
        dk_ctr[0] += 1
        return "c%d" % dk_ctr[0]

    def phase_A(src_d, dst_d):
        ar.reset(CONST_END)
        win = ar.alloc([8, 4096], BF16)
        wout = ar.alloc([16, 1024], BF16)
        wsp_f = ar.alloc([8, 128], F32)
        wmT = ar.alloc([8, 128], BF16)
        tri = ar.alloc([128], F32)
        biasT = ar.alloc([8, 128], F32)
        gcol = ar.alloc([8], F32)
        gv = ar.alloc([16], F32)
        xt_ring = [ar.alloc([1024], F32) for _ in range(2)]
        xr_ring = [ar.alloc([1024], F32) for _ in range(2)]
        xs_ring = [{"bf": ar.alloc([1024], BF16), "junk": None} for _ in range(2)]
        junk = ar.alloc([1024], BF16)
        for d_ in xs_ring:
            d_["junk"] = junk
        ss_r = [ar.alloc([1], F32) for _ in range(2)]
        rstd_r = [ar.alloc([1], F32) for _ in range(2)]
        xnT = ar.alloc([8, 512], BF16)
        uT = ar.alloc([16, 512], BF16)
        vbf = ar.alloc([4, 2048], BF16)
        ssv_r = [ar.alloc([4], F32) for _ in range(2)]
        rsv_r = [ar.alloc([1], F32) for _ in range(2)]
        wms_r = [ar.alloc([8, 128], BF16) for _ in range(4)]
        yT = ar.alloc([16, 512], BF16)
        tmp_r = [ar.alloc([512], F32) for _ in range(2)]

        w_in = w["l0_sg_w_in"].rearrange("(k p) n -> p k n", p=128)
        for k in range(8):
            P.add("gpsimd", (lambda k: lambda e: e.dma_start(out=win[:, k, :], in_=w_in[:, k, :]))(k),
                  w=[("win", k)], dkey="w%d" % k)
        w_o = w["l0_sg_w_out"].rearrange("(k p) n -> p k n", p=128)
        for q in range(4):
            P.add("gpsimd", (lambda q: lambda e: e.dma_start(out=wout[:, 4 * q:4 * q + 4, :], in_=w_o[:, 4 * q:4 * q + 4, :]))(q),
                  w=[("wout", q)], dkey="w%d" % (8 + q))
        P.add("sync", lambda e: e.dma_start(out=wsp_f, in_=w["l0_sg_w_spatial"].rearrange("g t s -> t g s")), w=["wsp_f"], dkey="c0")
        P.add("sync", lambda e: e.dma_start(out=tri, in_=tri_d[:, :]), w=["tri"], dkey="c1")
        P.add("sync", lambda e: e.dma_start(out=biasT.rearrange("p g t -> p (g t)"),
                                            in_=w["l0_sg_b_spatial"].rearrange("g t -> (g t)").partition_broadcast(128)),
              w=["biasT"], dkey="c2")
        P.add("sync", col_load(gcol, w["l0_mix_norm"], 8), w=["gcol"], dkey="c3")
        P.add("sync", col_load(gv, w["l0_sg_v_norm"], 16), w=["gv"], dkey="c4")

        bT = 0

        def wtr(e):
            last = None
            for g in range(8):
                last = e.transpose(ps_t[:, g // 4, (g % 4) * 128:(g % 4 + 1) * 128], wsp_f[:, g, :], ident_f)
            return last
        P.add("tensor", wtr, r=["wsp_f", "ident_f"], w=[("ps", 0), ("ps", 1)])

        def wmk(e):
            return e.tensor_tensor(out=wmT, in0=ps_t[:, 0:2, :].rearrange("p a (b t) -> p (a b) t", b=4),
                                   in1=tri.unsqueeze(1).to_broadcast([128, 8, 128]), op=ALU.mult)
        P.add("vector", wmk, r=[("ps", 0), ("ps", 1), "tri"], w=["wmT"])

        psT = PsRing([0, 1])
        psM = PsRing([2, 3, 4, 5, 6, 7])

        for st in range(NST):
            for c in range(4):
                T = st * 4 + c
                xt = xt_ring[T % 2]
                P.add("sync", (lambda xt, T: lambda e: e.dma_start(out=xt, in_=src_d[T * 128:(T + 1) * 128, :]))(xt, T),
                      w=[("xt", T % 2)], dkey="ld%d" % (T % 2))
                norm_tile(xt, gcol, xs_ring[T % 2], ss_r[T % 2], rstd_r[T % 2],
                          xnT[:, :, c * 128:(c + 1) * 128], ("A", T % 2), psT,
                          keys_r=[("xt", T % 2)], keys_w=[("xnT", c)])
            for m in range(16):
                b = psM.next()

                def mm(e, m=m, b=b):
                    last = None
                    for k in range(8):
                        last = e.matmul(psf(b), lhsT=win[:, k, m * 128:(m + 1) * 128], rhs=xnT[:, k, :],
                                        start=(k == 0), stop=(k == 7))
                    return last
                P.add("tensor", mm, r=[("win", k) for k in range(8)] + [("xnT", c) for c in range(4)], w=[("ps", b)])
                P.add("scalar", (lambda m, b: lambda e: e.activation(out=uT[:, m, :], in_=psf(b), func=AF.Gelu_apprx_tanh))(m, b),
                      r=[("ps", b)], w=[("uT", m)])
            for c in range(4):
                T = st * 4 + c
                ssv = ssv_r[T % 2]
                rsv = rsv_r[T % 2]
                P.add("gpsimd", (lambda ssv: lambda e: e.memset(ssv, 0.0))(ssv), w=[("ssv", T % 2)])
                for j in range(4):
                    b = psM.next()

                    def mmv(e, c=c, j=j, b=b):
                        last = None
                        for k in range(8):
                            last = e.matmul(psf(b), lhsT=xnT[:, k, c * 128:(c + 1) * 128],
                                            rhs=win[:, k, 2048 + j * 512:2048 + (j + 1) * 512],
                                            start=(k == 0), stop=(k == 7))
                        return last
                    P.add("tensor", mmv, r=[("win", k) for k in range(8)] + [("xnT", c)], w=[("ps", b)])
                    P.add("scalar", (lambda c, j, b: lambda e: e.activation(out=vbf[:, c, j * 512:(j + 1) * 512], in_=psf(b),
                                                                             func=AF.Gelu_apprx_tanh))(c, j, b),
                          r=[("ps", b)], w=[("vbf", c, j)])
                    P.add("scalar", (lambda c, j, ssv: lambda e: e.activation(out=junk[:, 0:512], in_=vbf[:, c, j * 512:(j + 1) * 512],
                                                                               func=AF.Square, accum_out=ssv[:, j:j + 1]))(c, j, ssv),
                          r=[("vbf", c, j), ("ssv", T % 2)], w=[("ssv", T % 2), ("junkA",)])

                P.chain("gpsimd", [
                    (lambda ssv, rsv: lambda e: e.tensor_tensor(out=rsv, in0=ssv[:, 0:1], in1=ssv[:, 1:2], op=ALU.add))(ssv, rsv),
                    (lambda ssv, rsv: lambda e: e.tensor_tensor(out=rsv, in0=rsv, in1=ssv[:, 2:3], op=ALU.add))(ssv, rsv),
                    (lambda ssv, rsv: lambda e: e.tensor_tensor(out=rsv, in0=rsv, in1=ssv[:, 3:4], op=ALU.add))(ssv, rsv),
                    (lambda ssv, rsv: lambda e: e.tensor_scalar(out=rsv, in0=rsv, scalar1=1.0 / 2048, scalar2=EPS, op0=ALU.mult, op1=ALU.add))(ssv, rsv),
                    (lambda ssv, rsv: lambda e: e.tensor_tensor(out=rsv, in0=rsv, in1=neghalf[:, 0:1], op=ALU.pow))(ssv, rsv),
                ], r=[("ssv", T % 2)], w=[("rsv", T % 2)])
                wms = wms_r[T % 4]
                P.add("gpsimd", (lambda wms, rsv: lambda e: e.tensor_scalar(out=wms, in0=wmT, scalar1=rsv[:, 0:1], scalar2=None,
                                                                             op0=ALU.mult))(wms, rsv),
                      r=["wmT", ("rsv", T % 2)], w=[("wms", T % 4)], strict=True)
            for m in range(16):
                g = m // 2
                b = psM.next()

                def mmx(e, m=m, g=g, b=b):
                    last = None
                    for c in range(4):
                        T = st * 4 + c
                        last = e.matmul(psf(b)[:, c * 128:(c + 1) * 128], lhsT=vbf[:, c, m * 128:(m + 1) * 128],
                                        rhs=wms_r[T % 4][:, g, :], start=True, stop=True)
                    return last
                P.add("tensor", mmx, r=[("vbf", c, m // 4) for c in range(4)] + [("wms", (st * 4 + c) % 4) for c in range(4)],
                      w=[("ps", b)])
                tmp = tmp_r[m % 2]

                def e1(e, m=m, g=g, b=b, tmp=tmp):
                    return e.scalar_tensor_tensor(out=tmp.rearrange("p (c t) -> p c t", c=4),
                                                  in0=psf(b).rearrange("p (c t) -> p c t", c=4),
                                                  scalar=gv[:, m:m + 1],
                                                  in1=biasT[:, g, :].unsqueeze(1).to_broadcast([128, 4, 128]),
                                                  op0=ALU.mult, op1=ALU.add)
                P.add("vector", e1, r=[("ps", b), "gv", "biasT"], w=[("tmp", m % 2)])
                P.add("vector", (lambda m, tmp: lambda e: e.tensor_tensor(out=yT[:, m, :], in0=tmp, in1=uT[:, m, :], op=ALU.mult))(m, tmp),
                      r=[("tmp", m % 2), ("uT", m)], w=[("yT", m)])
            for c in range(4):
                T = st * 4 + c
                xr = xr_ring[T % 2]
                P.add("sync", (lambda xr, T: lambda e: e.dma_start(out=xr, in_=src_d[T * 128:(T + 1) * 128, :]))(xr, T),
                      w=[("xr", T % 2)], dkey="lr%d" % (T % 2))
                for j in range(2):
                    b = psM.next()

                    def mmo(e, c=c, j=j, b=b):
                        last = None
                        for k in range(16):
                            last = e.matmul(psf(b), lhsT=yT[:, k, c * 128:(c + 1) * 128], rhs=wout[:, k, j * 512:(j + 1) * 512],
                                            start=(k == 0), stop=(k == 15))
                        return last
                    P.add("tensor", mmo, r=[("yT", m) for m in range(16)] + [("wout", q) for q in range(4)], w=[("ps", b)])
                    P.add("vector", (lambda xr, j, b: lambda e: e.tensor_tensor(out=xr[:, j * 512:(j + 1) * 512], in0=psf(b),
                                                                                 in1=xr[:, j * 512:(j + 1) * 512], op=ALU.add))(xr, j, b),
                          r=[("ps", b), ("xr", T % 2)], w=[("xr", T % 2)])
                P.add("sync", (lambda xr, T: lambda e: e.dma_start(out=dst_d[T * 128:(T + 1) * 128, :], in_=xr))(xr, T),
                      r=[("xr", T % 2)], w=[("hA", T)], dkey="st%d" % (T % 2))


    def phase_B(src_d, dst_d):
        ar.reset(CONST_END)
        dk_ctr[0] = 0
        wg = ar.alloc([8, FFN], BF16)
        wu = ar.alloc([8, FFN], BF16)
        wd = ar.alloc([NF0, 1024], BF16)
        gcol = ar.alloc([8], F32)
        xt_ring = [ar.alloc([1024], F32) for _ in range(2)]
        xr_ring = [ar.alloc([1024], F32) for _ in range(2)]
        junk = ar.alloc([1024], BF16)
        xs_ring = [{"bf": ar.alloc([1024], BF16), "junk": junk} for _ in range(4)]
        ss_r = [ar.alloc([1], F32) for _ in range(4)]
        rstd_r = [ar.alloc([1], F32) for _ in range(4)]
        xnT = ar.alloc([8, 512], BF16)
        HT = ar.alloc([NF0, 512], BF16)
        tmp_r = [ar.alloc([512], F32) for _ in range(2)]

        wgd = w["l0_ffn_w_gate"].rearrange("(k p) n -> p k n", p=128)
        wud = w["l0_ffn_w_up"].rearrange("(k p) n -> p k n", p=128)
        wdd = w["l0_ffn_w_down"].rearrange("(k p) n -> p k n", p=128)
        P.add("sync", col_load(gcol, w["l0_ffn_norm"], 8), w=["gcol"], dkey=dk())
        for k in range(8):
            P.add("gpsimd", (lambda k: lambda e: e.dma_start(out=wg[:, k, :], in_=wgd[:, k, :]))(k), w=[("wg", k)], dkey="w%d" % k)
            P.add("gpsimd", (lambda k: lambda e: e.dma_start(out=wu[:, k, :], in_=wud[:, k, :]))(k), w=[("wu", k)], dkey="w%d" % (8 + k))
        for q in range(2):
            P.add("gpsimd", (lambda q: lambda e: e.dma_start(out=wd[:, 11 * q:11 * q + 11, :], in_=wdd[:, 11 * q:11 * q + 11, :]))(q),
                  w=[("wd", q)], dkey="w%d" % (16 + q))
        psT = PsRing([0, 1])
        psM = PsRing([2, 3, 4, 5, 6, 7])

        def p1(st):
            for c in range(4):
                T = st * 4 + c
                xt = xt_ring[T % 2]
                P.add("sync", (lambda xt, T: lambda e: e.dma_start(out=xt, in_=src_d[T * 128:(T + 1) * 128, :]))(xt, T),
                      w=[("xt", T % 2)], dkey="ld%d" % (T % 2))
                norm_p1(xt, xs_ring[c], ss_r[c], rstd_r[c], ("B", c), [("xt", T % 2)])

        def p2(st):
            for c in range(4):
                norm_p2(gcol, xs_ring[c], xnT[:, :, c * 128:(c + 1) * 128], ("B", c), psT, [("xnT", c)])

        p1(0)
        for st in range(NST):
            p2(st)
            for m in range(NF0):
                bg = psM.next()
                bu = psM.next()

                def mmg(e, m=m, bg=bg):
                    last = None
                    for k in range(8):
                        last = e.matmul(psf(bg), lhsT=wg[:, k, m * 128:(m + 1) * 128], rhs=xnT[:, k, :], start=(k == 0), stop=(k == 7))
                    return last

                def mmu(e, m=m, bu=bu):
                    last = None
                    for k in range(8):
                        last = e.matmul(psf(bu), lhsT=wu[:, k, m * 128:(m + 1) * 128], rhs=xnT[:, k, :], start=(k == 0), stop=(k == 7))
                    return last
                xk = [("xnT", c) for c in range(4)]
                P.add("tensor", mmg, r=[("wg", k) for k in range(8)] + xk, w=[("ps", bg)])
                P.add("tensor", mmu, r=[("wu", k) for k in range(8)] + xk, w=[("ps", bu)])
                tmp = tmp_r[m % 2]
                P.add("scalar", (lambda tmp, bg: lambda e: e.activation(out=tmp, in_=psf(bg), func=AF.Silu))(tmp, bg),
                      r=[("ps", bg)], w=[("tmp", m % 2)])
                P.add("vector", (lambda tmp, bu, m: lambda e: e.tensor_tensor(out=HT[:, m, :], in0=psf(bu), in1=tmp, op=ALU.mult))(tmp, bu, m),
                      r=[("ps", bu), ("tmp", m % 2)], w=[("HT", m)])
            if st + 1 < NST:
                p1(st + 1)
            for c in range(4):
                T = st * 4 + c
                xr = xr_ring[T % 2]
                P.add("sync", (lambda xr, T: lambda e: e.dma_start(out=xr, in_=src_d[T * 128:(T + 1) * 128, :]))(xr, T),
                      w=[("xr", T % 2)], dkey="lr%d" % (T % 2))
                for j in range(2):
                    b = psM.next()

                    def mmd(e, c=c, j=j, b=b):
                        last = None
                        for k in range(NF0):
                            last = e.matmul(psf(b), lhsT=HT[:, k, c * 128:(c + 1) * 128], rhs=wd[:, k, j * 512:(j + 1) * 512],
                                            start=(k == 0), stop=(k == NF0 - 1))
                        return last
                    P.add("tensor", mmd, r=[("HT", m) for m in range(NF0)] + [("wd", 0), ("wd", 1)], w=[("ps", b)])
                    P.add("vector", (lambda xr, j, b: lambda e: e.tensor_tensor(out=xr[:, j * 512:(j + 1) * 512], in0=psf(b),
                                                                                 in1=xr[:, j * 512:(j + 1) * 512], op=ALU.add))(xr, j, b),
                          r=[("ps", b), ("xr", T % 2)], w=[("xr", T % 2)])
                P.add("sync", (lambda xr, T: lambda e: e.dma_start(out=dst_d[T * 128:(T + 1) * 128, :], in_=xr))(xr, T),
                      r=[("xr", T % 2)], w=[("hB", T)], dkey="st%d" % (T % 2))


    def phase_C(src_d, dst_d):
        ar.reset(CONST_END)
        dk_ctr[0] = 0
        wqkv = ar.alloc([8, 3072], BF16)
        gcol = ar.alloc([8], F32)
        gqk = ar.alloc([2, 64], F32)
        posi = ar.alloc([NT], I32)
        posf = ar.alloc([NT], F32)
        invf = ar.alloc([8], F32)
        ang = ar.alloc([NT, 8], F32)
        angk = ar.alloc([NT, 8], I32)
        angf = ar.alloc([NT, 8], F32)
        angm = ar.alloc([NT, 8], F32)
        sinT = ar.alloc([NT, 8], F32)
        cosT = ar.alloc([NT, 8], F32)
        xt_ring = [ar.alloc([1024], F32) for _ in range(2)]
        junk = ar.alloc([1024], BF16)
        xs_ring = [{"bf": ar.alloc([1024], BF16), "junk": junk} for _ in range(2)]
        ss_r = [ar.alloc([1], F32) for _ in range(2)]
        rstd_r = [ar.alloc([1], F32) for _ in range(2)]
        xnT_r = [ar.alloc([8, 128], BF16) for _ in range(2)]
        qk_r = [ar.alloc([32, 64], F32) for _ in range(2)]
        sq_r = [ar.alloc([32, 64], F32) for _ in range(2)]
        ssq_r = [ar.alloc([32], F32) for _ in range(2)]
        qkb_r = [ar.alloc([32, 64], BF16) for _ in range(2)]
        rt = [ar.alloc([32, 8], F32) for _ in range(4)]
        qT_st = [ar.alloc([8, 512], BF16) for _ in range(2)]
        kT_st = [ar.alloc([8, 512], BF16) for _ in range(2)]
        vb_r = [ar.alloc([1024], BF16) for _ in range(2)]

        wq_d = w["l1_da_w_qkv"].rearrange("(k p) n -> p k n", p=128)
        for k in range(8):
            P.add("gpsimd", (lambda k: lambda e: e.dma_start(out=wqkv[:, k, :], in_=wq_d[:, k, :]))(k), w=[("wqkv", k)], dkey="w%d" % k)
        P.add("sync", col_load(gcol, w["l1_mix_norm"], 8), w=["gcol"], dkey=dk())
        P.add("sync", lambda e: e.dma_start(out=gqk[:, 0, :], in_=w["l1_da_q_norm"].partition_broadcast(128)), w=["gqk0"], dkey=dk())
        P.add("sync", lambda e: e.dma_start(out=gqk[:, 1, :], in_=w["l1_da_k_norm"].partition_broadcast(128)), w=["gqk1"], dkey=dk())
        P.add("sync", lambda e: e.dma_start(out=invf, in_=w["c_invf"].partition_broadcast(128)), w=["invf"], dkey=dk())

        def posld(e):
            with nc.allow_non_contiguous_dma(reason="positions to token-major columns"):
                return e.dma_start(out=posi, in_=w["positions"].rearrange("(t p) -> p t", p=128))
        P.add("sync", posld, w=["posi"], dkey=dk())
        P.add("vector", lambda e: e.tensor_scalar(out=gqk[:, 0, :], in0=gqk[:, 0, :], scalar1=0.125, scalar2=None, op0=ALU.mult),
              r=["gqk0"], w=["gqk0"])

        TWO_PI = 2.0 * math.pi

        def trig(dst, shift, tagk):
            def f(e):
                a3 = ang.rearrange("p t i -> p (t i)")
                k3 = angk.rearrange("p t i -> p (t i)")
                f3 = angf.rearrange("p t i -> p (t i)")
                m3 = angm.rearrange("p t i -> p (t i)")
                e.tensor_scalar(out=f3, in0=a3, scalar1=shift, scalar2=None, op0=ALU.add)
                e.tensor_scalar(out=k3, in0=f3, scalar1=1.0 / TWO_PI, scalar2=None, op0=ALU.mult)
                e.tensor_copy(out=m3, in_=k3)
                e.scalar_tensor_tensor(out=f3, in0=m3, scalar=-TWO_PI, in1=f3, op0=ALU.mult, op1=ALU.add)
                e.tensor_scalar(out=m3, in0=f3, scalar1=math.pi, scalar2=-TWO_PI, op0=ALU.is_gt, op1=ALU.mult)
                e.tensor_tensor(out=f3, in0=f3, in1=m3, op=ALU.add)
                e.tensor_scalar(out=m3, in0=f3, scalar1=-math.pi, scalar2=TWO_PI, op0=ALU.is_lt, op1=ALU.mult)
                e.tensor_tensor(out=f3, in0=f3, in1=m3, op=ALU.add)
                return e.tensor_scalar(out=f3, in0=f3, scalar1=3.1415925, scalar2=-3.1415925, op0=ALU.min, op1=ALU.max)
            P.add("vector", f, r=["ang"], w=["angf"])
            P.add("scalar", lambda e: e.activation(out=dst.rearrange("p t i -> p (t i)"), in_=angf.rearrange("p t i -> p (t i)"), func=AF.Sin),
                  r=["angf"], w=[tagk])

        P.add("vector", lambda e: e.tensor_copy(out=posf, in_=posi), r=["posi"], w=["posf"])
        P.add("vector", lambda e: e.tensor_tensor(out=ang, in0=posf.unsqueeze(2).to_broadcast([128, NT, 8]),
                                                  in1=invf.unsqueeze(1).to_broadcast([128, NT, 8]), op=ALU.mult),
              r=["posf", "invf"], w=["ang"])
        trig(sinT, 0.0, "sinT")
        trig(cosT, 0.5 * math.pi, "cosT")

        psT = PsRing([0, 1])
        psQ = PsRing([2, 3])
        psM = PsRing([4, 5, 6, 7])
        qT_v = qT_d.rearrange("h p s -> p h s")
        kT_v = kT_d.rearrange("h p s -> p h s")
        v_v = v_d.rearrange("s h d -> s (h d)")

        def c1_stageA(T):
            st, c = T // 4, T % 4
            s2 = T % 2
            xt = xt_ring[s2]
            P.add("sync", (lambda xt, T: lambda e: e.dma_start(out=xt, in_=src_d[T * 128:(T + 1) * 128, :]))(xt, T),
                  w=[("xt", s2)], dkey="ld%d" % s2)
            xnT = xnT_r[s2]
            norm_tile(xt, gcol, xs_ring[s2], ss_r[s2], rstd_r[s2], xnT, ("C", s2), psT, [("xt", s2)], [("xnT", s2)])
            qk = qk_r[s2]
            qkf = qk.rearrange("p g d -> p (g d)")
            sqf = sq_r[s2].rearrange("p g d -> p (g d)")
            vb = vb_r[s2]
            for jb in range(6):
                b = psM.next()

                def mm(e, jb=jb, b=b, xnT=xnT):
                    last = None
                    for k in range(8):
                        last = e.matmul(psf(b), lhsT=xnT[:, k, :], rhs=wqkv[:, k, jb * 512:(jb + 1) * 512], start=(k == 0), stop=(k == 7))
                    return last
                P.add("tensor", mm, r=[("wqkv", k) for k in range(8)] + [("xnT", s2)], w=[("ps", b)])
                if jb < 4:
                    P.add("scalar", (lambda jb, b, qkf: lambda e: e.activation(out=qkf[:, jb * 512:(jb + 1) * 512], in_=psf(b), func=AF.Copy))(jb, b, qkf),
                          r=[("ps", b)], w=[("qk", s2, jb)])
                    P.add("scalar", (lambda jb, b: lambda e: e.activation(out=sqf[:, jb * 512:(jb + 1) * 512], in_=psf(b), func=AF.Square))(jb, b),
                          r=[("ps", b)], w=[("sq", s2, jb)])
                else:
                    P.add("scalar", (lambda jb, b, vb: lambda e: e.activation(out=vb[:, (jb - 4) * 512:(jb - 3) * 512], in_=psf(b), func=AF.Copy))(jb, b, vb),
                          r=[("ps", b)], w=[("vb", s2, jb)])
        def c1_stageB(T):
            st, c = T // 4, T % 4
            s2 = T % 2
            qk = qk_r[s2]
            qkf = qk.rearrange("p g d -> p (g d)")
            vb = vb_r[s2]
            sq = sq_r[s2]
            ssq = ssq_r[s2]
            P.add("vector", (lambda ssq: lambda e: e.tensor_reduce(out=ssq, in_=sq, axis=AX.X, op=ALU.add))(ssq),
                  r=[("sq", s2, j) for j in range(4)], w=[("ssq", s2)])

            P.chain("gpsimd", [
                (lambda ssq: lambda e: e.tensor_scalar(out=ssq, in0=ssq, scalar1=1.0 / 64, scalar2=EPS, op0=ALU.mult, op1=ALU.add))(ssq),
                (lambda ssq: lambda e: e.tensor_tensor(out=ssq, in0=ssq, in1=neghalf[:, 0:32], op=ALU.pow))(ssq),
            ], r=[("ssq", s2)], w=[("ssq", s2)])
            qkeys = [("qk", s2, j) for j in range(4)]
            P.add("vector", (lambda qk, ssq: lambda e: e.tensor_tensor(out=qk, in0=qk, in1=ssq.unsqueeze(2).to_broadcast([128, 32, 64]), op=ALU.mult))(qk, ssq),
                  r=qkeys + [("ssq", s2)], w=qkeys)

            def gmul(e, qk=qk):
                q4 = qk.rearrange("p (a g) d -> p a g d", a=2)
                return e.tensor_tensor(out=q4, in0=q4, in1=gqk.unsqueeze(2).to_broadcast([128, 2, 16, 64]), op=ALU.mult)
            P.add("gpsimd", gmul, r=qkeys + ["gqk0", "gqk1"], w=qkeys)
            qkb = qkb_r[s2]
            P.add("scalar", (lambda qkb, qkf: lambda e: e.activation(out=qkb.rearrange("p g d -> p (g d)"), in_=qkf, func=AF.Copy))(qkb, qkf),
                  r=qkeys, w=[("qkb", s2)])

            def rope(e, qk=qk, qkb=qkb, T=T):
                cs = cosT[:, T, :].unsqueeze(1).to_broadcast([128, 32, 8])
                sn = sinT[:, T, :].unsqueeze(1).to_broadcast([128, 32, 8])
                x1 = qk[:, :, 0:8]
                x2 = qk[:, :, 8:16]
                e.tensor_tensor(out=rt[0], in0=x1, in1=cs, op=ALU.mult)
                e.tensor_tensor(out=rt[1], in0=x2, in1=sn, op=ALU.mult)
                e.tensor_tensor(out=rt[2], in0=x2, in1=cs, op=ALU.mult)
                e.tensor_tensor(out=rt[3], in0=x1, in1=sn, op=ALU.mult)
                e.tensor_tensor(out=qkb[:, :, 0:8], in0=rt[0], in1=rt[1], op=ALU.subtract)
                return e.tensor_tensor(out=qkb[:, :, 8:16], in0=rt[2], in1=rt[3], op=ALU.add)
            P.add("gpsimd", rope, r=qkeys + [("qkb", s2), "sinT", "cosT"], w=[("qkb", s2)])
            bq = psQ.next()
            bk = psQ.next()

            def trq(e, qkb=qkb, bq=bq, bk=bk):
                last = None
                for h in range(8):
                    e.transpose(psb(bq)[:, h * 128:(h + 1) * 128], qkb[:, 2 * h:2 * h + 2, :].rearrange("p a d -> p (a d)"), ident_bf)
                    last = e.transpose(psb(bk)[:, h * 128:(h + 1) * 128], qkb[:, 16 + 2 * h:18 + 2 * h, :].rearrange("p a d -> p (a d)"), ident_bf)
                return last
            P.add("tensor", trq, r=[("qkb", s2), "ident_bf"], w=[("ps", bq), ("ps", bk)])
            qs = qT_st[st % 2]
            ks = kT_st[st % 2]
            P.add("scalar", (lambda qs, bq, c: lambda e: e.activation(out=qs[:, :, c * 128:(c + 1) * 128],
                                                                       in_=psb(bq).rearrange("p (h t) -> p h t", h=8), func=AF.Copy))(qs, bq, c),
                  r=[("ps", bq)], w=[("qs", st % 2, c)])
            P.add("vector", (lambda ks, bk, c: lambda e: e.tensor_copy(out=ks[:, :, c * 128:(c + 1) * 128],
                                                                        in_=psb(bk).rearrange("p (h t) -> p h t", h=8)))(ks, bk, c),
                  r=[("ps", bk)], w=[("ks", st % 2, c)])
            P.add("sync", (lambda vb, T: lambda e: e.dma_start(out=v_v[T * 128:(T + 1) * 128, :], in_=vb))(vb, T),
                  r=[("vb", s2, 4), ("vb", s2, 5)], w=[("v_d", T)], dkey="sv%d" % s2)
            if c == 3:
                P.add("sync", (lambda qs, st: lambda e: e.dma_start(out=qT_v[:, :, st * 512:(st + 1) * 512], in_=qs))(qs, st),
                      r=[("qs", st % 2, cc) for cc in range(4)], w=[("qT_d", st)], dkey="sq%d" % (st % 2))
                P.add("sync", (lambda ks, st: lambda e: e.dma_start(out=kT_v[:, :, st * 512:(st + 1) * 512], in_=ks))(ks, st),
                      r=[("ks", st % 2, cc) for cc in range(4)], w=[("kT_d", st)], dkey="sk%d" % (st % 2))
        c1_stageA(0)
        for T in range(NT):
            if T + 1 < NT:
                c1_stageA(T + 1)
            c1_stageB(T)
        P.barrier()

        ar.reset(CONST_END)
        dk_ctr[0] = 0
        OnT = ar.alloc([8, S], BF16)
        KT_r = [ar.alloc([S], BF16) for _ in range(2)]
        Q0_r = [ar.alloc([S], BF16) for _ in range(2)]
        Q1_r = [ar.alloc([S], BF16) for _ in range(2)]
        V_r = [ar.alloc([NT, 128], BF16) for _ in range(2)]
        E_r = [ar.alloc([512], BF16) for _ in range(8)]
        Es = [ar.alloc([512], F32) for _ in range(2)]
        tri_f = ar.alloc([128], F32)
        maskneg = ar.alloc([128], BF16)
        ones_b = ar.alloc([128], BF16)
        ones_f = ar.alloc([128], F32)
        lamv = ar.alloc([4, 64], F32)
        lamp = ar.alloc([2, 64], F32)
        lsum = [ar.alloc([1], F32) for _ in range(2)]
        lexp = [ar.alloc([1], F32) for _ in range(2)]
        nlam = ar.alloc([1], F32)
        gb = ar.alloc([2, 64], F32)
        mx = [ar.alloc([1], F32) for _ in range(2)]
        negB = ar.alloc([1], F32)
        sg = ar.alloc([1], F32)
        rinv = [ar.alloc([512], F32) for _ in range(2)]
        tA = ar.alloc([512], F32)
        tB = ar.alloc([512], F32)
        sqb = ar.alloc([512], BF16)
        rstd5 = ar.alloc([512], F32)
        epsb = ar.alloc([1], F32)
        wo = ar.alloc([8, 1024], BF16)
        xr_ring = [ar.alloc([1024], F32) for _ in range(2)]

        wo_d = w["l1_da_w_out"].rearrange("(k p) n -> p k n", p=128)
        P.add("gpsimd", lambda e: e.dma_start(out=wo, in_=wo_d), w=["wo"], dkey="w0")
        P.add("sync", lambda e: e.dma_start(out=tri_f, in_=tri_d[:, :]), w=["tri_f"], dkey=dk())
        P.add("vector", lambda e: e.tensor_scalar(out=maskneg, in0=tri_f, scalar1=-1.0, scalar2=30000.0, op0=ALU.add, op1=ALU.mult),
              r=["tri_f"], w=["maskneg"])
        P.add("gpsimd", lambda e: e.memset(ones_b, 1.0), w=["ones_b"])
        P.add("gpsimd", lambda e: e.memset(ones_f, 1.0), w=["ones_f"])
        P.add("gpsimd", lambda e: e.memset(epsb, EPS), w=["epsb"])
        for s2 in range(2):
            P.add("gpsimd", (lambda s2: lambda e: e.memset(Q0_r[s2][64:128, :], 0.0))(s2), w=[("Q0", s2)])
            P.add("gpsimd", (lambda s2: lambda e: e.memset(Q1_r[s2][0:64, :], 0.0))(s2), w=[("Q1", s2)])
        for i, nm in enumerate(["l1_da_lambda_q1", "l1_da_lambda_q2", "l1_da_lambda_k1", "l1_da_lambda_k2"]):
            P.add("sync", (lambda i, nm: lambda e: e.dma_start(out=lamv[:, i, :], in_=w[nm].partition_broadcast(128)))(i, nm),
                  w=[("lamv", i)], dkey=dk())
        P.add("sync", lambda e: e.dma_start(out=gb[:, 0, :], in_=w["l1_da_q_norm"].partition_broadcast(128)), w=[("gb", 0)], dkey=dk())
        P.add("sync", lambda e: e.dma_start(out=gb[:, 1, :], in_=w["l1_da_k_norm"].partition_broadcast(128)), w=[("gb", 1)], dkey=dk())

        def sgld(e):
            with nc.allow_non_contiguous_dma(reason="tiny gain column"):
                return e.dma_start(out=sg, in_=w["l1_da_subln"].rearrange("(p o) -> p o", o=1))
        P.add("sync", sgld, w=["sg"], dkey=dk())
        P.add("vector", lambda e: e.tensor_scalar(out=sg, in0=sg, scalar1=1.0 - LAMBDA_INIT, scalar2=None, op0=ALU.mult), r=["sg"], w=["sg"])
        P.chain("vector", [
            lambda e: e.tensor_tensor(out=lamp[:, 0, :], in0=lamv[:, 0, :], in1=lamv[:, 2, :], op=ALU.mult),
            lambda e: e.tensor_tensor(out=lamp[:, 1, :], in0=lamv[:, 1, :], in1=lamv[:, 3, :], op=ALU.mult),
            lambda e: e.tensor_reduce(out=lsum[0], in_=lamp[:, 0, :], axis=AX.X, op=ALU.add),
            lambda e: e.tensor_reduce(out=lsum[1], in_=lamp[:, 1, :], axis=AX.X, op=ALU.add),
        ], r=[("lamv", i) for i in range(4)], w=["lsum"])

        def lexpf(e):
            e.activation(out=lexp[0], in_=lsum[0], func=AF.Exp)
            return e.activation(out=lexp[1], in_=lsum[1], func=AF.Exp)
        P.add("scalar", lexpf, r=["lsum"], w=["lexp"])
        P.chain("vector", [
            lambda e: e.tensor_tensor(out=nlam, in0=lexp[1], in1=lexp[0], op=ALU.subtract),
            lambda e: e.tensor_scalar(out=nlam, in0=nlam, scalar1=-LAMBDA_INIT, scalar2=None, op0=ALU.add),
        ], r=["lexp"], w=["nlam"])
        P.chain("vector", [
            lambda e: e.tensor_tensor(out=gb, in0=gb, in1=gb, op=ALU.mult),
            lambda e: e.tensor_reduce(out=mx[0], in_=gb[:, 0, :], axis=AX.X, op=ALU.max),
            lambda e: e.tensor_reduce(out=mx[1], in_=gb[:, 1, :], axis=AX.X, op=ALU.max),
            lambda e: e.tensor_tensor(out=mx[0], in0=mx[0], in1=mx[1], op=ALU.max),
            lambda e: e.tensor_scalar(out=negB, in0=mx[0], scalar1=1.0, scalar2=-8.0, op0=ALU.max, op1=ALU.mult),
        ], r=[("gb", 0), ("gb", 1)], w=["negB"])

        psS = PsRing([2, 3, 4, 5, 6, 7])
        v_h = v_d.rearrange("(j p) h d -> p j h d", p=128)

        def head_loads(h):
            s2 = h % 2
            P.add("sync", lambda e: e.dma_start(out=KT_r[s2], in_=kT_d[h]), w=[("KT", s2)], dkey="lk%d" % s2)
            P.add("sync", lambda e: e.dma_start(out=Q0_r[s2][0:64, :], in_=qT_d[h][0:64, :]), w=[("Q0", s2)], dkey="lq%d" % s2)
            P.add("sync", lambda e: e.dma_start(out=Q1_r[s2][64:128, :], in_=qT_d[h][64:128, :]), w=[("Q1", s2)], dkey="lp%d" % s2)
            P.add("sync", lambda e: e.dma_start(out=V_r[s2], in_=v_h[:, :, h, :]), w=[("V", s2)], dkey="lv%d" % s2)

        items = [(h, Q, j) for h in range(8) for Q in range(NST) for j in range(4 * Q + 4)]
        slot_of = {}
        ectr = [0]

        def front(it):
            h, Q, j = it
            s2 = h % 2
            KT, Q0, Q1 = KT_r[s2], Q0_r[s2], Q1_r[s2]
            if h == 0 and Q == 0 and j == 0:
                head_loads(0)
            lo = max(0, j - 4 * Q)
            diag = j >= 4 * Q
            bA = psS.next()
            bB = psS.next()

            def smm(e):
                ins = None
                for bS, Qz in ((bA, Q0), (bB, Q1)):
                    ins = e.matmul(psf(bS)[:, lo * 128:512], lhsT=KT[:, j * 128:(j + 1) * 128],
                                   rhs=Qz[:, Q * 512 + lo * 128:(Q + 1) * 512], start=True, stop=not diag)
                    if diag:
                        ins = e.matmul(psf(bS)[:, lo * 128:(lo + 1) * 128], lhsT=ident_bf, rhs=maskneg, start=False, stop=True)
                return ins
            P.add("tensor", smm, r=[("KT", s2), ("Q0", s2), ("Q1", s2), "maskneg", "ident_bf"], w=[("ps", bA), ("ps", bB)])
            sl = []
            for bS in (bA, bB):
                es_ = ectr[0] % 8
                ectr[0] += 1
                sl.append(es_)
                E = E_r[es_]
                P.add("scalar", (lambda E, bS: lambda e: e.activation(out=E[:, lo * 128:512], in_=psf(bS)[:, lo * 128:512],
                                                                      func=AF.Exp, bias=negB[:, 0:1]))(E, bS),
                      r=[("ps", bS), "negB"], w=[("E", es_)])
            slot_of[it] = sl

        def back(it):
            h, Q, j = it
            s2 = h % 2
            V = V_r[s2]
            lo = max(0, j - 4 * Q)
            sl = slot_of.pop(it)
            if Q == 0 and j == 0 and h + 1 < 8:
                head_loads(h + 1)

            def av(e):
                ins = None
                for c_, es_ in enumerate(sl):
                    E = E_r[es_]
                    ins = e.matmul(ps_t[:, c_, lo * 128:512], lhsT=V[:, j, :], rhs=E[:, lo * 128:512], start=(j == 0), stop=(j == 4 * Q + 3))
                return ins
            P.add("tensor", av, r=[("E", sl[0]), ("E", sl[1]), ("V", s2)], w=[("ps", 0), ("ps", 1)])
            for c_, es_ in enumerate(sl):
                E = E_r[es_]
                eng_ = "vector"
                if j == 0:
                    P.add(eng_, (lambda c_, E: lambda e: e.tensor_copy(out=Es[c_], in_=E))(c_, E), r=[("E", es_)], w=[("Es", c_)])
                else:
                    P.add(eng_, (lambda c_, E: lambda e: e.tensor_tensor(out=Es[c_][:, lo * 128:512], in0=Es[c_][:, lo * 128:512],
                                                                         in1=E[:, lo * 128:512], op=ALU.add))(c_, E),
                          r=[("E", es_), ("Es", c_)], w=[("Es", c_)])
            if j != 4 * Q + 3:
                return
            b1 = psS.next()
            b2 = psS.next()
            P.add("tensor", lambda e: e.matmul(psf(b1), lhsT=ones_f, rhs=Es[0], start=True, stop=True), r=[("Es", 0), "ones_f"], w=[("ps", b1)])
            P.add("tensor", lambda e: e.matmul(psf(b2), lhsT=ones_f, rhs=Es[1], start=True, stop=True), r=[("Es", 1), "ones_f"], w=[("ps", b2)])
            P.add("scalar", lambda e: e.activation(out=rinv[0], in_=psf(b1), func=AF.Ln), r=[("ps", b1)], w=["rinv0"])
            P.add("scalar", lambda e: e.activation(out=rinv[1], in_=psf(b2), func=AF.Ln), r=[("ps", b2)], w=["rinv1"])
            P.add("scalar", lambda e: e.activation(out=rinv[0], in_=rinv[0], func=AF.Exp, scale=-1.0), r=["rinv0"], w=["rinv0"])
            P.add("scalar", lambda e: e.activation(out=rinv[1], in_=rinv[1], func=AF.Exp, scale=-1.0), r=["rinv1"], w=["rinv1"])
            P.add("vector", lambda e: e.tensor_copy(out=tA, in_=ps_t[:, 0, :]), r=[("ps", 0)], w=["tA"])
            P.add("vector", lambda e: e.tensor_copy(out=tB, in_=ps_t[:, 1, :]), r=[("ps", 1)], w=["tB"])
            P.add("vector", lambda e: e.tensor_tensor(out=tA, in0=tA, in1=rinv[0], op=ALU.mult), r=["tA", "rinv0"], w=["tA"])
            P.add("vector", lambda e: e.tensor_tensor(out=tB, in0=tB, in1=rinv[1], op=ALU.mult), r=["tB", "rinv1"], w=["tB"])
            P.add("vector", lambda e: e.scalar_tensor_tensor(out=tA, in0=tB, scalar=nlam[:, 0:1], in1=tA, op0=ALU.mult, op1=ALU.add),
                  r=["tA", "tB", "nlam"], w=["tA"])
            P.add("scalar", lambda e: e.activation(out=sqb, in_=tA, func=AF.Square), r=["tA"], w=["sqb"])
            bt = psS.next()
            P.add("tensor", lambda e: e.matmul(psf(bt), lhsT=ones_b, rhs=sqb, start=True, stop=True), r=["sqb", "ones_b"], w=[("ps", bt)])
            P.add("scalar", lambda e: e.activation(out=rstd5, in_=psf(bt), func=AF.Ln, scale=1.0 / 128, bias=epsb[:, 0:1]),
                  r=[("ps", bt), "epsb"], w=["rstd5"])
            P.add("scalar", lambda e: e.activation(out=rstd5, in_=rstd5, func=AF.Exp, scale=-0.5), r=["rstd5"], w=["rstd5"])
            P.add("vector", lambda e: e.scalar_tensor_tensor(out=OnT[:, h, Q * 512:(Q + 1) * 512], in0=tA, scalar=sg[:, 0:1], in1=rstd5,
                                                             op0=ALU.mult, op1=ALU.mult),
                  r=["tA", "rstd5", "sg"], w=[("OnT", h, 4 * Q + i) for i in range(4)])

        LA = 2
        for n in range(len(items) + LA):
            if n < len(items):
                front(items[n])
            if n >= LA:
                back(items[n - LA])

        for T in range(NT):
            s2 = T % 2
            xr = xr_ring[s2]
            P.add("sync", (lambda xr, T: lambda e: e.dma_start(out=xr, in_=src_d[T * 128:(T + 1) * 128, :]))(xr, T),
                  w=[("xr", s2)], dkey="lr%d" % s2)
            for j in range(2):
                b = psS.next()

                def mmo(e, T=T, j=j, b=b):
                    last = None
                    for k in range(8):
                        last = e.matmul(psf(b), lhsT=OnT[:, k, T * 128:(T + 1) * 128], rhs=wo[:, k, j * 512:(j + 1) * 512],
                                        start=(k == 0), stop=(k == 7))
                    return last
                P.add("tensor", mmo, r=[("OnT", k, T) for k in range(8)] + ["wo"], w=[("ps", b)])
                P.add("vector", (lambda xr, j, b: lambda e: e.tensor_tensor(out=xr[:, j * 512:(j + 1) * 512], in0=psf(b),
                                                                             in1=xr[:, j * 512:(j + 1) * 512], op=ALU.add))(xr, j, b),
                      r=[("ps", b), ("xr", s2)], w=[("xr", s2)])
            P.add("sync", (lambda xr, T: lambda e: e.dma_start(out=dst_d[T * 128:(T + 1) * 128, :], in_=xr))(xr, T),
                  r=[("xr", s2)], w=[("hC", T)], dkey="st%d" % s2)


    def phase_D_dense(src_d, dst_d):
        ar.reset(CONST_END)
        dk_ctr[0] = 0
        gcol = ar.alloc([8], F32)
        wr = ar.alloc([8, 8], F32)
        acc = ar.alloc([16, 1024], F32)
        xnT = ar.alloc([8, 2048], BF16)
        gates = ar.alloc([16, 8], F32)
        xs32_r = [ar.alloc([1024], F32) for _ in range(2)]
        junk = ar.alloc([1024], BF16)
        xs_ring = [{"bf": ar.alloc([1024], BF16), "junk": junk} for _ in range(2)]
        ss_r = [ar.alloc([1], F32) for _ in range(2)]
        rstd_r = [ar.alloc([1], F32) for _ in range(2)]
        xn32T = ar.alloc([8, 128], F32)
        lg = ar.alloc([8], F32)
        eq = ar.alloc([8], F32)
        l2 = ar.alloc([8], F32)
        sel = ar.alloc([8], F32)
        ex = ar.alloc([8], F32)
        m1 = ar.alloc([1], F32)
        m2 = ar.alloc([1], F32)
        nm1 = ar.alloc([1], F32)
        den = ar.alloc([1], F32)
        rden = ar.alloc([1], F32)
        wg_r = [ar.alloc([8, 512], BF16) for _ in range(2)]
        wu_r = [ar.alloc([8, 512], BF16) for _ in range(2)]
        wd_r = [ar.alloc([4, 1024], BF16) for _ in range(2)]
        HT_r = [ar.alloc([4, 512], BF16) for _ in range(2)]
        tmp_r = [ar.alloc([512], F32) for _ in range(2)]

        P.add("sync", col_load(gcol, w["l1_moe_norm"], 8), w=["gcol"], dkey=dk())
        P.add("sync", lambda e: e.dma_start(out=wr, in_=w["l1_moe_w_router"].rearrange("(k p) e -> p k e", p=128)), w=["wr"], dkey=dk())
        wg_d = w["l1_moe_w_gate"]
        wu_d = w["l1_moe_w_up"]
        wd_d = w["l1_moe_w_down"]
        psT = PsRing([0, 1])

        groups = [(hf, e_, fg) for hf in range(2) for e_ in range(NE) for fg in range(FE // 512)]

        def load_group(gi):
            hf, e_, fg = groups[gi]
            sl = gi % 2
            P.add("gpsimd", (lambda sl, e_, fg: lambda e: e.dma_start(
                out=wg_r[sl], in_=wg_d[e_].rearrange("(k p) f -> p k f", p=128)[:, :, fg * 512:(fg + 1) * 512]))(sl, e_, fg),
                w=[("wg", sl)], dkey="wg%d" % sl)
            P.add("gpsimd", (lambda sl, e_, fg: lambda e: e.dma_start(
                out=wu_r[sl], in_=wu_d[e_].rearrange("(k p) f -> p k f", p=128)[:, :, fg * 512:(fg + 1) * 512]))(sl, e_, fg),
                w=[("wu", sl)], dkey="wu%d" % sl)
            P.add("gpsimd", (lambda sl, e_, fg: lambda e: e.dma_start(
                out=wd_r[sl], in_=wd_d[e_][fg * 512:(fg + 1) * 512, :].rearrange("(k p) n -> p k n", p=128)))(sl, e_, fg),
                w=[("wd", sl)], dkey="wd%d" % sl)

        gi = 0
        hi = 0
        for hf in range(2):
            for t in range(16):
                T = hf * 16 + t
                s2 = t % 2
                at = acc[:, t, :]
                P.add("sync", (lambda at, T: lambda e: e.dma_start(out=at, in_=src_d[T * 128:(T + 1) * 128, :]))(at, T),
                      w=[("acc", t)], dkey="la%d" % t)
                xs32 = xs32_r[s2]
                norm_p1(at, xs_ring[s2], ss_r[s2], rstd_r[s2], ("D", s2), [("acc", t)], xs32=xs32)
                norm_p2(gcol, xs_ring[s2], xnT[:, :, t * 128:(t + 1) * 128], ("D", s2), psT, [("xnT", t)])

                def trf(e, xs32=xs32):
                    last = None
                    for k in range(8):
                        last = e.transpose(ps_t[:, 2 + k // 4, (k % 4) * 128:(k % 4 + 1) * 128], xs32[:, k * 128:(k + 1) * 128], ident_f)
                    return last
                P.add("tensor", trf, r=[("xs32", ("D", s2)), "ident_f"], w=[("ps", 2), ("ps", 3)])
                P.add("vector", lambda e: e.tensor_tensor(out=xn32T, in0=ps_t[:, 2:4, :].rearrange("p a (b t) -> p (a b) t", b=4),
                                                          in1=gcol.unsqueeze(2).to_broadcast([128, 8, 128]), op=ALU.mult),
                      r=[("ps", 2), ("ps", 3), "gcol"], w=["xn32T"])

                def lgm(e):
                    last = None
                    for k in range(8):
                        last = e.matmul(ps_t[:, 4, 0:8], lhsT=xn32T[:, k, :], rhs=wr[:, k, :], start=(k == 0), stop=(k == 7))
                    return last
                P.add("tensor", lgm, r=["xn32T", "wr"], w=[("ps", 4)])
                P.chain("vector", [
                    lambda e: e.tensor_copy(out=lg, in_=ps_t[:, 4, 0:8]),
                    lambda e: e.tensor_reduce(out=m1, in_=lg, axis=AX.X, op=ALU.max),
                    lambda e: e.tensor_scalar(out=eq, in0=lg, scalar1=m1[:, 0:1], scalar2=None, op0=ALU.is_equal),
                    lambda e: e.scalar_tensor_tensor(out=l2, in0=eq, scalar=-1.0e30, in1=lg, op0=ALU.mult, op1=ALU.add),
                    lambda e: e.tensor_reduce(out=m2, in_=l2, axis=AX.X, op=ALU.max),
                    lambda e: e.tensor_scalar(out=sel, in0=lg, scalar1=m2[:, 0:1], scalar2=None, op0=ALU.is_ge),
                    lambda e: e.tensor_scalar(out=nm1, in0=m1, scalar1=-1.0, scalar2=None, op0=ALU.mult),
                ], r=[("ps", 4)], w=["lg", "sel", "nm1"])
                P.add("scalar", lambda e: e.activation(out=ex, in_=lg, func=AF.Exp, bias=nm1[:, 0:1]), r=["lg", "nm1"], w=["ex"])
                P.chain("vector", [
                    lambda e: e.tensor_tensor(out=ex, in0=ex, in1=sel, op=ALU.mult),
                    lambda e: e.tensor_reduce(out=den, in_=ex, axis=AX.X, op=ALU.add),
                    lambda e: e.reciprocal(out=rden, in_=den),
                    (lambda t: lambda e: e.tensor_scalar(out=gates[:, t, :], in0=ex, scalar1=rden[:, 0:1], scalar2=None, op0=ALU.mult))(t),
                ], r=["ex", "sel"], w=[("gates", t), "ex"])

            psM = PsRing([0, 1, 2, 3, 4, 5, 6, 7])
            if hf == 0:
                load_group(0)
            for e_ in range(NE):
                for fg in range(FE // 512):
                    if gi + 1 < len(groups):
                        load_group(gi + 1)
                    sl = gi % 2
                    wg, wu, wd = wg_r[sl], wu_r[sl], wd_r[sl]
                    for st in range(4):
                        HT = HT_r[hi % 2]
                        hk = hi % 2
                        hi += 1
                        xk = [("xnT", st * 4 + c) for c in range(4)]
                        for fc in range(4):
                            bg = psM.next()
                            bu = psM.next()

                            def mmg(e, fc=fc, bg=bg, wg=wg, st=st):
                                last = None
                                for k in range(8):
                                    last = e.matmul(psf(bg), lhsT=wg[:, k, fc * 128:(fc + 1) * 128], rhs=xnT[:, k, st * 512:(st + 1) * 512],
                                                    start=(k == 0), stop=(k == 7))
                                return last

                            def mmu(e, fc=fc, bu=bu, wu=wu, st=st):
                                last = None
                                for k in range(8):
                                    last = e.matmul(psf(bu), lhsT=wu[:, k, fc * 128:(fc + 1) * 128], rhs=xnT[:, k, st * 512:(st + 1) * 512],
                                                    start=(k == 0), stop=(k == 7))
                                return last
                            P.add("tensor", mmg, r=[("wg", sl)] + xk, w=[("ps", bg)])
                            P.add("tensor", mmu, r=[("wu", sl)] + xk, w=[("ps", bu)])
                            tmp = tmp_r[fc % 2]
                            P.add("scalar", (lambda tmp, bg: lambda e: e.activation(out=tmp, in_=psf(bg), func=AF.Silu))(tmp, bg),
                                  r=[("ps", bg)], w=[("tmp", fc % 2)])
                            P.add("vector", (lambda tmp, bu, fc, HT: lambda e: e.tensor_tensor(out=HT[:, fc, :], in0=psf(bu), in1=tmp, op=ALU.mult))(tmp, bu, fc, HT),
                                  r=[("ps", bu), ("tmp", fc % 2)], w=[("HT", hk, fc)])
                        for c in range(4):
                            t = st * 4 + c
                            for j in range(2):
                                b = psM.next()

                                def mmd(e, c=c, j=j, b=b, HT=HT, wd=wd):
                                    last = None
                                    for k in range(4):
                                        last = e.matmul(psf(b), lhsT=HT[:, k, c * 128:(c + 1) * 128], rhs=wd[:, k, j * 512:(j + 1) * 512],
                                                        start=(k == 0), stop=(k == 3))
                                    return last
                                P.add("tensor", mmd, r=[("HT", hk, fc) for fc in range(4)] + [("wd", sl)], w=[("ps", b)])
                                P.add("vector", (lambda t, j, b, e_: lambda e: e.scalar_tensor_tensor(
                                    out=acc[:, t, j * 512:(j + 1) * 512], in0=psf(b), scalar=gates[:, t, e_:e_ + 1],
                                    in1=acc[:, t, j * 512:(j + 1) * 512], op0=ALU.mult, op1=ALU.add))(t, j, b, e_),
                                    r=[("ps", b), ("gates", t), ("acc", t)], w=[("acc", t)])
                    gi += 1
            for t in range(16):
                T = hf * 16 + t
                P.add("sync", (lambda t, T: lambda e: e.dma_start(out=dst_d[T * 128:(T + 1) * 128, :], in_=acc[:, t, :]))(t, T),
                      r=[("acc", t)], w=[("hD", T)], dkey="sa%d" % t)


    def phase_D(src_d, dst_d):
        BIG = 1.0e6
        ar.reset(CONST_END)
        dk_ctr[0] = 0
        idxA = ar.alloc([NT, 2], I32)
        idxB = ar.alloc([NT, 2], I32)
        gAB = ar.alloc([NT, 2], F32)
        gBB = ar.alloc([NT, 2], F32)
        gcol = ar.alloc([8], F32)
        P_END = ar.off
        wr = ar.alloc([8, 8], F32)
        tris = ar.alloc([128], F32)
        ones_f = ar.alloc([128], F32)
        eoff = ar.alloc([8], F32)
        Mcum = ar.alloc([8], F32)
        xt_ring = [ar.alloc([1024], F32) for _ in range(2)]
        xs32_r = [ar.alloc([1024], F32) for _ in range(2)]
        junk = ar.alloc([1024], BF16)
        xs_ring = [{"bf": ar.alloc([1024], BF16), "junk": junk} for _ in range(2)]
        ss_r = [ar.alloc([1], F32) for _ in range(2)]
        rstd_r = [ar.alloc([1], F32) for _ in range(2)]
        xn32T = ar.alloc([8, 128], F32)
        lg = ar.alloc([8], F32)
        eq = ar.alloc([8], F32)
        l2 = ar.alloc([8], F32)
        sel = ar.alloc([8], F32)
        ex = ar.alloc([8], F32)
        gt = ar.alloc([8], F32)
        rk = ar.alloc([8], F32)
        sf = ar.alloc([8], F32)
        sm = ar.alloc([8], F32)
        t2 = ar.alloc([8], F32)
        oh = ar.alloc([8], F32)
        tg = ar.alloc([8], F32)
        tg2 = ar.alloc([8], F32)
        cnti = ar.alloc([8], I32)
        m1 = ar.alloc([1], F32)
        m2 = ar.alloc([1], F32)
        nm1 = ar.alloc([1], F32)
        den = ar.alloc([1], F32)
        rden = ar.alloc([1], F32)
        sA = ar.alloc([1], F32)
        sB = ar.alloc([1], F32)
        XT = [ar.alloc([8, 512], BF16) for _ in range(4)]
        acc = [ar.alloc([4, 1024], F32) for _ in range(4)]
        wg_r = [ar.alloc([8, 512], BF16) for _ in range(2)]
        wu_r = [ar.alloc([8, 512], BF16) for _ in range(2)]
        wd_r = [ar.alloc([4, 1024], BF16) for _ in range(2)]
        HT_r = [ar.alloc([4, 512], BF16) for _ in range(2)]
        tmp_r = [ar.alloc([512], F32) for _ in range(2)]
        xg_r = [ar.alloc([1024], BF16) for _ in range(2)]

        P.add("sync", col_load(gcol, w["l1_moe_norm"], 8), w=["gcol"], dkey=dk())
        P.add("sync", lambda e: e.dma_start(out=wr, in_=w["l1_moe_w_router"].rearrange("(k p) e -> p k e", p=128)), w=["wr"], dkey=dk())
        P.add("sync", lambda e: e.dma_start(out=tris, in_=w["c_tris"][:, :]), w=["tris"], dkey=dk())
        P.add("sync", lambda e: e.dma_start(out=eoff, in_=w["c_eoff"].partition_broadcast(128)), w=["eoff"], dkey=dk())
        P.add("gpsimd", lambda e: e.memset(ones_f, 1.0), w=["ones_f"])
        P.add("gpsimd", lambda e: e.memset(Mcum, 0.0), w=["Mcum"])
        psT = PsRing([0, 1])

        for T in range(NT):
            s2 = T % 2
            xt = xt_ring[s2]
            P.add("sync", (lambda xt, T: lambda e: e.dma_start(out=xt, in_=src_d[T * 128:(T + 1) * 128, :]))(xt, T),
                  w=[("xt", s2)], dkey="ld%d" % s2)
            xs32 = xs32_r[s2]
            xsb = xs_ring[s2]["bf"]
            norm_p1(xt, xs_ring[s2], ss_r[s2], rstd_r[s2], ("D", s2), [("xt", s2)], xs32=xs32)

            def trf(e, xs32=xs32):
                last = None
                for k in range(8):
                    last = e.transpose(ps_t[:, 2 + k // 4, (k % 4) * 128:(k % 4 + 1) * 128], xs32[:, k * 128:(k + 1) * 128], ident_f)
                return last
            P.add("tensor", trf, r=[("xs32", ("D", s2)), "ident_f"], w=[("ps", 2), ("ps", 3)])
            P.add("vector", lambda e: e.tensor_tensor(out=xn32T, in0=ps_t[:, 2:4, :].rearrange("p a (b t) -> p (a b) t", b=4),
                                                      in1=gcol.unsqueeze(2).to_broadcast([128, 8, 128]), op=ALU.mult),
                  r=[("ps", 2), ("ps", 3), "gcol"], w=["xn32T"])

            def lgm(e):
                last = None
                for k in range(8):
                    last = e.matmul(ps_t[:, 4, 0:8], lhsT=xn32T[:, k, :], rhs=wr[:, k, :], start=(k == 0), stop=(k == 7))
                return last
            P.add("tensor", lgm, r=["xn32T", "wr"], w=[("ps", 4)])
            P.chain("vector", [
                lambda e: e.tensor_copy(out=lg, in_=ps_t[:, 4, 0:8]),
                lambda e: e.tensor_reduce(out=m1, in_=lg, axis=AX.X, op=ALU.max),
                lambda e: e.tensor_scalar(out=eq, in0=lg, scalar1=m1[:, 0:1], scalar2=None, op0=ALU.is_equal),
                lambda e: e.scalar_tensor_tensor(out=l2, in0=eq, scalar=-1.0e30, in1=lg, op0=ALU.mult, op1=ALU.add),
                lambda e: e.tensor_reduce(out=m2, in_=l2, axis=AX.X, op=ALU.max),
                lambda e: e.tensor_scalar(out=sel, in0=lg, scalar1=m2[:, 0:1], scalar2=None, op0=ALU.is_ge),
                lambda e: e.tensor_scalar(out=nm1, in0=m1, scalar1=-1.0, scalar2=None, op0=ALU.mult),
            ], r=[("ps", 4)], w=["lg", "sel", "nm1"])
            P.add("scalar", lambda e: e.activation(out=ex, in_=lg, func=AF.Exp, bias=nm1[:, 0:1]), r=["lg", "nm1"], w=["ex"])

            def rkm(e):
                e.matmul(ps_t[:, 5, 0:8], lhsT=tris, rhs=sel, start=True, stop=False)
                return e.matmul(ps_t[:, 5, 0:8], lhsT=ones_f, rhs=Mcum, start=False, stop=True)
            P.add("tensor", rkm, r=["sel", "Mcum", "tris", "ones_f"], w=[("ps", 5)])
            P.chain("vector", [
                lambda e: e.tensor_tensor(out=ex, in0=ex, in1=sel, op=ALU.mult),
                lambda e: e.tensor_reduce(out=den, in_=ex, axis=AX.X, op=ALU.add),
                lambda e: e.reciprocal(out=rden, in_=den),
                lambda e: e.tensor_scalar(out=gt, in0=ex, scalar1=rden[:, 0:1], scalar2=None, op0=ALU.mult),
                lambda e: e.tensor_copy(out=rk, in_=ps_t[:, 5, 0:8]),
                lambda e: e.tensor_tensor(out=Mcum, in0=Mcum, in1=sel, op=ALU.add),
                lambda e: e.tensor_tensor(out=sf, in0=rk, in1=eoff, op=ALU.add),
                lambda e: e.scalar_tensor_tensor(out=sm, in0=sf, scalar=-BIG, in1=sel, op0=ALU.add, op1=ALU.mult),
                lambda e: e.tensor_scalar(out=sm, in0=sm, scalar1=BIG, scalar2=None, op0=ALU.add),
                lambda e: e.tensor_reduce(out=sA, in_=sm, axis=AX.X, op=ALU.min),
                lambda e: e.tensor_tensor(out=t2, in0=sf, in1=sel, op=ALU.mult),
                lambda e: e.tensor_reduce(out=sB, in_=t2, axis=AX.X, op=ALU.max),
                lambda e: e.tensor_scalar(out=oh, in0=sm, scalar1=sA[:, 0:1], scalar2=None, op0=ALU.is_equal),
                lambda e: e.tensor_tensor(out=tg, in0=gt, in1=oh, op=ALU.mult),
                (lambda T: lambda e: e.tensor_reduce(out=gAB[:, T, 0:1], in_=tg, axis=AX.X, op=ALU.add))(T),
                lambda e: e.tensor_tensor(out=tg2, in0=gt, in1=tg, op=ALU.subtract),
                (lambda T: lambda e: e.tensor_reduce(out=gBB[:, T, 0:1], in_=tg2, axis=AX.X, op=ALU.add))(T),
                (lambda T: lambda e: e.tensor_copy(out=idxA[:, T, 0:1], in_=sA))(T),
                (lambda T: lambda e: e.tensor_copy(out=idxB[:, T, 0:1], in_=sB))(T),
            ], r=["ex", "sel", ("ps", 5), "eoff", "Mcum"], w=["ex", "Mcum", ("idx", T), ("gab", T)])
            for which, idx_t in ((0, idxA), (1, idxB)):
                P.add("gpsimd", (lambda idx_t, T, xsb: lambda e: e.indirect_dma_start(
                    out=Xg_d[:, :], out_offset=bass.IndirectOffsetOnAxis(ap=idx_t[:, T, 0:1], axis=0),
                    in_=xsb, in_offset=None))(idx_t, T, xsb),
                    r=[("idx", T), ("xsbf", ("D", s2))], w=[("Xg", T, which)], dkey="sc%d%d" % (s2, which))
        P.add("tensor", lambda e: e.matmul(ps_t[:, 5, 0:8], lhsT=ones_f, rhs=Mcum, start=True, stop=True), r=["Mcum", "ones_f"], w=[("ps", 5)])
        P.add("vector", lambda e: e.tensor_copy(out=cnti, in_=ps_t[:, 5, 0:8]), r=[("ps", 5)], w=["cnti"])
        P.add("sync", lambda e: e.dma_start(out=cnt_d[0:1, :], in_=cnti[0:1, :]), r=["cnti"], w=["cnt_d"], dkey=dk())
        P.barrier()

        for e_ in range(NE):
            P.regload("cnt%d" % e_, cnt_d[0:1, e_:e_ + 1])
        wg_d = w["l1_moe_w_gate"]
        wu_d = w["l1_moe_w_up"]
        wd_d = w["l1_moe_w_down"]
        NFG = FE // 512
        psM = PsRing([0, 1, 2, 3, 4, 5, 6, 7])
        ctr = {"hi": 0, "xg": 0}

        def load_w(e_, fg, sl):
            P.add("gpsimd", lambda e: e.dma_start(
                out=wg_r[sl], in_=wg_d[e_].rearrange("(k p) f -> p k f", p=128)[:, :, fg * 512:(fg + 1) * 512]),
                w=[("wg", sl)], dkey="wg%d" % sl)
            P.add("gpsimd", lambda e: e.dma_start(
                out=wu_r[sl], in_=wu_d[e_].rearrange("(k p) f -> p k f", p=128)[:, :, fg * 512:(fg + 1) * 512]),
                w=[("wu", sl)], dkey="wu%d" % sl)
            P.add("gpsimd", lambda e: e.dma_start(
                out=wd_r[sl], in_=wd_d[e_][fg * 512:(fg + 1) * 512, :].rearrange("(k p) n -> p k n", p=128)),
                w=[("wd", sl)], dkey="wd%d" % sl)

        def prep(e_, rnd):
            ck = "cnt%d" % e_
            for k in range(4):
                kk = rnd * 4 + k
                P.pred_begin(ck, kk * 512 + 1)
                for c in range(4):
                    xs_ = ctr["xg"] % 2
                    ctr["xg"] += 1
                    xg = xg_r[xs_]
                    row0 = e_ * S + kk * 512 + c * 128
                    P.add("sync", (lambda xg, row0: lambda e: e.dma_start(out=xg, in_=Xg_d[row0:row0 + 128, :]))(xg, row0),
                          w=[("xg", xs_)], dkey="xg%d" % xs_)
                    b = psM.next()

                    def trx(e, xg=xg, b=b):
                        last = None
                        for kq in range(8):
                            last = e.transpose(psb(b)[:, kq * 128:(kq + 1) * 128], xg[:, kq * 128:(kq + 1) * 128], ident_bf)
                        return last
                    P.add("tensor", trx, r=[("xg", xs_), "ident_bf"], w=[("ps", b)])
                    P.add("vector", (lambda k, c, b: lambda e: e.tensor_tensor(
                        out=XT[k][:, :, c * 128:(c + 1) * 128], in0=psb(b).rearrange("p (k t) -> p k t", k=8),
                        in1=gcol.unsqueeze(2).to_broadcast([128, 8, 128]), op=ALU.mult))(k, c, b),
                        r=[("ps", b), "gcol"], w=[("XT", k, c)])
            for k in range(4):
                P.pred_end()

        def compute(e_, rnd, fg, sl):
            ck = "cnt%d" % e_
            wg, wu, wd = wg_r[sl], wu_r[sl], wd_r[sl]
            for k in range(4):
                kk = rnd * 4 + k
                P.pred_begin(ck, kk * 512 + 1)
                HT = HT_r[ctr["hi"] % 2]
                hk = ctr["hi"] % 2
                ctr["hi"] += 1
                xk = [("XT", k, c) for c in range(4)]
                for fc in range(4):
                    bg = psM.next()
                    bu = psM.next()

                    def mmg(e, fc=fc, bg=bg, wg=wg, k=k):
                        last = None
                        for kq in range(8):
                            last = e.matmul(psf(bg), lhsT=wg[:, kq, fc * 128:(fc + 1) * 128], rhs=XT[k][:, kq, :],
                                            start=(kq == 0), stop=(kq == 7))
                        return last

                    def mmu(e, fc=fc, bu=bu, wu=wu, k=k):
                        last = None
                        for kq in range(8):
                            last = e.matmul(psf(bu), lhsT=wu[:, kq, fc * 128:(fc + 1) * 128], rhs=XT[k][:, kq, :],
                                            start=(kq == 0), stop=(kq == 7))
                        return last
                    P.add("tensor", mmg, r=[("wg", sl)] + xk, w=[("ps", bg)])
                    P.add("tensor", mmu, r=[("wu", sl)] + xk, w=[("ps", bu)])
                    tmp = tmp_r[fc % 2]
                    P.add("scalar", (lambda tmp, bg: lambda e: e.activation(out=tmp, in_=psf(bg), func=AF.Silu))(tmp, bg),
                          r=[("ps", bg)], w=[("tmp", fc % 2)])
                    P.add("vector", (lambda tmp, bu, fc, HT: lambda e: e.tensor_tensor(out=HT[:, fc, :], in0=psf(bu), in1=tmp, op=ALU.mult))(tmp, bu, fc, HT),
                          r=[("ps", bu), ("tmp", fc % 2)], w=[("HT", hk, fc)])
                for c in range(4):
                    for j in range(2):
                        b = psM.next()

                        def mmd(e, c=c, j=j, b=b, HT=HT, wd=wd):
                            last = None
                            for kq in range(4):
                                last = e.matmul(psf(b), lhsT=HT[:, kq, c * 128:(c + 1) * 128], rhs=wd[:, kq, j * 512:(j + 1) * 512],
                                                start=(kq == 0), stop=(kq == 3))
                            return last
                        P.add("tensor", mmd, r=[("HT", hk, fc) for fc in range(4)] + [("wd", sl)], w=[("ps", b)])
                        dst = acc[k][:, c, j * 512:(j + 1) * 512]
                        if fg == 0:
                            P.add("vector", (lambda dst, b: lambda e: e.tensor_copy(out=dst, in_=psf(b)))(dst, b),
                                  r=[("ps", b)], w=[("acc", k, c)])
                        else:
                            P.add("vector", (lambda dst, b: lambda e: e.tensor_tensor(out=dst, in0=psf(b), in1=dst, op=ALU.add))(dst, b),
                                  r=[("ps", b), ("acc", k, c)], w=[("acc", k, c)])
            for k in range(4):
                P.pred_end()

        def stores(e_, rnd):
            ck = "cnt%d" % e_
            for k in range(4):
                kk = rnd * 4 + k
                P.pred_begin(ck, kk * 512 + 1)
                row0 = e_ * S + kk * 512
                P.add("sync", (lambda k, row0: lambda e: e.dma_start(
                    out=Yg_d[row0:row0 + 512, :].rearrange("(c p) n -> p c n", p=128), in_=acc[k]))(k, row0),
                    r=[("acc", k, c) for c in range(4)], w=[("Yg", e_, kk)], dkey="ys%d" % k)
            for k in range(4):
                P.pred_end()

        order0 = [(e_, fg) for e_ in range(NE) for fg in range(NFG)]
        load_w(order0[0][0], order0[0][1], 0)
        for n, (e_, fg) in enumerate(order0):
            if fg == 0:
                prep(e_, 0)
            if n + 1 < len(order0):
                load_w(order0[n + 1][0], order0[n + 1][1], (n + 1) % 2)
            compute(e_, 0, fg, n % 2)
            if fg == NFG - 1:
                stores(e_, 0)
        g = len(order0)
        for e_ in range(NE):
            P.pred_begin("cnt%d" % e_, 2049)
            prep(e_, 1)
            load_w(e_, 0, g % 2)
            for fg in range(NFG):
                if fg + 1 < NFG:
                    load_w(e_, fg + 1, (g + 1) % 2)
                compute(e_, 1, fg, g % 2)
                g += 1
            stores(e_, 1)
            P.pred_end()
        P.barrier()

        ar.reset(P_END)
        ya_r = [ar.alloc([1024], F32) for _ in range(2)]
        yb_r = [ar.alloc([1024], F32) for _ in range(2)]
        xr_r = [ar.alloc([1024], F32) for _ in range(2)]
        for T in range(NT):
            s2 = T % 2
            ya, yb, xr = ya_r[s2], yb_r[s2], xr_r[s2]
            P.add("gpsimd", (lambda ya, T: lambda e: e.indirect_dma_start(
                out=ya, out_offset=None, in_=Yg_d[:, :], in_offset=bass.IndirectOffsetOnAxis(ap=idxA[:, T, 0:1], axis=0)))(ya, T),
                w=[("ya", s2)], dkey="ga%d" % s2)
            P.add("gpsimd", (lambda yb, T: lambda e: e.indirect_dma_start(
                out=yb, out_offset=None, in_=Yg_d[:, :], in_offset=bass.IndirectOffsetOnAxis(ap=idxB[:, T, 0:1], axis=0)))(yb, T),
                w=[("yb", s2)], dkey="gb%d" % s2)
            P.add("sync", (lambda xr, T: lambda e: e.dma_start(out=xr, in_=src_d[T * 128:(T + 1) * 128, :]))(xr, T),
                  w=[("xr", s2)], dkey="lr%d" % s2)
            P.add("vector", (lambda ya, xr, T: lambda e: e.scalar_tensor_tensor(out=xr, in0=ya, scalar=gAB[:, T, 0:1], in1=xr, op0=ALU.mult, op1=ALU.add))(ya, xr, T),
                  r=[("ya", s2), ("xr", s2)], w=[("xr", s2)])
            P.add("vector", (lambda yb, xr, T: lambda e: e.scalar_tensor_tensor(out=xr, in0=yb, scalar=gBB[:, T, 0:1], in1=xr, op0=ALU.mult, op1=ALU.add))(yb, xr, T),
                  r=[("yb", s2), ("xr", s2)], w=[("xr", s2)])
            P.add("sync", (lambda xr, T: lambda e: e.dma_start(out=dst_d[T * 128:(T + 1) * 128, :], in_=xr))(xr, T),
                  r=[("xr", s2)], w=[("hD", T)], dkey="st%d" % s2)


    if os.environ.get("MOE_DENSE"):
        phase_D = phase_D_dense

    order = ["A", "B", "C", "D"]
    nph = order.index(stop_after) + 1
    srcs = [x_d, h1_d, h2_d, h3_d]
    dsts = [h1_d, h2_d, h3_d, out_d]
    dsts[nph - 1] = out_d
    fns = [phase_A, phase_B, phase_C, phase_D]
    for i in range(nph):
        fns[i](srcs[i], dsts[i])
        P.barrier()

    P.emit(es)
    es.close()
    return nc, list(declared.keys())


_CACHE = {}


def _consts():
    ident = np.eye(128, dtype=np.float32)
    tri = np.triu(np.ones((128, 128), dtype=np.float32))
    invf = (1.0 / (np.float32(500000.0) ** (np.arange(0, 16, 2, dtype=np.float32) / np.float32(16)))).astype(np.float32)
    return {
        "c_ident_bf": ident.astype(ml_dtypes.bfloat16),
        "c_ident_f": ident,
        "c_tri": tri,
        "c_invf": invf,
        "c_tris": np.triu(np.ones((128, 128), dtype=np.float32), 1),
        "c_eoff": (np.arange(8, dtype=np.float32) * 4096.0).astype(np.float32),
    }


def kernel(stop_after="D", **inputs):
    key = stop_after
    if key not in _CACHE:
        _CACHE[key] = build_program(stop_after)
    nc, used = _CACHE[key]
    consts = _consts()
    in_maps = []
    for b in range(8):
        m = {}
        for k, v in inputs.items():
            if k not in used:
                continue
            a = np.asarray(v)
            if k == "x":
                m[k] = np.ascontiguousarray(a[b])
            elif k == "positions":
                m[k] = np.ascontiguousarray(a[b]).astype(np.int32, copy=False)
            else:
                m[k] = np.ascontiguousarray(a)
        m.update({k: v for k, v in consts.items() if k in used})
        in_maps.append(m)
    res = run_bass_kernel_spmd(nc, in_maps, core_ids=list(range(8)))
    global _LAST
    _LAST = res
    return np.stack([np.asarray(r["out"]) for r in res.results], axis=0).astype(np.float32, copy=False)
```
